# Optimizing a Trainium2 kernel written in Bass

```python
import jax, jax.numpy as jnp
from jax import lax
import numpy as np

D_MODEL = 1024
BATCH = 8
SEQ = 2048
DEPTH = 1
DEC_BATCH = 32
DEC_SEQ = 8
PAST_LEN = 16384
PAGE_SIZE = 128

CONV_WIDTH = D_MODEL // 2
CONV_K = 3
N_HEADS = 8
NOPE_DIM = 64
ROPE_DIM = 32
V_DIM = 64
Q_RANK = D_MODEL // 4
KV_RANK = D_MODEL // 8
ROPE_BASE = 10000.0
MIX_WIDTH = CONV_WIDTH + N_HEADS * V_DIM
OUT_GROUP_DIM = 64
N_OUT_GROUPS = MIX_WIDTH // OUT_GROUP_DIM
PROJ_WIDTH = 3 * CONV_WIDTH + Q_RANK + KV_RANK + ROPE_DIM
N_GROUPS = 4
EXPERTS_PER_GROUP = 8
N_EXPERTS = N_GROUPS * EXPERTS_PER_GROUP
TOP_K = 2
D_EXPERT = D_MODEL // 4
EPS = 1e-6
Q_BLOCK = 128

kernel_name = "hymba_conv_mla_hmoe_step"


def _rmsnorm(x, g):
    xf = x.astype(jnp.float32)
    y = xf * lax.rsqrt(jnp.mean(xf * xf, axis=-1, keepdims=True) + EPS)
    return (y * g.astype(jnp.float32)).astype(x.dtype)


def _rope(x, pos):
    half = ROPE_DIM // 2
    inv_freq = ROPE_BASE ** (-jnp.arange(half, dtype=jnp.float32) / half)
    ang = pos.astype(jnp.float32)[:, None] * inv_freq[None, :]
    ang = ang.reshape(ang.shape[:1] + (1,) * (x.ndim - 3) + (half,))
    cos, sin = jnp.cos(ang), jnp.sin(ang)
    xf = x.astype(jnp.float32)
    x1, x2 = xf[..., :half], xf[..., half:]
    return jnp.concatenate([x1 * cos - x2 * sin, x1 * sin + x2 * cos], axis=-1).astype(x.dtype)


def _project(h, pos, w_in, g_q_lat, w_uq, g_kv_lat, g_q_nope, g_q_rope, g_k_rope):
    z = h @ w_in
    c = CONV_WIDTH
    u_in, gate_b, gate_c = z[..., :c], z[..., c:2 * c], z[..., 2 * c:3 * c]
    o = 3 * c
    q_lat = z[..., o:o + Q_RANK]
    o += Q_RANK
    kv_lat = z[..., o:o + KV_RANK]
    o += KV_RANK
    k_rope_raw = z[..., o:o + ROPE_DIM]
    q = (_rmsnorm(q_lat, g_q_lat) @ w_uq).reshape(q_lat.shape[:-1] + (N_HEADS, NOPE_DIM + ROPE_DIM))
    q_nope = _rmsnorm(q[..., :NOPE_DIM], g_q_nope)
    q_rope = _rope(_rmsnorm(q[..., NOPE_DIM:], g_q_rope), pos)
    c_kv = _rmsnorm(kv_lat, g_kv_lat)
    k_rope = _rope(_rmsnorm(k_rope_raw, g_k_rope), pos)
    return u_in, gate_b, gate_c, q_nope, q_rope, c_kv, k_rope


def _short_conv(u_in, gate_b, gate_c, buf, conv_w, conv_b):
    u = gate_c * u_in
    ext = jnp.concatenate([buf.astype(u.dtype), u], axis=1)
    t = u.shape[1]
    v = conv_b
    for k in range(CONV_K):
        v = v + conv_w[k] * ext[:, k:k + t]
    return gate_b * v, ext[:, t:]


def _kv_expand(c_kv, w_uk, w_uv, g_k_nope):
    lead = c_kv.shape[:-1]
    k_nope = _rmsnorm((c_kv @ w_uk).reshape(lead + (N_HEADS, NOPE_DIM)), g_k_nope)
    v = (c_kv @ w_uv).reshape(lead + (N_HEADS, V_DIM))
    return k_nope, v


def _attend(q_nope, q_rope, k_nope, k_rope, v, q_pos, k_pos):
    scale = (NOPE_DIM + ROPE_DIM) ** -0.5
    s = (jnp.einsum('bthd,bshd->bhts', q_nope, k_nope)
         + jnp.einsum('bthd,bsd->bhts', q_rope, k_rope)).astype(jnp.float32) * scale
    s = jnp.where(k_pos[None, :] <= q_pos[:, None], s, -1e30)
    p = jax.nn.softmax(s, axis=-1).astype(v.dtype)
    return jnp.einsum('bhts,bshd->bthd', p, v)


def _merge(y_conv, y_attn, g_out, w_out):
    y = jnp.concatenate([y_conv, y_attn.reshape(y_attn.shape[:-2] + (N_HEADS * V_DIM,))], axis=-1)
    lead = y.shape[:-1]
    y = _rmsnorm(y.reshape(lead + (N_OUT_GROUPS, OUT_GROUP_DIM)),
                 g_out.reshape(N_OUT_GROUPS, OUT_GROUP_DIM)).reshape(lead + (MIX_WIDTH,))
    return y @ w_out


def _hier_moe(h, w_rg, b_rg, w_re, b_re, w_gate, w_up, w_down):
    lead = h.shape[:-1]
    t = h.reshape(-1, D_MODEL)
    p_group = jax.nn.softmax((t @ w_rg + b_rg).astype(jnp.float32), axis=-1)
    p_sel, g_sel = lax.top_k(p_group, 1)
    e_logits = (t @ w_re + b_re).astype(jnp.float32).reshape(-1, N_GROUPS, EXPERTS_PER_GROUP)
    e_in_group = jnp.take_along_axis(e_logits, g_sel[:, :, None], axis=1)[:, 0]
    top_vals, top_idx = lax.top_k(e_in_group, TOP_K)
    w = jax.nn.softmax(top_vals, axis=-1) * p_sel
    e_idx = g_sel * EXPERTS_PER_GROUP + top_idx
    gate = jnp.sum(w[..., None] * jax.nn.one_hot(e_idx, N_EXPERTS, dtype=jnp.float32), axis=1).astype(h.dtype)
    y = jnp.zeros_like(t)
    for e in range(N_EXPERTS):
        a = jax.nn.silu(t @ w_gate[e]) * (t @ w_up[e])
        y = y + gate[:, e:e + 1] * (a @ w_down[e])
    return y.reshape(lead + (D_MODEL,))


def setup_inputs(seed: int = 0) -> dict:
    key = jax.random.key(seed)
    ks = jax.random.split(key, 32)
    n_pages = PAST_LEN // PAGE_SIZE
    n_pool = (DEC_BATCH * n_pages * 5) // 4
    L = DEPTH

    def nrm(k, shape, scale):
        return jax.random.normal(k, shape, jnp.float32) * scale

    def gain(k, shape):
        return 1.0 + 0.01 * jax.random.normal(k, shape, jnp.float32)

    page_table = jax.random.permutation(ks[5], n_pool)[:DEC_BATCH * n_pages]
    page_table = page_table.reshape(DEC_BATCH, n_pages).astype(jnp.int32)
    return {
        "x_prompt": nrm(ks[0], (BATCH, SEQ, D_MODEL), 1.0),
        "x_sample": nrm(ks[1], (DEC_BATCH, DEC_SEQ, D_MODEL), 1.0),
        "state_conv": nrm(ks[2], (L, DEC_BATCH, CONV_K - 1, CONV_WIDTH), 1.0),
        "cache_ckv": nrm(ks[3], (L, n_pool, PAGE_SIZE, KV_RANK), 1.0),
        "cache_krope": nrm(ks[4], (L, n_pool, PAGE_SIZE, ROPE_DIM), 1.0),
        "page_table": page_table,
        "g_mix": gain(ks[6], (L, D_MODEL)),
        "w_in": nrm(ks[7], (L, D_MODEL, PROJ_WIDTH), D_MODEL ** -0.5),
        "conv_w": nrm(ks[8], (L, CONV_K, CONV_WIDTH), 0.5),
        "conv_b": nrm(ks[9], (L, CONV_WIDTH), 0.01),
        "g_q_lat": gain(ks[10], (L, Q_RANK)),
        "w_uq": nrm(ks[11], (L, Q_RANK, N_HEADS * (NOPE_DIM + ROPE_DIM)), Q_RANK ** -0.5),
        "g_kv_lat": gain(ks[12], (L, KV_RANK)),
        "w_uk": nrm(ks[13], (L, KV_RANK, N_HEADS * NOPE_DIM), KV_RANK ** -0.5),
        "w_uv": nrm(ks[14], (L, KV_RANK, N_HEADS * V_DIM), KV_RANK ** -0.5),
        "g_q_nope": gain(ks[15], (L, NOPE_DIM)),
        "g_q_rope": gain(ks[16], (L, ROPE_DIM)),
        "g_k_nope": gain(ks[17], (L, NOPE_DIM)),
        "g_k_rope": gain(ks[18], (L, ROPE_DIM)),
        "g_out": gain(ks[19], (L, MIX_WIDTH)),
        "w_out": nrm(ks[20], (L, MIX_WIDTH, D_MODEL), MIX_WIDTH ** -0.5),
        "g_ffn": gain(ks[21], (L, D_MODEL)),
        "w_router_group": nrm(ks[22], (L, D_MODEL, N_GROUPS), D_MODEL ** -0.5),
        "b_router_group": nrm(ks[23], (L, N_GROUPS), 0.01),
        "w_router_expert": nrm(ks[24], (L, D_MODEL, N_EXPERTS), D_MODEL ** -0.5),
        "b_router_expert": nrm(ks[25], (L, N_EXPERTS), 0.01),
        "w_gate": nrm(ks[26], (L, N_EXPERTS, D_MODEL, D_EXPERT), D_MODEL ** -0.5),
        "w_up": nrm(ks[27], (L, N_EXPERTS, D_MODEL, D_EXPERT), D_MODEL ** -0.5),
        "w_down": nrm(ks[28], (L, N_EXPERTS, D_EXPERT, D_MODEL), D_EXPERT ** -0.5),
    }


def reference(x_prompt, x_sample, state_conv, cache_ckv, cache_krope, page_table,
              g_mix, w_in, conv_w, conv_b, g_q_lat, w_uq, g_kv_lat, w_uk, w_uv,
              g_q_nope, g_q_rope, g_k_nope, g_k_rope, g_out, w_out,
              g_ffn, w_router_group, b_router_group, w_router_expert, b_router_expert,
              w_gate, w_up, w_down):
    b_p, s_p = x_prompt.shape[0], x_prompt.shape[1]
    t_s = x_sample.shape[1]
    past_len = page_table.shape[1] * cache_ckv.shape[2]
    pos_p = jnp.arange(s_p, dtype=jnp.int32)
    pos_s = past_len + jnp.arange(t_s, dtype=jnp.int32)
    kpos_s = jnp.arange(past_len + t_s, dtype=jnp.int32)
    n_blk = s_p // Q_BLOCK

    xp, xs = x_prompt, x_sample
    ckv_p_l, kr_p_l, conv_p_l, ckv_s_l, kr_s_l, conv_s_l = [], [], [], [], [], []
    for l in range(DEPTH):
        proj_w = (w_in[l], g_q_lat[l], w_uq[l], g_kv_lat[l], g_q_nope[l], g_q_rope[l], g_k_rope[l])
        wuk, wuv, gkn = w_uk[l], w_uv[l], g_k_nope[l]

        u_p, gb_p, gc_p, qn_p, qr_p, ckv_p, kr_p = _project(_rmsnorm(xp, g_mix[l]), pos_p, *proj_w)
        yconv_p, buf_p = _short_conv(u_p, gb_p, gc_p, jnp.zeros((b_p, CONV_K - 1, CONV_WIDTH), xp.dtype),
                                     conv_w[l], conv_b[l])
        kn_p, v_p = _kv_expand(ckv_p, wuk, wuv, gkn)

        def q_block(args, kn_p=kn_p, kr_p=kr_p, v_p=v_p):
            qn, qr, qpos = args
            return _attend(qn, qr, kn_p, kr_p, v_p, qpos, pos_p)

        def to_blocks(a):
            return jnp.moveaxis(a.reshape((b_p, n_blk, Q_BLOCK) + a.shape[2:]), 1, 0)

        ya = lax.map(q_block, (to_blocks(qn_p), to_blocks(qr_p), pos_p.reshape(n_blk, Q_BLOCK)))
        yattn_p = jnp.moveaxis(ya, 0, 1).reshape(b_p, s_p, N_HEADS, V_DIM)
        xp = xp + _merge(yconv_p, yattn_p, g_out[l], w_out[l])
        xp = xp + _hier_moe(_rmsnorm(xp, g_ffn[l]), w_router_group[l], b_router_group[l],
                            w_router_expert[l], b_router_expert[l], w_gate[l], w_up[l], w_down[l])

        u_s, gb_s, gc_s, qn_s, qr_s, ckv_s, kr_s = _project(_rmsnorm(xs, g_mix[l]), pos_s, *proj_w)
        yconv_s, buf_s = _short_conv(u_s, gb_s, gc_s, state_conv[l], conv_w[l], conv_b[l])
        pool_ckv, pool_kr = cache_ckv[l], cache_krope[l]

        def one_seq(args, pool_ckv=pool_ckv, pool_kr=pool_kr):
            qn, qr, ckv_new, kr_new, pages = args
            ckv_all = jnp.concatenate([pool_ckv[pages].reshape(-1, KV_RANK).astype(ckv_new.dtype), ckv_new], axis=0)
            kr_all = jnp.concatenate([pool_kr[pages].reshape(-1, ROPE_DIM).astype(kr_new.dtype), kr_new], axis=0)
            kn, v = _kv_expand(ckv_all, wuk, wuv, gkn)
            return _attend(qn[None], qr[None], kn[None], kr_all[None], v[None], pos_s, kpos_s)[0]

        yattn_s = lax.map(one_seq, (qn_s, qr_s, ckv_s, kr_s, page_table))
        xs = xs + _merge(yconv_s, yattn_s, g_out[l], w_out[l])
        xs = xs + _hier_moe(_rmsnorm(xs, g_ffn[l]), w_router_group[l], b_router_group[l],
                            w_router_expert[l], b_router_expert[l], w_gate[l], w_up[l], w_down[l])

        ckv_p_l.append(ckv_p)
        kr_p_l.append(kr_p)
        conv_p_l.append(buf_p)
        ckv_s_l.append(ckv_s)
        kr_s_l.append(kr_s)
        conv_s_l.append(buf_s)

    return (xp, xs, jnp.stack(ckv_p_l), jnp.stack(kr_p_l), jnp.stack(conv_p_l),
            jnp.stack(ckv_s_l), jnp.stack(kr_s_l), jnp.stack(conv_s_l))
```

```python
import numpy as np
from contextlib import ExitStack
import concourse.bass as bass
import concourse.mybir as mybir
from concourse.bass_utils import run_bass_kernel_spmd

F32 = mybir.dt.float32
BF16 = mybir.dt.bfloat16
I32 = mybir.dt.int32
ALU = mybir.AluOpType
AF = mybir.ActivationFunctionType
AX = mybir.AxisListType

NCORES = 8
D = 1024
SEQ = 2048
NS = 32
NTOK = SEQ + NS
CW = 512
QR = 256
KVR = 128
RD = 32
NH = 8
ND = 64
VD = 64
PW = 3 * CW + QR + KVR + RD
NE = 32
DE = 256
EPS = 1e-6
DEBUG = False
NPAGE = 128
PAGE = 128
NPOOL = 5120
PAST = NPAGE * PAGE
SCALE = float((ND + RD) ** -0.5)


class Rec:
    __slots__ = ("eng", "fn", "deps", "dma", "needed", "sem", "val", "seq", "stream", "pdeps")

    def __init__(self, eng, fn, deps, dma):
        self.eng, self.fn, self.deps, self.dma = eng, fn, deps, dma
        self.needed = False
        self.sem = None
        self.val = 0
        self.seq = -1
        self.stream = 0
        self.pdeps = ()


class Sched:
    ENGS = ("sync", "gpsimd", "vector", "scalar", "tensor")

    def __init__(self):
        self.ops = {e: [] for e in self.ENGS}
        self.buf = {}
        self.streams = {}
        self.mute = False
        self.seq = 0
        self.cur_stream = 0
        self.win = None

    def op(self, eng, fn, reads=(), writes=(), dma=None):
        if self.mute:
            return Rec(None, None, [], None)
        deps = []
        seen = set()

        def add(r):
            if r is not None and id(r) not in seen:
                seen.add(id(r))
                deps.append(r)

        for k in reads:
            st = self.buf.get(k)
            if st is not None:
                add(st[0])
        for k in writes:
            st = self.buf.get(k)
            if st is not None:
                add(st[0])
                for r in st[1]:
                    add(r)
        pdeps = ()
        if eng == "tensor":
            pdeps = [d for d in deps if d.eng == "tensor" and not d.dma]
            deps = [d for d in deps if d.eng != "tensor" or d.dma]
        rec = Rec(eng, fn, deps, dma)
        rec.pdeps = pdeps
        rec.seq = self.seq
        rec.stream = self.cur_stream
        self.seq += 1
        for d in deps:
            d.needed = True
        self.ops[eng].append(rec)
        if self.win is not None:
            self.win.append(rec)
        for k in reads:
            self.buf.setdefault(k, [None, []])[1].append(rec)
        for k in writes:
            self.buf[k] = [rec, []]
        if dma is not None:
            self.streams.setdefault(dma, 0)
        return rec

    def window_begin(self):
        self.win = []

    def window_end(self):
        win, self.win = self.win, None
        if not win:
            return
        inwin = set(id(r) for r in win)
        first_seq = win[0].seq
        for e in self.ENGS:
            self.ops[e] = [r for r in self.ops[e] if id(r) not in inwin]
        by_stream = {}
        for r in win:
            by_stream.setdefault(r.stream, []).append(r)
        keys = sorted(by_stream)
        ptr = {k: 0 for k in keys}
        pe_order = [r for r in win if r.eng == "tensor"]
        pe_next = 0
        done = set()
        out = []
        turn = 0
        total = len(win)

        def ready(r):
            for d in r.deps:
                if id(d) in inwin and id(d) not in done:
                    return False
            for d in r.pdeps:
                if id(d) in inwin and id(d) not in done:
                    return False
            return True
        while len(out) < total:
            picked = None
            for off in range(len(keys)):
                k = keys[(turn + off) % len(keys)]
                if ptr[k] < len(by_stream[k]) and ready(by_stream[k][ptr[k]]):
                    picked = k
                    break
            assert picked is not None, "window_end: no ready op (dependency cycle?)"
            r = by_stream[picked][ptr[picked]]
            ptr[picked] += 1
            if r.eng == "tensor":
                pe_next += 1
            done.add(id(r))
            out.append(r)
            turn = (keys.index(picked) + 1) % len(keys)
        for r in out:
            self.ops[r.eng].append(r)

    def barrier(self):
        lasts = []
        last_dma = {}
        for e in self.ENGS:
            last_c = None
            for r in self.ops[e]:
                if r.dma is not None:
                    last_dma[r.dma] = r
                elif r.fn is not None:
                    last_c = r
            if last_c is not None:
                lasts.append(last_c)
        lasts += list(last_dma.values())
        for d in lasts:
            d.needed = True
        for e in self.ENGS:
            rec = Rec(e, None, list(lasts), None)
            self.ops[e].append(rec)
        self.buf = {}

    def dma(self, eng, stream, out, in_, reads=(), writes=(), **kw):
        return self.op(eng, lambda e: e.dma_start(out=out, in_=in_, **kw), reads, writes, dma=stream)

    def emit(self, nc, es):
        sems = {}
        for e in self.ENGS:
            sems[e] = es.enter_context(nc.semaphore("p_" + e))
        dsem = {}
        for s in self.streams:
            dsem[s] = es.enter_context(nc.semaphore("d_" + s))
        cnt = {s: 0 for s in self.streams}
        for e in self.ENGS:
            c = 0
            for r in self.ops[e]:
                if r.dma is not None:
                    cnt[r.dma] += 16
                    r.sem, r.val = dsem[r.dma], cnt[r.dma]
                elif r.needed:
                    c += 1
                    r.sem, r.val = sems[e], c
        block = es.enter_context(nc.Block())

        def run(eng_name):
            def body(eng):
                waited = {}
                for r in self.ops[eng_name]:
                    need = {}
                    for d in r.deps:
                        key = id(d.sem)
                        if key not in need or need[key][1] < d.val:
                            need[key] = (d.sem, d.val)
                    for key, (sem_, val_) in need.items():
                        if waited.get(key, 0) < val_:
                            eng.wait_ge(sem_, val_)
                            waited[key] = val_
                    if r.fn is None:
                        continue
                    ins = r.fn(eng)
                    if r.dma is not None:
                        ins.then_inc(r.sem, 16)
                    elif r.needed:
                        ins.then_inc(r.sem, 1)
            return body

        block.sync(run("sync"))
        block.gpsimd(run("gpsimd"))
        block.vector(run("vector"))
        block.scalar(run("scalar"))
        block.tensor(run("tensor"))


def build_program():
    nc = bass.Bass("TRN2", target_bir_lowering=False)
    S = Sched()
    es = ExitStack()

    def din(name, shape, dt=F32):
        return nc.dram_tensor(name, list(shape), dt, kind="ExternalInput").ap()

    def dout(name, shape, dt=F32):
        return nc.dram_tensor(name, list(shape), dt, kind="ExternalOutput").ap()

    x_p = din("x_p", [SEQ, D])
    x_s = din("x_s", [NS, D])
    st_conv = din("st_conv", [4, 2, CW])
    cache_ckv = din("cache_ckv", [NPOOL, PAGE, KVR])
    cache_kr = din("cache_kr", [NPOOL, PAGE, RD])
    ptab = din("ptab", [4, NPAGE], I32)
    g_mix = din("g_mix", [1, D])
    w_in = din("w_in", [D, PW])
    conv_w = din("conv_w", [3, CW])
    conv_b = din("conv_b", [1, CW])
    g_q_lat = din("g_q_lat", [1, QR])
    w_uq = din("w_uq", [QR, NH * (ND + RD)])
    g_kv_lat = din("g_kv_lat", [1, KVR])
    w_uk = din("w_uk", [KVR, NH * ND])
    w_uv = din("w_uv", [KVR, NH * VD])
    g_q_nope = din("g_q_nope", [1, ND])
    g_q_rope = din("g_q_rope", [1, RD])
    g_k_nope = din("g_k_nope", [1, ND])
    g_k_rope = din("g_k_rope", [1, RD])
    g_out = din("g_out", [1, D])
    w_out = din("w_out", [D, D])
    g_ffn = din("g_ffn", [1, D])
    w_rg = din("w_rg", [D, 4])
    b_rg = din("b_rg", [1, 4])
    w_re = din("w_re", [D, NE])
    b_re = din("b_re", [1, NE])
    w_gate = din("w_gate", [NE * 128, 8 * DE])
    w_up = din("w_up", [NE * 128, 8 * DE])
    w_down = din("w_down", [NE * 128, 2 * D])

    y_p = dout("y_p", [SEQ, D])
    y_s = dout("y_s", [NS, D])
    o_ckv_p = dout("o_ckv_p", [SEQ, KVR])
    o_kr_p = dout("o_kr_p", [SEQ, RD])
    o_conv_p = dout("o_conv_p", [2, CW])
    o_ckv_s = dout("o_ckv_s", [NS, KVR])
    o_kr_s = dout("o_kr_s", [NS, RD])
    o_conv_s = dout("o_conv_s", [4, 2, CW])

    def sb(name, shape, dt=F32):
        return es.enter_context(nc.sbuf_tensor(name, list(shape), dt))

    def dbg(name, ap, keys):
        if not DEBUG:
            return
        shp = list(ap.shape)
        o = nc.dram_tensor("dbg_" + name, shp, ap.dtype, kind="ExternalOutput").ap()
        out_recs.append(S.dma("sync", "st", o, ap, reads=keys))

    out_recs = []

    ARENA_BYTES = 61440
    arena_t = sb("arena", [128, ARENA_BYTES // 4], F32)
    arena_off = [0]

    def arena_reset():
        arena_off[0] = 0

    def wk(name, shape, dt=F32):
        esz = 2 if dt == BF16 else 4
        n = 1
        for d in shape[1:]:
            n *= d
        nb = (n * esz + 31) // 32 * 32
        off = arena_off[0]
        assert off + nb <= ARENA_BYTES, (name, off, nb)
        arena_off[0] = off + nb
        ap = arena_t[:, off // 4:(off + nb) // 4]
        if dt != F32:
            ap = ap.bitcast(dt)
        ap = ap[:, 0:n]
        if len(shape) == 3:
            ap = ap.rearrange("p (a b) -> p a b", a=shape[1])
        elif len(shape) == 4:
            ap = ap.rearrange("p (a b c) -> p a b c", a=shape[1], b=shape[2])
        return ap

    ps_big_t = es.enter_context(nc.psum_tensor("ps_big", [128, 4096], F32))
    ps_big = ps_big_t[:, :]
    ps = [ps_big[:, i * 512:(i + 1) * 512] for i in range(8)]

    def psb(i):
        return ps[i].bitcast(BF16)

    ident = sb("ident", [128, 128], BF16)
    tri = sb("tri", [128, 128], BF16)
    bconv = sb("bconv", [128, 128], BF16)
    onesb = sb("onesb", [128, 128], BF16)
    zero_c = sb("zero_c", [128, 1], F32)
    eps_c = sb("eps_c", [128, 1], F32)
    gmix_b = sb("gmix_b", [128, D], F32)
    gql_b = sb("gql_b", [128, QR], F32)
    gkv_b = sb("gkv_b", [128, KVR], F32)
    gq_b = sb("gq_b", [128, ND + RD], F32)
    gkn_b = sb("gkn_b", [128, ND], F32)
    gkr_b = sb("gkr_b", [128, RD], F32)
    convw_c = sb("convw_c", [128, 3, 4], F32)
    convb_c = sb("convb_c", [128, 4], F32)
    gout_c = sb("gout_c", [128, 8], F32)
    gattn_c = sb("gattn_c", [64, 8], F32)
    gkn_c = sb("gkn_c", [64, 1], F32)
    cosT = sb("cosT", [128, 17, 16], F32)
    sinT = sb("sinT", [128, 17, 16], F32)

    ckv_s_b = sb("ckv_s_b", [NS, KVR + 1], BF16)
    ya_s = sb("ya_s", [128, 4, NS], BF16)
    arenaX = sb("arenaX", [128, 8 * NTOK], BF16)
    w_in_sb = arenaX[:, 0:8 * PW].rearrange("p (k n) -> p k n", k=8)
    w_uq_sb = sb("w_uq_sb", [128, 2, NH * (ND + RD)], BF16)
    w_uk_sb = sb("w_uk_sb", [128, NH * ND], BF16)
    w_uv_sb = sb("w_uv_sb", [128, NH * VD], BF16)

    QT = sb("QT", [128, NH, NTOK], BF16)
    arenaK = sb("arenaK", [128, NH * NTOK + 16 * NH * (VD + 1) + 4 * NTOK], BF16)
    KT = arenaK[:, 0:NH * NTOK].rearrange("p (h t) -> p h t", h=NH)
    _o = NH * NTOK
    Vaug = arenaK[:, _o:_o + 16 * NH * (VD + 1)].rearrange("p (j h d) -> p j h d", j=16, h=NH)
    _o += 16 * NH * (VD + 1)
    yconvT = arenaK[:, _o:_o + 4 * NTOK].rearrange("p (c t) -> p c t", c=4)

    xin = [wk("xin%d" % i, [128, D], F32) for i in range(2)]
    junk = wk("junk", [128, D], BF16)
    hbf = [wk("hbf%d" % i, [128, D], BF16) for i in range(2)]
    hT = [wk("hT%d" % i, [128, 8, 256], BF16) for i in range(2)]
    st1 = [wk("st1_%d" % i, [128, 96], F32) for i in range(2)]
    qln = wk("qln", [128, QR], BF16)
    qlnT = wk("qlnT", [128, 2, 128], BF16)
    ckv_f = [wk("ckv_f%d" % i, [128, KVR], F32) for i in range(2)]
    ckv_b = wk("ckv_b", [128, KVR], BF16)
    ckvT = wk("ckvT", [128, 128], BF16)
    krn = wk("krn", [128, RD], F32)
    kr_f = [wk("kr_f%d" % i, [128, RD], F32) for i in range(2)]
    rtmp = wk("rtmp", [128, 4, 16], F32)
    qsq = wk("qsq", [128, 768], F32)
    ksq = wk("ksq", [128, 512], F32)
    qn = wk("qn", [128, NH, ND + RD], F32)
    qrt = wk("qrt", [128, 4, NH, 16], F32)
    Qb = wk("Qb", [128, NH, ND + RD], BF16)
    Kb = wk("Kb", [128, NH, ND + RD], BF16)
    kn_t = wk("kn_t", [128, NH, ND], F32)
    gc_sb = wk("gc_sb", [128, 256], F32)
    ubuf = [wk("ubuf%d" % c, [128, 2 + 256], F32) for c in range(4)]
    usam = [wk("usam%d" % c, [128, 4, 10], F32) for c in range(4)]
    vconv = wk("vconv", [128, 256], F32)
    yconv = wk("yconv", [128, 256], F32)
    csq = wk("csq", [128, 256], BF16)
    crs = wk("crs", [128, 256], F32)

    S.op("gpsimd", lambda e: e.memset(ident[:], 0.0), writes=["ident"])
    S.op("gpsimd", lambda e: e.affine_select(out=ident[:], in_=ident[:], pattern=[[-1, 128]],
                                            compare_op=ALU.not_equal, fill=1.0, base=0,
                                            channel_multiplier=1), reads=["ident"], writes=["ident"])
    S.op("gpsimd", lambda e: e.memset(onesb[:], 1.0), writes=["onesb"])
    S.op("gpsimd", lambda e: e.affine_select(out=tri[:], in_=onesb[:], pattern=[[1, 128]],
                                            compare_op=ALU.is_ge, fill=0.0, base=0,
                                            channel_multiplier=-1), reads=["onesb"], writes=["tri"])
    S.op("gpsimd", lambda e: e.memset(bconv[:], 0.0), writes=["bconv"])
    S.op("gpsimd", lambda e: e.memset(bconv[0:64, 0:64], 1.0 / 64), reads=["bconv"], writes=["bconv"])
    S.op("gpsimd", lambda e: e.memset(bconv[64:128, 64:128], 1.0 / 64), reads=["bconv"], writes=["bconv"])
    S.op("gpsimd", lambda e: e.memset(zero_c[:], 0.0), writes=["zero_c"])
    S.op("gpsimd", lambda e: e.memset(eps_c[:], EPS), writes=["eps_c"])
    S.op("gpsimd", lambda e: e.memset(Vaug[:, :, :, VD:VD + 1], 1.0), writes=["Vaug_ones"])
    for c in range(4):
        S.op("gpsimd", lambda e, c=c: e.memset(ubuf[c][:, 0:2], 0.0), writes=[("ubuf", c)])

    def bload(dst, src, n, key):
        S.dma("sync", "ld", dst[:], src[0:1, :].to_broadcast([128, n]), writes=[key])

    bload(gmix_b, g_mix, D, "gmix_b")
    bload(gql_b, g_q_lat, QR, "gql_b")
    bload(gkv_b, g_kv_lat, KVR, "gkv_b")
    bload(gkn_b, g_k_nope, ND, "gkn_b")
    bload(gkr_b, g_k_rope, RD, "gkr_b")
    S.dma("sync", "ld", gq_b[:, 0:ND], g_q_nope[0:1, :].to_broadcast([128, ND]), writes=["gq_b0"])
    S.dma("sync", "ld", gq_b[:, ND:ND + RD], g_q_rope[0:1, :].to_broadcast([128, RD]), writes=["gq_b1"])
    if True:
        for k in range(3):
            S.dma("sync", "ld", convw_c[:, k, :], conv_w[k:k + 1, :].rearrange("o (c p) -> p (o c)", p=128),
                  writes=[("convw_c", k)], allow_slow_non_contiguous=True)
        S.dma("sync", "ld", convb_c[:], conv_b.rearrange("o (c p) -> p (o c)", p=128), writes=["convb_c"], allow_slow_non_contiguous=True)
        S.dma("sync", "ld", gout_c[:], g_out.rearrange("o (c p) -> p (o c)", p=128), writes=["gout_c"], allow_slow_non_contiguous=True)
        S.dma("sync", "ld", gattn_c[:], g_out[:, CW:D].rearrange("o (h p) -> p (o h)", p=64), writes=["gattn_c"], allow_slow_non_contiguous=True)
        S.dma("sync", "ld", gkn_c[:], g_k_nope.rearrange("o p -> p o"), writes=["gkn_c"], allow_slow_non_contiguous=True)
        for c in range(4):
            for sq_ in range(4):
                S.dma("sync", "ld", usam[c][:, sq_, 0:2],
                      st_conv[sq_, :, c * 128:(c + 1) * 128].rearrange("t p -> p t"), writes=[("usam", c, sq_)],
                      allow_slow_non_contiguous=True)

    w_in_v = w_in.rearrange("(k p) n -> p k n", p=128)
    for k in range(8):
        S.dma("gpsimd", "wg", w_in_sb[:, k, :], w_in_v[:, k, :], writes=[("w_in", k)])
    S.dma("gpsimd", "wg", w_uq_sb[:], w_uq.rearrange("(k p) n -> p k n", p=128), writes=["w_uq"])
    S.dma("gpsimd", "wg", w_uk_sb[:], w_uk[:, :], writes=["w_uk"])
    S.dma("gpsimd", "wg", w_uv_sb[:], w_uv[:, :], writes=["w_uv"])

    wgb = nc.dram_tensor("wgb", [NE * 128, 8 * DE], BF16, kind="Internal").ap()
    wub = nc.dram_tensor("wub", [NE * 128, 8 * DE], BF16, kind="Internal").ap()
    wdb = nc.dram_tensor("wdb", [NE * 128, 2 * D], BF16, kind="Internal").ap()
    conv_jobs = [(srcw, dstw, q_) for srcw, dstw in ((w_gate, wgb), (w_up, wub), (w_down, wdb)) for q_ in range(8)]

    def issue_conv(n):
        for _ in range(n):
            if conv_jobs:
                srcw, dstw, q_ = conv_jobs.pop(0)
                S.dma("gpsimd", "cv", dstw[q_ * 512:(q_ + 1) * 512, :], srcw[q_ * 512:(q_ + 1) * 512, :], writes=[("wconv", id(dstw), q_)])

    MARK_SETUP = arena_off[0]
    posf = wk("posf", [128, 17], F32)
    posi = wk("posi", [128, 17], I32)
    invf = wk("invf", [128, 16], F32)
    ang = wk("ang", [128, 17, 16], F32)
    ang2 = wk("ang2", [128, 17, 16], F32)
    S.op("gpsimd", lambda e: e.iota(posi[:, 0:16], pattern=[[128, 16]], base=0, channel_multiplier=1),
         writes=["posi0"])
    S.op("gpsimd", lambda e: e.iota(posi[:, 16:17], pattern=[[0, 1]], base=0, channel_multiplier=1),
         writes=["posi1"])
    S.op("vector", lambda e: e.tensor_single_scalar(out=posi[:, 16:17], in_=posi[:, 16:17], scalar=7,
                                                   op=ALU.bitwise_and), reads=["posi1"], writes=["posi1"])
    S.op("vector", lambda e: e.tensor_single_scalar(out=posi[:, 16:17], in_=posi[:, 16:17], scalar=PAST,
                                                   op=ALU.add), reads=["posi1"], writes=["posi1"])
    S.op("vector", lambda e: e.tensor_copy(out=posf[:], in_=posi[:]), reads=["posi0", "posi1"], writes=["posf"])
    invf_np = (np.float32(10000.0) ** (-(np.arange(16, dtype=np.float32) / np.float32(16)))).astype(np.float32)
    for i in range(16):
        S.op("gpsimd", lambda e, i=i: e.memset(invf[:, i:i + 1], float(invf_np[i])), writes=[("invf", i)])
    invk = [("invf", i) for i in range(16)]
    for t in range(17):
        S.op("vector", lambda e, t=t: e.tensor_scalar(out=ang[:, t, :], in0=invf[:], scalar1=posf[:, t:t + 1],
                                                      scalar2=None, op0=ALU.mult),
             reads=invk + ["posf"], writes=[("ang", t)])
    angkeys = [("ang", t) for t in range(17)]
    PI = float(np.pi)
    C1 = 6.28125
    C2 = float(2 * np.pi - 6.28125)
    angk = wk("angk", [128, 17, 16], I32)
    angkf = wk("angkf", [128, 17, 16], F32)
    angm = wk("angm", [128, 17, 16], F32)
    S.op("vector", lambda e: e.tensor_scalar(out=ang2[:], in0=ang[:], scalar1=float(1.0 / (2 * np.pi)), scalar2=None,
                                             op0=ALU.mult), reads=angkeys, writes=["ang2"])
    S.op("vector", lambda e: e.tensor_copy(out=angk[:], in_=ang2[:]), reads=["ang2"], writes=["angk"])
    S.op("vector", lambda e: e.tensor_copy(out=angkf[:], in_=angk[:]), reads=["angk"], writes=["angkf"])
    S.op("vector", lambda e: e.scalar_tensor_tensor(out=ang2[:], in0=angkf[:], scalar=-C1, in1=ang[:],
                                                    op0=ALU.mult, op1=ALU.add), reads=["angkf"] + angkeys, writes=["ang2"])
    S.op("vector", lambda e: e.scalar_tensor_tensor(out=ang2[:], in0=angkf[:], scalar=-C2, in1=ang2[:],
                                                    op0=ALU.mult, op1=ALU.add), reads=["angkf", "ang2"], writes=["ang2"])

    def wrap_and_sin(dst, key):
        S.op("vector", lambda e: e.tensor_single_scalar(out=angm[:], in_=ang2[:], scalar=PI, op=ALU.is_gt),
             reads=["ang2"], writes=["angm"])
        S.op("vector", lambda e: e.scalar_tensor_tensor(out=ang2[:], in0=angm[:], scalar=-2 * PI, in1=ang2[:],
                                                        op0=ALU.mult, op1=ALU.add), reads=["angm", "ang2"], writes=["ang2"])
        S.op("vector", lambda e: e.tensor_scalar(out=angm[:], in0=ang2[:], scalar1=-PI, scalar2=PI,
                                                 op0=ALU.max, op1=ALU.min), reads=["ang2"], writes=["angm"])
        S.op("scalar", lambda e: e.activation(out=dst[:], in_=angm[:], func=AF.Sin), reads=["angm"], writes=[key])

    wrap_and_sin(sinT, "sinT")
    S.op("vector", lambda e: e.tensor_scalar(out=ang2[:], in0=ang2[:], scalar1=PI / 2, scalar2=None, op0=ALU.add),
         reads=["ang2", "sinT"], writes=["ang2"])
    wrap_and_sin(cosT, "cosT")

    S.barrier()
    arena_off[0] = MARK_SETUP
    gc_sbL = [gc_sb, wk("gc_sb2", [128, 256], F32)]
    vconvL = [vconv, wk("vconv2", [128, 256], F32)]
    yconvL = [yconv, wk("yconv2", [128, 256], F32)]
    csqL = [csq, wk("csq2", [128, 256], BF16)]
    crsL = [crs, wk("crs2", [128, 256], F32)]
    qlnTL = [qlnT, wk("qlnT2", [128, 2, 128], BF16)]
    ckvTL = [ckvT, wk("ckvT2", [128, 128], BF16)]
    def rstd_from_ss(ss_ap, out_ap, n, T, keys_r, keys_w, tmp_ap, tmpkey):
        S.op("scalar", lambda e: e.activation(out=tmp_ap, in_=ss_ap, func=AF.Ln, bias=eps_c[0:T, :], scale=1.0 / n),
             reads=keys_r + ["eps_c"], writes=[tmpkey])
        S.op("scalar", lambda e: e.activation(out=out_ap, in_=tmp_ap, func=AF.Exp, scale=-0.5),
             reads=[tmpkey], writes=keys_w)

    def rope(dst1, dst2, x1, x2, cos, sin, tmp, rkeys, wkeys, tkey):
        S.op("vector", lambda e: e.tensor_tensor(out=tmp[0], in0=x1, in1=cos, op=ALU.mult), reads=rkeys, writes=[(tkey, 0)])
        S.op("vector", lambda e: e.tensor_tensor(out=tmp[1], in0=x2, in1=sin, op=ALU.mult), reads=rkeys, writes=[(tkey, 1)])
        S.op("vector", lambda e: e.tensor_tensor(out=tmp[2], in0=x1, in1=sin, op=ALU.mult), reads=rkeys, writes=[(tkey, 2)])
        S.op("vector", lambda e: e.tensor_tensor(out=tmp[3], in0=x2, in1=cos, op=ALU.mult), reads=rkeys, writes=[(tkey, 3)])
        S.op("vector", lambda e: e.tensor_tensor(out=dst1, in0=tmp[0], in1=tmp[1], op=ALU.subtract),
             reads=[(tkey, 0), (tkey, 1)], writes=[wkeys[0]])
        S.op("vector", lambda e: e.tensor_tensor(out=dst2, in0=tmp[2], in1=tmp[3], op=ALU.add),
             reads=[(tkey, 2), (tkey, 3)], writes=[wkeys[1]])

    def token_tile(ti, part):
        S.mute = (part == "B")
        T = 128 if ti < 16 else NS
        tok0 = ti * 128
        sl = ti % 2
        src = x_p[tok0:tok0 + 128, :] if ti < 16 else x_s[:, :]
        X, H, ST = xin[sl], hbf[sl], st1[sl]
        hTc = hT[(ti // 2) % 2]
        col0 = (ti % 2) * 128
        if ti in (16, 0):
            S.dma("sync", "ldx%d" % sl, X[0:T, :], src, writes=[("xin", sl)])
        if ti < 15:
            nsl = (ti + 1) % 2
            S.dma("sync", "ldx%d" % nsl, xin[nsl][:, :], x_p[(ti + 1) * 128:(ti + 2) * 128, :], writes=[("xin", nsl)])
        S.op("scalar", lambda e: e.activation(out=junk[0:T, :], in_=X[0:T, :], func=AF.Square, accum_out=ST[0:T, 0:1]),
             reads=[("xin", sl)], writes=[("st", sl, 0)])
        rstd_from_ss(ST[0:T, 0:1], ST[0:T, 1:2], D, T, [("st", sl, 0)], [("st", sl, 1)], ST[0:T, 2:3], ("st", sl, 2))
        S.op("vector", lambda e: e.scalar_tensor_tensor(out=H[0:T, :], in0=X[0:T, :], scalar=ST[0:T, 1:2],
                                                        in1=gmix_b[0:T, :], op0=ALU.mult, op1=ALU.mult),
             reads=[("xin", sl), ("st", sl, 1), "gmix_b"], writes=[("hbf", sl)])
        pT = psb(0)

        def tr_h(e):
            ins = None
            for k in range(8):
                ins = e.transpose(out=pT[:, k * 128:k * 128 + T], in_=H[0:T, k * 128:(k + 1) * 128], identity=ident[0:T, 0:T])
            return ins
        S.op("tensor", tr_h, reads=[("hbf", sl), "ident"], writes=["ps0"])
        S.op("scalar", lambda e: e.copy(out=hTc[:, :, col0:col0 + T],
                                        in_=pT[:, 0:1024].rearrange("p (k t) -> p k t", k=8)[:, :, 0:T]),
             reads=["ps0"], writes=[("hT", (ti // 2) % 2, ti % 2)])
        hkey = ("hT", (ti // 2) % 2, ti % 2)

        def mm_small(e):
            ins = None
            for k in range(8):
                ins = e.matmul(ps[1][0:T, 0:416], lhsT=hTc[:, k, col0:col0 + T], rhs=w_in_sb[:, k, 3 * CW:PW],
                               start=(k == 0), stop=(k == 7))
            return ins
        S.op("tensor", mm_small, reads=[hkey] + [("w_in", k) for k in range(8)], writes=["ps1"])
        zs = ps[1]
        for j, (a, b) in enumerate(((0, QR), (QR, QR + KVR), (QR + KVR, QR + KVR + RD))):
            S.op("scalar", lambda e, a=a, b=b, j=j: e.activation(out=junk[0:T, a:b], in_=zs[0:T, a:b], func=AF.Square,
                                                                 accum_out=ST[0:T, 4 + j:5 + j]),
                 reads=["ps1"], writes=[("st", sl, 4 + j)])
        rstd_from_ss(ST[0:T, 4:5], ST[0:T, 8:9], QR, T, [("st", sl, 4)], [("st", sl, 8)], ST[0:T, 12:13], ("st", sl, 12))
        rstd_from_ss(ST[0:T, 5:6], ST[0:T, 9:10], KVR, T, [("st", sl, 5)], [("st", sl, 9)], ST[0:T, 13:14], ("st", sl, 13))
        rstd_from_ss(ST[0:T, 6:7], ST[0:T, 10:11], RD, T, [("st", sl, 6)], [("st", sl, 10)], ST[0:T, 14:15], ("st", sl, 14))
        S.op("vector", lambda e: e.scalar_tensor_tensor(out=qln[0:T, :], in0=zs[0:T, 0:QR], scalar=ST[0:T, 8:9],
                                                        in1=gql_b[0:T, :], op0=ALU.mult, op1=ALU.mult),
             reads=["ps1", ("st", sl, 8), "gql_b"], writes=["qln"])
        CK = ckv_f[sl]
        S.op("vector", lambda e: e.scalar_tensor_tensor(out=CK[0:T, :], in0=zs[0:T, QR:QR + KVR], scalar=ST[0:T, 9:10],
                                                        in1=gkv_b[0:T, :], op0=ALU.mult, op1=ALU.mult),
             reads=["ps1", ("st", sl, 9), "gkv_b"], writes=[("ckv_f", sl)])
        S.op("vector", lambda e: e.scalar_tensor_tensor(out=krn[0:T, :], in0=zs[0:T, QR + KVR:QR + KVR + RD],
                                                        scalar=ST[0:T, 10:11], in1=gkr_b[0:T, :], op0=ALU.mult, op1=ALU.mult),
             reads=["ps1", ("st", sl, 10), "gkr_b"], writes=["krn"])
        dst_ckv = o_ckv_p[tok0:tok0 + 128, :] if ti < 16 else o_ckv_s[:, :]
        out_recs.append(S.dma("sync", "stc%d" % sl, dst_ckv, CK[0:T, :], reads=[("ckv_f", sl)]))
        S.op("gpsimd", lambda e: e.tensor_copy(out=ckv_b[0:T, :], in_=CK[0:T, :]), reads=[("ckv_f", sl)], writes=["ckv_b"])
        if ti == 16:
            S.op("gpsimd", lambda e: e.tensor_copy(out=ckv_s_b[0:T, 0:KVR], in_=CK[0:T, :]), reads=[("ckv_f", sl)], writes=["ckv_s_b"])
            S.op("gpsimd", lambda e: e.memset(ckv_s_b[0:T, KVR:KVR + 1], 1.0), writes=["ckv_s_b1"])
        KR = kr_f[sl]
        rope(KR[0:T, 0:16], KR[0:T, 16:32], krn[0:T, 0:16], krn[0:T, 16:32], cosT[0:T, ti, :], sinT[0:T, ti, :],
             [rtmp[0:T, i, :] for i in range(4)], ["krn", "cosT", "sinT"], [("kr_f", sl, 0), ("kr_f", sl, 1)], "rtmp")
        dst_kr = o_kr_p[tok0:tok0 + 128, :] if ti < 16 else o_kr_s[:, :]
        out_recs.append(S.dma("sync", "stk%d" % sl, dst_kr, KR[0:T, :], reads=[("kr_f", sl, 0), ("kr_f", sl, 1)]))

        pT2 = psb(2)

        def tr_q(e):
            e.transpose(out=pT2[:, 0:T], in_=qln[0:T, 0:128], identity=ident[0:T, 0:T])
            e.transpose(out=pT2[:, 128:128 + T], in_=qln[0:T, 128:256], identity=ident[0:T, 0:T])
            return e.transpose(out=pT2[:, 256:256 + T], in_=ckv_b[0:T, :], identity=ident[0:T, 0:T])
        S.op("tensor", tr_q, reads=["qln", "ckv_b", "ident"], writes=["ps2"])
        qlnT, ckvT = qlnTL[sl], ckvTL[sl]
        S.op("vector", lambda e: e.tensor_copy(out=qlnT[:, :, 0:T],
                                               in_=pT2[:, 0:256].rearrange("p (k t) -> p k t", k=2)[:, :, 0:T]),
             reads=["ps2"], writes=[("qlnT", sl)])
        S.op("vector", lambda e: e.tensor_copy(out=ckvT[:, 0:T], in_=pT2[:, 256:256 + T]), reads=["ps2"], writes=[("ckvT", sl)])
        S.mute = (part == "A")

        def mm_q(e):
            ins = None
            for half in range(2):
                for k in range(2):
                    ins = e.matmul(ps[3 + half][0:T, 0:384], lhsT=qlnT[:, k, 0:T], rhs=w_uq_sb[:, k, half * 384:(half + 1) * 384],
                                   start=(k == 0), stop=(k == 1))
            return ins
        S.op("tensor", mm_q, reads=[("qlnT", sl), "w_uq"], writes=["ps3", "ps4"])

        def mm_kv(e):
            e.matmul(ps[5][0:T, :], lhsT=ckvT[:, 0:T], rhs=w_uk_sb[:, :], start=True, stop=True)
            return e.matmul(ps[6][0:T, :], lhsT=ckvT[:, 0:T], rhs=w_uv_sb[:, :], start=True, stop=True)
        S.op("tensor", mm_kv, reads=[("ckvT", sl), "w_uk", "w_uv"], writes=["ps5", "ps6"])

        for half in range(2):
            S.op("scalar", lambda e, half=half: e.activation(out=qsq[0:T, half * 384:(half + 1) * 384],
                                                             in_=ps[3 + half][0:T, 0:384], func=AF.Square),
                 reads=["ps%d" % (3 + half)], writes=[("qsq", half)])
        qsq_v = qsq[:, :].rearrange("p (h d) -> p h d", h=NH)
        S.op("vector", lambda e: e.reduce_sum(out=ST[0:T, 16:24], in_=qsq_v[0:T, :, 0:ND], axis=AX.X),
             reads=[("qsq", 0), ("qsq", 1)], writes=[("st", sl, 16)])
        S.op("vector", lambda e: e.reduce_sum(out=ST[0:T, 24:32], in_=qsq_v[0:T, :, ND:ND + RD], axis=AX.X),
             reads=[("qsq", 0), ("qsq", 1)], writes=[("st", sl, 24)])
        rstd_from_ss(ST[0:T, 16:24], ST[0:T, 32:40], ND, T, [("st", sl, 16)], [("st", sl, 32)], ST[0:T, 48:56], ("st", sl, 48))
        rstd_from_ss(ST[0:T, 24:32], ST[0:T, 40:48], RD, T, [("st", sl, 24)], [("st", sl, 40)], ST[0:T, 56:64], ("st", sl, 56))
        for half in range(2):
            hs = slice(half * 4, half * 4 + 4)
            qps = ps[3 + half][0:T, 0:384].rearrange("p (h d) -> p h d", h=4)
            S.op("vector", lambda e, hs=hs, qps=qps: e.tensor_tensor(
                out=qn[0:T, hs, 0:ND], in0=qps[:, :, 0:ND],
                in1=ST[0:T, 32 + hs.start:32 + hs.stop].unsqueeze(2).to_broadcast([T, 4, ND]), op=ALU.mult),
                reads=["ps%d" % (3 + half), ("st", sl, 32)], writes=[("qn", half, 0)])
            S.op("vector", lambda e, hs=hs, qps=qps: e.tensor_tensor(
                out=qn[0:T, hs, ND:ND + RD], in0=qps[:, :, ND:ND + RD],
                in1=ST[0:T, 40 + hs.start:40 + hs.stop].unsqueeze(2).to_broadcast([T, 4, RD]), op=ALU.mult),
                reads=["ps%d" % (3 + half), ("st", sl, 40)], writes=[("qn", half, 1)])
        qnk = [("qn", h2, j) for h2 in range(2) for j in range(2)]
        S.op("vector", lambda e: e.tensor_tensor(out=qn[0:T, :, :], in0=qn[0:T, :, :],
                                                 in1=gq_b[0:T, :].unsqueeze(1).to_broadcast([T, NH, ND + RD]), op=ALU.mult),
             reads=qnk + ["gq_b0", "gq_b1"], writes=["qn_g"])
        S.op("gpsimd", lambda e: e.tensor_copy(out=Qb[0:T, :, 0:ND], in_=qn[0:T, :, 0:ND]), reads=["qn_g"], writes=["Qb_n"])
        cosb = cosT[0:T, ti, :].unsqueeze(1).to_broadcast([T, NH, 16])
        sinb = sinT[0:T, ti, :].unsqueeze(1).to_broadcast([T, NH, 16])
        rope(Qb[0:T, :, ND:ND + 16], Qb[0:T, :, ND + 16:ND + 32], qn[0:T, :, ND:ND + 16], qn[0:T, :, ND + 16:ND + 32],
             cosb, sinb, [qrt[0:T, i, :, :] for i in range(4)], ["qn_g", "cosT", "sinT"], ["Qb_r0", "Qb_r1"], "qrt")

        S.op("scalar", lambda e: e.activation(out=ksq[0:T, 0:512], in_=ps[5][0:T, :], func=AF.Square),
             reads=["ps5"], writes=["ksq"])
        S.op("vector", lambda e: e.reduce_sum(out=ST[0:T, 64:72], in_=ksq[0:T, 0:512].rearrange("p (h d) -> p h d", h=NH), axis=AX.X),
             reads=["ksq"], writes=[("st", sl, 64)])
        rstd_from_ss(ST[0:T, 64:72], ST[0:T, 72:80], ND, T, [("st", sl, 64)], [("st", sl, 72)], ST[0:T, 80:88], ("st", sl, 80))
        S.op("vector", lambda e: e.tensor_tensor(out=kn_t[0:T, :, :], in0=ps[5][0:T, :].rearrange("p (h d) -> p h d", h=NH),
                                                 in1=ST[0:T, 72:80].unsqueeze(2).to_broadcast([T, NH, ND]), op=ALU.mult),
             reads=["ps5", ("st", sl, 72)], writes=["kn_t"])
        S.op("vector", lambda e: e.tensor_tensor(out=Kb[0:T, :, 0:ND], in0=kn_t[0:T, :, :],
                                                 in1=gkn_b[0:T, :].unsqueeze(1).to_broadcast([T, NH, ND]), op=ALU.mult),
             reads=["kn_t", "gkn_b"], writes=["Kb_n"])
        S.op("gpsimd", lambda e: e.tensor_copy(out=Kb[0:T, :, ND:ND + RD], in_=KR[0:T, :].unsqueeze(1).to_broadcast([T, NH, RD])),
             reads=[("kr_f", sl, 0), ("kr_f", sl, 1)], writes=["Kb_r"])
        if ti < 16:
            S.op("scalar", lambda e: e.copy(out=Vaug[:, ti, :, 0:VD], in_=ps[6][:, :].rearrange("p (h d) -> p h d", h=NH)),
                 reads=["ps6"], writes=[("Vaug", ti)])

        pQ = psb(7)

        def tr_Q(e):
            ins = None
            for h in range(NH):
                ins = e.transpose(out=pQ[0:96, h * 128:h * 128 + T], in_=Qb[0:T, h, :], identity=ident[0:T, 0:T])
            return ins
        S.op("tensor", tr_Q, reads=["Qb_n", "Qb_r0", "Qb_r1", "ident"], writes=["ps7"])
        S.op("vector", lambda e: e.tensor_copy(out=QT[0:96, :, tok0:tok0 + T],
                                               in_=pQ[0:96, 0:1024].rearrange("p (h t) -> p h t", h=NH)[:, :, 0:T]),
             reads=["ps7"], writes=[("QT", ti)])
        pK = psb(0)

        def tr_K(e):
            ins = None
            for h in range(NH):
                ins = e.transpose(out=pK[0:96, h * 128:h * 128 + T], in_=Kb[0:T, h, :], identity=ident[0:T, 0:T])
            return ins
        S.op("tensor", tr_K, reads=["Kb_n", "Kb_r", "ident"], writes=["ps0"])
        S.op("scalar", lambda e: e.copy(out=KT[0:96, :, tok0:tok0 + T],
                                        in_=pK[0:96, 0:1024].rearrange("p (h t) -> p h t", h=NH)[:, :, 0:T]),
             reads=["ps0"], writes=[("KT", ti)])
        S.mute = False

    def conv_chunk(ci):
        if ci < 8:
            nseg, L = 1, 256
            hTc = hT[ci % 2]
            hkeys = [("hT", ci % 2, j) for j in range(2)]
            tok0 = ci * 256
        else:
            nseg, L = 4, 8
            hTc = hT[0]
            hkeys = [("hT", 0, 0)]
            tok0 = SEQ
        N = nseg * L
        for c in range(4):
            U = ubuf[c] if ci < 8 else usam[c]
            ukey = ("ubuf", c) if ci < 8 else ("usam", c)
            ukeys = [ukey] if ci < 8 else [("usam", c, q_) for q_ in range(4)]
            cwk = [("convw_c", k) for k in range(3)]
            if ci < 8:
                ufull = U[:, :].rearrange("p (s l) -> p s l", s=1)
            else:
                ufull = U[:, :, :]

            if S.win is not None:
                S.cur_stream = 2 + c
            pc = c % 2
            bk = (1, 2, 3, 4) if pc == 0 else (5, 6, 7, 0)
            gc_, vc_, yc_, cs_, cr_ = gc_sbL[pc], vconvL[pc], yconvL[pc], csqL[pc], crsL[pc]
            kx = lambda nm: (nm, pc)

            def mm(e, c=c, bk=bk):
                ins = None
                for j, off in enumerate((0, 2 * CW, CW)):
                    for k in range(8):
                        ins = e.matmul(ps[bk[j]][:, 0:N], lhsT=w_in_sb[:, k, off + c * 128:off + (c + 1) * 128],
                                       rhs=hTc[:, k, 0:N], start=(k == 0), stop=(k == 7))
                return ins
            S.op("tensor", mm, reads=hkeys + [("w_in", k) for k in range(8)], writes=["ps%d" % bk[0], "ps%d" % bk[1], "ps%d" % bk[2]])
            S.op("scalar", lambda e, bk=bk, gc_=gc_: e.copy(out=gc_[:, 0:N], in_=ps[bk[1]][:, 0:N]), reads=["ps%d" % bk[1]], writes=[kx("gc_sb")])
            S.op("vector", lambda e, ufull=ufull, bk=bk, gc_=gc_: e.tensor_tensor(
                out=ufull[:, :, 2:2 + L], in0=ps[bk[0]][:, 0:N].rearrange("p (s l) -> p s l", s=nseg),
                in1=gc_[:, 0:N].rearrange("p (s l) -> p s l", s=nseg), op=ALU.mult),
                reads=["ps%d" % bk[0], kx("gc_sb")], writes=[(ukey, "body")])
            v3 = vc_[:, 0:N].rearrange("p (s l) -> p s l", s=nseg)
            S.op("vector", lambda e, c=c, ufull=ufull, v3=v3: e.tensor_scalar(
                out=v3, in0=ufull[:, :, 2:2 + L], scalar1=convw_c[:, 2, c:c + 1], scalar2=convb_c[:, c:c + 1],
                op0=ALU.mult, op1=ALU.add), reads=[(ukey, "body")] + cwk + ["convb_c"], writes=[kx("vconv")])
            for tap in (1, 0):
                S.op("vector", lambda e, c=c, tap=tap, ufull=ufull, v3=v3: e.scalar_tensor_tensor(
                    out=v3, in0=ufull[:, :, tap:tap + L], scalar=convw_c[:, tap, c:c + 1], in1=v3,
                    op0=ALU.mult, op1=ALU.add), reads=[(ukey, "body"), kx("vconv")] + ukeys + cwk, writes=[kx("vconv")])
            S.op("vector", lambda e, bk=bk, yc_=yc_, vc_=vc_: e.tensor_tensor(out=yc_[:, 0:N], in0=ps[bk[2]][:, 0:N], in1=vc_[:, 0:N], op=ALU.mult),
                 reads=["ps%d" % bk[2], kx("vconv")], writes=[kx("yconv")])
            S.op("scalar", lambda e, cs_=cs_, yc_=yc_: e.activation(out=cs_[:, 0:N], in_=yc_[:, 0:N], func=AF.Square),
                 reads=[kx("yconv")], writes=[kx("csq")])
            S.op("tensor", lambda e, bk=bk, cs_=cs_: e.matmul(ps[bk[3]][:, 0:N], lhsT=bconv[:, :], rhs=cs_[:, 0:N], start=True, stop=True),
                 reads=[kx("csq"), "bconv"], writes=["ps%d" % bk[3]])
            S.op("scalar", lambda e, bk=bk, cr_=cr_: e.activation(out=cr_[:, 0:N], in_=ps[bk[3]][:, 0:N], func=AF.Ln, bias=eps_c[:, :], scale=1.0),
                 reads=["ps%d" % bk[3], "eps_c"], writes=[kx("crs")])
            S.op("scalar", lambda e, cr_=cr_: e.activation(out=cr_[:, 0:N], in_=cr_[:, 0:N], func=AF.Exp, scale=-0.5),
                 reads=[kx("crs")], writes=[kx("crs")])
            S.op("vector", lambda e, c=c, yc_=yc_, cr_=cr_: e.scalar_tensor_tensor(out=yconvT[:, c, tok0:tok0 + N], in0=yc_[:, 0:N],
                                                                                  scalar=gout_c[:, c:c + 1], in1=cr_[:, 0:N],
                                                                                  op0=ALU.mult, op1=ALU.mult),
                 reads=[kx("yconv"), kx("crs"), "gout_c"], writes=[("yconvT", c, ci)])
            if True:
                if ci == 7:
                    out_recs.append(S.dma("sync", "st", o_conv_p[:, c * 128:(c + 1) * 128].rearrange("t p -> p t"),
                                          U[:, 256:258], reads=[(ukey, "body")], allow_slow_non_contiguous=True))
                elif ci == 8:
                    for q_ in range(4):
                        out_recs.append(S.dma("sync", "st", o_conv_s[q_, :, c * 128:(c + 1) * 128].rearrange("t p -> p t"),
                                              U[:, q_, 8:10], reads=[(ukey, "body")], allow_slow_non_contiguous=True))
            if ci < 7:
                S.op("gpsimd", lambda e, U=U: e.tensor_copy(out=U[:, 0:2], in_=U[:, 256:258]),
                     reads=[(ukey, "body")], writes=[ukey])

    token_tile(16, "A")
    token_tile(16, "B")
    conv_chunk(8)
    token_tile(0, "A")
    for ti in range(16):
        S.window_begin()
        if ti + 1 < 16:
            S.cur_stream = 0
            token_tile(ti + 1, "A")
        S.cur_stream = 1
        token_tile(ti, "B")
        if ti % 2 == 1:
            S.cur_stream = 2
            conv_chunk(ti // 2)
        S.cur_stream = 0
        S.window_end()

    S.barrier()
    arena_reset()
    RG = 16
    c2ckv = cache_ckv.rearrange("n (r t) d -> (n r) (t d)", r=RG)
    c2kr = cache_kr.rearrange("n (r t) d -> (n r) (t d)", r=2)
    idx_i = wk("idx_i", [128, 4], I32)
    idx16 = wk("idx16", [128, 4], I32)
    idx2 = wk("idx2", [128, 4], I32)
    riota = wk("riota", [128, RG], I32)
    idxc = wk("idxc", [128, 4, RG], I32)
    idxk = wk("idxk", [128, 4, 2], I32)
    ckv_g = [wk("ckv_g%d" % i, [128, 1024], BF16) for i in range(3)]
    kr_g = [wk("kr_g%d" % i, [128, 2048], BF16) for i in range(2)]
    cgx = [wk("cgx%d" % i, [128, 8, KVR + 1], BF16) for i in range(2)]
    ckvT_g = [wk("ckvT_g%d" % i, [128, 1024], BF16) for i in range(2)]
    krT_g = [wk("krT_g%d" % i, [128, 1024], BF16) for i in range(2)]
    ksq_s = [wk("ksq_s%d" % i, [128, 2048], BF16) for i in range(2)]
    ss_s = wk("ss_s", [128, 64], F32)
    ln_s = wk("ln_s", [128, 64], F32)
    rs_s = [wk("rs_s%d" % i, [128, 64], F32) for i in range(2)]
    tmpS = [wk("tmpS%d" % i, [128, 512], F32) for i in range(2)]
    Es = [wk("Es%d" % i, [128, 512], BF16) for i in range(2)]
    QabsT = wk("QabsT", [128, 4, 64], BF16)
    QrT = wk("QrT", [128, 4, 64], BF16)
    Qg = wk("Qg", [128, NH, NS], BF16)
    w_ukT = wk("w_ukT", [128, NH, 128], BF16)
    En = wk("En", [128, NH, NS], F32)
    En_s = wk("En_s", [128, 4, 64], BF16)
    mk_i = wk("mk_i", [128, 4], I32)
    mk_f = wk("mk_f", [128, 4], F32)
    mcol_i = wk("mcol_i", [128, 2, NS], I32)
    mcol_f = wk("mcol_f", [128, 2, NS], F32)
    mnew = wk("mnew", [128, 2, NS], F32)
    rsum = wk("rsum", [128, 1], F32)
    ctxn = wk("ctxn", [128, 128], BF16)
    ctxT = wk("ctxT", [128, 64], BF16)
    ysq = wk("ysq", [128, 256], BF16)
    yr = wk("yr", [128, 256], F32)
    battn_s = wk("battn_s", [128, 64], BF16)

    V = "vector"
    S.dma("sync", "ld", idx_i[:, :], ptab.rearrange("s p -> p s"), writes=["idx_i"], allow_slow_non_contiguous=True)
    S.op("gpsimd", lambda e: e.iota(riota[:, :], pattern=[[1, RG]], base=0, channel_multiplier=0), writes=["riota"])
    S.op("gpsimd", lambda e: e.memset(battn_s[:, :], 1.0 / 64), writes=["battn_s"])
    for q_ in range(2):
        S.op("gpsimd", lambda e, q_=q_: e.memset(cgx[q_][:, :, KVR:KVR + 1], 1.0), writes=[("cgx1", q_)])
    idx_f = wk("idx_f", [128, 4], F32)
    riota_f = wk("riota_f", [128, RG], F32)
    idxc_f = wk("idxc_f", [128, 4, RG], F32)
    idxk_f = wk("idxk_f", [128, 4, 2], F32)
    S.op(V, lambda e: e.tensor_copy(out=idx_f[:, :], in_=idx_i[:, :]), reads=["idx_i"], writes=["idx_f"])
    S.op(V, lambda e: e.tensor_copy(out=riota_f[:, :], in_=riota[:, :]), reads=["riota"], writes=["riota_f"])
    for q_ in range(4):
        S.op(V, lambda e, q_=q_: e.scalar_tensor_tensor(out=idxc_f[:, q_, :], in0=idx_f[:, q_:q_ + 1].to_broadcast([128, RG]),
                                                        scalar=float(RG), in1=riota_f[:, :], op0=ALU.mult, op1=ALU.add),
             reads=["idx_f", "riota_f"], writes=[("idxc_f", q_)])
        S.op(V, lambda e, q_=q_: e.scalar_tensor_tensor(out=idxk_f[:, q_, :], in0=idx_f[:, q_:q_ + 1].to_broadcast([128, 2]),
                                                        scalar=2.0, in1=riota_f[:, 0:2], op0=ALU.mult, op1=ALU.add),
             reads=["idx_f", "riota_f"], writes=[("idxk_f", q_)])
    S.op(V, lambda e: e.tensor_copy(out=idxc[:, :, :], in_=idxc_f[:, :, :]), reads=[("idxc_f", q_) for q_ in range(4)], writes=["idxc"])
    S.op(V, lambda e: e.tensor_copy(out=idxk[:, :, :], in_=idxk_f[:, :, :]), reads=[("idxk_f", q_) for q_ in range(4)], writes=["idxk"])

    def tr_wuk(e):
        ins = None
        for h in range(NH):
            ins = e.transpose(out=psb(0)[0:64, h * 128:(h + 1) * 128], in_=w_uk_sb[:, h * 64:(h + 1) * 64], identity=ident[:, :])
        return ins
    S.op("tensor", tr_wuk, reads=["w_uk", "ident"], writes=["ps0"])
    S.op(V, lambda e: e.tensor_copy(out=w_ukT[0:64, :, :], in_=psb(0)[0:64, 0:1024].rearrange("p (h d) -> p h d", h=NH)),
         reads=["ps0"], writes=["w_ukT"])
    S.op(V, lambda e: e.tensor_scalar(out=Qg[0:64, :, :], in0=QT[0:64, :, SEQ:SEQ + NS], scalar1=gkn_c[0:64, 0:1], scalar2=None,
                                      op0=ALU.mult), reads=[("QT", 16), "gkn_c"], writes=["Qg"])

    def mm_qabs(e):
        ins = None
        for h in range(NH):
            ins = e.matmul(ps[2][:, h * NS:(h + 1) * NS], lhsT=w_ukT[0:64, h, :], rhs=Qg[0:64, h, :], start=True, stop=True)
        return ins
    S.op("tensor", mm_qabs, reads=["w_ukT", "Qg"], writes=["ps2"])
    S.op(V, lambda e: e.tensor_copy(out=QabsT[:, :, :].rearrange("p s (h t) -> p h s t", h=NH),
                                    in_=ps[2][:, 0:NH * NS].rearrange("p (h s t) -> p h s t", h=NH, s=4)),
         reads=["ps2"], writes=["QabsT"])
    S.op(V, lambda e: e.tensor_copy(out=QrT[0:32, :, :].rearrange("p s (h t) -> p h s t", h=NH),
                                    in_=QT[64:96, :, SEQ:SEQ + NS].rearrange("p h (s t) -> p h s t", s=4)),
         reads=[("QT", 16)], writes=["QrT"])

    def mm_new(e):
        ins = None
        for h in range(NH):
            ins = e.matmul(ps[3][0:NS, h * NS:(h + 1) * NS], lhsT=KT[0:96, h, SEQ:SEQ + NS], rhs=QT[0:96, h, SEQ:SEQ + NS],
                           start=True, stop=True)
        return ins
    S.op("tensor", mm_new, reads=[("KT", 16), ("QT", 16)], writes=["ps3"])
    S.op("scalar", lambda e: e.activation(out=En[0:NS, :, :], in_=ps[3][0:NS, 0:NH * NS].rearrange("p (h q) -> p h q", h=NH),
                                          func=AF.Exp, scale=SCALE), reads=["ps3"], writes=["En"])
    S.op("gpsimd", lambda e: e.iota(mk_i[:, 0:1], pattern=[[0, 1]], base=0, channel_multiplier=1), writes=["mk_i0"])
    S.op(V, lambda e: e.tensor_single_scalar(out=mk_i[:, 1:2], in_=mk_i[:, 0:1], scalar=3, op=ALU.arith_shift_right),
         reads=["mk_i0"], writes=["mk_i1"])
    S.op(V, lambda e: e.tensor_single_scalar(out=mk_i[:, 2:3], in_=mk_i[:, 0:1], scalar=7, op=ALU.bitwise_and),
         reads=["mk_i0"], writes=["mk_i2"])
    S.op(V, lambda e: e.tensor_copy(out=mk_f[:, :], in_=mk_i[:, :]), reads=["mk_i0", "mk_i1", "mk_i2"], writes=["mk_f"])
    S.op("gpsimd", lambda e: e.iota(mcol_i[:, 0, :], pattern=[[1, 4], [0, 8]], base=0, channel_multiplier=0), writes=["mcol0"])
    S.op("gpsimd", lambda e: e.iota(mcol_i[:, 1, :], pattern=[[0, 4], [1, 8]], base=0, channel_multiplier=0), writes=["mcol1"])
    S.op(V, lambda e: e.tensor_copy(out=mcol_f[:, :, :], in_=mcol_i[:, :, :]), reads=["mcol0", "mcol1"], writes=["mcol_f"])
    S.op(V, lambda e: e.tensor_scalar(out=mnew[:, 0, :], in0=mcol_f[:, 0, :], scalar1=mk_f[:, 1:2], scalar2=None, op0=ALU.is_equal),
         reads=["mcol_f", "mk_f"], writes=["mnew0"])
    S.op(V, lambda e: e.tensor_scalar(out=mnew[:, 1, :], in0=mcol_f[:, 1, :], scalar1=mk_f[:, 2:3], scalar2=None, op0=ALU.is_ge),
         reads=["mcol_f", "mk_f"], writes=["mnew1"])
    S.op(V, lambda e: e.tensor_tensor(out=mnew[:, 0, :], in0=mnew[:, 0, :], in1=mnew[:, 1, :], op=ALU.mult),
         reads=["mnew0", "mnew1"], writes=["mnew0"])
    S.op(V, lambda e: e.tensor_tensor(out=En[0:NS, :, :], in0=En[0:NS, :, :],
                                      in1=mnew[0:NS, 0, :].unsqueeze(1).to_broadcast([NS, NH, NS]), op=ALU.mult),
         reads=["En", "mnew0"], writes=["En"])
    S.op(V, lambda e: e.tensor_copy(out=En_s[0:NS, :, :].rearrange("p s (h t) -> p h s t", h=NH),
                                    in_=En[0:NS, :, :].rearrange("p h (s t) -> p h s t", s=4)),
         reads=["En"], writes=["En_s"])

    NG = 4 * RG

    def gather_group(g):
        s_, r = divmod(g, RG)
        cg = ckv_g[g % 3]
        S.op("gpsimd", lambda e: e.indirect_dma_start(
            out=cg[:, :], out_offset=None, in_=c2ckv[:, :],
            in_offset=bass.IndirectOffsetOnAxis(ap=idxc[:, s_, r:r + 1], axis=0)),
            reads=["idxc"], writes=[("ckv_g", g % 3)], dma="gc%d" % (g % 3))
        if r % 8 == 0:
            kb = (g // 8) % 2
            S.op("gpsimd", lambda e: e.indirect_dma_start(
                out=kr_g[kb][:, :], out_offset=None, in_=c2kr[:, :],
                in_offset=bass.IndirectOffsetOnAxis(ap=idxk[:, s_, r // 8:r // 8 + 1], axis=0)),
                reads=["idxk"], writes=[("kr_g", kb)], dma="gk%d" % kb)

    def front(g):
        s_, r = divmod(g, RG)
        if g + 2 < NG:
            gather_group(g + 2)
        if g % 3 == 2:
            issue_conv(1)
        cg = ckv_g[g % 3]
        kb = (g // 8) % 2
        cT, kT_ = ckvT_g[g % 2], krT_g[g % 2]
        rs_ = rs_s[g % 2]
        cx = cgx[g % 2]
        S.op("gpsimd", lambda e: e.tensor_copy(out=cx[:, :, 0:KVR], in_=cg[:, :].rearrange("p (t d) -> p t d", t=8)),
             reads=[("ckv_g", g % 3)], writes=[("cgx", g % 2)])

        def tr_c(e):
            ins = None
            for t in range(8):
                ins = e.transpose(out=psb(0)[:, t * 128:(t + 1) * 128], in_=cg[:, t * 128:(t + 1) * 128], identity=ident[:, :])
            return ins
        S.op("tensor", tr_c, reads=[("ckv_g", g % 3), "ident"], writes=["ps0"])
        S.op("scalar", lambda e: e.copy(out=cT[:, :], in_=psb(0)[:, 0:1024]), reads=["ps0"], writes=[("ckvT_g", g % 2)])

        def tr_k(e):
            ins = None
            for t in range(8):
                tl = (r % 8) * 8 + t
                ins = e.transpose(out=psb(1)[0:32, t * 128:(t + 1) * 128], in_=kr_g[kb][:, tl * 32:(tl + 1) * 32], identity=ident[:, :])
            return ins
        S.op("tensor", tr_k, reads=[("kr_g", kb), "ident"], writes=["ps1"])
        S.op(V, lambda e: e.tensor_copy(out=kT_[0:32, :], in_=psb(1)[0:32, 0:1024]), reads=["ps1"], writes=[("krT_g", g % 2)])
        for rd in range(4):
            b0 = 2 + 2 * (rd % 2)

            def mm_kn(e, rd=rd, b0=b0):
                ins = None
                for tt_ in range(2):
                    t = rd * 2 + tt_
                    ins = e.matmul(ps[b0 + tt_][:, :], lhsT=cT[:, t * 128:(t + 1) * 128], rhs=w_uk_sb[:, :], start=True, stop=True)
                return ins
            S.op("tensor", mm_kn, reads=[("ckvT_g", g % 2), "w_uk"], writes=["ps%d" % b0, "ps%d" % (b0 + 1)])
            S.op("scalar", lambda e, rd=rd, b0=b0: e.activation(out=ksq_s[rd % 2][:, 0:1024], in_=ps_big[:, b0 * 512:(b0 + 2) * 512],
                                                                func=AF.Square),
                 reads=["ps%d" % b0, "ps%d" % (b0 + 1)], writes=[("ksq_s", rd % 2)])
            S.op(V, lambda e, rd=rd: e.reduce_sum(out=ss_s[:, rd * 16:(rd + 1) * 16],
                                                  in_=ksq_s[rd % 2][:, 0:1024].rearrange("p (a d) -> p a d", d=ND), axis=AX.X),
                 reads=[("ksq_s", rd % 2)], writes=[("ss_s", rd)])
        S.op("scalar", lambda e: e.activation(out=ln_s[:, :], in_=ss_s[:, :], func=AF.Ln, bias=eps_c[:, :], scale=1.0 / ND),
             reads=[("ss_s", q_) for q_ in range(4)] + ["eps_c"], writes=["ln_s"])
        S.op("scalar", lambda e: e.activation(out=rs_[:, :], in_=ln_s[:, :], func=AF.Exp, scale=-0.5),
             reads=["ln_s"], writes=[("rs_s", g % 2)])

    def back(g):
        s_, r = divmod(g, RG)
        cT, kT_ = ckvT_g[g % 2], krT_g[g % 2]
        rs_ = rs_s[g % 2]
        cx = cgx[g % 2]
        if r == 0:
            S.op("tensor", lambda e: e.matmul(ps[7][0:64, 0:KVR + 1], lhsT=En_s[0:NS, s_, :], rhs=ckv_s_b[0:NS, :], start=True, stop=False),
                 reads=["En_s", "ckv_s_b", "ckv_s_b1"], writes=["ps7"])

        def mm_sc(e):
            ins = None
            for t in range(8):
                e.matmul(ps[6][:, t * 64:(t + 1) * 64], lhsT=cT[:, t * 128:(t + 1) * 128], rhs=QabsT[:, s_, :], start=True, stop=True)
            for t in range(8):
                ins = e.matmul(ps[1][:, t * 64:(t + 1) * 64], lhsT=kT_[0:32, t * 128:(t + 1) * 128], rhs=QrT[0:32, s_, :],
                               start=True, stop=True)
            return ins
        S.op("tensor", mm_sc, reads=[("ckvT_g", g % 2), ("krT_g", g % 2), "QabsT", "QrT"], writes=["ps6", "ps1"])
        tS, E_ = tmpS[g % 2], Es[g % 2]
        S.op(V, lambda e: e.tensor_tensor(out=tS[:, :].rearrange("p (a t) -> p a t", t=8),
                                          in0=ps[6][:, :].rearrange("p (a t) -> p a t", t=8),
                                          in1=rs_[:, :].unsqueeze(2).to_broadcast([128, 64, 8]), op=ALU.mult),
             reads=["ps6", ("rs_s", g % 2)], writes=[("tmpS", g % 2)])
        S.op(V, lambda e: e.tensor_tensor(out=tS[:, :], in0=ps[1][:, :], in1=tS[:, :], op=ALU.add),
             reads=["ps1", ("tmpS", g % 2)], writes=[("tmpS", g % 2)])
        S.op("scalar", lambda e: e.activation(out=E_[:, :], in_=tS[:, :], func=AF.Exp, scale=SCALE),
             reads=[("tmpS", g % 2)], writes=[("Es", g % 2)])

        def mm_ctx(e):
            ins = None
            for t in range(8):
                last = (r == RG - 1 and t == 7)
                ins = e.matmul(ps[7][0:64, 0:KVR + 1], lhsT=E_[:, t * 64:(t + 1) * 64], rhs=cx[:, t, :], start=False, stop=last)
            return ins
        S.op("tensor", mm_ctx, reads=[("Es", g % 2), ("cgx", g % 2), ("cgx1", g % 2)], writes=["ps7"])
        if r == RG - 1:
            S.op(V, lambda e: e.reciprocal(out=rsum[0:64, :], in_=ps[7][0:64, 128:129]), reads=["ps7"], writes=["rsum"])
            S.op(V, lambda e: e.tensor_scalar(out=ctxn[0:64, :], in0=ps[7][0:64, 0:128], scalar1=rsum[0:64, 0:1], scalar2=None, op0=ALU.mult),
                 reads=["ps7", "rsum"], writes=["ctxn"])
            S.op("tensor", lambda e: e.transpose(out=ps_ct[:, 0:64], in_=ctxn[0:64, :], identity=ident[0:64, 0:64]),
                 reads=["ctxn", "ident"], writes=["ps7b"])
            S.op(V, lambda e: e.tensor_copy(out=ctxT[:, :], in_=ps_ct[:, 0:64]), reads=["ps7b"], writes=["ctxT"])

            def mm_yv(e):
                ins = None
                for h in range(NH):
                    ins = e.matmul(ps_yv[0:64, h * NS + s_ * 8:h * NS + s_ * 8 + 8], lhsT=w_uv_sb[:, h * 64:(h + 1) * 64],
                                   rhs=ctxT[:, h * 8:(h + 1) * 8], start=True, stop=True)
                return ins
            S.op("tensor", mm_yv, reads=["ctxT", "w_uv"], writes=[("psyv", s_)])

    ps_yv = ps_big[:, 7 * 512 + 256:7 * 512 + 512]
    ps_ct = ps[7].bitcast(BF16)[:, 320:384]
    gather_group(0)
    gather_group(1)
    front(0)
    for g in range(NG):
        S.window_begin()
        if g + 1 < NG:
            S.cur_stream = 0
            front(g + 1)
        S.cur_stream = 1
        back(g)
        S.cur_stream = 0
        S.window_end()
    S.op("scalar", lambda e: e.activation(out=ysq[0:64, :], in_=ps_yv[0:64, 0:256], func=AF.Square),
         reads=[("psyv", q) for q in range(4)], writes=["ysq"])
    S.op("tensor", lambda e: e.matmul(ps[2][0:64, 0:256], lhsT=battn_s[0:64, 0:64], rhs=ysq[0:64, :], start=True, stop=True),
         reads=["ysq", "battn_s"], writes=["ps2"])
    S.op("scalar", lambda e: e.activation(out=yr[0:64, :], in_=ps[2][0:64, 0:256], func=AF.Ln, bias=eps_c[0:64, :], scale=1.0),
         reads=["ps2", "eps_c"], writes=["yr"])
    S.op("scalar", lambda e: e.activation(out=yr[0:64, :], in_=yr[0:64, :], func=AF.Exp, scale=-0.5), reads=["yr"], writes=["yr"])
    for h in range(NH):
        po = (h % 2) * 64
        S.op(V, lambda e, h=h, po=po: e.scalar_tensor_tensor(out=ya_s[po:po + 64, h // 2, :], in0=ps_yv[0:64, h * NS:(h + 1) * NS],
                                                             scalar=gattn_c[0:64, h:h + 1], in1=yr[0:64, h * NS:(h + 1) * NS],
                                                             op0=ALU.mult, op1=ALU.mult),
             reads=[("psyv", q) for q in range(4)] + ["yr", "gattn_c"], writes=["ya_s"])

    S.barrier()
    arena_reset()
    x1s = nc.dram_tensor("x1s", [NTOK, D], F32, kind="Internal").ap()
    OH = wk("OH", [128, 17, 64], F32)
    RK = wk("RK", [128, 17, 4], F32)
    carry = wk("carry", [128, NE], F32)
    P23_MARK = arena_off[0]
    LG = wk("LG", [128, 17, 36], F32)
    MARK_R = arena_off[0]
    H2 = nc.dram_tensor("H2", [NTOK, D], BF16, kind="Internal").ap()
    w_out_sb = wk("w_out_sb", [128, 8, D], BF16)
    Et = [wk("Et%d" % i, [128, 512], BF16) for i in range(3)]
    sqa = wk("sqa", [128, 512], BF16)
    lnr = wk("lnr", [128, 512], F32)
    yattnT = [wk("yattnT%d" % i, [128, 4, 512], BF16) for i in range(2)]
    xr = [wk("xr%d" % i, [128, D], F32) for i in range(2)]
    h2b = wk("h2b", [128, D], BF16)
    junk2 = wk("junk2", [128, D], BF16)
    st2 = [wk("st2_%d" % i, [128, 32], F32) for i in range(2)]
    brb = wk("brb", [128, 36], F32)
    wr_sb = wk("wr_sb", [128, 8, 36], BF16)
    battn = wk("battn", [128, 64], BF16)

    S.dma("gpsimd", "wo", w_out_sb, w_out.rearrange("(k p) n -> p k n", p=128), writes=["w_out"])
    S.dma("gpsimd", "wr0", wr_sb[:, :, 0:4], w_rg.rearrange("(k p) n -> p k n", p=128), writes=["wr0"])
    S.dma("gpsimd", "wr1", wr_sb[:, :, 4:36], w_re.rearrange("(k p) n -> p k n", p=128), writes=["wr1"])
    S.dma("sync", "lg", gmix_b[:], g_ffn[0:1, :].to_broadcast([128, D]), writes=["gffn_b"])
    S.dma("sync", "lb0", brb[:, 0:4], b_rg[0:1, :].to_broadcast([128, 4]), writes=["brb0"])
    S.dma("sync", "lb1", brb[:, 4:36], b_re[0:1, :].to_broadcast([128, NE]), writes=["brb1"])
    S.op("gpsimd", lambda e: e.memset(battn[0:64, :], 1.0 / 64), writes=["battn0"])
    S.op("gpsimd", lambda e: e.memset(battn[64:65, :], EPS), writes=["battn1"])

    def attention_chunk(c):
        ya = yattnT[c % 2]
        nj = 4 * c + 4
        for h in range(NH):
            ob = 2 + (h % 2)

            def q0n(j):
                q0 = max(c * 512, j * 128)
                return q0, (c + 1) * 512 - q0

            def issue_S(j, h=h):
                q0, n = q0n(j)
                bank = j % 2
                S.op("tensor", lambda e: e.matmul(ps[bank][:, 0:n], lhsT=KT[0:96, h, j * 128:(j + 1) * 128],
                                                  rhs=QT[0:96, h, q0:q0 + n], start=True, stop=True),
                     reads=[("KT", j)] + [("QT", t) for t in range(q0 // 128, 4 * c + 4)], writes=["ps%d" % bank])
            issue_S(0)
            for j in range(nj):
                if j + 1 < nj:
                    issue_S(j + 1)
                q0, n = q0n(j)
                bank, eb = j % 2, j % 3
                S.op("scalar", lambda e, n=n, bank=bank, eb=eb: e.activation(out=Et[eb][:, 0:n], in_=ps[bank][:, 0:n],
                                                                             func=AF.Exp, scale=SCALE),
                     reads=["ps%d" % bank], writes=[("E", eb)])
                if j >= 4 * c:
                    S.op("gpsimd", lambda e, eb=eb: e.tensor_tensor(out=Et[eb][:, 0:128], in0=Et[eb][:, 0:128], in1=tri[:, :],
                                                                    op=ALU.mult),
                         reads=[("E", eb), "tri"], writes=[("E", eb)])
                S.op("tensor", lambda e, j=j, h=h, n=n, q0=q0, eb=eb, ob=ob: e.matmul(
                    ps[ob][0:65, q0 - c * 512:512], lhsT=Vaug[:, j, h, :], rhs=Et[eb][:, 0:n],
                    start=(j == 0), stop=(j == nj - 1)),
                    reads=[("E", eb), ("Vaug", j), "Vaug_ones"], writes=["ps%d" % ob])
            S.op("scalar", lambda e, ob=ob: e.activation(out=sqa[0:65, :], in_=ps[ob][0:65, :], func=AF.Square),
                 reads=["ps%d" % ob], writes=["sqa"])
            S.op("tensor", lambda e: e.matmul(ps[4][0:64, :], lhsT=battn[0:65, :], rhs=sqa[0:65, :], start=True, stop=True),
                 reads=["sqa", "battn0", "battn1"], writes=["ps4"])
            S.op("scalar", lambda e: e.activation(out=lnr[0:64, :], in_=ps[4][0:64, :], func=AF.Ln),
                 reads=["ps4", "lnr", "lnr2"], writes=["lnr"])
            S.op("scalar", lambda e: e.activation(out=lnr[0:64, :], in_=lnr[0:64, :], func=AF.Exp, scale=-0.5),
                 reads=["lnr"], writes=["lnr"])
            po = (h % 2) * 64
            S.op("vector", lambda e, h=h, ob=ob, po=po, ya=ya: e.scalar_tensor_tensor(
                out=ya[po:po + 64, h // 2, :], in0=ps[ob][0:64, :], scalar=gattn_c[0:64, h:h + 1], in1=lnr[0:64, :],
                op0=ALU.mult, op1=ALU.mult),
                reads=["ps%d" % ob, "lnr", "gattn_c"], writes=[("ya", c % 2, h)])

    def merge_tile(ti):
        T = 128 if ti < 16 else NS
        tok0 = ti * 128
        sl = ti % 2
        X, X1, ST = xr[sl], xr[sl], st2[sl]
        src = x_p[tok0:tok0 + 128, :] if ti < 16 else x_s[:, :]
        if ti == 0:
            S.dma("sync", "ldr%d" % sl, X[0:T, :], src, writes=[("xr", sl), ("x1b", sl, 0), ("x1b", sl, 1)])
        if ti < 16:
            nt_ = ti + 1
            nsl = nt_ % 2
            nT = 128 if nt_ < 16 else NS
            nsrc = x_p[nt_ * 128:(nt_ + 1) * 128, :] if nt_ < 16 else x_s[:, :]
            S.dma("sync", "ldr%d" % nsl, xr[nsl][0:nT, :], nsrc, writes=[("xr", nsl), ("x1b", nsl, 0), ("x1b", nsl, 1)])
        if ti < 16:
            ya = yattnT[(ti // 4) % 2]
            acol = (ti % 4) * 128
            yakeys = [("ya", (ti // 4) % 2, h) for h in range(NH)]
        else:
            ya = ya_s
            acol = 0
            yakeys = ["ya_s"]

        def mm(e):
            ins = None
            for half in range(2):
                for k in range(4):
                    ins = e.matmul(ps[5 + half][0:T, :], lhsT=yconvT[:, k, tok0:tok0 + T],
                                   rhs=w_out_sb[:, k, half * 512:(half + 1) * 512], start=(k == 0), stop=False)
                for k in range(4):
                    ins = e.matmul(ps[5 + half][0:T, :], lhsT=ya[:, k, acol:acol + T],
                                   rhs=w_out_sb[:, 4 + k, half * 512:(half + 1) * 512], start=False, stop=(k == 3))
            return ins
        S.op("tensor", mm, reads=yakeys + ["w_out"] + [("yconvT", c, ci) for c in range(4) for ci in range(9)],
             writes=["ps5", "ps6"])
        for half in range(2):
            S.op("vector", lambda e, half=half: e.tensor_tensor(out=X1[0:T, half * 512:(half + 1) * 512],
                                                                in0=ps[5 + half][0:T, :], in1=X[0:T, half * 512:(half + 1) * 512],
                                                                op=ALU.add),
                 reads=["ps%d" % (5 + half), ("xr", sl)], writes=[("x1b", sl, half)])
        x1k = [("x1b", sl, 0), ("x1b", sl, 1)]
        S.dma("sync", "stx%d" % sl, x1s[tok0:tok0 + T, :], X1[0:T, :], reads=x1k, writes=[("x1s", ti)])
        S.op("scalar", lambda e: e.activation(out=junk2[0:T, :], in_=X1[0:T, :], func=AF.Square, accum_out=ST[0:T, 0:1]),
             reads=x1k, writes=[("st2", sl, 0)])
        rstd_from_ss(ST[0:T, 0:1], ST[0:T, 1:2], D, T, [("st2", sl, 0)], [("st2", sl, 1)], ST[0:T, 2:3], ("st2", sl, 2))
        S.op("vector", lambda e: e.scalar_tensor_tensor(out=h2b[0:T, :], in0=X1[0:T, :], scalar=ST[0:T, 1:2],
                                                        in1=gmix_b[0:T, :], op0=ALU.mult, op1=ALU.mult),
             reads=x1k + [("st2", sl, 1), "gffn_b"], writes=["h2b"])
        S.dma("sync", "sth", H2[tok0:tok0 + T, :], h2b[0:T, :], reads=["h2b"], writes=[("H2", ti)])
        pT = psb(7)

        def tr(e):
            ins = None
            for k in range(8):
                ins = e.transpose(out=pT[:, k * 128:k * 128 + T], in_=h2b[0:T, k * 128:(k + 1) * 128], identity=ident[0:T, 0:T])
            return ins
        S.op("tensor", tr, reads=["h2b", "ident"], writes=["ps7"])
        S.op("scalar", lambda e: e.copy(out=QT[:, :, tok0:tok0 + T],
                                        in_=pT[:, 0:1024].rearrange("p (k t) -> p k t", k=8)[:, :, 0:T]),
             reads=["ps7"], writes=[("QT", ti)])

        def mmr(e):
            ins = None
            for k in range(8):
                ins = e.matmul(ps[7][0:T, 0:36], lhsT=QT[:, k, tok0:tok0 + T], rhs=wr_sb[:, k, :], start=(k == 0), stop=(k == 7))
            return ins
        S.op("tensor", mmr, reads=[("QT", ti), "wr0", "wr1"], writes=["ps7"])
        S.op("vector", lambda e: e.tensor_tensor(out=LG[0:T, ti, :], in0=ps[7][0:T, 0:36], in1=brb[0:T, :], op=ALU.add),
             reads=["ps7", "brb0", "brb1"], writes=[("LG", ti)])

    for c in range(4):
        attention_chunk(c)
        for t in range(4):
            merge_tile(4 * c + t)
    merge_tile(16)
    issue_conv(100)

    S.barrier()
    arena_off[0] = MARK_R
    V = "vector"
    NTL = 17
    g4 = wk("g4", [128, NTL, 4], F32)
    goh = wk("goh", [128, NTL, 4], F32)
    pen = wk("pen", [128, NTL, 4], F32)
    sc = wk("sc", [128, 12, NTL], F32)
    em = wk("em", [128, NTL, NE], F32)
    em2 = wk("em2", [128, NTL, NE], F32)
    R7 = wk("R7", [128, NTL, NE], F32)
    CAR = wk("CAR", [128, NTL, NE], F32)
    Mb = wk("Mb", [128, NTL, NE], BF16)
    lst_b = wk("lst_b", [128, 128], BF16)
    S.op("gpsimd", lambda e: e.affine_select(out=lst_b[:, :], in_=onesb[:, :], pattern=[[1, 128]], compare_op=ALU.is_gt, fill=0.0,
                                            base=0, channel_multiplier=-1), reads=["onesb"], writes=["lst_b"])
    SC = lambda i: sc[:, i, :]
    bc4 = lambda ap: ap.unsqueeze(2).to_broadcast([128, NTL, 4])
    bc32 = lambda ap: ap.unsqueeze(2).to_broadcast([128, NTL, NE])
    OH1a, OH2a = OH[:, :, 0:32], OH[:, :, 32:64]
    S.op(V, lambda e: e.reduce_max(out=SC(0), in_=LG[:, :, 0:4], axis=AX.X), writes=["sc0"])
    S.op(V, lambda e: e.tensor_tensor(out=goh[:, :, :], in0=LG[:, :, 0:4], in1=bc4(SC(0)), op=ALU.is_equal), reads=["sc0"], writes=["goh"])
    S.op(V, lambda e: e.tensor_tensor(out=g4[:, :, :], in0=LG[:, :, 0:4], in1=bc4(SC(0)), op=ALU.subtract), reads=["sc0"], writes=["g4"])
    S.op("scalar", lambda e: e.activation(out=g4[:, :, :], in_=g4[:, :, :], func=AF.Exp), reads=["g4"], writes=["g4"])
    S.op(V, lambda e: e.reduce_sum(out=SC(1), in_=g4[:, :, :], axis=AX.X), reads=["g4"], writes=["sc1"])
    S.op(V, lambda e: e.reciprocal(out=SC(2), in_=SC(1)), reads=["sc1"], writes=["sc2"])
    S.op(V, lambda e: e.tensor_scalar(out=pen[:, :, :], in0=goh[:, :, :], scalar1=-1.0, scalar2=1e30, op0=ALU.add, op1=ALU.mult),
         reads=["goh"], writes=["pen"])
    S.op(V, lambda e: e.tensor_tensor(out=em[:, :, :].rearrange("p t (g x) -> p t g x", g=4),
                                      in0=LG[:, :, 4:36].rearrange("p t (g x) -> p t g x", g=4),
                                      in1=pen[:, :, :].unsqueeze(3).to_broadcast([128, NTL, 4, 8]), op=ALU.add),
         reads=["pen"], writes=["em"])
    S.op(V, lambda e: e.reduce_max(out=SC(3), in_=em[:, :, :], axis=AX.X), reads=["em"], writes=["sc3"])
    S.op(V, lambda e: e.tensor_tensor(out=OH1a, in0=em[:, :, :], in1=bc32(SC(3)), op=ALU.is_equal), reads=["em", "sc3"], writes=["oh1"])
    S.op(V, lambda e: e.scalar_tensor_tensor(out=em2[:, :, :], in0=OH1a, scalar=-1e30, in1=em[:, :, :], op0=ALU.mult, op1=ALU.add),
         reads=["oh1", "em"], writes=["em2"])
    S.op(V, lambda e: e.reduce_max(out=SC(4), in_=em2[:, :, :], axis=AX.X), reads=["em2"], writes=["sc4"])
    S.op(V, lambda e: e.tensor_tensor(out=OH2a, in0=em2[:, :, :], in1=bc32(SC(4)), op=ALU.is_equal), reads=["em2", "sc4"], writes=["oh2"])
    S.op(V, lambda e: e.tensor_tensor(out=SC(5), in0=SC(4), in1=SC(3), op=ALU.subtract), reads=["sc3", "sc4"], writes=["sc5"])
    S.op("scalar", lambda e: e.activation(out=SC(6), in_=SC(5), func=AF.Exp), reads=["sc5"], writes=["sc6"])
    S.op(V, lambda e: e.tensor_scalar(out=SC(7), in0=SC(6), scalar1=1.0, scalar2=None, op0=ALU.add), reads=["sc6"], writes=["sc7"])
    S.op(V, lambda e: e.reciprocal(out=SC(8), in_=SC(7)), reads=["sc7"], writes=["sc8"])
    S.op(V, lambda e: e.tensor_tensor(out=RK[:, :, 2], in0=SC(8), in1=SC(2), op=ALU.mult), reads=["sc8", "sc2"], writes=["rk2"])
    S.op(V, lambda e: e.tensor_tensor(out=RK[:, :, 3], in0=SC(2), in1=RK[:, :, 2], op=ALU.subtract), reads=["rk2", "sc2"], writes=["rk3"])
    S.op(V, lambda e: e.tensor_tensor(out=Mb[:, :, :], in0=OH1a, in1=OH2a, op=ALU.add), reads=["oh1", "oh2"], writes=["Mb"])

    def mmc(e):
        ins = None
        for ti in range(NTL):
            T = 128 if ti < 16 else NS
            if ti < 16:
                e.matmul(ps[6][:, ti * NE:(ti + 1) * NE], lhsT=lst_b[0:T, 0:T], rhs=Mb[0:T, ti, :], start=True, stop=True)
                ins = e.matmul(ps[5][:, ti * NE:(ti + 1) * NE], lhsT=onesb[0:T, :], rhs=Mb[0:T, ti, :], start=True, stop=True)
            else:
                e.matmul(ps[7][0:T, 0:NE], lhsT=lst_b[0:T, 0:T], rhs=Mb[0:T, ti, :], start=True, stop=True)
                ins = e.matmul(ps[7][:, 64:64 + NE], lhsT=onesb[0:T, :], rhs=Mb[0:T, ti, :], start=True, stop=True)
        return ins
    S.op("tensor", mmc, reads=["Mb", "lst_b", "onesb"], writes=["ps5", "ps6", "ps7"])
    S.op("gpsimd", lambda e: e.memset(CAR[:, 0, :], 0.0), writes=[("car", 0)])
    for ti in range(16):
        S.op(V, lambda e, ti=ti: e.tensor_tensor(out=CAR[:, ti + 1, :], in0=ps[5][:, ti * NE:(ti + 1) * NE], in1=CAR[:, ti, :], op=ALU.add),
             reads=["ps5", ("car", ti)], writes=[("car", ti + 1)])
    S.op(V, lambda e: e.tensor_tensor(out=carry[:, :], in0=ps[7][:, 64:64 + NE], in1=CAR[:, 16, :], op=ALU.add),
         reads=["ps7", ("car", 16)], writes=["carry"])
    cark = [("car", t) for t in range(17)]
    S.op(V, lambda e: e.tensor_tensor(out=R7[:, 0:16, :], in0=ps[6][:, :].rearrange("p (t x) -> p t x", t=16), in1=CAR[:, 0:16, :], op=ALU.add),
         reads=["ps6"] + cark, writes=["R7a"])
    S.op(V, lambda e: e.tensor_tensor(out=R7[0:NS, 16, :], in0=ps[7][0:NS, 0:NE], in1=CAR[0:NS, 16, :], op=ALU.add),
         reads=["ps7"] + cark, writes=["R7b"])
    S.op(V, lambda e: e.tensor_tensor(out=em[:, :, :], in0=R7[:, :, :], in1=OH1a, op=ALU.mult), reads=["R7a", "R7b", "oh1", "em2"], writes=["em"])
    S.op(V, lambda e: e.reduce_sum(out=RK[:, :, 0], in_=em[:, :, :], axis=AX.X), reads=["em"], writes=["rk0"])
    S.op(V, lambda e: e.tensor_tensor(out=em2[:, :, :], in0=R7[:, :, :], in1=OH2a, op=ALU.mult), reads=["R7a", "R7b", "oh2"], writes=["em2"])
    S.op(V, lambda e: e.reduce_sum(out=RK[:, :, 1], in_=em2[:, :, :], axis=AX.X), reads=["em2"], writes=["rk1"])

    S.barrier()
    arena_off[0] = P23_MARK
    TM = 256
    NT = (2 * NTOK + TM - 1) // TM + NE
    NSL = NT * TM
    Xs = nc.dram_tensor("Xs", [NSL, D], BF16, kind="Internal").ap()
    Ys = nc.dram_tensor("Ys", [NSL, D], BF16, kind="Internal").ap()
    V = "vector"
    cnt_i = wk("cnt_i", [128, NE], I32)
    nt_f = wk("nt_f", [128, NE], F32)
    scn = [wk("scn%d" % i, [128, NE], F32) for i in range(2)]
    bt = wk("bt", [128, NE], F32)
    bslot = wk("bslot", [128, NE], F32)
    ee_i = wk("ee_i", [128, NE], I32)
    ee_f = wk("ee_f", [128, NE], F32)
    QTf = QT[:, :, :].rearrange("p h t -> p (h t)").bitcast(F32)
    ii_f = QTf[:, 0:NT * NE].rearrange("p (i e) -> p i e", i=NT)
    ii_i = QTf[:, NT * NE:2 * NT * NE].bitcast(I32).rearrange("p (i e) -> p i e", i=NT)
    indA = QTf[:, 2 * NT * NE:3 * NT * NE].rearrange("p (i e) -> p i e", i=NT)
    texp = wk("texp", [128, NT], F32)
    tval = wk("tval", [128, NT], F32)
    pp_i = wk("pp_i", [128, 1], I32)
    pp_f = wk("pp_f", [128, 1], F32)
    idxw_f = wk("idxw_f", [128, NT], F32)
    idxw = wk("idxw", [128, NT], I32)
    POSf = wk("POSf", [128, 17, 2], F32)
    POS = wk("POS", [128, 17, 2], I32)
    ptmp = wk("ptmp", [128, NE], F32)

    S.op(V, lambda e: e.tensor_copy(out=cnt_i[:, :], in_=carry[:, :]), reads=["carry"], writes=["cnt_i"])
    S.op(V, lambda e: e.tensor_single_scalar(out=cnt_i[:, :], in_=cnt_i[:, :], scalar=TM - 1, op=ALU.add), reads=["cnt_i"], writes=["cnt_i"])
    S.op(V, lambda e: e.tensor_single_scalar(out=cnt_i[:, :], in_=cnt_i[:, :], scalar=8, op=ALU.arith_shift_right),
         reads=["cnt_i"], writes=["cnt_i"])
    S.op(V, lambda e: e.tensor_copy(out=nt_f[:, :], in_=cnt_i[:, :]), reads=["cnt_i"], writes=["nt_f"])
    cur, curk = nt_f, ["nt_f"]
    for si, sh in enumerate((1, 2, 4, 8, 16)):
        nxt = scn[si % 2]
        k0, k1 = ("scn", si % 2, 0), ("scn", si % 2, 1)
        S.op(V, lambda e, cur=cur, nxt=nxt, sh=sh: e.tensor_copy(out=nxt[:, 0:sh], in_=cur[:, 0:sh]), reads=curk, writes=[k0])
        S.op(V, lambda e, cur=cur, nxt=nxt, sh=sh: e.tensor_tensor(out=nxt[:, sh:NE], in0=cur[:, sh:NE], in1=cur[:, 0:NE - sh], op=ALU.add),
             reads=curk, writes=[k1])
        cur, curk = nxt, [k0, k1]
    bti, btik = cur, curk
    S.op(V, lambda e: e.tensor_tensor(out=bt[:, :], in0=bti[:, :], in1=nt_f[:, :], op=ALU.subtract), reads=btik + ["nt_f"], writes=["bt"])
    S.op(V, lambda e: e.tensor_single_scalar(out=bslot[:, :], in_=bt[:, :], scalar=float(TM), op=ALU.mult), reads=["bt"], writes=["bslot"])
    S.op("gpsimd", lambda e: e.iota(ee_i[:, :], pattern=[[1, NE]], base=0, channel_multiplier=0), writes=["ee_i"])
    S.op("gpsimd", lambda e: e.iota(ii_i[:, :, :], pattern=[[1, NT], [0, NE]], base=0, channel_multiplier=0), writes=["ii_i"])
    S.op("gpsimd", lambda e: e.iota(pp_i[:, :], pattern=[[0, 1]], base=0, channel_multiplier=1), writes=["pp_i"])
    S.op(V, lambda e: e.tensor_copy(out=ee_f[:, :], in_=ee_i[:, :]), reads=["ee_i"], writes=["ee_f"])
    S.op(V, lambda e: e.tensor_copy(out=ii_f[:, :, :], in_=ii_i[:, :, :]), reads=["ii_i"], writes=["ii_f"])
    S.op(V, lambda e: e.tensor_copy(out=pp_f[:, :], in_=pp_i[:, :]), reads=["pp_i"], writes=["pp_f"])
    S.op(V, lambda e: e.tensor_tensor(out=indA[:, :, :], in0=ii_f[:, :, :], in1=bt[:, :].unsqueeze(1).to_broadcast([128, NT, NE]), op=ALU.is_ge),
         reads=["ii_f", "bt"], writes=["indA"])
    S.op(V, lambda e: e.tensor_tensor(out=ii_f[:, :, :], in0=ii_f[:, :, :], in1=bti[:, :].unsqueeze(1).to_broadcast([128, NT, NE]), op=ALU.is_lt),
         reads=["ii_f"] + btik, writes=["ii_f"])
    S.op(V, lambda e: e.tensor_tensor(out=indA[:, :, :], in0=indA[:, :, :], in1=ii_f[:, :, :], op=ALU.mult), reads=["indA", "ii_f"], writes=["indA"])
    S.op(V, lambda e: e.reduce_sum(out=tval[:, :], in_=indA[:, :, :], axis=AX.X), reads=["indA"], writes=["tval"])
    S.op(V, lambda e: e.tensor_tensor(out=indA[:, :, :], in0=indA[:, :, :], in1=ee_f[:, :].unsqueeze(1).to_broadcast([128, NT, NE]), op=ALU.mult),
         reads=["indA", "ee_f", "tval"], writes=["indA"])
    S.op(V, lambda e: e.reduce_sum(out=texp[:, :], in_=indA[:, :, :], axis=AX.X), reads=["indA"], writes=["texp"])
    S.op(V, lambda e: e.tensor_scalar(out=idxw_f[:, :], in0=texp[:, :], scalar1=128.0, scalar2=pp_f[:, 0:1], op0=ALU.mult, op1=ALU.add),
         reads=["texp", "pp_f"], writes=["idxw_f"])
    S.op(V, lambda e: e.tensor_copy(out=idxw[:, :], in_=idxw_f[:, :]), reads=["idxw_f"], writes=["idxw"])

    h2t = [wk("h2t%d" % i, [128, D], BF16) for i in range(2)]
    last_sc = None
    for ti in range(17):
        T = 128 if ti < 16 else NS
        for k in range(2):
            S.op(V, lambda e, ti=ti, k=k, T=T: e.tensor_tensor(out=ptmp[0:T, :], in0=OH[0:T, ti, 32 * k:32 * k + 32], in1=bslot[0:T, :], op=ALU.mult),
                 reads=["bslot"], writes=["ptmp"])
            S.op(V, lambda e, ti=ti, k=k, T=T: e.reduce_sum(out=POSf[0:T, ti, k:k + 1], in_=ptmp[0:T, :], axis=AX.X),
                 reads=["ptmp"], writes=[("posf", ti, k)])
        S.op(V, lambda e, ti=ti, T=T: e.tensor_tensor(out=POSf[0:T, ti, :], in0=POSf[0:T, ti, :], in1=RK[0:T, ti, 0:2], op=ALU.add),
             reads=[("posf", ti, 0), ("posf", ti, 1)], writes=[("posf2", ti)])
        S.op(V, lambda e, ti=ti, T=T: e.tensor_copy(out=POS[0:T, ti, :], in_=POSf[0:T, ti, :]), reads=[("posf2", ti)], writes=[("pos", ti)])
        sl = ti % 2
        tok0 = ti * 128
        S.dma("sync", "lh%d" % sl, h2t[sl][0:T, :], H2[tok0:tok0 + T, :], writes=[("h2t", sl)])
        if ti == 0:
            dbg("h2t0", h2t[0][:, :], [("h2t", 0)])
        for k in range(2):
            last_sc = S.op("gpsimd", lambda e, ti=ti, k=k, T=T, sl=sl: e.indirect_dma_start(
                out=Xs[:, :], out_offset=bass.IndirectOffsetOnAxis(ap=POS[0:T, ti, k:k + 1], axis=0),
                in_=h2t[sl][0:T, :], in_offset=None), reads=[("h2t", sl), ("pos", ti)], writes=[("xs_sc", ti, k)], dma="sc%d" % sl)
    S.barrier()

    NWS = 5
    WSZ = 8 * DE + 8 * DE + 2 * D
    wslots = [arenaX[:, i * WSZ:(i + 1) * WSZ] for i in range(2)] + [arenaK[:, i * WSZ:(i + 1) * WSZ] for i in range(3)]
    xs = [wk("xs%d" % i, [128, 2, D], BF16) for i in range(2)]
    xsT = [wk("xsT%d" % i, [128, 8, TM], BF16) for i in range(2)]
    sgm = [wk("sgm%d" % i, [128, TM], F32) for i in range(2)]
    aTm = [wk("aTm%d" % i, [128, 2, TM], BF16) for i in range(2)]
    ysb = [wk("ysb%d" % i, [128, 2, D], BF16) for i in range(2)]
    ys_recs = []

    def wviews(slot):
        a = wslots[slot]
        return (a[:, 0:8 * DE], a[:, 8 * DE:16 * DE], a[:, 16 * DE:16 * DE + 2 * D])

    def fetch_tile(i):
        slot = i % NWS
        wg2, wu2, wd2 = wviews(slot)
        for j, (dst, srcw) in enumerate(((wg2, wgb), (wu2, wub), (wd2, wdb))):
            S.op("gpsimd", lambda e, dst=dst, srcw=srcw, i=i: e.indirect_dma_start(
                out=dst, out_offset=None, in_=srcw[:, :], in_offset=bass.IndirectOffsetOnAxis(ap=idxw[:, i:i + 1], axis=0)),
                reads=["idxw"], writes=[("wsl", slot, j)], dma="we%d_%d" % (slot, j))

    def fetch_x(i):
        sl = i % 2
        S.dma("sync", "lx%d" % sl, xs[sl][:, :, :], Xs[i * TM:(i + 1) * TM, :].rearrange("(h p) d -> p h d", p=128), writes=[("xs", sl)])

    def moe_tile(i):
        slot, sl = i % NWS, i % 2
        wg2, wu2, wd2 = wviews(slot)
        wg = wg2.rearrange("p (k n) -> p k n", k=8)
        wu = wu2.rearrange("p (k n) -> p k n", k=8)
        wd = wd2.rearrange("p (k n) -> p k n", k=2)
        X, XT, A, Y = xs[sl], xsT[sl], aTm[sl], ysb[sl]
        for hh in range(2):
            def tr(e, hh=hh):
                ins = None
                for k in range(8):
                    ins = e.transpose(out=psb(hh)[:, k * 128:(k + 1) * 128], in_=X[:, hh, k * 128:(k + 1) * 128], identity=ident[:, :])
                return ins
            S.op("tensor", tr, reads=[("xs", sl), "ident"], writes=["ps%d" % hh])
            eng = "scalar" if hh == 0 else "vector"
            if hh == 0:
                S.op("scalar", lambda e, hh=hh: e.copy(out=XT[:, :, hh * 128:(hh + 1) * 128],
                                                       in_=psb(hh)[:, 0:1024].rearrange("p (k t) -> p k t", k=8)),
                     reads=["ps%d" % hh], writes=[("xsT", sl, hh)])
            else:
                S.op("vector", lambda e, hh=hh: e.tensor_copy(out=XT[:, :, hh * 128:(hh + 1) * 128],
                                                              in_=psb(hh)[:, 0:1024].rearrange("p (k t) -> p k t", k=8)),
                     reads=["ps%d" % hh], writes=[("xsT", sl, hh)])
        for kc in range(2):
            def mm(e, kc=kc):
                ins = None
                for k in range(8):
                    ins = e.matmul(ps[2 + kc][:, 0:TM], lhsT=wg[:, k, kc * 128:(kc + 1) * 128], rhs=XT[:, k, :], start=(k == 0), stop=(k == 7))
                for k in range(8):
                    ins = e.matmul(ps[2 + kc][:, TM:2 * TM], lhsT=wu[:, k, kc * 128:(kc + 1) * 128], rhs=XT[:, k, :], start=(k == 0), stop=(k == 7))
                return ins
            S.op("tensor", mm, reads=[("xsT", sl, 0), ("xsT", sl, 1), ("wsl", slot, 0), ("wsl", slot, 1)], writes=["ps%d" % (2 + kc)])
            S.op("scalar", lambda e, kc=kc: e.activation(out=sgm[kc][:, :], in_=ps[2 + kc][:, 0:TM], func=AF.Silu),
                 reads=["ps%d" % (2 + kc)], writes=[("sgm", kc)])
            S.op("vector", lambda e, kc=kc: e.tensor_tensor(out=A[:, kc, :], in0=ps[2 + kc][:, TM:2 * TM], in1=sgm[kc][:, :], op=ALU.mult),
                 reads=["ps%d" % (2 + kc), ("sgm", kc)], writes=[("aTm", sl, kc)])
        for hh in range(2):
            for half in range(2):
                pb = 4 + hh * 2 + half

                def mmd(e, hh=hh, half=half, pb=pb):
                    ins = None
                    for kc in range(2):
                        ins = e.matmul(ps[pb][:, :], lhsT=A[:, kc, hh * 128:(hh + 1) * 128], rhs=wd[:, kc, half * 512:(half + 1) * 512],
                                       start=(kc == 0), stop=(kc == 1))
                    return ins
                S.op("tensor", mmd, reads=[("aTm", sl, 0), ("aTm", sl, 1), ("wsl", slot, 2)], writes=["ps%d" % pb])
                if half == 0:
                    S.op("scalar", lambda e, hh=hh, half=half, pb=pb: e.copy(out=Y[:, hh, half * 512:(half + 1) * 512], in_=ps[pb][:, :]),
                         reads=["ps%d" % pb], writes=[("ysb", sl, hh, half)])
                else:
                    S.op("vector", lambda e, hh=hh, half=half, pb=pb: e.tensor_copy(out=Y[:, hh, half * 512:(half + 1) * 512], in_=ps[pb][:, :]),
                         reads=["ps%d" % pb], writes=[("ysb", sl, hh, half)])
        ys_recs.append(S.dma("sync", "sys%d" % sl, Ys[i * TM:(i + 1) * TM, :].rearrange("(h p) d -> p h d", p=128), Y[:, :, :],
                             reads=[("ysb", sl, hh, half) for hh in range(2) for half in range(2)]))

    PRE = 3
    for i in range(min(PRE, NT)):
        fetch_tile(i)
    fetch_x(0)
    dbg("xs_t0", xs[0][:, :, :], [("xs", 0)])
    dbg("wg_t0", wslots[0][:, 0:2048], [("wsl", 0, 0)])
    dbg("wd_t0", wslots[0][:, 4096:6144], [("wsl", 0, 2)])
    for i in range(NT):
        if i + PRE < NT:
            fetch_tile(i + PRE)
        if i + 1 < NT:
            fetch_x(i + 1)
        moe_tile(i)
        if i == 0:
            dbg("ysb_t0", ysb[0][:, :, :], [("ysb", 0, hh, half) for hh in range(2) for half in range(2)])
            dbg("xsT_t0", xsT[0][:, :, :], [("xsT", 0, 0), ("xsT", 0, 1)])
            dbg("aT_t0", aTm[0][:, :, :], [("aTm", 0, 0), ("aTm", 0, 1)])
    S.barrier()

    xf = [QTf[:, i * D:(i + 1) * D] for i in range(2)]
    QTb = QT[:, :, :].rearrange("p h t -> p (h t)")
    g1 = [QTb[:, (4 + i) * D:(5 + i) * D] for i in range(2)]
    g2 = [QTb[:, (6 + i) * D:(7 + i) * D] for i in range(2)]
    for ti in range(17):
        T = 128 if ti < 16 else NS
        tok0 = ti * 128
        sl = ti % 2
        S.dma("sync", "lf%d" % sl, xf[sl][0:T, :], x1s[tok0:tok0 + T, :], writes=[("xf", sl)])
        for k, G in enumerate((g1, g2)):
            S.op("gpsimd", lambda e, ti=ti, k=k, T=T, G=G, sl=sl: e.indirect_dma_start(
                out=G[sl][0:T, :], out_offset=None, in_=Ys[:, :],
                in_offset=bass.IndirectOffsetOnAxis(ap=POS[0:T, ti, k:k + 1], axis=0)),
                reads=[("pos", ti)], writes=[("gg", k, sl)], dma="gy%d_%d" % (k, sl))
        if ti == 0:
            dbg("g1_0", g1[0][:, :], [("gg", 0, 0)])
            dbg("g2_0", g2[0][:, :], [("gg", 1, 0)])
            dbg("xf_0", xf[0][:, :], [("xf", 0)])
        S.op(V, lambda e, ti=ti, T=T, sl=sl: e.scalar_tensor_tensor(out=xf[sl][0:T, :], in0=g1[sl][0:T, :], scalar=RK[0:T, ti, 2:3],
                                                                    in1=xf[sl][0:T, :], op0=ALU.mult, op1=ALU.add),
             reads=[("gg", 0, sl), ("xf", sl)], writes=[("xf", sl)])
        S.op(V, lambda e, ti=ti, T=T, sl=sl: e.scalar_tensor_tensor(out=xf[sl][0:T, :], in0=g2[sl][0:T, :], scalar=RK[0:T, ti, 3:4],
                                                                    in1=xf[sl][0:T, :], op0=ALU.mult, op1=ALU.add),
             reads=[("gg", 1, sl), ("xf", sl)], writes=[("xf", sl)])
        dst = y_p[tok0:tok0 + 128, :] if ti < 16 else y_s[:, :]
        out_recs.append(S.dma("sync", "sf%d" % sl, dst, xf[sl][0:T, :], reads=[("xf", sl)]))

    dbg("OH", OH[:, :, :], [])
    dbg("RK", RK[:, :, :], [])
    dbg("carry", carry[:, :], [])
    dbg("nt_f", nt_f[:, :], [])
    dbg("bt", bt[:, :], [])
    dbg("bti", bti[:, :], [])
    dbg("texp", texp[:, :], [])
    dbg("idxw", idxw[:, :], [])
    dbg("POS", POS[:, :, :], [])
    dbg("xsT0", xsT[0][:, :, :], [])
    dbg("ysb0", ysb[0][:, :, :], [])
    S.op("sync", None, reads=[], writes=[])
    fin = S.ops["sync"][-1]
    fin.deps = [r_ for r_ in out_recs if r_.eng is not None]
    for d in fin.deps:
        d.needed = True

    S.emit(nc, es)
    es.close()
    return nc


_CACHE = {}


def kernel(**inputs):
    f = lambda a: np.ascontiguousarray(a)
    nc = _CACHE.get("nc")
    if nc is None:
        nc = build_program()
        _CACHE["nc"] = nc
    shared = {
        "cache_ckv": f(inputs["cache_ckv"][0]),
        "cache_kr": f(inputs["cache_krope"][0]),
        "g_mix": f(inputs["g_mix"]), "w_in": f(inputs["w_in"][0]),
        "conv_w": f(inputs["conv_w"][0]), "conv_b": f(inputs["conv_b"]),
        "g_q_lat": f(inputs["g_q_lat"]), "w_uq": f(inputs["w_uq"][0]),
        "g_kv_lat": f(inputs["g_kv_lat"]), "w_uk": f(inputs["w_uk"][0]), "w_uv": f(inputs["w_uv"][0]),
        "g_q_nope": f(inputs["g_q_nope"]), "g_q_rope": f(inputs["g_q_rope"]),
        "g_k_nope": f(inputs["g_k_nope"]), "g_k_rope": f(inputs["g_k_rope"]),
        "g_out": f(inputs["g_out"]), "w_out": f(inputs["w_out"][0]), "g_ffn": f(inputs["g_ffn"]),
        "w_rg": f(inputs["w_router_group"][0]), "b_rg": f(inputs["b_router_group"]),
        "w_re": f(inputs["w_router_expert"][0]), "b_re": f(inputs["b_router_expert"]),
        "w_gate": f(inputs["w_gate"][0].reshape(NE, 8, 128, DE).transpose(0, 2, 1, 3).reshape(NE * 128, 8 * DE)),
        "w_up": f(inputs["w_up"][0].reshape(NE, 8, 128, DE).transpose(0, 2, 1, 3).reshape(NE * 128, 8 * DE)),
        "w_down": f(inputs["w_down"][0].reshape(NE, 2, 128, D).transpose(0, 2, 1, 3).reshape(NE * 128, 2 * D)),
    }
    in_maps = []
    for c in range(NCORES):
        m = dict(shared)
        m["x_p"] = f(inputs["x_prompt"][c])
        m["x_s"] = f(inputs["x_sample"][4 * c:4 * c + 4].reshape(NS, D))
        m["st_conv"] = f(inputs["state_conv"][0, 4 * c:4 * c + 4])
        m["ptab"] = f(inputs["page_table"][4 * c:4 * c + 4])
        in_maps.append(m)
    res = run_bass_kernel_spmd(nc, in_maps, core_ids=list(range(NCORES)))
    R = res.results
    if DEBUG:
        _CACHE["dbg"] = R
    y_p = np.stack([R[c]["y_p"] for c in range(NCORES)], 0)
    y_s = np.concatenate([R[c]["y_s"].reshape(4, 8, D) for c in range(NCORES)], 0)
    ckv_p = np.stack([R[c]["o_ckv_p"] for c in range(NCORES)], 0)[None]
    kr_p = np.stack([R[c]["o_kr_p"] for c in range(NCORES)], 0)[None]
    conv_p = np.stack([R[c]["o_conv_p"] for c in range(NCORES)], 0)[None]
    ckv_s = np.concatenate([R[c]["o_ckv_s"].reshape(4, 8, KVR) for c in range(NCORES)], 0)[None]
    kr_s = np.concatenate([R[c]["o_kr_s"].reshape(4, 8, RD) for c in range(NCORES)], 0)[None]
    conv_s = np.concatenate([R[c]["o_conv_s"] for c in range(NCORES)], 0)[None]
    return (y_p, y_s, ckv_p, kr_p, conv_p, ckv_s, kr_s, conv_s)
```

```python
import numpy as np
from contextlib import ExitStack
import concourse.bass as bass
import concourse.mybir as mybir
from concourse.bass_utils import run_bass_kernel_spmd

F32 = mybir.dt.float32
BF16 = mybir.dt.bfloat16
I32 = mybir.dt.int32
ALU = mybir.AluOpType
AF = mybir.ActivationFunctionType
AX = mybir.AxisListType

NCORES = 8
D = 1024
SEQ = 2048
NS = 32
NTOK = SEQ + NS
CW = 512
QR = 256
KVR = 128
RD = 32
NH = 8
ND = 64
VD = 64
PW = 3 * CW + QR + KVR + RD
NE = 32
DE = 256
EPS = 1e-6
DEBUG = False
NPAGE = 128
PAGE = 128
NPOOL = 5120
PAST = NPAGE * PAGE
SCALE = float((ND + RD) ** -0.5)


class Rec:
    __slots__ = ("eng", "fn", "deps", "dma", "needed", "sem", "val", "seq", "stream", "pdeps")

    def __init__(self, eng, fn, deps, dma):
        self.eng, self.fn, self.deps, self.dma = eng, fn, deps, dma
        self.needed = False
        self.sem = None
        self.val = 0
        self.seq = -1
        self.stream = 0
        self.pdeps = ()


class Sched:
    ENGS = ("sync", "gpsimd", "vector", "scalar", "tensor")

    def __init__(self):
        self.ops = {e: [] for e in self.ENGS}
        self.buf = {}
        self.streams = {}
        self.mute = False
        self.seq = 0
        self.cur_stream = 0
        self.win = None

    def op(self, eng, fn, reads=(), writes=(), dma=None):
        if self.mute:
            return Rec(None, None, [], None)
        deps = []
        seen = set()

        def add(r):
            if r is not None and id(r) not in seen:
                seen.add(id(r))
                deps.append(r)

        for k in reads:
            st = self.buf.get(k)
            if st is not None:
                add(st[0])
        for k in writes:
            st = self.buf.get(k)
            if st is not None:
                add(st[0])
                for r in st[1]:
                    add(r)
        pdeps = ()
        if eng == "tensor":
            pdeps = [d for d in deps if d.eng == "tensor" and not d.dma]
            deps = [d for d in deps if d.eng != "tensor" or d.dma]
        rec = Rec(eng, fn, deps, dma)
        rec.pdeps = pdeps
        rec.seq = self.seq
        rec.stream = self.cur_stream
        self.seq += 1
        for d in deps:
            d.needed = True
        self.ops[eng].append(rec)
        if self.win is not None:
            self.win.append(rec)
        for k in reads:
            self.buf.setdefault(k, [None, []])[1].append(rec)
        for k in writes:
            self.buf[k] = [rec, []]
        if dma is not None:
            self.streams.setdefault(dma, 0)
        return rec

    def window_begin(self):
        self.win = []

    def window_end(self):
        win, self.win = self.win, None
        if not win:
            return
        inwin = set(id(r) for r in win)
        first_seq = win[0].seq
        for e in self.ENGS:
            self.ops[e] = [r for r in self.ops[e] if id(r) not in inwin]
        by_stream = {}
        for r in win:
            by_stream.setdefault(r.stream, []).append(r)
        keys = sorted(by_stream)
        ptr = {k: 0 for k in keys}
        pe_order = [r for r in win if r.eng == "tensor"]
        pe_next = 0
        done = set()
        out = []
        turn = 0
        total = len(win)

        def ready(r):
            for d in r.deps:
                if id(d) in inwin and id(d) not in done:
                    return False
            for d in r.pdeps:
                if id(d) in inwin and id(d) not in done:
                    return False
            return True
        while len(out) < total:
            picked = None
            for off in range(len(keys)):
                k = keys[(turn + off) % len(keys)]
                if ptr[k] < len(by_stream[k]) and ready(by_stream[k][ptr[k]]):
                    picked = k
                    break
            assert picked is not None, "window_end: no ready op (dependency cycle?)"
            r = by_stream[picked][ptr[picked]]
            ptr[picked] += 1
            if r.eng == "tensor":
                pe_next += 1
            done.add(id(r))
            out.append(r)
            turn = (keys.index(picked) + 1) % len(keys)
        for r in out:
            self.ops[r.eng].append(r)

    def barrier(self):
        lasts = []
        last_dma = {}
        for e in self.ENGS:
            last_c = None
            for r in self.ops[e]:
                if r.dma is not None:
                    last_dma[r.dma] = r
                elif r.fn is not None:
                    last_c = r
            if last_c is not None:
                lasts.append(last_c)
        lasts += list(last_dma.values())
        for d in lasts:
            d.needed = True
        for e in self.ENGS:
            rec = Rec(e, None, list(lasts), None)
            self.ops[e].append(rec)
        self.buf = {}

    def dma(self, eng, stream, out, in_, reads=(), writes=(), **kw):
        return self.op(eng, lambda e: e.dma_start(out=out, in_=in_, **kw), reads, writes, dma=stream)

    def emit(self, nc, es):
        sems = {}
        for e in self.ENGS:
            sems[e] = es.enter_context(nc.semaphore("p_" + e))
        dsem = {}
        for s in self.streams:
            dsem[s] = es.enter_context(nc.semaphore("d_" + s))
        cnt = {s: 0 for s in self.streams}
        for e in self.ENGS:
            c = 0
            for r in self.ops[e]:
                if r.dma is not None:
                    cnt[r.dma] += 16
                    r.sem, r.val = dsem[r.dma], cnt[r.dma]
                elif r.needed:
                    c += 1
                    r.sem, r.val = sems[e], c
        block = es.enter_context(nc.Block())

        def run(eng_name):
            def body(eng):
                waited = {}
                for r in self.ops[eng_name]:
                    need = {}
                    for d in r.deps:
                        key = id(d.sem)
                        if key not in need or need[key][1] < d.val:
                            need[key] = (d.sem, d.val)
                    for key, (sem_, val_) in need.items():
                        if waited.get(key, 0) < val_:
                            eng.wait_ge(sem_, val_)
                            waited[key] = val_
                    if r.fn is None:
                        continue
                    ins = r.fn(eng)
                    if r.dma is not None:
                        ins.then_inc(r.sem, 16)
                    elif r.needed:
                        ins.then_inc(r.sem, 1)
            return body

        block.sync(run("sync"))
        block.gpsimd(run("gpsimd"))
        block.vector(run("vector"))
        block.scalar(run("scalar"))
        block.tensor(run("tensor"))


def build_program():
    nc = bass.Bass("TRN2", target_bir_lowering=False)
    S = Sched()
    es = ExitStack()

    def din(name, shape, dt=F32):
        return nc.dram_tensor(name, list(shape), dt, kind="ExternalInput").ap()

    def dout(name, shape, dt=F32):
        return nc.dram_tensor(name, list(shape), dt, kind="ExternalOutput").ap()

    x_p = din("x_p", [SEQ, D])
    x_s = din("x_s", [NS, D])
    st_conv = din("st_conv", [4, 2, CW])
    cache_ckv = din("cache_ckv", [NPOOL, PAGE, KVR])
    cache_kr = din("cache_kr", [NPOOL, PAGE, RD])
    ptab = din("ptab", [4, NPAGE], I32)
    g_mix = din("g_mix", [1, D])
    w_in = din("w_in", [D, PW])
    conv_w = din("conv_w", [3, CW])
    conv_b = din("conv_b", [1, CW])
    g_q_lat = din("g_q_lat", [1, QR])
    w_uq = din("w_uq", [QR, NH * (ND + RD)])
    g_kv_lat = din("g_kv_lat", [1, KVR])
    w_uk = din("w_uk", [KVR, NH * ND])
    w_uv = din("w_uv", [KVR, NH * VD])
    g_q_nope = din("g_q_nope", [1, ND])
    g_q_rope = din("g_q_rope", [1, RD])
    g_k_nope = din("g_k_nope", [1, ND])
    g_k_rope = din("g_k_rope", [1, RD])
    g_out = din("g_out", [1, D])
    w_out = din("w_out", [D, D])
    g_ffn = din("g_ffn", [1, D])
    w_rg = din("w_rg", [D, 4])
    b_rg = din("b_rg", [1, 4])
    w_re = din("w_re", [D, NE])
    b_re = din("b_re", [1, NE])
    w_gate = din("w_gate", [NE * 128, 8 * DE])
    w_up = din("w_up", [NE * 128, 8 * DE])
    w_down = din("w_down", [NE * 128, 2 * D])

    y_p = dout("y_p", [SEQ, D])
    y_s = dout("y_s", [NS, D])
    o_ckv_p = dout("o_ckv_p", [SEQ, KVR])
    o_kr_p = dout("o_kr_p", [SEQ, RD])
    o_conv_p = dout("o_conv_p", [2, CW])
    o_ckv_s = dout("o_ckv_s", [NS, KVR])
    o_kr_s = dout("o_kr_s", [NS, RD])
    o_conv_s = dout("o_conv_s", [4, 2, CW])

    def sb(name, shape, dt=F32):
        return es.enter_context(nc.sbuf_tensor(name, list(shape), dt))

    def dbg(name, ap, keys):
        if not DEBUG:
            return
        shp = list(ap.shape)
        o = nc.dram_tensor("dbg_" + name, shp, ap.dtype, kind="ExternalOutput").ap()
        out_recs.append(S.dma("sync", "st", o, ap, reads=keys))

    out_recs = []

    ARENA_BYTES = 61440
    arena_t = sb("arena", [128, ARENA_BYTES // 4], F32)
    arena_off = [0]

    def arena_reset():
        arena_off[0] = 0

    def wk(name, shape, dt=F32):
        esz = 2 if dt == BF16 else 4
        n = 1
        for d in shape[1:]:
            n *= d
        nb = (n * esz + 31) // 32 * 32
        off = arena_off[0]
        assert off + nb <= ARENA_BYTES, (name, off, nb)
        arena_off[0] = off + nb
        ap = arena_t[:, off // 4:(off + nb) // 4]
        if dt != F32:
            ap = ap.bitcast(dt)
        ap = ap[:, 0:n]
        if len(shape) == 3:
            ap = ap.rearrange("p (a b) -> p a b", a=shape[1])
        elif len(shape) == 4:
            ap = ap.rearrange("p (a b c) -> p a b c", a=shape[1], b=shape[2])
        return ap

    ps_big_t = es.enter_context(nc.psum_tensor("ps_big", [128, 4096], F32))
    ps_big = ps_big_t[:, :]
    ps = [ps_big[:, i * 512:(i + 1) * 512] for i in range(8)]

    def psb(i):
        return ps[i].bitcast(BF16)

    ident = sb("ident", [128, 128], BF16)
    tri = sb("tri", [128, 128], BF16)
    bconv = sb("bconv", [128, 128], BF16)
    onesb = sb("onesb", [128, 128], BF16)
    zero_c = sb("zero_c", [128, 1], F32)
    eps_c = sb("eps_c", [128, 1], F32)
    gmix_b = sb("gmix_b", [128, D], F32)
    gql_b = sb("gql_b", [128, QR], F32)
    gkv_b = sb("gkv_b", [128, KVR], F32)
    gq_b = sb("gq_b", [128, ND + RD], F32)
    gkn_b = sb("gkn_b", [128, ND], F32)
    gkr_b = sb("gkr_b", [128, RD], F32)
    convw_c = sb("convw_c", [128, 3, 4], F32)
    convb_c = sb("convb_c", [128, 4], F32)
    gout_c = sb("gout_c", [128, 8], F32)
    gattn_c = sb("gattn_c", [64, 8], F32)
    gkn_c = sb("gkn_c", [64, 1], F32)
    cosT = sb("cosT", [128, 17, 16], F32)
    sinT = sb("sinT", [128, 17, 16], F32)

    ckv_s_b = sb("ckv_s_b", [NS, KVR + 1], BF16)
    ya_s = sb("ya_s", [128, 4, NS], BF16)
    arenaX = sb("arenaX", [128, 8 * NTOK], BF16)
    w_in_sb = arenaX[:, 0:8 * PW].rearrange("p (k n) -> p k n", k=8)
    w_uq_sb = sb("w_uq_sb", [128, 2, NH * (ND + RD)], BF16)
    w_uk_sb = sb("w_uk_sb", [128, NH * ND], BF16)
    w_uv_sb = sb("w_uv_sb", [128, NH * VD], BF16)

    QT = sb("QT", [128, NH, NTOK], BF16)
    arenaK = sb("arenaK", [128, NH * NTOK + 16 * NH * (VD + 1) + 4 * NTOK], BF16)
    KT = arenaK[:, 0:NH * NTOK].rearrange("p (h t) -> p h t", h=NH)
    _o = NH * NTOK
    Vaug = arenaK[:, _o:_o + 16 * NH * (VD + 1)].rearrange("p (j h d) -> p j h d", j=16, h=NH)
    _o += 16 * NH * (VD + 1)
    yconvT = arenaK[:, _o:_o + 4 * NTOK].rearrange("p (c t) -> p c t", c=4)

    xin = [wk("xin%d" % i, [128, D], F32) for i in range(2)]
    junk = wk("junk", [128, D], BF16)
    hbf = [wk("hbf%d" % i, [128, D], BF16) for i in range(2)]
    hT = [wk("hT%d" % i, [128, 8, 256], BF16) for i in range(2)]
    st1 = [wk("st1_%d" % i, [128, 96], F32) for i in range(2)]
    qln = wk("qln", [128, QR], BF16)
    qlnT = wk("qlnT", [128, 2, 128], BF16)
    ckv_f = [wk("ckv_f%d" % i, [128, KVR], F32) for i in range(2)]
    ckv_b = wk("ckv_b", [128, KVR], BF16)
    ckvT = wk("ckvT", [128, 128], BF16)
    krn = wk("krn", [128, RD], F32)
    kr_f = [wk("kr_f%d" % i, [128, RD], F32) for i in range(2)]
    rtmp = wk("rtmp", [128, 4, 16], F32)
    qsq = wk("qsq", [128, 768], F32)
    ksq = wk("ksq", [128, 512], F32)
    qn = wk("qn", [128, NH, ND + RD], F32)
    qrt = wk("qrt", [128, 4, NH, 16], F32)
    Qb = wk("Qb", [128, NH, ND + RD], BF16)
    Kb = wk("Kb", [128, NH, ND + RD], BF16)
    kn_t = wk("kn_t", [128, NH, ND], F32)
    gc_sb = wk("gc_sb", [128, 256], F32)
    ubuf = [wk("ubuf%d" % c, [128, 2 + 256], F32) for c in range(4)]
    usam = [wk("usam%d" % c, [128, 4, 10], F32) for c in range(4)]
    vconv = wk("vconv", [128, 256], F32)
    yconv = wk("yconv", [128, 256], F32)
    csq = wk("csq", [128, 256], BF16)
    crs = wk("crs", [128, 256], F32)

    S.op("gpsimd", lambda e: e.memset(ident[:], 0.0), writes=["ident"])
    S.op("gpsimd", lambda e: e.affine_select(out=ident[:], in_=ident[:], pattern=[[-1, 128]],
                                            compare_op=ALU.not_equal, fill=1.0, base=0,
                                            channel_multiplier=1), reads=["ident"], writes=["ident"])
    S.op("gpsimd", lambda e: e.memset(onesb[:], 1.0), writes=["onesb"])
    S.op("gpsimd", lambda e: e.affine_select(out=tri[:], in_=onesb[:], pattern=[[1, 128]],
                                            compare_op=ALU.is_ge, fill=0.0, base=0,
                                            channel_multiplier=-1), reads=["onesb"], writes=["tri"])
    S.op("gpsimd", lambda e: e.memset(bconv[:], 0.0), writes=["bconv"])
    S.op("gpsimd", lambda e: e.memset(bconv[0:64, 0:64], 1.0 / 64), reads=["bconv"], writes=["bconv"])
    S.op("gpsimd", lambda e: e.memset(bconv[64:128, 64:128], 1.0 / 64), reads=["bconv"], writes=["bconv"])
    S.op("gpsimd", lambda e: e.memset(zero_c[:], 0.0), writes=["zero_c"])
    S.op("gpsimd", lambda e: e.memset(eps_c[:], EPS), writes=["eps_c"])
    S.op("gpsimd", lambda e: e.memset(Vaug[:, :, :, VD:VD + 1], 1.0), writes=["Vaug_ones"])
    for c in range(4):
        S.op("gpsimd", lambda e, c=c: e.memset(ubuf[c][:, 0:2], 0.0), writes=[("ubuf", c)])

    def bload(dst, src, n, key):
        S.dma("sync", "ld", dst[:], src[0:1, :].to_broadcast([128, n]), writes=[key])

    bload(gmix_b, g_mix, D, "gmix_b")
    bload(gql_b, g_q_lat, QR, "gql_b")
    bload(gkv_b, g_kv_lat, KVR, "gkv_b")
    bload(gkn_b, g_k_nope, ND, "gkn_b")
    bload(gkr_b, g_k_rope, RD, "gkr_b")
    S.dma("sync", "ld", gq_b[:, 0:ND], g_q_nope[0:1, :].to_broadcast([128, ND]), writes=["gq_b0"])
    S.dma("sync", "ld", gq_b[:, ND:ND + RD], g_q_rope[0:1, :].to_broadcast([128, RD]), writes=["gq_b1"])
    if True:
        for k in range(3):
            S.dma("sync", "ld", convw_c[:, k, :], conv_w[k:k + 1, :].rearrange("o (c p) -> p (o c)", p=128),
                  writes=[("convw_c", k)], allow_slow_non_contiguous=True)
        S.dma("sync", "ld", convb_c[:], conv_b.rearrange("o (c p) -> p (o c)", p=128), writes=["convb_c"], allow_slow_non_contiguous=True)
        S.dma("sync", "ld", gout_c[:], g_out.rearrange("o (c p) -> p (o c)", p=128), writes=["gout_c"], allow_slow_non_contiguous=True)
        S.dma("sync", "ld", gattn_c[:], g_out[:, CW:D].rearrange("o (h p) -> p (o h)", p=64), writes=["gattn_c"], allow_slow_non_contiguous=True)
        S.dma("sync", "ld", gkn_c[:], g_k_nope.rearrange("o p -> p o"), writes=["gkn_c"], allow_slow_non_contiguous=True)
        for c in range(4):
            for sq_ in range(4):
                S.dma("sync", "ld", usam[c][:, sq_, 0:2],
                      st_conv[sq_, :, c * 128:(c + 1) * 128].rearrange("t p -> p t"), writes=[("usam", c, sq_)],
                      allow_slow_non_contiguous=True)

    w_in_v = w_in.rearrange("(k p) n -> p k n", p=128)
    for k in range(8):
        S.dma("gpsimd", "wg", w_in_sb[:, k, :], w_in_v[:, k, :], writes=[("w_in", k)])
    S.dma("gpsimd", "wg", w_uq_sb[:], w_uq.rearrange("(k p) n -> p k n", p=128), writes=["w_uq"])
    S.dma("gpsimd", "wg", w_uk_sb[:], w_uk[:, :], writes=["w_uk"])
    S.dma("gpsimd", "wg", w_uv_sb[:], w_uv[:, :], writes=["w_uv"])

    wgb = nc.dram_tensor("wgb", [NE * 128, 8 * DE], BF16, kind="Internal").ap()
    wub = nc.dram_tensor("wub", [NE * 128, 8 * DE], BF16, kind="Internal").ap()
    wdb = nc.dram_tensor("wdb", [NE * 128, 2 * D], BF16, kind="Internal").ap()
    conv_jobs = [(srcw, dstw, q_) for srcw, dstw in ((w_gate, wgb), (w_up, wub), (w_down, wdb)) for q_ in range(8)]

    def issue_conv(n):
        for _ in range(n):
            if conv_jobs:
                srcw, dstw, q_ = conv_jobs.pop(0)
                S.dma("gpsimd", "cv", dstw[q_ * 512:(q_ + 1) * 512, :], srcw[q_ * 512:(q_ + 1) * 512, :], writes=[("wconv", id(dstw), q_)])

    MARK_SETUP = arena_off[0]
    posf = wk("posf", [128, 17], F32)
    posi = wk("posi", [128, 17], I32)
    invf = wk("invf", [128, 16], F32)
    ang = wk("ang", [128, 17, 16], F32)
    ang2 = wk("ang2", [128, 17, 16], F32)
    S.op("gpsimd", lambda e: e.iota(posi[:, 0:16], pattern=[[128, 16]], base=0, channel_multiplier=1),
         writes=["posi0"])
    S.op("gpsimd", lambda e: e.iota(posi[:, 16:17], pattern=[[0, 1]], base=0, channel_multiplier=1),
         writes=["posi1"])
    S.op("vector", lambda e: e.tensor_single_scalar(out=posi[:, 16:17], in_=posi[:, 16:17], scalar=7,
                                                   op=ALU.bitwise_and), reads=["posi1"], writes=["posi1"])
    S.op("vector", lambda e: e.tensor_single_scalar(out=posi[:, 16:17], in_=posi[:, 16:17], scalar=PAST,
                                                   op=ALU.add), reads=["posi1"], writes=["posi1"])
    S.op("vector", lambda e: e.tensor_copy(out=posf[:], in_=posi[:]), reads=["posi0", "posi1"], writes=["posf"])
    invf_np = (np.float32(10000.0) ** (-(np.arange(16, dtype=np.float32) / np.float32(16)))).astype(np.float32)
    for i in range(16):
        S.op("gpsimd", lambda e, i=i: e.memset(invf[:, i:i + 1], float(invf_np[i])), writes=[("invf", i)])
    invk = [("invf", i) for i in range(16)]
    for t in range(17):
        S.op("vector", lambda e, t=t: e.tensor_scalar(out=ang[:, t, :], in0=invf[:], scalar1=posf[:, t:t + 1],
                                                      scalar2=None, op0=ALU.mult),
             reads=invk + ["posf"], writes=[("ang", t)])
    angkeys = [("ang", t) for t in range(17)]
    PI = float(np.pi)
    C1 = 6.28125
    C2 = float(2 * np.pi - 6.28125)
    angk = wk("angk", [128, 17, 16], I32)
    angkf = wk("angkf", [128, 17, 16], F32)
    angm = wk("angm", [128, 17, 16], F32)
    S.op("vector", lambda e: e.tensor_scalar(out=ang2[:], in0=ang[:], scalar1=float(1.0 / (2 * np.pi)), scalar2=None,
                                             op0=ALU.mult), reads=angkeys, writes=["ang2"])
    S.op("vector", lambda e: e.tensor_copy(out=angk[:], in_=ang2[:]), reads=["ang2"], writes=["angk"])
    S.op("vector", lambda e: e.tensor_copy(out=angkf[:], in_=angk[:]), reads=["angk"], writes=["angkf"])
    S.op("vector", lambda e: e.scalar_tensor_tensor(out=ang2[:], in0=angkf[:], scalar=-C1, in1=ang[:],
                                                    op0=ALU.mult, op1=ALU.add), reads=["angkf"] + angkeys, writes=["ang2"])
    S.op("vector", lambda e: e.scalar_tensor_tensor(out=ang2[:], in0=angkf[:], scalar=-C2, in1=ang2[:],
                                                    op0=ALU.mult, op1=ALU.add), reads=["angkf", "ang2"], writes=["ang2"])

    def wrap_and_sin(dst, key):
        S.op("vector", lambda e: e.tensor_single_scalar(out=angm[:], in_=ang2[:], scalar=PI, op=ALU.is_gt),
             reads=["ang2"], writes=["angm"])
        S.op("vector", lambda e: e.scalar_tensor_tensor(out=ang2[:], in0=angm[:], scalar=-2 * PI, in1=ang2[:],
                                                        op0=ALU.mult, op1=ALU.add), reads=["angm", "ang2"], writes=["ang2"])
        S.op("vector", lambda e: e.tensor_scalar(out=angm[:], in0=ang2[:], scalar1=-PI, scalar2=PI,
                                                 op0=ALU.max, op1=ALU.min), reads=["ang2"], writes=["angm"])
        S.op("scalar", lambda e: e.activation(out=dst[:], in_=angm[:], func=AF.Sin), reads=["angm"], writes=[key])

    wrap_and_sin(sinT, "sinT")
    S.op("vector", lambda e: e.tensor_scalar(out=ang2[:], in0=ang2[:], scalar1=PI / 2, scalar2=None, op0=ALU.add),
         reads=["ang2", "sinT"], writes=["ang2"])
    wrap_and_sin(cosT, "cosT")

    S.barrier()
    arena_off[0] = MARK_SETUP
    gc_sbL = [gc_sb, wk("gc_sb2", [128, 256], F32)]
    vconvL = [vconv, wk("vconv2", [128, 256], F32)]
    yconvL = [yconv, wk("yconv2", [128, 256], F32)]
    csqL = [csq, wk("csq2", [128, 256], BF16)]
    crsL = [crs, wk("crs2", [128, 256], F32)]
    qlnTL = [qlnT, wk("qlnT2", [128, 2, 128], BF16)]
    ckvTL = [ckvT, wk("ckvT2", [128, 128], BF16)]
    def rstd_from_ss(ss_ap, out_ap, n, T, keys_r, keys_w, tmp_ap, tmpkey):
        S.op("scalar", lambda e: e.activation(out=tmp_ap, in_=ss_ap, func=AF.Ln, bias=eps_c[0:T, :], scale=1.0 / n),
             reads=keys_r + ["eps_c"], writes=[tmpkey])
        S.op("scalar", lambda e: e.activation(out=out_ap, in_=tmp_ap, func=AF.Exp, scale=-0.5),
             reads=[tmpkey], writes=keys_w)

    def rope(dst1, dst2, x1, x2, cos, sin, tmp, rkeys, wkeys, tkey):
        S.op("vector", lambda e: e.tensor_tensor(out=tmp[0], in0=x1, in1=cos, op=ALU.mult), reads=rkeys, writes=[(tkey, 0)])
        S.op("vector", lambda e: e.tensor_tensor(out=tmp[1], in0=x2, in1=sin, op=ALU.mult), reads=rkeys, writes=[(tkey, 1)])
        S.op("vector", lambda e: e.tensor_tensor(out=tmp[2], in0=x1, in1=sin, op=ALU.mult), reads=rkeys, writes=[(tkey, 2)])
        S.op("vector", lambda e: e.tensor_tensor(out=tmp[3], in0=x2, in1=cos, op=ALU.mult), reads=rkeys, writes=[(tkey, 3)])
        S.op("vector", lambda e: e.tensor_tensor(out=dst1, in0=tmp[0], in1=tmp[1], op=ALU.subtract),
             reads=[(tkey, 0), (tkey, 1)], writes=[wkeys[0]])
        S.op("vector", lambda e: e.tensor_tensor(out=dst2, in0=tmp[2], in1=tmp[3], op=ALU.add),
             reads=[(tkey, 2), (tkey, 3)], writes=[wkeys[1]])

    def token_tile(ti, part):
        S.mute = (part == "B")
        T = 128 if ti < 16 else NS
        tok0 = ti * 128
        sl = ti % 2
        src = x_p[tok0:tok0 + 128, :] if ti < 16 else x_s[:, :]
        X, H, ST = xin[sl], hbf[sl], st1[sl]
        hTc = hT[(ti // 2) % 2]
        col0 = (ti % 2) * 128
        if ti in (16, 0):
            S.dma("sync", "ldx%d" % sl, X[0:T, :], src, writes=[("xin", sl)])
        if ti < 15:
            nsl = (ti + 1) % 2
            S.dma("sync", "ldx%d" % nsl, xin[nsl][:, :], x_p[(ti + 1) * 128:(ti + 2) * 128, :], writes=[("xin", nsl)])
        S.op("scalar", lambda e: e.activation(out=junk[0:T, :], in_=X[0:T, :], func=AF.Square, accum_out=ST[0:T, 0:1]),
             reads=[("xin", sl)], writes=[("st", sl, 0)])
        rstd_from_ss(ST[0:T, 0:1], ST[0:T, 1:2], D, T, [("st", sl, 0)], [("st", sl, 1)], ST[0:T, 2:3], ("st", sl, 2))
        S.op("vector", lambda e: e.scalar_tensor_tensor(out=H[0:T, :], in0=X[0:T, :], scalar=ST[0:T, 1:2],
                                                        in1=gmix_b[0:T, :], op0=ALU.mult, op1=ALU.mult),
             reads=[("xin", sl), ("st", sl, 1), "gmix_b"], writes=[("hbf", sl)])
        pT = psb(0)

        def tr_h(e):
            ins = None
            for k in range(8):
                ins = e.transpose(out=pT[:, k * 128:k * 128 + T], in_=H[0:T, k * 128:(k + 1) * 128], identity=ident[0:T, 0:T])
            return ins
        S.op("tensor", tr_h, reads=[("hbf", sl), "ident"], writes=["ps0"])
        S.op("scalar", lambda e: e.copy(out=hTc[:, :, col0:col0 + T],
                                        in_=pT[:, 0:1024].rearrange("p (k t) -> p k t", k=8)[:, :, 0:T]),
             reads=["ps0"], writes=[("hT", (ti // 2) % 2, ti % 2)])
        hkey = ("hT", (ti // 2) % 2, ti % 2)

        def mm_small(e):
            ins = None
            for k in range(8):
                ins = e.matmul(ps[1][0:T, 0:416], lhsT=hTc[:, k, col0:col0 + T], rhs=w_in_sb[:, k, 3 * CW:PW],
                               start=(k == 0), stop=(k == 7))
            return ins
        S.op("tensor", mm_small, reads=[hkey] + [("w_in", k) for k in range(8)], writes=["ps1"])
        zs = ps[1]
        for j, (a, b) in enumerate(((0, QR), (QR, QR + KVR), (QR + KVR, QR + KVR + RD))):
            S.op("scalar", lambda e, a=a, b=b, j=j: e.activation(out=junk[0:T, a:b], in_=zs[0:T, a:b], func=AF.Square,
                                                                 accum_out=ST[0:T, 4 + j:5 + j]),
                 reads=["ps1"], writes=[("st", sl, 4 + j)])
        rstd_from_ss(ST[0:T, 4:5], ST[0:T, 8:9], QR, T, [("st", sl, 4)], [("st", sl, 8)], ST[0:T, 12:13], ("st", sl, 12))
        rstd_from_ss(ST[0:T, 5:6], ST[0:T, 9:10], KVR, T, [("st", sl, 5)], [("st", sl, 9)], ST[0:T, 13:14], ("st", sl, 13))
        rstd_from_ss(ST[0:T, 6:7], ST[0:T, 10:11], RD, T, [("st", sl, 6)], [("st", sl, 10)], ST[0:T, 14:15], ("st", sl, 14))
        S.op("vector", lambda e: e.scalar_tensor_tensor(out=qln[0:T, :], in0=zs[0:T, 0:QR], scalar=ST[0:T, 8:9],
                                                        in1=gql_b[0:T, :], op0=ALU.mult, op1=ALU.mult),
             reads=["ps1", ("st", sl, 8), "gql_b"], writes=["qln"])
        CK = ckv_f[sl]
        S.op("vector", lambda e: e.scalar_tensor_tensor(out=CK[0:T, :], in0=zs[0:T, QR:QR + KVR], scalar=ST[0:T, 9:10],
                                                        in1=gkv_b[0:T, :], op0=ALU.mult, op1=ALU.mult),
             reads=["ps1", ("st", sl, 9), "gkv_b"], writes=[("ckv_f", sl)])
        S.op("vector", lambda e: e.scalar_tensor_tensor(out=krn[0:T, :], in0=zs[0:T, QR + KVR:QR + KVR + RD],
                                                        scalar=ST[0:T, 10:11], in1=gkr_b[0:T, :], op0=ALU.mult, op1=ALU.mult),
             reads=["ps1", ("st", sl, 10), "gkr_b"], writes=["krn"])
        dst_ckv = o_ckv_p[tok0:tok0 + 128, :] if ti < 16 else o_ckv_s[:, :]
        out_recs.append(S.dma("sync", "stc%d" % sl, dst_ckv, CK[0:T, :], reads=[("ckv_f", sl)]))
        S.op("gpsimd", lambda e: e.tensor_copy(out=ckv_b[0:T, :], in_=CK[0:T, :]), reads=[("ckv_f", sl)], writes=["ckv_b"])
        if ti == 16:
            S.op("gpsimd", lambda e: e.tensor_copy(out=ckv_s_b[0:T, 0:KVR], in_=CK[0:T, :]), reads=[("ckv_f", sl)], writes=["ckv_s_b"])
            S.op("gpsimd", lambda e: e.memset(ckv_s_b[0:T, KVR:KVR + 1], 1.0), writes=["ckv_s_b1"])
        KR = kr_f[sl]
        rope(KR[0:T, 0:16], KR[0:T, 16:32], krn[0:T, 0:16], krn[0:T, 16:32], cosT[0:T, ti, :], sinT[0:T, ti, :],
             [rtmp[0:T, i, :] for i in range(4)], ["krn", "cosT", "sinT"], [("kr_f", sl, 0), ("kr_f", sl, 1)], "rtmp")
        dst_kr = o_kr_p[tok0:tok0 + 128, :] if ti < 16 else o_kr_s[:, :]
        out_recs.append(S.dma("sync", "stk%d" % sl, dst_kr, KR[0:T, :], reads=[("kr_f", sl, 0), ("kr_f", sl, 1)]))

        pT2 = psb(2)

        def tr_q(e):
            e.transpose(out=pT2[:, 0:T], in_=qln[0:T, 0:128], identity=ident[0:T, 0:T])
            e.transpose(out=pT2[:, 128:128 + T], in_=qln[0:T, 128:256], identity=ident[0:T, 0:T])
            return e.transpose(out=pT2[:, 256:256 + T], in_=ckv_b[0:T, :], identity=ident[0:T, 0:T])
        S.op("tensor", tr_q, reads=["qln", "ckv_b", "ident"], writes=["ps2"])
        qlnT, ckvT = qlnTL[sl], ckvTL[sl]
        S.op("vector", lambda e: e.tensor_copy(out=qlnT[:, :, 0:T],
                                               in_=pT2[:, 0:256].rearrange("p (k t) -> p k t", k=2)[:, :, 0:T]),
             reads=["ps2"], writes=[("qlnT", sl)])
        S.op("vector", lambda e: e.tensor_copy(out=ckvT[:, 0:T], in_=pT2[:, 256:256 + T]), reads=["ps2"], writes=[("ckvT", sl)])
        S.mute = (part == "A")

        def mm_q(e):
            ins = None
            for half in range(2):
                for k in range(2):
                    ins = e.matmul(ps[3 + half][0:T, 0:384], lhsT=qlnT[:, k, 0:T], rhs=w_uq_sb[:, k, half * 384:(half + 1) * 384],
                                   start=(k == 0), stop=(k == 1))
            return ins
        S.op("tensor", mm_q, reads=[("qlnT", sl), "w_uq"], writes=["ps3", "ps4"])

        def mm_kv(e):
            e.matmul(ps[5][0:T, :], lhsT=ckvT[:, 0:T], rhs=w_uk_sb[:, :], start=True, stop=True)
            return e.matmul(ps[6][0:T, :], lhsT=ckvT[:, 0:T], rhs=w_uv_sb[:, :], start=True, stop=True)
        S.op("tensor", mm_kv, reads=[("ckvT", sl), "w_uk", "w_uv"], writes=["ps5", "ps6"])

        for half in range(2):
            S.op("scalar", lambda e, half=half: e.activation(out=qsq[0:T, half * 384:(half + 1) * 384],
                                                             in_=ps[3 + half][0:T, 0:384], func=AF.Square),
                 reads=["ps%d" % (3 + half)], writes=[("qsq", half)])
        qsq_v = qsq[:, :].rearrange("p (h d) -> p h d", h=NH)
        S.op("vector", lambda e: e.reduce_sum(out=ST[0:T, 16:24], in_=qsq_v[0:T, :, 0:ND], axis=AX.X),
             reads=[("qsq", 0), ("qsq", 1)], writes=[("st", sl, 16)])
        S.op("vector", lambda e: e.reduce_sum(out=ST[0:T, 24:32], in_=qsq_v[0:T, :, ND:ND + RD], axis=AX.X),
             reads=[("qsq", 0), ("qsq", 1)], writes=[("st", sl, 24)])
        rstd_from_ss(ST[0:T, 16:24], ST[0:T, 32:40], ND, T, [("st", sl, 16)], [("st", sl, 32)], ST[0:T, 48:56], ("st", sl, 48))
        rstd_from_ss(ST[0:T, 24:32], ST[0:T, 40:48], RD, T, [("st", sl, 24)], [("st", sl, 40)], ST[0:T, 56:64], ("st", sl, 56))
        for half in range(2):
            hs = slice(half * 4, half * 4 + 4)
            qps = ps[3 + half][0:T, 0:384].rearrange("p (h d) -> p h d", h=4)
            S.op("vector", lambda e, hs=hs, qps=qps: e.tensor_tensor(
                out=qn[0:T, hs, 0:ND], in0=qps[:, :, 0:ND],
                in1=ST[0:T, 32 + hs.start:32 + hs.stop].unsqueeze(2).to_broadcast([T, 4, ND]), op=ALU.mult),
                reads=["ps%d" % (3 + half), ("st", sl, 32)], writes=[("qn", half, 0)])
            S.op("vector", lambda e, hs=hs, qps=qps: e.tensor_tensor(
                out=qn[0:T, hs, ND:ND + RD], in0=qps[:, :, ND:ND + RD],
                in1=ST[0:T, 40 + hs.start:40 + hs.stop].unsqueeze(2).to_broadcast([T, 4, RD]), op=ALU.mult),
                reads=["ps%d" % (3 + half), ("st", sl, 40)], writes=[("qn", half, 1)])
        qnk = [("qn", h2, j) for h2 in range(2) for j in range(2)]
        S.op("vector", lambda e: e.tensor_tensor(out=qn[0:T, :, :], in0=qn[0:T, :, :],
                                                 in1=gq_b[0:T, :].unsqueeze(1).to_broadcast([T, NH, ND + RD]), op=ALU.mult),
             reads=qnk + ["gq_b0", "gq_b1"], writes=["qn_g"])
        S.op("gpsimd", lambda e: e.tensor_copy(out=Qb[0:T, :, 0:ND], in_=qn[0:T, :, 0:ND]), reads=["qn_g"], writes=["Qb_n"])
        cosb = cosT[0:T, ti, :].unsqueeze(1).to_broadcast([T, NH, 16])
        sinb = sinT[0:T, ti, :].unsqueeze(1).to_broadcast([T, NH, 16])
        rope(Qb[0:T, :, ND:ND + 16], Qb[0:T, :, ND + 16:ND + 32], qn[0:T, :, ND:ND + 16], qn[0:T, :, ND + 16:ND + 32],
             cosb, sinb, [qrt[0:T, i, :, :] for i in range(4)], ["qn_g", "cosT", "sinT"], ["Qb_r0", "Qb_r1"], "qrt")

        S.op("scalar", lambda e: e.activation(out=ksq[0:T, 0:512], in_=ps[5][0:T, :], func=AF.Square),
             reads=["ps5"], writes=["ksq"])
        S.op("vector", lambda e: e.reduce_sum(out=ST[0:T, 64:72], in_=ksq[0:T, 0:512].rearrange("p (h d) -> p h d", h=NH), axis=AX.X),
             reads=["ksq"], writes=[("st", sl, 64)])
        rstd_from_ss(ST[0:T, 64:72], ST[0:T, 72:80], ND, T, [("st", sl, 64)], [("st", sl, 72)], ST[0:T, 80:88], ("st", sl, 80))
        S.op("vector", lambda e: e.tensor_tensor(out=kn_t[0:T, :, :], in0=ps[5][0:T, :].rearrange("p (h d) -> p h d", h=NH),
                                                 in1=ST[0:T, 72:80].unsqueeze(2).to_broadcast([T, NH, ND]), op=ALU.mult),
             reads=["ps5", ("st", sl, 72)], writes=["kn_t"])
        S.op("vector", lambda e: e.tensor_tensor(out=Kb[0:T, :, 0:ND], in0=kn_t[0:T, :, :],
                                                 in1=gkn_b[0:T, :].unsqueeze(1).to_broadcast([T, NH, ND]), op=ALU.mult),
             reads=["kn_t", "gkn_b"], writes=["Kb_n"])
        S.op("gpsimd", lambda e: e.tensor_copy(out=Kb[0:T, :, ND:ND + RD], in_=KR[0:T, :].unsqueeze(1).to_broadcast([T, NH, RD])),
             reads=[("kr_f", sl, 0), ("kr_f", sl, 1)], writes=["Kb_r"])
        if ti < 16:
            S.op("scalar", lambda e: e.copy(out=Vaug[:, ti, :, 0:VD], in_=ps[6][:, :].rearrange("p (h d) -> p h d", h=NH)),
                 reads=["ps6"], writes=[("Vaug", ti)])

        pQ = psb(7)

        def tr_Q(e):
            ins = None
            for h in range(NH):
                ins = e.transpose(out=pQ[0:96, h * 128:h * 128 + T], in_=Qb[0:T, h, :], identity=ident[0:T, 0:T])
            return ins
        S.op("tensor", tr_Q, reads=["Qb_n", "Qb_r0", "Qb_r1", "ident"], writes=["ps7"])
        S.op("vector", lambda e: e.tensor_copy(out=QT[0:96, :, tok0:tok0 + T],
                                               in_=pQ[0:96, 0:1024].rearrange("p (h t) -> p h t", h=NH)[:, :, 0:T]),
             reads=["ps7"], writes=[("QT", ti)])
        pK = psb(0)

        def tr_K(e):
            ins = None
            for h in range(NH):
                ins = e.transpose(out=pK[0:96, h * 128:h * 128 + T], in_=Kb[0:T, h, :], identity=ident[0:T, 0:T])
            return ins
        S.op("tensor", tr_K, reads=["Kb_n", "Kb_r", "ident"], writes=["ps0"])
        S.op("scalar", lambda e: e.copy(out=KT[0:96, :, tok0:tok0 + T],
                                        in_=pK[0:96, 0:1024].rearrange("p (h t) -> p h t", h=NH)[:, :, 0:T]),
             reads=["ps0"], writes=[("KT", ti)])
        S.mute = False

    def conv_chunk(ci):
        if ci < 8:
            nseg, L = 1, 256
            hTc = hT[ci % 2]
            hkeys = [("hT", ci % 2, j) for j in range(2)]
            tok0 = ci * 256
        else:
            nseg, L = 4, 8
            hTc = hT[0]
            hkeys = [("hT", 0, 0)]
            tok0 = SEQ
        N = nseg * L
        for c in range(4):
            U = ubuf[c] if ci < 8 else usam[c]
            ukey = ("ubuf", c) if ci < 8 else ("usam", c)
            ukeys = [ukey] if ci < 8 else [("usam", c, q_) for q_ in range(4)]
            cwk = [("convw_c", k) for k in range(3)]
            if ci < 8:
                ufull = U[:, :].rearrange("p (s l) -> p s l", s=1)
            else:
                ufull = U[:, :, :]

            if S.win is not None:
                S.cur_stream = 2 + c
            pc = c % 2
            bk = (1, 2, 3, 4) if pc == 0 else (5, 6, 7, 0)
            gc_, vc_, yc_, cs_, cr_ = gc_sbL[pc], vconvL[pc], yconvL[pc], csqL[pc], crsL[pc]
            kx = lambda nm: (nm, pc)

            def mm(e, c=c, bk=bk):
                ins = None
                for j, off in enumerate((0, 2 * CW, CW)):
                    for k in range(8):
                        ins = e.matmul(ps[bk[j]][:, 0:N], lhsT=w_in_sb[:, k, off + c * 128:off + (c + 1) * 128],
                                       rhs=hTc[:, k, 0:N], start=(k == 0), stop=(k == 7))
                return ins
            S.op("tensor", mm, reads=hkeys + [("w_in", k) for k in range(8)], writes=["ps%d" % bk[0], "ps%d" % bk[1], "ps%d" % bk[2]])
            S.op("scalar", lambda e, bk=bk, gc_=gc_: e.copy(out=gc_[:, 0:N], in_=ps[bk[1]][:, 0:N]), reads=["ps%d" % bk[1]], writes=[kx("gc_sb")])
            S.op("vector", lambda e, ufull=ufull, bk=bk, gc_=gc_: e.tensor_tensor(
                out=ufull[:, :, 2:2 + L], in0=ps[bk[0]][:, 0:N].rearrange("p (s l) -> p s l", s=nseg),
                in1=gc_[:, 0:N].rearrange("p (s l) -> p s l", s=nseg), op=ALU.mult),
                reads=["ps%d" % bk[0], kx("gc_sb")], writes=[(ukey, "body")])
            v3 = vc_[:, 0:N].rearrange("p (s l) -> p s l", s=nseg)
            S.op("vector", lambda e, c=c, ufull=ufull, v3=v3: e.tensor_scalar(
                out=v3, in0=ufull[:, :, 2:2 + L], scalar1=convw_c[:, 2, c:c + 1], scalar2=convb_c[:, c:c + 1],
                op0=ALU.mult, op1=ALU.add), reads=[(ukey, "body")] + cwk + ["convb_c"], writes=[kx("vconv")])
            for tap in (1, 0):
                S.op("vector", lambda e, c=c, tap=tap, ufull=ufull, v3=v3: e.scalar_tensor_tensor(
                    out=v3, in0=ufull[:, :, tap:tap + L], scalar=convw_c[:, tap, c:c + 1], in1=v3,
                    op0=ALU.mult, op1=ALU.add), reads=[(ukey, "body"), kx("vconv")] + ukeys + cwk, writes=[kx("vconv")])
            S.op("vector", lambda e, bk=bk, yc_=yc_, vc_=vc_: e.tensor_tensor(out=yc_[:, 0:N], in0=ps[bk[2]][:, 0:N], in1=vc_[:, 0:N], op=ALU.mult),
                 reads=["ps%d" % bk[2], kx("vconv")], writes=[kx("yconv")])
            S.op("scalar", lambda e, cs_=cs_, yc_=yc_: e.activation(out=cs_[:, 0:N], in_=yc_[:, 0:N], func=AF.Square),
                 reads=[kx("yconv")], writes=[kx("csq")])
            S.op("tensor", lambda e, bk=bk, cs_=cs_: e.matmul(ps[bk[3]][:, 0:N], lhsT=bconv[:, :], rhs=cs_[:, 0:N], start=True, stop=True),
                 reads=[kx("csq"), "bconv"], writes=["ps%d" % bk[3]])
            S.op("scalar", lambda e, bk=bk, cr_=cr_: e.activation(out=cr_[:, 0:N], in_=ps[bk[3]][:, 0:N], func=AF.Ln, bias=eps_c[:, :], scale=1.0),
                 reads=["ps%d" % bk[3], "eps_c"], writes=[kx("crs")])
            S.op("scalar", lambda e, cr_=cr_: e.activation(out=cr_[:, 0:N], in_=cr_[:, 0:N], func=AF.Exp, scale=-0.5),
                 reads=[kx("crs")], writes=[kx("crs")])
            S.op("vector", lambda e, c=c, yc_=yc_, cr_=cr_: e.scalar_tensor_tensor(out=yconvT[:, c, tok0:tok0 + N], in0=yc_[:, 0:N],
                                                                                  scalar=gout_c[:, c:c + 1], in1=cr_[:, 0:N],
                                                                                  op0=ALU.mult, op1=ALU.mult),
                 reads=[kx("yconv"), kx("crs"), "gout_c"], writes=[("yconvT", c, ci)])
            if True:
                if ci == 7:
                    out_recs.append(S.dma("sync", "st", o_conv_p[:, c * 128:(c + 1) * 128].rearrange("t p -> p t"),
                                          U[:, 256:258], reads=[(ukey, "body")], allow_slow_non_contiguous=True))
                elif ci == 8:
                    for q_ in range(4):
                        out_recs.append(S.dma("sync", "st", o_conv_s[q_, :, c * 128:(c + 1) * 128].rearrange("t p -> p t"),
                                              U[:, q_, 8:10], reads=[(ukey, "body")], allow_slow_non_contiguous=True))
            if ci < 7:
                S.op("gpsimd", lambda e, U=U: e.tensor_copy(out=U[:, 0:2], in_=U[:, 256:258]),
                     reads=[(ukey, "body")], writes=[ukey])

    token_tile(16, "A")
    token_tile(16, "B")
    conv_chunk(8)
    token_tile(0, "A")
    for ti in range(16):
        S.window_begin()
        if ti + 1 < 16:
            S.cur_stream = 0
            token_tile(ti + 1, "A")
        S.cur_stream = 1
        token_tile(ti, "B")
        if ti % 2 == 1:
            S.cur_stream = 2
            conv_chunk(ti // 2)
        S.cur_stream = 0
        S.window_end()

    S.barrier()
    arena_reset()
    RG = 16
    c2ckv = cache_ckv.rearrange("n (r t) d -> (n r) (t d)", r=RG)
    c2kr = cache_kr.rearrange("n (r t) d -> (n r) (t d)", r=2)
    idx_i = wk("idx_i", [128, 4], I32)
    idx16 = wk("idx16", [128, 4], I32)
    idx2 = wk("idx2", [128, 4], I32)
    riota = wk("riota", [128, RG], I32)
    idxc = wk("idxc", [128, 4, RG], I32)
    idxk = wk("idxk", [128, 4, 2], I32)
    ckv_g = [wk("ckv_g%d" % i, [128, 1024], BF16) for i in range(3)]
    kr_g = [wk("kr_g%d" % i, [128, 2048], BF16) for i in range(2)]
    cgx = [wk("cgx%d" % i, [128, 8, KVR + 1], BF16) for i in range(2)]
    ckvT_g = [wk("ckvT_g%d" % i, [128, 1024], BF16) for i in range(2)]
    krT_g = [wk("krT_g%d" % i, [128, 1024], BF16) for i in range(2)]
    ksq_s = [wk("ksq_s%d" % i, [128, 2048], BF16) for i in range(2)]
    ss_s = wk("ss_s", [128, 64], F32)
    ln_s = wk("ln_s", [128, 64], F32)
    rs_s = [wk("rs_s%d" % i, [128, 64], F32) for i in range(2)]
    tmpS = [wk("tmpS%d" % i, [128, 512], F32) for i in range(2)]
    Es = [wk("Es%d" % i, [128, 512], BF16) for i in range(2)]
    QabsT = wk("QabsT", [128, 4, 64], BF16)
    QrT = wk("QrT", [128, 4, 64], BF16)
    Qg = wk("Qg", [128, NH, NS], BF16)
    w_ukT = wk("w_ukT", [128, NH, 128], BF16)
    En = wk("En", [128, NH, NS], F32)
    En_s = wk("En_s", [128, 4, 64], BF16)
    mk_i = wk("mk_i", [128, 4], I32)
    mk_f = wk("mk_f", [128, 4], F32)
    mcol_i = wk("mcol_i", [128, 2, NS], I32)
    mcol_f = wk("mcol_f", [128, 2, NS], F32)
    mnew = wk("mnew", [128, 2, NS], F32)
    rsum = wk("rsum", [128, 1], F32)
    ctxn = wk("ctxn", [128, 128], BF16)
    ctxT = wk("ctxT", [128, 64], BF16)
    ysq = wk("ysq", [128, 256], BF16)
    yr = wk("yr", [128, 256], F32)
    battn_s = wk("battn_s", [128, 64], BF16)

    V = "vector"
    S.dma("sync", "ld", idx_i[:, :], ptab.rearrange("s p -> p s"), writes=["idx_i"], allow_slow_non_contiguous=True)
    S.op("gpsimd", lambda e: e.iota(riota[:, :], pattern=[[1, RG]], base=0, channel_multiplier=0), writes=["riota"])
    S.op("gpsimd", lambda e: e.memset(battn_s[:, :], 1.0 / 64), writes=["battn_s"])
    for q_ in range(2):
        S.op("gpsimd", lambda e, q_=q_: e.memset(cgx[q_][:, :, KVR:KVR + 1], 1.0), writes=[("cgx1", q_)])
    idx_f = wk("idx_f", [128, 4], F32)
    riota_f = wk("riota_f", [128, RG], F32)
    idxc_f = wk("idxc_f", [128, 4, RG], F32)
    idxk_f = wk("idxk_f", [128, 4, 2], F32)
    S.op(V, lambda e: e.tensor_copy(out=idx_f[:, :], in_=idx_i[:, :]), reads=["idx_i"], writes=["idx_f"])
    S.op(V, lambda e: e.tensor_copy(out=riota_f[:, :], in_=riota[:, :]), reads=["riota"], writes=["riota_f"])
    for q_ in range(4):
        S.op(V, lambda e, q_=q_: e.scalar_tensor_tensor(out=idxc_f[:, q_, :], in0=idx_f[:, q_:q_ + 1].to_broadcast([128, RG]),
                                                        scalar=float(RG), in1=riota_f[:, :], op0=ALU.mult, op1=ALU.add),
             reads=["idx_f", "riota_f"], writes=[("idxc_f", q_)])
        S.op(V, lambda e, q_=q_: e.scalar_tensor_tensor(out=idxk_f[:, q_, :], in0=idx_f[:, q_:q_ + 1].to_broadcast([128, 2]),
                                                        scalar=2.0, in1=riota_f[:, 0:2], op0=ALU.mult, op1=ALU.add),
             reads=["idx_f", "riota_f"], writes=[("idxk_f", q_)])
    S.op(V, lambda e: e.tensor_copy(out=idxc[:, :, :], in_=idxc_f[:, :, :]), reads=[("idxc_f", q_) for q_ in range(4)], writes=["idxc"])
    S.op(V, lambda e: e.tensor_copy(out=idxk[:, :, :], in_=idxk_f[:, :, :]), reads=[("idxk_f", q_) for q_ in range(4)], writes=["idxk"])

    def tr_wuk(e):
        ins = None
        for h in range(NH):
            ins = e.transpose(out=psb(0)[0:64, h * 128:(h + 1) * 128], in_=w_uk_sb[:, h * 64:(h + 1) * 64], identity=ident[:, :])
        return ins
    S.op("tensor", tr_wuk, reads=["w_uk", "ident"], writes=["ps0"])
    S.op(V, lambda e: e.tensor_copy(out=w_ukT[0:64, :, :], in_=psb(0)[0:64, 0:1024].rearrange("p (h d) -> p h d", h=NH)),
         reads=["ps0"], writes=["w_ukT"])
    S.op(V, lambda e: e.tensor_scalar(out=Qg[0:64, :, :], in0=QT[0:64, :, SEQ:SEQ + NS], scalar1=gkn_c[0:64, 0:1], scalar2=None,
                                      op0=ALU.mult), reads=[("QT", 16), "gkn_c"], writes=["Qg"])

    def mm_qabs(e):
        ins = None
        for h in range(NH):
            ins = e.matmul(ps[2][:, h * NS:(h + 1) * NS], lhsT=w_ukT[0:64, h, :], rhs=Qg[0:64, h, :], start=True, stop=True)
        return ins
    S.op("tensor", mm_qabs, reads=["w_ukT", "Qg"], writes=["ps2"])
    S.op(V, lambda e: e.tensor_copy(out=QabsT[:, :, :].rearrange("p s (h t) -> p h s t", h=NH),
                                    in_=ps[2][:, 0:NH * NS].rearrange("p (h s t) -> p h s t", h=NH, s=4)),
         reads=["ps2"], writes=["QabsT"])
    S.op(V, lambda e: e.tensor_copy(out=QrT[0:32, :, :].rearrange("p s (h t) -> p h s t", h=NH),
                                    in_=QT[64:96, :, SEQ:SEQ + NS].rearrange("p h (s t) -> p h s t", s=4)),
         reads=[("QT", 16)], writes=["QrT"])

    def mm_new(e):
        ins = None
        for h in range(NH):
            ins = e.matmul(ps[3][0:NS, h * NS:(h + 1) * NS], lhsT=KT[0:96, h, SEQ:SEQ + NS], rhs=QT[0:96, h, SEQ:SEQ + NS],
                           start=True, stop=True)
        return ins
    S.op("tensor", mm_new, reads=[("KT", 16), ("QT", 16)], writes=["ps3"])
    S.op("scalar", lambda e: e.activation(out=En[0:NS, :, :], in_=ps[3][0:NS, 0:NH * NS].rearrange("p (h q) -> p h q", h=NH),
                                          func=AF.Exp, scale=SCALE), reads=["ps3"], writes=["En"])
    S.op("gpsimd", lambda e: e.iota(mk_i[:, 0:1], pattern=[[0, 1]], base=0, channel_multiplier=1), writes=["mk_i0"])
    S.op(V, lambda e: e.tensor_single_scalar(out=mk_i[:, 1:2], in_=mk_i[:, 0:1], scalar=3, op=ALU.arith_shift_right),
         reads=["mk_i0"], writes=["mk_i1"])
    S.op(V, lambda e: e.tensor_single_scalar(out=mk_i[:, 2:3], in_=mk_i[:, 0:1], scalar=7, op=ALU.bitwise_and),
         reads=["mk_i0"], writes=["mk_i2"])
    S.op(V, lambda e: e.tensor_copy(out=mk_f[:, :], in_=mk_i[:, :]), reads=["mk_i0", "mk_i1", "mk_i2"], writes=["mk_f"])
    S.op("gpsimd", lambda e: e.iota(mcol_i[:, 0, :], pattern=[[1, 4], [0, 8]], base=0, channel_multiplier=0), writes=["mcol0"])
    S.op("gpsimd", lambda e: e.iota(mcol_i[:, 1, :], pattern=[[0, 4], [1, 8]], base=0, channel_multiplier=0), writes=["mcol1"])
    S.op(V, lambda e: e.tensor_copy(out=mcol_f[:, :, :], in_=mcol_i[:, :, :]), reads=["mcol0", "mcol1"], writes=["mcol_f"])
    S.op(V, lambda e: e.tensor_scalar(out=mnew[:, 0, :], in0=mcol_f[:, 0, :], scalar1=mk_f[:, 1:2], scalar2=None, op0=ALU.is_equal),
         reads=["mcol_f", "mk_f"], writes=["mnew0"])
    S.op(V, lambda e: e.tensor_scalar(out=mnew[:, 1, :], in0=mcol_f[:, 1, :], scalar1=mk_f[:, 2:3], scalar2=None, op0=ALU.is_ge),
         reads=["mcol_f", "mk_f"], writes=["mnew1"])
    S.op(V, lambda e: e.tensor_tensor(out=mnew[:, 0, :], in0=mnew[:, 0, :], in1=mnew[:, 1, :], op=ALU.mult),
         reads=["mnew0", "mnew1"], writes=["mnew0"])
    S.op(V, lambda e: e.tensor_tensor(out=En[0:NS, :, :], in0=En[0:NS, :, :],
                                      in1=mnew[0:NS, 0, :].unsqueeze(1).to_broadcast([NS, NH, NS]), op=ALU.mult),
         reads=["En", "mnew0"], writes=["En"])
    S.op(V, lambda e: e.tensor_copy(out=En_s[0:NS, :, :].rearrange("p s (h t) -> p h s t", h=NH),
                                    in_=En[0:NS, :, :].rearrange("p h (s t) -> p h s t", s=4)),
         reads=["En"], writes=["En_s"])

    NG = 4 * RG

    def gather_group(g):
        s_, r = divmod(g, RG)
        cg = ckv_g[g % 3]
        S.op("gpsimd", lambda e: e.indirect_dma_start(
            out=cg[:, :], out_offset=None, in_=c2ckv[:, :],
            in_offset=bass.IndirectOffsetOnAxis(ap=idxc[:, s_, r:r + 1], axis=0)),
            reads=["idxc"], writes=[("ckv_g", g % 3)], dma="gc%d" % (g % 3))
        if r % 8 == 0:
            kb = (g // 8) % 2
            S.op("gpsimd", lambda e: e.indirect_dma_start(
                out=kr_g[kb][:, :], out_offset=None, in_=c2kr[:, :],
                in_offset=bass.IndirectOffsetOnAxis(ap=idxk[:, s_, r // 8:r // 8 + 1], axis=0)),
                reads=["idxk"], writes=[("kr_g", kb)], dma="gk%d" % kb)

    def front(g):
        s_, r = divmod(g, RG)
        if g + 2 < NG:
            gather_group(g + 2)
        if g % 3 == 2:
            issue_conv(1)
        cg = ckv_g[g % 3]
        kb = (g // 8) % 2
        cT, kT_ = ckvT_g[g % 2], krT_g[g % 2]
        rs_ = rs_s[g % 2]
        cx = cgx[g % 2]
        S.op("gpsimd", lambda e: e.tensor_copy(out=cx[:, :, 0:KVR], in_=cg[:, :].rearrange("p (t d) -> p t d", t=8)),
             reads=[("ckv_g", g % 3)], writes=[("cgx", g % 2)])

        def tr_c(e):
            ins = None
            for t in range(8):
                ins = e.transpose(out=psb(0)[:, t * 128:(t + 1) * 128], in_=cg[:, t * 128:(t + 1) * 128], identity=ident[:, :])
            return ins
        S.op("tensor", tr_c, reads=[("ckv_g", g % 3), "ident"], writes=["ps0"])
        S.op("scalar", lambda e: e.copy(out=cT[:, :], in_=psb(0)[:, 0:1024]), reads=["ps0"], writes=[("ckvT_g", g % 2)])

        def tr_k(e):
            ins = None
            for t in range(8):
                tl = (r % 8) * 8 + t
                ins = e.transpose(out=psb(1)[0:32, t * 128:(t + 1) * 128], in_=kr_g[kb][:, tl * 32:(tl + 1) * 32], identity=ident[:, :])
            return ins
        S.op("tensor", tr_k, reads=[("kr_g", kb), "ident"], writes=["ps1"])
        S.op(V, lambda e: e.tensor_copy(out=kT_[0:32, :], in_=psb(1)[0:32, 0:1024]), reads=["ps1"], writes=[("krT_g", g % 2)])
        for rd in range(4):
            b0 = 2 + 2 * (rd % 2)

            def mm_kn(e, rd=rd, b0=b0):
                ins = None
                for tt_ in range(2):
                    t = rd * 2 + tt_
                    ins = e.matmul(ps[b0 + tt_][:, :], lhsT=cT[:, t * 128:(t + 1) * 128], rhs=w_uk_sb[:, :], start=True, stop=True)
                return ins
            S.op("tensor", mm_kn, reads=[("ckvT_g", g % 2), "w_uk"], writes=["ps%d" % b0, "ps%d" % (b0 + 1)])
            S.op("scalar", lambda e, rd=rd, b0=b0: e.activation(out=ksq_s[rd % 2][:, 0:1024], in_=ps_big[:, b0 * 512:(b0 + 2) * 512],
                                                                func=AF.Square),
                 reads=["ps%d" % b0, "ps%d" % (b0 + 1)], writes=[("ksq_s", rd % 2)])
            S.op(V, lambda e, rd=rd: e.reduce_sum(out=ss_s[:, rd * 16:(rd + 1) * 16],
                                                  in_=ksq_s[rd % 2][:, 0:1024].rearrange("p (a d) -> p a d", d=ND), axis=AX.X),
                 reads=[("ksq_s", rd % 2)], writes=[("ss_s", rd)])
        S.op("scalar", lambda e: e.activation(out=ln_s[:, :], in_=ss_s[:, :], func=AF.Ln, bias=eps_c[:, :], scale=1.0 / ND),
             reads=[("ss_s", q_) for q_ in range(4)] + ["eps_c"], writes=["ln_s"])
        S.op("scalar", lambda e: e.activation(out=rs_[:, :], in_=ln_s[:, :], func=AF.Exp, scale=-0.5),
             reads=["ln_s"], writes=[("rs_s", g % 2)])

    def back(g):
        s_, r = divmod(g, RG)
        cT, kT_ = ckvT_g[g % 2], krT_g[g % 2]
        rs_ = rs_s[g % 2]
        cx = cgx[g % 2]
        if r == 0:
            S.op("tensor", lambda e: e.matmul(ps[7][0:64, 0:KVR + 1], lhsT=En_s[0:NS, s_, :], rhs=ckv_s_b[0:NS, :], start=True, stop=False),
                 reads=["En_s", "ckv_s_b", "ckv_s_b1"], writes=["ps7"])

        def mm_sc(e):
            ins = None
            for t in range(8):
                e.matmul(ps[6][:, t * 64:(t + 1) * 64], lhsT=cT[:, t * 128:(t + 1) * 128], rhs=QabsT[:, s_, :], start=True, stop=True)
            for t in range(8):
                ins = e.matmul(ps[1][:, t * 64:(t + 1) * 64], lhsT=kT_[0:32, t * 128:(t + 1) * 128], rhs=QrT[0:32, s_, :],
                               start=True, stop=True)
            return ins
        S.op("tensor", mm_sc, reads=[("ckvT_g", g % 2), ("krT_g", g % 2), "QabsT", "QrT"], writes=["ps6", "ps1"])
        tS, E_ = tmpS[g % 2], Es[g % 2]
        S.op(V, lambda e: e.tensor_tensor(out=tS[:, :].rearrange("p (a t) -> p a t", t=8),
                                          in0=ps[6][:, :].rearrange("p (a t) -> p a t", t=8),
                                          in1=rs_[:, :].unsqueeze(2).to_broadcast([128, 64, 8]), op=ALU.mult),
             reads=["ps6", ("rs_s", g % 2)], writes=[("tmpS", g % 2)])
        S.op(V, lambda e: e.tensor_tensor(out=tS[:, :], in0=ps[1][:, :], in1=tS[:, :], op=ALU.add),
             reads=["ps1", ("tmpS", g % 2)], writes=[("tmpS", g % 2)])
        S.op("scalar", lambda e: e.activation(out=E_[:, :], in_=tS[:, :], func=AF.Exp, scale=SCALE),
             reads=[("tmpS", g % 2)], writes=[("Es", g % 2)])

        def mm_ctx(e):
            ins = None
            for t in range(8):
                last = (r == RG - 1 and t == 7)
                ins = e.matmul(ps[7][0:64, 0:KVR + 1], lhsT=E_[:, t * 64:(t + 1) * 64], rhs=cx[:, t, :], start=False, stop=last)
            return ins
        S.op("tensor", mm_ctx, reads=[("Es", g % 2), ("cgx", g % 2), ("cgx1", g % 2)], writes=["ps7"])
        if r == RG - 1:
            S.op(V, lambda e: e.reciprocal(out=rsum[0:64, :], in_=ps[7][0:64, 128:129]), reads=["ps7"], writes=["rsum"])
            S.op(V, lambda e: e.tensor_scalar(out=ctxn[0:64, :], in0=ps[7][0:64, 0:128], scalar1=rsum[0:64, 0:1], scalar2=None, op0=ALU.mult),
                 reads=["ps7", "rsum"], writes=["ctxn"])
            S.op("tensor", lambda e: e.transpose(out=ps_ct[:, 0:64], in_=ctxn[0:64, :], identity=ident[0:64, 0:64]),
                 reads=["ctxn", "ident"], writes=["ps7b"])
            S.op(V, lambda e: e.tensor_copy(out=ctxT[:, :], in_=ps_ct[:, 0:64]), reads=["ps7b"], writes=["ctxT"])

            def mm_yv(e):
                ins = None
                for h in range(NH):
                    ins = e.matmul(ps_yv[0:64, h * NS + s_ * 8:h * NS + s_ * 8 + 8], lhsT=w_uv_sb[:, h * 64:(h + 1) * 64],
                                   rhs=ctxT[:, h * 8:(h + 1) * 8], start=True, stop=True)
                return ins
            S.op("tensor", mm_yv, reads=["ctxT", "w_uv"], writes=[("psyv", s_)])

    ps_yv = ps_big[:, 7 * 512 + 256:7 * 512 + 512]
    ps_ct = ps[7].bitcast(BF16)[:, 320:384]
    gather_group(0)
    gather_group(1)
    front(0)
    for g in range(NG):
        S.window_begin()
        if g + 1 < NG:
            S.cur_stream = 0
            front(g + 1)
        S.cur_stream = 1
        back(g)
        S.cur_stream = 0
        S.window_end()
    S.op("scalar", lambda e: e.activation(out=ysq[0:64, :], in_=ps_yv[0:64, 0:256], func=AF.Square),
         reads=[("psyv", q) for q in range(4)], writes=["ysq"])
    S.op("tensor", lambda e: e.matmul(ps[2][0:64, 0:256], lhsT=battn_s[0:64, 0:64], rhs=ysq[0:64, :], start=True, stop=True),
         reads=["ysq", "battn_s"], writes=["ps2"])
    S.op("scalar", lambda e: e.activation(out=yr[0:64, :], in_=ps[2][0:64, 0:256], func=AF.Ln, bias=eps_c[0:64, :], scale=1.0),
         reads=["ps2", "eps_c"], writes=["yr"])
    S.op("scalar", lambda e: e.activation(out=yr[0:64, :], in_=yr[0:64, :], func=AF.Exp, scale=-0.5), reads=["yr"], writes=["yr"])
    for h in range(NH):
        po = (h % 2) * 64
        S.op(V, lambda e, h=h, po=po: e.scalar_tensor_tensor(out=ya_s[po:po + 64, h // 2, :], in0=ps_yv[0:64, h * NS:(h + 1) * NS],
                                                             scalar=gattn_c[0:64, h:h + 1], in1=yr[0:64, h * NS:(h + 1) * NS],
                                                             op0=ALU.mult, op1=ALU.mult),
             reads=[("psyv", q) for q in range(4)] + ["yr", "gattn_c"], writes=["ya_s"])

    S.barrier()
    arena_reset()
    x1s = nc.dram_tensor("x1s", [NTOK, D], F32, kind="Internal").ap()
    OH = wk("OH", [128, 17, 64], F32)
    RK = wk("RK", [128, 17, 4], F32)
    carry = wk("carry", [128, NE], F32)
    P23_MARK = arena_off[0]
    LG = wk("LG", [128, 17, 36], F32)
    MARK_R = arena_off[0]
    H2 = nc.dram_tensor("H2", [NTOK, D], BF16, kind="Internal").ap()
    w_out_sb = wk("w_out_sb", [128, 8, D], BF16)
    Et = [wk("Et%d" % i, [128, 512], BF16) for i in range(6)]
    sqa = wk("sqa", [128, 512], BF16)
    lnr = wk("lnr", [128, 512], F32)
    yattnT = [wk("yattnT%d" % i, [128, 4, 512], BF16) for i in range(2)]
    xr = [wk("xr%d" % i, [128, D], F32) for i in range(2)]
    h2b = wk("h2b", [128, D], BF16)
    junk2 = wk("junk2", [128, D], BF16)
    st2 = [wk("st2_%d" % i, [128, 32], F32) for i in range(2)]
    brb = wk("brb", [128, 36], F32)
    wr_sb = wk("wr_sb", [128, 8, 36], BF16)
    battn = wk("battn", [128, 64], BF16)

    S.dma("gpsimd", "wo", w_out_sb, w_out.rearrange("(k p) n -> p k n", p=128), writes=["w_out"])
    S.dma("gpsimd", "wr0", wr_sb[:, :, 0:4], w_rg.rearrange("(k p) n -> p k n", p=128), writes=["wr0"])
    S.dma("gpsimd", "wr1", wr_sb[:, :, 4:36], w_re.rearrange("(k p) n -> p k n", p=128), writes=["wr1"])
    S.dma("sync", "lg", gmix_b[:], g_ffn[0:1, :].to_broadcast([128, D]), writes=["gffn_b"])
    S.dma("sync", "lb0", brb[:, 0:4], b_rg[0:1, :].to_broadcast([128, 4]), writes=["brb0"])
    S.dma("sync", "lb1", brb[:, 4:36], b_re[0:1, :].to_broadcast([128, NE]), writes=["brb1"])
    S.op("gpsimd", lambda e: e.memset(battn[0:64, :], 1.0 / 64), writes=["battn0"])
    S.op("gpsimd", lambda e: e.memset(battn[64:65, :], EPS), writes=["battn1"])

    def attention_chunk(c):
        ya = yattnT[c % 2]
        nj = 4 * c + 4
        S.window_begin()
        for h in range(NH):
            S.cur_stream = h % 2
            ob = 2 + (h % 2)
            sb0 = 0 if h % 2 == 0 else 5
            eb0 = 3 * (h % 2)

            def q0n(j):
                q0 = max(c * 512, j * 128)
                return q0, (c + 1) * 512 - q0

            def issue_S(j, h=h, sb0=sb0):
                q0, n = q0n(j)
                bank = sb0 + j % 2
                S.op("tensor", lambda e: e.matmul(ps[bank][:, 0:n], lhsT=KT[0:96, h, j * 128:(j + 1) * 128],
                                                  rhs=QT[0:96, h, q0:q0 + n], start=True, stop=True),
                     reads=[("KT", j)] + [("QT", t) for t in range(q0 // 128, 4 * c + 4)], writes=["ps%d" % bank])
            issue_S(0)
            for j in range(nj):
                if j + 1 < nj:
                    issue_S(j + 1)
                q0, n = q0n(j)
                bank, eb = sb0 + j % 2, eb0 + j % 3
                S.op("scalar", lambda e, n=n, bank=bank, eb=eb: e.activation(out=Et[eb][:, 0:n], in_=ps[bank][:, 0:n],
                                                                             func=AF.Exp, scale=SCALE),
                     reads=["ps%d" % bank], writes=[("E", eb)])
                if j >= 4 * c:
                    S.op("gpsimd", lambda e, eb=eb: e.tensor_tensor(out=Et[eb][:, 0:128], in0=Et[eb][:, 0:128], in1=tri[:, :],
                                                                    op=ALU.mult),
                         reads=[("E", eb), "tri"], writes=[("E", eb)])
                S.op("tensor", lambda e, j=j, h=h, n=n, q0=q0, eb=eb, ob=ob: e.matmul(
                    ps[ob][0:65, q0 - c * 512:512], lhsT=Vaug[:, j, h, :], rhs=Et[eb][:, 0:n],
                    start=(j == 0), stop=(j == nj - 1)),
                    reads=[("E", eb), ("Vaug", j), "Vaug_ones"], writes=["ps%d" % ob])
            S.op("scalar", lambda e, ob=ob: e.activation(out=sqa[0:65, :], in_=ps[ob][0:65, :], func=AF.Square),
                 reads=["ps%d" % ob], writes=["sqa"])
            S.op("tensor", lambda e: e.matmul(ps[4][0:64, :], lhsT=battn[0:65, :], rhs=sqa[0:65, :], start=True, stop=True),
                 reads=["sqa", "battn0", "battn1"], writes=["ps4"])
            S.op("scalar", lambda e: e.activation(out=lnr[0:64, :], in_=ps[4][0:64, :], func=AF.Ln),
                 reads=["ps4", "lnr", "lnr2"], writes=["lnr"])
            S.op("scalar", lambda e: e.activation(out=lnr[0:64, :], in_=lnr[0:64, :], func=AF.Exp, scale=-0.5),
                 reads=["lnr"], writes=["lnr"])
            po = (h % 2) * 64
            S.op("vector", lambda e, h=h, ob=ob, po=po, ya=ya: e.scalar_tensor_tensor(
                out=ya[po:po + 64, h // 2, :], in0=ps[ob][0:64, :], scalar=gattn_c[0:64, h:h + 1], in1=lnr[0:64, :],
                op0=ALU.mult, op1=ALU.mult),
                reads=["ps%d" % ob, "lnr", "gattn_c"], writes=[("ya", c % 2, h)])
        S.cur_stream = 0
        S.window_end()

    def merge_tile(ti):
        T = 128 if ti < 16 else NS
        tok0 = ti * 128
        sl = ti % 2
        X, X1, ST = xr[sl], xr[sl], st2[sl]
        src = x_p[tok0:tok0 + 128, :] if ti < 16 else x_s[:, :]
        if ti == 0:
            S.dma("sync", "ldr%d" % sl, X[0:T, :], src, writes=[("xr", sl), ("x1b", sl, 0), ("x1b", sl, 1)])
        if ti < 16:
            nt_ = ti + 1
            nsl = nt_ % 2
            nT = 128 if nt_ < 16 else NS
            nsrc = x_p[nt_ * 128:(nt_ + 1) * 128, :] if nt_ < 16 else x_s[:, :]
            S.dma("sync", "ldr%d" % nsl, xr[nsl][0:nT, :], nsrc, writes=[("xr", nsl), ("x1b", nsl, 0), ("x1b", nsl, 1)])
        if ti < 16:
            ya = yattnT[(ti // 4) % 2]
            acol = (ti % 4) * 128
            yakeys = [("ya", (ti // 4) % 2, h) for h in range(NH)]
        else:
            ya = ya_s
            acol = 0
            yakeys = ["ya_s"]

        def mm(e):
            ins = None
            for half in range(2):
                for k in range(4):
                    ins = e.matmul(ps[5 + half][0:T, :], lhsT=yconvT[:, k, tok0:tok0 + T],
                                   rhs=w_out_sb[:, k, half * 512:(half + 1) * 512], start=(k == 0), stop=False)
                for k in range(4):
                    ins = e.matmul(ps[5 + half][0:T, :], lhsT=ya[:, k, acol:acol + T],
                                   rhs=w_out_sb[:, 4 + k, half * 512:(half + 1) * 512], start=False, stop=(k == 3))
            return ins
        S.op("tensor", mm, reads=yakeys + ["w_out"] + [("yconvT", c, ci) for c in range(4) for ci in range(9)],
             writes=["ps5", "ps6"])
        for half in range(2):
            S.op("vector", lambda e, half=half: e.tensor_tensor(out=X1[0:T, half * 512:(half + 1) * 512],
                                                                in0=ps[5 + half][0:T, :], in1=X[0:T, half * 512:(half + 1) * 512],
                                                                op=ALU.add),
                 reads=["ps%d" % (5 + half), ("xr", sl)], writes=[("x1b", sl, half)])
        x1k = [("x1b", sl, 0), ("x1b", sl, 1)]
        S.dma("sync", "stx%d" % sl, x1s[tok0:tok0 + T, :], X1[0:T, :], reads=x1k, writes=[("x1s", ti)])
        S.op("scalar", lambda e: e.activation(out=junk2[0:T, :], in_=X1[0:T, :], func=AF.Square, accum_out=ST[0:T, 0:1]),
             reads=x1k, writes=[("st2", sl, 0)])
        rstd_from_ss(ST[0:T, 0:1], ST[0:T, 1:2], D, T, [("st2", sl, 0)], [("st2", sl, 1)], ST[0:T, 2:3], ("st2", sl, 2))
        S.op("vector", lambda e: e.scalar_tensor_tensor(out=h2b[0:T, :], in0=X1[0:T, :], scalar=ST[0:T, 1:2],
                                                        in1=gmix_b[0:T, :], op0=ALU.mult, op1=ALU.mult),
             reads=x1k + [("st2", sl, 1), "gffn_b"], writes=["h2b"])
        S.dma("sync", "sth", H2[tok0:tok0 + T, :], h2b[0:T, :], reads=["h2b"], writes=[("H2", ti)])
        pT = psb(7)

        def tr(e):
            ins = None
            for k in range(8):
                ins = e.transpose(out=pT[:, k * 128:k * 128 + T], in_=h2b[0:T, k * 128:(k + 1) * 128], identity=ident[0:T, 0:T])
            return ins
        S.op("tensor", tr, reads=["h2b", "ident"], writes=["ps7"])
        S.op("scalar", lambda e: e.copy(out=QT[:, :, tok0:tok0 + T],
                                        in_=pT[:, 0:1024].rearrange("p (k t) -> p k t", k=8)[:, :, 0:T]),
             reads=["ps7"], writes=[("QT", ti)])

        def mmr(e):
            ins = None
            for k in range(8):
                ins = e.matmul(ps[7][0:T, 0:36], lhsT=QT[:, k, tok0:tok0 + T], rhs=wr_sb[:, k, :], start=(k == 0), stop=(k == 7))
            return ins
        S.op("tensor", mmr, reads=[("QT", ti), "wr0", "wr1"], writes=["ps7"])
        S.op("vector", lambda e: e.tensor_tensor(out=LG[0:T, ti, :], in0=ps[7][0:T, 0:36], in1=brb[0:T, :], op=ALU.add),
             reads=["ps7", "brb0", "brb1"], writes=[("LG", ti)])

    for c in range(4):
        attention_chunk(c)
        for t in range(4):
            merge_tile(4 * c + t)
    merge_tile(16)
    issue_conv(100)

    S.barrier()
    arena_off[0] = MARK_R
    V = "vector"
    NTL = 17
    g4 = wk("g4", [128, NTL, 4], F32)
    goh = wk("goh", [128, NTL, 4], F32)
    pen = wk("pen", [128, NTL, 4], F32)
    sc = wk("sc", [128, 12, NTL], F32)
    em = wk("em", [128, NTL, NE], F32)
    em2 = wk("em2", [128, NTL, NE], F32)
    R7 = wk("R7", [128, NTL, NE], F32)
    CAR = wk("CAR", [128, NTL, NE], F32)
    Mb = wk("Mb", [128, NTL, NE], BF16)
    lst_b = wk("lst_b", [128, 128], BF16)
    S.op("gpsimd", lambda e: e.affine_select(out=lst_b[:, :], in_=onesb[:, :], pattern=[[1, 128]], compare_op=ALU.is_gt, fill=0.0,
                                            base=0, channel_multiplier=-1), reads=["onesb"], writes=["lst_b"])
    SC = lambda i: sc[:, i, :]
    bc4 = lambda ap: ap.unsqueeze(2).to_broadcast([128, NTL, 4])
    bc32 = lambda ap: ap.unsqueeze(2).to_broadcast([128, NTL, NE])
    OH1a, OH2a = OH[:, :, 0:32], OH[:, :, 32:64]
    S.op(V, lambda e: e.reduce_max(out=SC(0), in_=LG[:, :, 0:4], axis=AX.X), writes=["sc0"])
    S.op(V, lambda e: e.tensor_tensor(out=goh[:, :, :], in0=LG[:, :, 0:4], in1=bc4(SC(0)), op=ALU.is_equal), reads=["sc0"], writes=["goh"])
    S.op(V, lambda e: e.tensor_tensor(out=g4[:, :, :], in0=LG[:, :, 0:4], in1=bc4(SC(0)), op=ALU.subtract), reads=["sc0"], writes=["g4"])
    S.op("scalar", lambda e: e.activation(out=g4[:, :, :], in_=g4[:, :, :], func=AF.Exp), reads=["g4"], writes=["g4"])
    S.op(V, lambda e: e.reduce_sum(out=SC(1), in_=g4[:, :, :], axis=AX.X), reads=["g4"], writes=["sc1"])
    S.op(V, lambda e: e.reciprocal(out=SC(2), in_=SC(1)), reads=["sc1"], writes=["sc2"])
    S.op(V, lambda e: e.tensor_scalar(out=pen[:, :, :], in0=goh[:, :, :], scalar1=-1.0, scalar2=1e30, op0=ALU.add, op1=ALU.mult),
         reads=["goh"], writes=["pen"])
    S.op(V, lambda e: e.tensor_tensor(out=em[:, :, :].rearrange("p t (g x) -> p t g x", g=4),
                                      in0=LG[:, :, 4:36].rearrange("p t (g x) -> p t g x", g=4),
                                      in1=pen[:, :, :].unsqueeze(3).to_broadcast([128, NTL, 4, 8]), op=ALU.add),
         reads=["pen"], writes=["em"])
    S.op(V, lambda e: e.reduce_max(out=SC(3), in_=em[:, :, :], axis=AX.X), reads=["em"], writes=["sc3"])
    S.op(V, lambda e: e.tensor_tensor(out=OH1a, in0=em[:, :, :], in1=bc32(SC(3)), op=ALU.is_equal), reads=["em", "sc3"], writes=["oh1"])
    S.op(V, lambda e: e.scalar_tensor_tensor(out=em2[:, :, :], in0=OH1a, scalar=-1e30, in1=em[:, :, :], op0=ALU.mult, op1=ALU.add),
         reads=["oh1", "em"], writes=["em2"])
    S.op(V, lambda e: e.reduce_max(out=SC(4), in_=em2[:, :, :], axis=AX.X), reads=["em2"], writes=["sc4"])
    S.op(V, lambda e: e.tensor_tensor(out=OH2a, in0=em2[:, :, :], in1=bc32(SC(4)), op=ALU.is_equal), reads=["em2", "sc4"], writes=["oh2"])
    S.op(V, lambda e: e.tensor_tensor(out=SC(5), in0=SC(4), in1=SC(3), op=ALU.subtract), reads=["sc3", "sc4"], writes=["sc5"])
    S.op("scalar", lambda e: e.activation(out=SC(6), in_=SC(5), func=AF.Exp), reads=["sc5"], writes=["sc6"])
    S.op(V, lambda e: e.tensor_scalar(out=SC(7), in0=SC(6), scalar1=1.0, scalar2=None, op0=ALU.add), reads=["sc6"], writes=["sc7"])
    S.op(V, lambda e: e.reciprocal(out=SC(8), in_=SC(7)), reads=["sc7"], writes=["sc8"])
    S.op(V, lambda e: e.tensor_tensor(out=RK[:, :, 2], in0=SC(8), in1=SC(2), op=ALU.mult), reads=["sc8", "sc2"], writes=["rk2"])
    S.op(V, lambda e: e.tensor_tensor(out=RK[:, :, 3], in0=SC(2), in1=RK[:, :, 2], op=ALU.subtract), reads=["rk2", "sc2"], writes=["rk3"])
    S.op(V, lambda e: e.tensor_tensor(out=Mb[:, :, :], in0=OH1a, in1=OH2a, op=ALU.add), reads=["oh1", "oh2"], writes=["Mb"])

    def mmc(e):
        ins = None
        for ti in range(NTL):
            T = 128 if ti < 16 else NS
            if ti < 16:
                e.matmul(ps[6][:, ti * NE:(ti + 1) * NE], lhsT=lst_b[0:T, 0:T], rhs=Mb[0:T, ti, :], start=True, stop=True)
                ins = e.matmul(ps[5][:, ti * NE:(ti + 1) * NE], lhsT=onesb[0:T, :], rhs=Mb[0:T, ti, :], start=True, stop=True)
            else:
                e.matmul(ps[7][0:T, 0:NE], lhsT=lst_b[0:T, 0:T], rhs=Mb[0:T, ti, :], start=True, stop=True)
                ins = e.matmul(ps[7][:, 64:64 + NE], lhsT=onesb[0:T, :], rhs=Mb[0:T, ti, :], start=True, stop=True)
        return ins
    S.op("tensor", mmc, reads=["Mb", "lst_b", "onesb"], writes=["ps5", "ps6", "ps7"])
    S.op("gpsimd", lambda e: e.memset(CAR[:, 0, :], 0.0), writes=[("car", 0)])
    for ti in range(16):
        S.op(V, lambda e, ti=ti: e.tensor_tensor(out=CAR[:, ti + 1, :], in0=ps[5][:, ti * NE:(ti + 1) * NE], in1=CAR[:, ti, :], op=ALU.add),
             reads=["ps5", ("car", ti)], writes=[("car", ti + 1)])
    S.op(V, lambda e: e.tensor_tensor(out=carry[:, :], in0=ps[7][:, 64:64 + NE], in1=CAR[:, 16, :], op=ALU.add),
         reads=["ps7", ("car", 16)], writes=["carry"])
    cark = [("car", t) for t in range(17)]
    S.op(V, lambda e: e.tensor_tensor(out=R7[:, 0:16, :], in0=ps[6][:, :].rearrange("p (t x) -> p t x", t=16), in1=CAR[:, 0:16, :], op=ALU.add),
         reads=["ps6"] + cark, writes=["R7a"])
    S.op(V, lambda e: e.tensor_tensor(out=R7[0:NS, 16, :], in0=ps[7][0:NS, 0:NE], in1=CAR[0:NS, 16, :], op=ALU.add),
         reads=["ps7"] + cark, writes=["R7b"])
    S.op(V, lambda e: e.tensor_tensor(out=em[:, :, :], in0=R7[:, :, :], in1=OH1a, op=ALU.mult), reads=["R7a", "R7b", "oh1", "em2"], writes=["em"])
    S.op(V, lambda e: e.reduce_sum(out=RK[:, :, 0], in_=em[:, :, :], axis=AX.X), reads=["em"], writes=["rk0"])
    S.op(V, lambda e: e.tensor_tensor(out=em2[:, :, :], in0=R7[:, :, :], in1=OH2a, op=ALU.mult), reads=["R7a", "R7b", "oh2"], writes=["em2"])
    S.op(V, lambda e: e.reduce_sum(out=RK[:, :, 1], in_=em2[:, :, :], axis=AX.X), reads=["em2"], writes=["rk1"])

    S.barrier()
    arena_off[0] = P23_MARK
    TM = 256
    NT = (2 * NTOK + TM - 1) // TM + NE
    NSL = NT * TM
    Xs = nc.dram_tensor("Xs", [NSL, D], BF16, kind="Internal").ap()
    Ys = nc.dram_tensor("Ys", [NSL, D], BF16, kind="Internal").ap()
    V = "vector"
    cnt_i = wk("cnt_i", [128, NE], I32)
    nt_f = wk("nt_f", [128, NE], F32)
    scn = [wk("scn%d" % i, [128, NE], F32) for i in range(2)]
    bt = wk("bt", [128, NE], F32)
    bslot = wk("bslot", [128, NE], F32)
    ee_i = wk("ee_i", [128, NE], I32)
    ee_f = wk("ee_f", [128, NE], F32)
    QTf = QT[:, :, :].rearrange("p h t -> p (h t)").bitcast(F32)
    ii_f = QTf[:, 0:NT * NE].rearrange("p (i e) -> p i e", i=NT)
    ii_i = QTf[:, NT * NE:2 * NT * NE].bitcast(I32).rearrange("p (i e) -> p i e", i=NT)
    indA = QTf[:, 2 * NT * NE:3 * NT * NE].rearrange("p (i e) -> p i e", i=NT)
    texp = wk("texp", [128, NT], F32)
    tval = wk("tval", [128, NT], F32)
    pp_i = wk("pp_i", [128, 1], I32)
    pp_f = wk("pp_f", [128, 1], F32)
    idxw_f = wk("idxw_f", [128, NT], F32)
    idxw = wk("idxw", [128, NT], I32)
    POSf = wk("POSf", [128, 17, 2], F32)
    POS = wk("POS", [128, 17, 2], I32)
    ptmp = wk("ptmp", [128, NE], F32)

    S.op(V, lambda e: e.tensor_copy(out=cnt_i[:, :], in_=carry[:, :]), reads=["carry"], writes=["cnt_i"])
    S.op(V, lambda e: e.tensor_single_scalar(out=cnt_i[:, :], in_=cnt_i[:, :], scalar=TM - 1, op=ALU.add), reads=["cnt_i"], writes=["cnt_i"])
    S.op(V, lambda e: e.tensor_single_scalar(out=cnt_i[:, :], in_=cnt_i[:, :], scalar=8, op=ALU.arith_shift_right),
         reads=["cnt_i"], writes=["cnt_i"])
    S.op(V, lambda e: e.tensor_copy(out=nt_f[:, :], in_=cnt_i[:, :]), reads=["cnt_i"], writes=["nt_f"])
    cur, curk = nt_f, ["nt_f"]
    for si, sh in enumerate((1, 2, 4, 8, 16)):
        nxt = scn[si % 2]
        k0, k1 = ("scn", si % 2, 0), ("scn", si % 2, 1)
        S.op(V, lambda e, cur=cur, nxt=nxt, sh=sh: e.tensor_copy(out=nxt[:, 0:sh], in_=cur[:, 0:sh]), reads=curk, writes=[k0])
        S.op(V, lambda e, cur=cur, nxt=nxt, sh=sh: e.tensor_tensor(out=nxt[:, sh:NE], in0=cur[:, sh:NE], in1=cur[:, 0:NE - sh], op=ALU.add),
             reads=curk, writes=[k1])
        cur, curk = nxt, [k0, k1]
    bti, btik = cur, curk
    S.op(V, lambda e: e.tensor_tensor(out=bt[:, :], in0=bti[:, :], in1=nt_f[:, :], op=ALU.subtract), reads=btik + ["nt_f"], writes=["bt"])
    S.op(V, lambda e: e.tensor_single_scalar(out=bslot[:, :], in_=bt[:, :], scalar=float(TM), op=ALU.mult), reads=["bt"], writes=["bslot"])
    S.op("gpsimd", lambda e: e.iota(ee_i[:, :], pattern=[[1, NE]], base=0, channel_multiplier=0), writes=["ee_i"])
    S.op("gpsimd", lambda e: e.iota(ii_i[:, :, :], pattern=[[1, NT], [0, NE]], base=0, channel_multiplier=0), writes=["ii_i"])
    S.op("gpsimd", lambda e: e.iota(pp_i[:, :], pattern=[[0, 1]], base=0, channel_multiplier=1), writes=["pp_i"])
    S.op(V, lambda e: e.tensor_copy(out=ee_f[:, :], in_=ee_i[:, :]), reads=["ee_i"], writes=["ee_f"])
    S.op(V, lambda e: e.tensor_copy(out=ii_f[:, :, :], in_=ii_i[:, :, :]), reads=["ii_i"], writes=["ii_f"])
    S.op(V, lambda e: e.tensor_copy(out=pp_f[:, :], in_=pp_i[:, :]), reads=["pp_i"], writes=["pp_f"])
    S.op(V, lambda e: e.tensor_tensor(out=indA[:, :, :], in0=ii_f[:, :, :], in1=bt[:, :].unsqueeze(1).to_broadcast([128, NT, NE]), op=ALU.is_ge),
         reads=["ii_f", "bt"], writes=["indA"])
    S.op(V, lambda e: e.tensor_tensor(out=ii_f[:, :, :], in0=ii_f[:, :, :], in1=bti[:, :].unsqueeze(1).to_broadcast([128, NT, NE]), op=ALU.is_lt),
         reads=["ii_f"] + btik, writes=["ii_f"])
    S.op(V, lambda e: e.tensor_tensor(out=indA[:, :, :], in0=indA[:, :, :], in1=ii_f[:, :, :], op=ALU.mult), reads=["indA", "ii_f"], writes=["indA"])
    S.op(V, lambda e: e.reduce_sum(out=tval[:, :], in_=indA[:, :, :], axis=AX.X), reads=["indA"], writes=["tval"])
    S.op(V, lambda e: e.tensor_tensor(out=indA[:, :, :], in0=indA[:, :, :], in1=ee_f[:, :].unsqueeze(1).to_broadcast([128, NT, NE]), op=ALU.mult),
         reads=["indA", "ee_f", "tval"], writes=["indA"])
    S.op(V, lambda e: e.reduce_sum(out=texp[:, :], in_=indA[:, :, :], axis=AX.X), reads=["indA"], writes=["texp"])
    S.op(V, lambda e: e.tensor_scalar(out=idxw_f[:, :], in0=texp[:, :], scalar1=128.0, scalar2=pp_f[:, 0:1], op0=ALU.mult, op1=ALU.add),
         reads=["texp", "pp_f"], writes=["idxw_f"])
    S.op(V, lambda e: e.tensor_copy(out=idxw[:, :], in_=idxw_f[:, :]), reads=["idxw_f"], writes=["idxw"])

    h2t = [wk("h2t%d" % i, [128, D], BF16) for i in range(4)]
    last_sc = None
    for ti in range(17):
        T = 128 if ti < 16 else NS
        for k in range(2):
            S.op(V, lambda e, ti=ti, k=k, T=T: e.tensor_tensor(out=ptmp[0:T, :], in0=OH[0:T, ti, 32 * k:32 * k + 32], in1=bslot[0:T, :], op=ALU.mult),
                 reads=["bslot"], writes=["ptmp"])
            S.op(V, lambda e, ti=ti, k=k, T=T: e.reduce_sum(out=POSf[0:T, ti, k:k + 1], in_=ptmp[0:T, :], axis=AX.X),
                 reads=["ptmp"], writes=[("posf", ti, k)])
        S.op(V, lambda e, ti=ti, T=T: e.tensor_tensor(out=POSf[0:T, ti, :], in0=POSf[0:T, ti, :], in1=RK[0:T, ti, 0:2], op=ALU.add),
             reads=[("posf", ti, 0), ("posf", ti, 1)], writes=[("posf2", ti)])
        S.op(V, lambda e, ti=ti, T=T: e.tensor_copy(out=POS[0:T, ti, :], in_=POSf[0:T, ti, :]), reads=[("posf2", ti)], writes=[("pos", ti)])
        sl = ti % 4
        tok0 = ti * 128
        S.dma("sync", "lh%d" % sl, h2t[sl][0:T, :], H2[tok0:tok0 + T, :], writes=[("h2t", sl)])
        if ti == 0:
            dbg("h2t0", h2t[0][:, :], [("h2t", 0)])
        for k in range(2):
            last_sc = S.op("gpsimd", lambda e, ti=ti, k=k, T=T, sl=sl: e.indirect_dma_start(
                out=Xs[:, :], out_offset=bass.IndirectOffsetOnAxis(ap=POS[0:T, ti, k:k + 1], axis=0),
                in_=h2t[sl][0:T, :], in_offset=None), reads=[("h2t", sl), ("pos", ti)], writes=[("xs_sc", ti, k)], dma="sc%d" % sl)
    S.barrier()

    NWS = 5
    WSZ = 8 * DE + 8 * DE + 2 * D
    wslots = [arenaX[:, i * WSZ:(i + 1) * WSZ] for i in range(2)] + [arenaK[:, i * WSZ:(i + 1) * WSZ] for i in range(3)]
    xs = [wk("xs%d" % i, [128, 2, D], BF16) for i in range(2)]
    xsT = [wk("xsT%d" % i, [128, 8, TM], BF16) for i in range(2)]
    sgm = [wk("sgm%d" % i, [128, TM], F32) for i in range(2)]
    aTm = [wk("aTm%d" % i, [128, 2, TM], BF16) for i in range(2)]
    ysb = [wk("ysb%d" % i, [128, 2, D], BF16) for i in range(2)]
    ys_recs = []

    def wviews(slot):
        a = wslots[slot]
        return (a[:, 0:8 * DE], a[:, 8 * DE:16 * DE], a[:, 16 * DE:16 * DE + 2 * D])

    def fetch_tile(i):
        slot = i % NWS
        wg2, wu2, wd2 = wviews(slot)
        for j, (dst, srcw) in enumerate(((wg2, wgb), (wu2, wub), (wd2, wdb))):
            S.op("gpsimd", lambda e, dst=dst, srcw=srcw, i=i: e.indirect_dma_start(
                out=dst, out_offset=None, in_=srcw[:, :], in_offset=bass.IndirectOffsetOnAxis(ap=idxw[:, i:i + 1], axis=0)),
                reads=["idxw"], writes=[("wsl", slot, j)], dma="we%d_%d" % (slot, j))

    def fetch_x(i):
        sl = i % 2
        S.dma("sync", "lx%d" % sl, xs[sl][:, :, :], Xs[i * TM:(i + 1) * TM, :].rearrange("(h p) d -> p h d", p=128), writes=[("xs", sl)])

    def moe_tile(i):
        slot, sl = i % NWS, i % 2
        wg2, wu2, wd2 = wviews(slot)
        wg = wg2.rearrange("p (k n) -> p k n", k=8)
        wu = wu2.rearrange("p (k n) -> p k n", k=8)
        wd = wd2.rearrange("p (k n) -> p k n", k=2)
        X, XT, A, Y = xs[sl], xsT[sl], aTm[sl], ysb[sl]
        for hh in range(2):
            def tr(e, hh=hh):
                ins = None
                for k in range(8):
                    ins = e.transpose(out=psb(hh)[:, k * 128:(k + 1) * 128], in_=X[:, hh, k * 128:(k + 1) * 128], identity=ident[:, :])
                return ins
            S.op("tensor", tr, reads=[("xs", sl), "ident"], writes=["ps%d" % hh])
            eng = "scalar" if hh == 0 else "vector"
            if hh == 0:
                S.op("scalar", lambda e, hh=hh: e.copy(out=XT[:, :, hh * 128:(hh + 1) * 128],
                                                       in_=psb(hh)[:, 0:1024].rearrange("p (k t) -> p k t", k=8)),
                     reads=["ps%d" % hh], writes=[("xsT", sl, hh)])
            else:
                S.op("vector", lambda e, hh=hh: e.tensor_copy(out=XT[:, :, hh * 128:(hh + 1) * 128],
                                                              in_=psb(hh)[:, 0:1024].rearrange("p (k t) -> p k t", k=8)),
                     reads=["ps%d" % hh], writes=[("xsT", sl, hh)])
        for kc in range(2):
            def mm(e, kc=kc):
                ins = None
                for k in range(8):
                    ins = e.matmul(ps[2 + kc][:, 0:TM], lhsT=wg[:, k, kc * 128:(kc + 1) * 128], rhs=XT[:, k, :], start=(k == 0), stop=(k == 7))
                for k in range(8):
                    ins = e.matmul(ps[2 + kc][:, TM:2 * TM], lhsT=wu[:, k, kc * 128:(kc + 1) * 128], rhs=XT[:, k, :], start=(k == 0), stop=(k == 7))
                return ins
            S.op("tensor", mm, reads=[("xsT", sl, 0), ("xsT", sl, 1), ("wsl", slot, 0), ("wsl", slot, 1)], writes=["ps%d" % (2 + kc)])
            S.op("scalar", lambda e, kc=kc: e.activation(out=sgm[kc][:, :], in_=ps[2 + kc][:, 0:TM], func=AF.Silu),
                 reads=["ps%d" % (2 + kc)], writes=[("sgm", kc)])
            S.op("vector", lambda e, kc=kc: e.tensor_tensor(out=A[:, kc, :], in0=ps[2 + kc][:, TM:2 * TM], in1=sgm[kc][:, :], op=ALU.mult),
                 reads=["ps%d" % (2 + kc), ("sgm", kc)], writes=[("aTm", sl, kc)])
        for hh in range(2):
            for half in range(2):
                pb = 4 + hh * 2 + half

                def mmd(e, hh=hh, half=half, pb=pb):
                    ins = None
                    for kc in range(2):
                        ins = e.matmul(ps[pb][:, :], lhsT=A[:, kc, hh * 128:(hh + 1) * 128], rhs=wd[:, kc, half * 512:(half + 1) * 512],
                                       start=(kc == 0), stop=(kc == 1))
                    return ins
                S.op("tensor", mmd, reads=[("aTm", sl, 0), ("aTm", sl, 1), ("wsl", slot, 2)], writes=["ps%d" % pb])
                if half == 0:
                    S.op("scalar", lambda e, hh=hh, half=half, pb=pb: e.copy(out=Y[:, hh, half * 512:(half + 1) * 512], in_=ps[pb][:, :]),
                         reads=["ps%d" % pb], writes=[("ysb", sl, hh, half)])
                else:
                    S.op("vector", lambda e, hh=hh, half=half, pb=pb: e.tensor_copy(out=Y[:, hh, half * 512:(half + 1) * 512], in_=ps[pb][:, :]),
                         reads=["ps%d" % pb], writes=[("ysb", sl, hh, half)])
        ys_recs.append(S.dma("sync", "sys%d" % sl, Ys[i * TM:(i + 1) * TM, :].rearrange("(h p) d -> p h d", p=128), Y[:, :, :],
                             reads=[("ysb", sl, hh, half) for hh in range(2) for half in range(2)]))

    PRE = 3
    for i in range(min(PRE, NT)):
        fetch_tile(i)
    fetch_x(0)
    dbg("xs_t0", xs[0][:, :, :], [("xs", 0)])
    dbg("wg_t0", wslots[0][:, 0:2048], [("wsl", 0, 0)])
    dbg("wd_t0", wslots[0][:, 4096:6144], [("wsl", 0, 2)])
    for i in range(NT):
        if i + PRE < NT:
            fetch_tile(i + PRE)
        if i + 1 < NT:
            fetch_x(i + 1)
        moe_tile(i)
        if i == 0:
            dbg("ysb_t0", ysb[0][:, :, :], [("ysb", 0, hh, half) for hh in range(2) for half in range(2)])
            dbg("xsT_t0", xsT[0][:, :, :], [("xsT", 0, 0), ("xsT", 0, 1)])
            dbg("aT_t0", aTm[0][:, :, :], [("aTm", 0, 0), ("aTm", 0, 1)])
    S.barrier()

    xf = [QTf[:, i * D:(i + 1) * D] for i in range(2)]
    QTb = QT[:, :, :].rearrange("p h t -> p (h t)")
    g1 = [QTb[:, (4 + i) * D:(5 + i) * D] for i in range(2)]
    g2 = [QTb[:, (6 + i) * D:(7 + i) * D] for i in range(2)]
    for ti in range(17):
        if ti % 2 == 0:
            S.window_begin()
        S.cur_stream = ti % 2
        T = 128 if ti < 16 else NS
        tok0 = ti * 128
        sl = ti % 2
        S.dma("sync", "lf%d" % sl, xf[sl][0:T, :], x1s[tok0:tok0 + T, :], writes=[("xf", sl)])
        for k, G in enumerate((g1, g2)):
            S.op("gpsimd", lambda e, ti=ti, k=k, T=T, G=G, sl=sl: e.indirect_dma_start(
                out=G[sl][0:T, :], out_offset=None, in_=Ys[:, :],
                in_offset=bass.IndirectOffsetOnAxis(ap=POS[0:T, ti, k:k + 1], axis=0)),
                reads=[("pos", ti)], writes=[("gg", k, sl)], dma="gy%d_%d" % (k, sl))
        if ti == 0:
            dbg("g1_0", g1[0][:, :], [("gg", 0, 0)])
            dbg("g2_0", g2[0][:, :], [("gg", 1, 0)])
            dbg("xf_0", xf[0][:, :], [("xf", 0)])
        S.op(V, lambda e, ti=ti, T=T, sl=sl: e.scalar_tensor_tensor(out=xf[sl][0:T, :], in0=g1[sl][0:T, :], scalar=RK[0:T, ti, 2:3],
                                                                    in1=xf[sl][0:T, :], op0=ALU.mult, op1=ALU.add),
             reads=[("gg", 0, sl), ("xf", sl)], writes=[("xf", sl)])
        S.op(V, lambda e, ti=ti, T=T, sl=sl: e.scalar_tensor_tensor(out=xf[sl][0:T, :], in0=g2[sl][0:T, :], scalar=RK[0:T, ti, 3:4],
                                                                    in1=xf[sl][0:T, :], op0=ALU.mult, op1=ALU.add),
             reads=[("gg", 1, sl), ("xf", sl)], writes=[("xf", sl)])
        dst = y_p[tok0:tok0 + 128, :] if ti < 16 else y_s[:, :]
        out_recs.append(S.dma("sync", "sf%d" % sl, dst, xf[sl][0:T, :], reads=[("xf", sl)]))
        if ti % 2 == 1 or ti == 16:
            S.cur_stream = 0
            S.window_end()

    dbg("OH", OH[:, :, :], [])
    dbg("RK", RK[:, :, :], [])
    dbg("carry", carry[:, :], [])
    dbg("nt_f", nt_f[:, :], [])
    dbg("bt", bt[:, :], [])
    dbg("bti", bti[:, :], [])
    dbg("texp", texp[:, :], [])
    dbg("idxw", idxw[:, :], [])
    dbg("POS", POS[:, :, :], [])
    dbg("xsT0", xsT[0][:, :, :], [])
    dbg("ysb0", ysb[0][:, :, :], [])
    S.op("sync", None, reads=[], writes=[])
    fin = S.ops["sync"][-1]
    fin.deps = [r_ for r_ in out_recs if r_.eng is not None]
    for d in fin.deps:
        d.needed = True

    S.emit(nc, es)
    es.close()
    return nc


_CACHE = {}


def kernel(**inputs):
    f = lambda a: np.ascontiguousarray(a)
    nc = _CACHE.get("nc")
    if nc is None:
        nc = build_program()
        _CACHE["nc"] = nc
    shared = {
        "cache_ckv": f(inputs["cache_ckv"][0]),
        "cache_kr": f(inputs["cache_krope"][0]),
        "g_mix": f(inputs["g_mix"]), "w_in": f(inputs["w_in"][0]),
        "conv_w": f(inputs["conv_w"][0]), "conv_b": f(inputs["conv_b"]),
        "g_q_lat": f(inputs["g_q_lat"]), "w_uq": f(inputs["w_uq"][0]),
        "g_kv_lat": f(inputs["g_kv_lat"]), "w_uk": f(inputs["w_uk"][0]), "w_uv": f(inputs["w_uv"][0]),
        "g_q_nope": f(inputs["g_q_nope"]), "g_q_rope": f(inputs["g_q_rope"]),
        "g_k_nope": f(inputs["g_k_nope"]), "g_k_rope": f(inputs["g_k_rope"]),
        "g_out": f(inputs["g_out"]), "w_out": f(inputs["w_out"][0]), "g_ffn": f(inputs["g_ffn"]),
        "w_rg": f(inputs["w_router_group"][0]), "b_rg": f(inputs["b_router_group"]),
        "w_re": f(inputs["w_router_expert"][0]), "b_re": f(inputs["b_router_expert"]),
        "w_gate": f(inputs["w_gate"][0].reshape(NE, 8, 128, DE).transpose(0, 2, 1, 3).reshape(NE * 128, 8 * DE)),
        "w_up": f(inputs["w_up"][0].reshape(NE, 8, 128, DE).transpose(0, 2, 1, 3).reshape(NE * 128, 8 * DE)),
        "w_down": f(inputs["w_down"][0].reshape(NE, 2, 128, D).transpose(0, 2, 1, 3).reshape(NE * 128, 2 * D)),
    }
    in_maps = []
    for c in range(NCORES):
        m = dict(shared)
        m["x_p"] = f(inputs["x_prompt"][c])
        m["x_s"] = f(inputs["x_sample"][4 * c:4 * c + 4].reshape(NS, D))
        m["st_conv"] = f(inputs["state_conv"][0, 4 * c:4 * c + 4])
        m["ptab"] = f(inputs["page_table"][4 * c:4 * c + 4])
        in_maps.append(m)
    res = run_bass_kernel_spmd(nc, in_maps, core_ids=list(range(NCORES)))
    R = res.results
    if DEBUG:
        _CACHE["dbg"] = R
    y_p = np.stack([R[c]["y_p"] for c in range(NCORES)], 0)
    y_s = np.concatenate([R[c]["y_s"].reshape(4, 8, D) for c in range(NCORES)], 0)
    ckv_p = np.stack([R[c]["o_ckv_p"] for c in range(NCORES)], 0)[None]
    kr_p = np.stack([R[c]["o_kr_p"] for c in range(NCORES)], 0)[None]
    conv_p = np.stack([R[c]["o_conv_p"] for c in range(NCORES)], 0)[None]
    ckv_s = np.concatenate([R[c]["o_ckv_s"].reshape(4, 8, KVR) for c in range(NCORES)], 0)[None]
    kr_s = np.concatenate([R[c]["o_kr_s"].reshape(4, 8, RD) for c in range(NCORES)], 0)[None]
    conv_s = np.concatenate([R[c]["o_conv_s"] for c in range(NCORES)], 0)[None]
    return (y_p, y_s, ckv_p, kr_p, conv_p, ckv_s, kr_s, conv_s)
```

```python
import numpy as np
from contextlib import ExitStack
import concourse.bass as bass
import concourse.mybir as mybir
from concourse.bass_utils import run_bass_kernel_spmd

F32 = mybir.dt.float32
BF16 = mybir.dt.bfloat16
I32 = mybir.dt.int32
ALU = mybir.AluOpType
AF = mybir.ActivationFunctionType
AX = mybir.AxisListType

NCORES = 8
D = 1024
SEQ = 2048
NS = 32
NTOK = SEQ + NS
CW = 512
QR = 256
KVR = 128
RD = 32
NH = 8
ND = 64
VD = 64
PW = 3 * CW + QR + KVR + RD
NE = 32
DE = 256
EPS = 1e-6
DEBUG = False
NPAGE = 128
PAGE = 128
NPOOL = 5120
PAST = NPAGE * PAGE
SCALE = float((ND + RD) ** -0.5)


class Rec:
    __slots__ = ("eng", "fn", "deps", "dma", "needed", "sem", "val", "seq", "stream", "pdeps")

    def __init__(self, eng, fn, deps, dma):
        self.eng, self.fn, self.deps, self.dma = eng, fn, deps, dma
        self.needed = False
        self.sem = None
        self.val = 0
        self.seq = -1
        self.stream = 0
        self.pdeps = ()


class Sched:
    ENGS = ("sync", "gpsimd", "vector", "scalar", "tensor")

    def __init__(self):
        self.ops = {e: [] for e in self.ENGS}
        self.buf = {}
        self.streams = {}
        self.mute = False
        self.seq = 0
        self.cur_stream = 0
        self.win = None

    def op(self, eng, fn, reads=(), writes=(), dma=None):
        if self.mute:
            return Rec(None, None, [], None)
        deps = []
        seen = set()

        def add(r):
            if r is not None and id(r) not in seen:
                seen.add(id(r))
                deps.append(r)

        for k in reads:
            st = self.buf.get(k)
            if st is not None:
                add(st[0])
        for k in writes:
            st = self.buf.get(k)
            if st is not None:
                add(st[0])
                for r in st[1]:
                    add(r)
        pdeps = ()
        if eng == "tensor":
            pdeps = [d for d in deps if d.eng == "tensor" and not d.dma]
            deps = [d for d in deps if d.eng != "tensor" or d.dma]
        rec = Rec(eng, fn, deps, dma)
        rec.pdeps = pdeps
        rec.seq = self.seq
        rec.stream = self.cur_stream
        self.seq += 1
        for d in deps:
            d.needed = True
        self.ops[eng].append(rec)
        if self.win is not None:
            self.win.append(rec)
        for k in reads:
            self.buf.setdefault(k, [None, []])[1].append(rec)
        for k in writes:
            self.buf[k] = [rec, []]
        if dma is not None:
            self.streams.setdefault(dma, 0)
        return rec

    def window_begin(self):
        self.win = []

    def window_end(self):
        win, self.win = self.win, None
        if not win:
            return
        inwin = set(id(r) for r in win)
        first_seq = win[0].seq
        for e in self.ENGS:
            self.ops[e] = [r for r in self.ops[e] if id(r) not in inwin]
        by_stream = {}
        for r in win:
            by_stream.setdefault(r.stream, []).append(r)
        keys = sorted(by_stream)
        ptr = {k: 0 for k in keys}
        pe_order = [r for r in win if r.eng == "tensor"]
        pe_next = 0
        done = set()
        out = []
        turn = 0
        total = len(win)

        def ready(r):
            for d in r.deps:
                if id(d) in inwin and id(d) not in done:
                    return False
            for d in r.pdeps:
                if id(d) in inwin and id(d) not in done:
                    return False
            return True
        while len(out) < total:
            picked = None
            for off in range(len(keys)):
                k = keys[(turn + off) % len(keys)]
                if ptr[k] < len(by_stream[k]) and ready(by_stream[k][ptr[k]]):
                    picked = k
                    break
            assert picked is not None, "window_end: no ready op (dependency cycle?)"
            r = by_stream[picked][ptr[picked]]
            ptr[picked] += 1
            if r.eng == "tensor":
                pe_next += 1
            done.add(id(r))
            out.append(r)
            turn = (keys.index(picked) + 1) % len(keys)
        for r in out:
            self.ops[r.eng].append(r)

    def barrier(self):
        lasts = []
        last_dma = {}
        for e in self.ENGS:
            last_c = None
            for r in self.ops[e]:
                if r.dma is not None:
                    last_dma[r.dma] = r
                elif r.fn is not None:
                    last_c = r
            if last_c is not None:
                lasts.append(last_c)
        lasts += list(last_dma.values())
        for d in lasts:
            d.needed = True
        for e in self.ENGS:
            rec = Rec(e, None, list(lasts), None)
            self.ops[e].append(rec)
        self.buf = {}

    def dma(self, eng, stream, out, in_, reads=(), writes=(), **kw):
        return self.op(eng, lambda e: e.dma_start(out=out, in_=in_, **kw), reads, writes, dma=stream)

    def emit(self, nc, es):
        sems = {}
        for e in self.ENGS:
            sems[e] = es.enter_context(nc.semaphore("p_" + e))
        dsem = {}
        for s in self.streams:
            dsem[s] = es.enter_context(nc.semaphore("d_" + s))
        cnt = {s: 0 for s in self.streams}
        for e in self.ENGS:
            c = 0
            for r in self.ops[e]:
                if r.dma is not None:
                    cnt[r.dma] += 16
                    r.sem, r.val = dsem[r.dma], cnt[r.dma]
                elif r.needed:
                    c += 1
                    r.sem, r.val = sems[e], c
        block = es.enter_context(nc.Block())

        def run(eng_name):
            def body(eng):
                waited = {}
                for r in self.ops[eng_name]:
                    need = {}
                    for d in r.deps:
                        key = id(d.sem)
                        if key not in need or need[key][1] < d.val:
                            need[key] = (d.sem, d.val)
                    for key, (sem_, val_) in need.items():
                        if waited.get(key, 0) < val_:
                            eng.wait_ge(sem_, val_)
                            waited[key] = val_
                    if r.fn is None:
                        continue
                    ins = r.fn(eng)
                    if r.dma is not None:
                        ins.then_inc(r.sem, 16)
                    elif r.needed:
                        ins.then_inc(r.sem, 1)
            return body

        block.sync(run("sync"))
        block.gpsimd(run("gpsimd"))
        block.vector(run("vector"))
        block.scalar(run("scalar"))
        block.tensor(run("tensor"))


def build_program():
    nc = bass.Bass("TRN2", target_bir_lowering=False)
    S = Sched()
    es = ExitStack()

    def din(name, shape, dt=F32):
        return nc.dram_tensor(name, list(shape), dt, kind="ExternalInput").ap()

    def dout(name, shape, dt=F32):
        return nc.dram_tensor(name, list(shape), dt, kind="ExternalOutput").ap()

    x_p = din("x_p", [SEQ, D])
    x_s = din("x_s", [NS, D])
    st_conv = din("st_conv", [4, 2, CW])
    cache_ckv = din("cache_ckv", [NPOOL, PAGE, KVR])
    cache_kr = din("cache_kr", [NPOOL, PAGE, RD])
    ptab = din("ptab", [4, NPAGE], I32)
    g_mix = din("g_mix", [1, D])
    w_in = din("w_in", [D, PW])
    conv_w = din("conv_w", [3, CW])
    conv_b = din("conv_b", [1, CW])
    g_q_lat = din("g_q_lat", [1, QR])
    w_uq = din("w_uq", [QR, NH * (ND + RD)])
    g_kv_lat = din("g_kv_lat", [1, KVR])
    w_uk = din("w_uk", [KVR, NH * ND])
    w_uv = din("w_uv", [KVR, NH * VD])
    g_q_nope = din("g_q_nope", [1, ND])
    g_q_rope = din("g_q_rope", [1, RD])
    g_k_nope = din("g_k_nope", [1, ND])
    g_k_rope = din("g_k_rope", [1, RD])
    g_out = din("g_out", [1, D])
    w_out = din("w_out", [D, D])
    g_ffn = din("g_ffn", [1, D])
    w_rg = din("w_rg", [D, 4])
    b_rg = din("b_rg", [1, 4])
    w_re = din("w_re", [D, NE])
    b_re = din("b_re", [1, NE])
    w_gate = din("w_gate", [NE * 128, 8 * DE])
    w_up = din("w_up", [NE * 128, 8 * DE])
    w_down = din("w_down", [NE * 128, 2 * D])

    y_p = dout("y_p", [SEQ, D])
    y_s = dout("y_s", [NS, D])
    o_ckv_p = dout("o_ckv_p", [SEQ, KVR])
    o_kr_p = dout("o_kr_p", [SEQ, RD])
    o_conv_p = dout("o_conv_p", [2, CW])
    o_ckv_s = dout("o_ckv_s", [NS, KVR])
    o_kr_s = dout("o_kr_s", [NS, RD])
    o_conv_s = dout("o_conv_s", [4, 2, CW])

    def sb(name, shape, dt=F32):
        return es.enter_context(nc.sbuf_tensor(name, list(shape), dt))

    def dbg(name, ap, keys):
        if not DEBUG:
            return
        shp = list(ap.shape)
        o = nc.dram_tensor("dbg_" + name, shp, ap.dtype, kind="ExternalOutput").ap()
        out_recs.append(S.dma("sync", "st", o, ap, reads=keys))

    out_recs = []

    ARENA_BYTES = 61440
    arena_t = sb("arena", [128, ARENA_BYTES // 4], F32)
    arena_off = [0]

    def arena_reset():
        arena_off[0] = 0

    def wk(name, shape, dt=F32):
        esz = 2 if dt == BF16 else 4
        n = 1
        for d in shape[1:]:
            n *= d
        nb = (n * esz + 31) // 32 * 32
        off = arena_off[0]
        assert off + nb <= ARENA_BYTES, (name, off, nb)
        arena_off[0] = off + nb
        ap = arena_t[:, off // 4:(off + nb) // 4]
        if dt != F32:
            ap = ap.bitcast(dt)
        ap = ap[:, 0:n]
        if len(shape) == 3:
            ap = ap.rearrange("p (a b) -> p a b", a=shape[1])
        elif len(shape) == 4:
            ap = ap.rearrange("p (a b c) -> p a b c", a=shape[1], b=shape[2])
        return ap

    ps_big_t = es.enter_context(nc.psum_tensor("ps_big", [128, 4096], F32))
    ps_big = ps_big_t[:, :]
    ps = [ps_big[:, i * 512:(i + 1) * 512] for i in range(8)]

    def psb(i):
        return ps[i].bitcast(BF16)

    ident = sb("ident", [128, 128], BF16)
    tri = sb("tri", [128, 128], BF16)
    bconv = sb("bconv", [128, 128], BF16)
    onesb = sb("onesb", [128, 128], BF16)
    zero_c = sb("zero_c", [128, 1], F32)
    eps_c = sb("eps_c", [128, 1], F32)
    gmix_b = sb("gmix_b", [128, D], F32)
    gql_b = sb("gql_b", [128, QR], F32)
    gkv_b = sb("gkv_b", [128, KVR], F32)
    gq_b = sb("gq_b", [128, ND + RD], F32)
    gkn_b = sb("gkn_b", [128, ND], F32)
    gkr_b = sb("gkr_b", [128, RD], F32)
    convw_c = sb("convw_c", [128, 3, 4], F32)
    convb_c = sb("convb_c", [128, 4], F32)
    gout_c = sb("gout_c", [128, 8], F32)
    gattn_c = sb("gattn_c", [64, 8], F32)
    gkn_c = sb("gkn_c", [64, 1], F32)
    cosT = sb("cosT", [128, 17, 16], F32)
    sinT = sb("sinT", [128, 17, 16], F32)

    ckv_s_b = sb("ckv_s_b", [NS, KVR + 1], BF16)
    ya_s = sb("ya_s", [128, 4, NS], BF16)
    arenaX = sb("arenaX", [128, 8 * NTOK], BF16)
    w_in_sb = arenaX[:, 0:8 * PW].rearrange("p (k n) -> p k n", k=8)
    w_uq_sb = sb("w_uq_sb", [128, 2, NH * (ND + RD)], BF16)
    w_uk_sb = sb("w_uk_sb", [128, NH * ND], BF16)
    w_uv_sb = sb("w_uv_sb", [128, NH * VD], BF16)

    QT = sb("QT", [128, NH, NTOK], BF16)
    arenaK = sb("arenaK", [128, NH * NTOK + 16 * NH * (VD + 1) + 4 * NTOK], BF16)
    KT = arenaK[:, 0:NH * NTOK].rearrange("p (h t) -> p h t", h=NH)
    _o = NH * NTOK
    Vaug = arenaK[:, _o:_o + 16 * NH * (VD + 1)].rearrange("p (j h d) -> p j h d", j=16, h=NH)
    _o += 16 * NH * (VD + 1)
    yconvT = arenaK[:, _o:_o + 4 * NTOK].rearrange("p (c t) -> p c t", c=4)

    xin = [wk("xin%d" % i, [128, D], F32) for i in range(2)]
    junk = wk("junk", [128, D], BF16)
    hbf = [wk("hbf%d" % i, [128, D], BF16) for i in range(2)]
    hT = [wk("hT%d" % i, [128, 8, 256], BF16) for i in range(2)]
    st1 = [wk("st1_%d" % i, [128, 96], F32) for i in range(2)]
    qln = wk("qln", [128, QR], BF16)
    qlnT = wk("qlnT", [128, 2, 128], BF16)
    ckv_f = [wk("ckv_f%d" % i, [128, KVR], F32) for i in range(2)]
    ckv_b = wk("ckv_b", [128, KVR], BF16)
    ckvT = wk("ckvT", [128, 128], BF16)
    krn = wk("krn", [128, RD], F32)
    kr_f = [wk("kr_f%d" % i, [128, RD], F32) for i in range(2)]
    rtmp = wk("rtmp", [128, 4, 16], F32)
    qsq = wk("qsq", [128, 768], F32)
    ksq = wk("ksq", [128, 512], F32)
    qn = wk("qn", [128, NH, ND + RD], F32)
    qrt = wk("qrt", [128, 4, NH, 16], F32)
    Qb = wk("Qb", [128, NH, ND + RD], BF16)
    Kb = wk("Kb", [128, NH, ND + RD], BF16)
    kn_t = wk("kn_t", [128, NH, ND], F32)
    gc_sb = wk("gc_sb", [128, 256], F32)
    ubuf = [wk("ubuf%d" % c, [128, 2 + 256], F32) for c in range(4)]
    usam = [wk("usam%d" % c, [128, 4, 10], F32) for c in range(4)]
    vconv = wk("vconv", [128, 256], F32)
    yconv = wk("yconv", [128, 256], F32)
    csq = wk("csq", [128, 256], BF16)
    crs = wk("crs", [128, 256], F32)

    S.op("gpsimd", lambda e: e.memset(ident[:], 0.0), writes=["ident"])
    S.op("gpsimd", lambda e: e.affine_select(out=ident[:], in_=ident[:], pattern=[[-1, 128]],
                                            compare_op=ALU.not_equal, fill=1.0, base=0,
                                            channel_multiplier=1), reads=["ident"], writes=["ident"])
    S.op("gpsimd", lambda e: e.memset(onesb[:], 1.0), writes=["onesb"])
    S.op("gpsimd", lambda e: e.affine_select(out=tri[:], in_=onesb[:], pattern=[[1, 128]],
                                            compare_op=ALU.is_ge, fill=0.0, base=0,
                                            channel_multiplier=-1), reads=["onesb"], writes=["tri"])
    S.op("gpsimd", lambda e: e.memset(bconv[:], 0.0), writes=["bconv"])
    S.op("gpsimd", lambda e: e.memset(bconv[0:64, 0:64], 1.0 / 64), reads=["bconv"], writes=["bconv"])
    S.op("gpsimd", lambda e: e.memset(bconv[64:128, 64:128], 1.0 / 64), reads=["bconv"], writes=["bconv"])
    S.op("gpsimd", lambda e: e.memset(zero_c[:], 0.0), writes=["zero_c"])
    S.op("gpsimd", lambda e: e.memset(eps_c[:], EPS), writes=["eps_c"])
    S.op("gpsimd", lambda e: e.memset(Vaug[:, :, :, VD:VD + 1], 1.0), writes=["Vaug_ones"])
    for c in range(4):
        S.op("gpsimd", lambda e, c=c: e.memset(ubuf[c][:, 0:2], 0.0), writes=[("ubuf", c)])

    def bload(dst, src, n, key):
        S.dma("sync", "ld", dst[:], src[0:1, :].to_broadcast([128, n]), writes=[key])

    bload(gmix_b, g_mix, D, "gmix_b")
    bload(gql_b, g_q_lat, QR, "gql_b")
    bload(gkv_b, g_kv_lat, KVR, "gkv_b")
    bload(gkn_b, g_k_nope, ND, "gkn_b")
    bload(gkr_b, g_k_rope, RD, "gkr_b")
    S.dma("sync", "ld", gq_b[:, 0:ND], g_q_nope[0:1, :].to_broadcast([128, ND]), writes=["gq_b0"])
    S.dma("sync", "ld", gq_b[:, ND:ND + RD], g_q_rope[0:1, :].to_broadcast([128, RD]), writes=["gq_b1"])
    if True:
        for k in range(3):
            S.dma("sync", "ld", convw_c[:, k, :], conv_w[k:k + 1, :].rearrange("o (c p) -> p (o c)", p=128),
                  writes=[("convw_c", k)], allow_slow_non_contiguous=True)
        S.dma("sync", "ld", convb_c[:], conv_b.rearrange("o (c p) -> p (o c)", p=128), writes=["convb_c"], allow_slow_non_contiguous=True)
        S.dma("sync", "ld", gout_c[:], g_out.rearrange("o (c p) -> p (o c)", p=128), writes=["gout_c"], allow_slow_non_contiguous=True)
        S.dma("sync", "ld", gattn_c[:], g_out[:, CW:D].rearrange("o (h p) -> p (o h)", p=64), writes=["gattn_c"], allow_slow_non_contiguous=True)
        S.dma("sync", "ld", gkn_c[:], g_k_nope.rearrange("o p -> p o"), writes=["gkn_c"], allow_slow_non_contiguous=True)
        for c in range(4):
            for sq_ in range(4):
                S.dma("sync", "ld", usam[c][:, sq_, 0:2],
                      st_conv[sq_, :, c * 128:(c + 1) * 128].rearrange("t p -> p t"), writes=[("usam", c, sq_)],
                      allow_slow_non_contiguous=True)

    w_in_v = w_in.rearrange("(k p) n -> p k n", p=128)
    for k in range(8):
        S.dma("gpsimd", "wg", w_in_sb[:, k, :], w_in_v[:, k, :], writes=[("w_in", k)])
    S.dma("gpsimd", "wg", w_uq_sb[:], w_uq.rearrange("(k p) n -> p k n", p=128), writes=["w_uq"])
    S.dma("gpsimd", "wg", w_uk_sb[:], w_uk[:, :], writes=["w_uk"])
    S.dma("gpsimd", "wg", w_uv_sb[:], w_uv[:, :], writes=["w_uv"])

    wgb = nc.dram_tensor("wgb", [NE * 128, 8 * DE], BF16, kind="Internal").ap()
    wub = nc.dram_tensor("wub", [NE * 128, 8 * DE], BF16, kind="Internal").ap()
    wdb = nc.dram_tensor("wdb", [NE * 128, 2 * D], BF16, kind="Internal").ap()
    conv_jobs = [(srcw, dstw, q_) for srcw, dstw in ((w_gate, wgb), (w_up, wub), (w_down, wdb)) for q_ in range(8)]

    def issue_conv(n):
        for _ in range(n):
            if conv_jobs:
                srcw, dstw, q_ = conv_jobs.pop(0)
                S.dma("gpsimd", "cv", dstw[q_ * 512:(q_ + 1) * 512, :], srcw[q_ * 512:(q_ + 1) * 512, :], writes=[("wconv", id(dstw), q_)])

    MARK_SETUP = arena_off[0]
    posf = wk("posf", [128, 17], F32)
    posi = wk("posi", [128, 17], I32)
    invf = wk("invf", [128, 16], F32)
    ang = wk("ang", [128, 17, 16], F32)
    ang2 = wk("ang2", [128, 17, 16], F32)
    S.op("gpsimd", lambda e: e.iota(posi[:, 0:16], pattern=[[128, 16]], base=0, channel_multiplier=1),
         writes=["posi0"])
    S.op("gpsimd", lambda e: e.iota(posi[:, 16:17], pattern=[[0, 1]], base=0, channel_multiplier=1),
         writes=["posi1"])
    S.op("vector", lambda e: e.tensor_single_scalar(out=posi[:, 16:17], in_=posi[:, 16:17], scalar=7,
                                                   op=ALU.bitwise_and), reads=["posi1"], writes=["posi1"])
    S.op("vector", lambda e: e.tensor_single_scalar(out=posi[:, 16:17], in_=posi[:, 16:17], scalar=PAST,
                                                   op=ALU.add), reads=["posi1"], writes=["posi1"])
    S.op("vector", lambda e: e.tensor_copy(out=posf[:], in_=posi[:]), reads=["posi0", "posi1"], writes=["posf"])
    invf_np = (np.float32(10000.0) ** (-(np.arange(16, dtype=np.float32) / np.float32(16)))).astype(np.float32)
    for i in range(16):
        S.op("gpsimd", lambda e, i=i: e.memset(invf[:, i:i + 1], float(invf_np[i])), writes=[("invf", i)])
    invk = [("invf", i) for i in range(16)]
    for t in range(17):
        S.op("vector", lambda e, t=t: e.tensor_scalar(out=ang[:, t, :], in0=invf[:], scalar1=posf[:, t:t + 1],
                                                      scalar2=None, op0=ALU.mult),
             reads=invk + ["posf"], writes=[("ang", t)])
    angkeys = [("ang", t) for t in range(17)]
    PI = float(np.pi)
    C1 = 6.28125
    C2 = float(2 * np.pi - 6.28125)
    angk = wk("angk", [128, 17, 16], I32)
    angkf = wk("angkf", [128, 17, 16], F32)
    angm = wk("angm", [128, 17, 16], F32)
    S.op("vector", lambda e: e.tensor_scalar(out=ang2[:], in0=ang[:], scalar1=float(1.0 / (2 * np.pi)), scalar2=None,
                                             op0=ALU.mult), reads=angkeys, writes=["ang2"])
    S.op("vector", lambda e: e.tensor_copy(out=angk[:], in_=ang2[:]), reads=["ang2"], writes=["angk"])
    S.op("vector", lambda e: e.tensor_copy(out=angkf[:], in_=angk[:]), reads=["angk"], writes=["angkf"])
    S.op("vector", lambda e: e.scalar_tensor_tensor(out=ang2[:], in0=angkf[:], scalar=-C1, in1=ang[:],
                                                    op0=ALU.mult, op1=ALU.add), reads=["angkf"] + angkeys, writes=["ang2"])
    S.op("vector", lambda e: e.scalar_tensor_tensor(out=ang2[:], in0=angkf[:], scalar=-C2, in1=ang2[:],
                                                    op0=ALU.mult, op1=ALU.add), reads=["angkf", "ang2"], writes=["ang2"])

    def wrap_and_sin(dst, key):
        S.op("vector", lambda e: e.tensor_single_scalar(out=angm[:], in_=ang2[:], scalar=PI, op=ALU.is_gt),
             reads=["ang2"], writes=["angm"])
        S.op("vector", lambda e: e.scalar_tensor_tensor(out=ang2[:], in0=angm[:], scalar=-2 * PI, in1=ang2[:],
                                                        op0=ALU.mult, op1=ALU.add), reads=["angm", "ang2"], writes=["ang2"])
        S.op("vector", lambda e: e.tensor_scalar(out=angm[:], in0=ang2[:], scalar1=-PI, scalar2=PI,
                                                 op0=ALU.max, op1=ALU.min), reads=["ang2"], writes=["angm"])
        S.op("scalar", lambda e: e.activation(out=dst[:], in_=angm[:], func=AF.Sin), reads=["angm"], writes=[key])

    wrap_and_sin(sinT, "sinT")
    S.op("vector", lambda e: e.tensor_scalar(out=ang2[:], in0=ang2[:], scalar1=PI / 2, scalar2=None, op0=ALU.add),
         reads=["ang2", "sinT"], writes=["ang2"])
    wrap_and_sin(cosT, "cosT")

    S.barrier()
    arena_off[0] = MARK_SETUP
    gc_sbL = [gc_sb, wk("gc_sb2", [128, 256], F32)]
    vconvL = [vconv, wk("vconv2", [128, 256], F32)]
    yconvL = [yconv, wk("yconv2", [128, 256], F32)]
    csqL = [csq, wk("csq2", [128, 256], BF16)]
    crsL = [crs, wk("crs2", [128, 256], F32)]
    qlnTL = [qlnT, wk("qlnT2", [128, 2, 128], BF16)]
    ckvTL = [ckvT, wk("ckvT2", [128, 128], BF16)]
    def rstd_from_ss(ss_ap, out_ap, n, T, keys_r, keys_w, tmp_ap, tmpkey):
        S.op("scalar", lambda e: e.activation(out=tmp_ap, in_=ss_ap, func=AF.Ln, bias=eps_c[0:T, :], scale=1.0 / n),
             reads=keys_r + ["eps_c"], writes=[tmpkey])
        S.op("scalar", lambda e: e.activation(out=out_ap, in_=tmp_ap, func=AF.Exp, scale=-0.5),
             reads=[tmpkey], writes=keys_w)

    def rope(dst1, dst2, x1, x2, cos, sin, tmp, rkeys, wkeys, tkey):
        S.op("vector", lambda e: e.tensor_tensor(out=tmp[0], in0=x1, in1=cos, op=ALU.mult), reads=rkeys, writes=[(tkey, 0)])
        S.op("vector", lambda e: e.tensor_tensor(out=tmp[1], in0=x2, in1=sin, op=ALU.mult), reads=rkeys, writes=[(tkey, 1)])
        S.op("vector", lambda e: e.tensor_tensor(out=tmp[2], in0=x1, in1=sin, op=ALU.mult), reads=rkeys, writes=[(tkey, 2)])
        S.op("vector", lambda e: e.tensor_tensor(out=tmp[3], in0=x2, in1=cos, op=ALU.mult), reads=rkeys, writes=[(tkey, 3)])
        S.op("vector", lambda e: e.tensor_tensor(out=dst1, in0=tmp[0], in1=tmp[1], op=ALU.subtract),
             reads=[(tkey, 0), (tkey, 1)], writes=[wkeys[0]])
        S.op("vector", lambda e: e.tensor_tensor(out=dst2, in0=tmp[2], in1=tmp[3], op=ALU.add),
             reads=[(tkey, 2), (tkey, 3)], writes=[wkeys[1]])

    def token_tile(ti, part):
        S.mute = (part == "B")
        T = 128 if ti < 16 else NS
        tok0 = ti * 128
        sl = ti % 2
        src = x_p[tok0:tok0 + 128, :] if ti < 16 else x_s[:, :]
        X, H, ST = xin[sl], hbf[sl], st1[sl]
        hTc = hT[(ti // 2) % 2]
        col0 = (ti % 2) * 128
        if ti in (16, 0):
            S.dma("sync", "ldx%d" % sl, X[0:T, :], src, writes=[("xin", sl)])
        if ti < 15:
            nsl = (ti + 1) % 2
            S.dma("sync", "ldx%d" % nsl, xin[nsl][:, :], x_p[(ti + 1) * 128:(ti + 2) * 128, :], writes=[("xin", nsl)])
        S.op("scalar", lambda e: e.activation(out=junk[0:T, :], in_=X[0:T, :], func=AF.Square, accum_out=ST[0:T, 0:1]),
             reads=[("xin", sl)], writes=[("st", sl, 0)])
        rstd_from_ss(ST[0:T, 0:1], ST[0:T, 1:2], D, T, [("st", sl, 0)], [("st", sl, 1)], ST[0:T, 2:3], ("st", sl, 2))
        S.op("vector", lambda e: e.scalar_tensor_tensor(out=H[0:T, :], in0=X[0:T, :], scalar=ST[0:T, 1:2],
                                                        in1=gmix_b[0:T, :], op0=ALU.mult, op1=ALU.mult),
             reads=[("xin", sl), ("st", sl, 1), "gmix_b"], writes=[("hbf", sl)])
        pT = psb(0)

        def tr_h(e):
            ins = None
            for k in range(8):
                ins = e.transpose(out=pT[:, k * 128:k * 128 + T], in_=H[0:T, k * 128:(k + 1) * 128], identity=ident[0:T, 0:T])
            return ins
        S.op("tensor", tr_h, reads=[("hbf", sl), "ident"], writes=["ps0"])
        S.op("scalar", lambda e: e.copy(out=hTc[:, :, col0:col0 + T],
                                        in_=pT[:, 0:1024].rearrange("p (k t) -> p k t", k=8)[:, :, 0:T]),
             reads=["ps0"], writes=[("hT", (ti // 2) % 2, ti % 2)])
        hkey = ("hT", (ti // 2) % 2, ti % 2)

        def mm_small(e):
            ins = None
            for k in range(8):
                ins = e.matmul(ps[1][0:T, 0:416], lhsT=hTc[:, k, col0:col0 + T], rhs=w_in_sb[:, k, 3 * CW:PW],
                               start=(k == 0), stop=(k == 7))
            return ins
        S.op("tensor", mm_small, reads=[hkey] + [("w_in", k) for k in range(8)], writes=["ps1"])
        zs = ps[1]
        for j, (a, b) in enumerate(((0, QR), (QR, QR + KVR), (QR + KVR, QR + KVR + RD))):
            S.op("scalar", lambda e, a=a, b=b, j=j: e.activation(out=junk[0:T, a:b], in_=zs[0:T, a:b], func=AF.Square,
                                                                 accum_out=ST[0:T, 4 + j:5 + j]),
                 reads=["ps1"], writes=[("st", sl, 4 + j)])
        rstd_from_ss(ST[0:T, 4:5], ST[0:T, 8:9], QR, T, [("st", sl, 4)], [("st", sl, 8)], ST[0:T, 12:13], ("st", sl, 12))
        rstd_from_ss(ST[0:T, 5:6], ST[0:T, 9:10], KVR, T, [("st", sl, 5)], [("st", sl, 9)], ST[0:T, 13:14], ("st", sl, 13))
        rstd_from_ss(ST[0:T, 6:7], ST[0:T, 10:11], RD, T, [("st", sl, 6)], [("st", sl, 10)], ST[0:T, 14:15], ("st", sl, 14))
        S.op("vector", lambda e: e.scalar_tensor_tensor(out=qln[0:T, :], in0=zs[0:T, 0:QR], scalar=ST[0:T, 8:9],
                                                        in1=gql_b[0:T, :], op0=ALU.mult, op1=ALU.mult),
             reads=["ps1", ("st", sl, 8), "gql_b"], writes=["qln"])
        CK = ckv_f[sl]
        S.op("vector", lambda e: e.scalar_tensor_tensor(out=CK[0:T, :], in0=zs[0:T, QR:QR + KVR], scalar=ST[0:T, 9:10],
                                                        in1=gkv_b[0:T, :], op0=ALU.mult, op1=ALU.mult),
             reads=["ps1", ("st", sl, 9), "gkv_b"], writes=[("ckv_f", sl)])
        S.op("vector", lambda e: e.scalar_tensor_tensor(out=krn[0:T, :], in0=zs[0:T, QR + KVR:QR + KVR + RD],
                                                        scalar=ST[0:T, 10:11], in1=gkr_b[0:T, :], op0=ALU.mult, op1=ALU.mult),
             reads=["ps1", ("st", sl, 10), "gkr_b"], writes=["krn"])
        dst_ckv = o_ckv_p[tok0:tok0 + 128, :] if ti < 16 else o_ckv_s[:, :]
        out_recs.append(S.dma("sync", "stc%d" % sl, dst_ckv, CK[0:T, :], reads=[("ckv_f", sl)]))
        S.op("gpsimd", lambda e: e.tensor_copy(out=ckv_b[0:T, :], in_=CK[0:T, :]), reads=[("ckv_f", sl)], writes=["ckv_b"])
        if ti == 16:
            S.op("gpsimd", lambda e: e.tensor_copy(out=ckv_s_b[0:T, 0:KVR], in_=CK[0:T, :]), reads=[("ckv_f", sl)], writes=["ckv_s_b"])
            S.op("gpsimd", lambda e: e.memset(ckv_s_b[0:T, KVR:KVR + 1], 1.0), writes=["ckv_s_b1"])
        KR = kr_f[sl]
        rope(KR[0:T, 0:16], KR[0:T, 16:32], krn[0:T, 0:16], krn[0:T, 16:32], cosT[0:T, ti, :], sinT[0:T, ti, :],
             [rtmp[0:T, i, :] for i in range(4)], ["krn", "cosT", "sinT"], [("kr_f", sl, 0), ("kr_f", sl, 1)], "rtmp")
        dst_kr = o_kr_p[tok0:tok0 + 128, :] if ti < 16 else o_kr_s[:, :]
        out_recs.append(S.dma("sync", "stk%d" % sl, dst_kr, KR[0:T, :], reads=[("kr_f", sl, 0), ("kr_f", sl, 1)]))

        pT2 = psb(2)

        def tr_q(e):
            e.transpose(out=pT2[:, 0:T], in_=qln[0:T, 0:128], identity=ident[0:T, 0:T])
            e.transpose(out=pT2[:, 128:128 + T], in_=qln[0:T, 128:256], identity=ident[0:T, 0:T])
            return e.transpose(out=pT2[:, 256:256 + T], in_=ckv_b[0:T, :], identity=ident[0:T, 0:T])
        S.op("tensor", tr_q, reads=["qln", "ckv_b", "ident"], writes=["ps2"])
        qlnT, ckvT = qlnTL[sl], ckvTL[sl]
        S.op("vector", lambda e: e.tensor_copy(out=qlnT[:, :, 0:T],
                                               in_=pT2[:, 0:256].rearrange("p (k t) -> p k t", k=2)[:, :, 0:T]),
             reads=["ps2"], writes=[("qlnT", sl)])
        S.op("vector", lambda e: e.tensor_copy(out=ckvT[:, 0:T], in_=pT2[:, 256:256 + T]), reads=["ps2"], writes=[("ckvT", sl)])
        S.mute = (part == "A")

        def mm_q(e):
            ins = None
            for half in range(2):
                for k in range(2):
                    ins = e.matmul(ps[3 + half][0:T, 0:384], lhsT=qlnT[:, k, 0:T], rhs=w_uq_sb[:, k, half * 384:(half + 1) * 384],
                                   start=(k == 0), stop=(k == 1))
            return ins
        S.op("tensor", mm_q, reads=[("qlnT", sl), "w_uq"], writes=["ps3", "ps4"])

        def mm_kv(e):
            e.matmul(ps[5][0:T, :], lhsT=ckvT[:, 0:T], rhs=w_uk_sb[:, :], start=True, stop=True)
            return e.matmul(ps[6][0:T, :], lhsT=ckvT[:, 0:T], rhs=w_uv_sb[:, :], start=True, stop=True)
        S.op("tensor", mm_kv, reads=[("ckvT", sl), "w_uk", "w_uv"], writes=["ps5", "ps6"])

        for half in range(2):
            S.op("scalar", lambda e, half=half: e.activation(out=qsq[0:T, half * 384:(half + 1) * 384],
                                                             in_=ps[3 + half][0:T, 0:384], func=AF.Square),
                 reads=["ps%d" % (3 + half)], writes=[("qsq", half)])
        qsq_v = qsq[:, :].rearrange("p (h d) -> p h d", h=NH)
        S.op("vector", lambda e: e.reduce_sum(out=ST[0:T, 16:24], in_=qsq_v[0:T, :, 0:ND], axis=AX.X),
             reads=[("qsq", 0), ("qsq", 1)], writes=[("st", sl, 16)])
        S.op("vector", lambda e: e.reduce_sum(out=ST[0:T, 24:32], in_=qsq_v[0:T, :, ND:ND + RD], axis=AX.X),
             reads=[("qsq", 0), ("qsq", 1)], writes=[("st", sl, 24)])
        rstd_from_ss(ST[0:T, 16:24], ST[0:T, 32:40], ND, T, [("st", sl, 16)], [("st", sl, 32)], ST[0:T, 48:56], ("st", sl, 48))
        rstd_from_ss(ST[0:T, 24:32], ST[0:T, 40:48], RD, T, [("st", sl, 24)], [("st", sl, 40)], ST[0:T, 56:64], ("st", sl, 56))
        for half in range(2):
            hs = slice(half * 4, half * 4 + 4)
            qps = ps[3 + half][0:T, 0:384].rearrange("p (h d) -> p h d", h=4)
            S.op("vector", lambda e, hs=hs, qps=qps: e.tensor_tensor(
                out=qn[0:T, hs, 0:ND], in0=qps[:, :, 0:ND],
                in1=ST[0:T, 32 + hs.start:32 + hs.stop].unsqueeze(2).to_broadcast([T, 4, ND]), op=ALU.mult),
                reads=["ps%d" % (3 + half), ("st", sl, 32)], writes=[("qn", half, 0)])
            S.op("vector", lambda e, hs=hs, qps=qps: e.tensor_tensor(
                out=qn[0:T, hs, ND:ND + RD], in0=qps[:, :, ND:ND + RD],
                in1=ST[0:T, 40 + hs.start:40 + hs.stop].unsqueeze(2).to_broadcast([T, 4, RD]), op=ALU.mult),
                reads=["ps%d" % (3 + half), ("st", sl, 40)], writes=[("qn", half, 1)])
        qnk = [("qn", h2, j) for h2 in range(2) for j in range(2)]
        S.op("vector", lambda e: e.tensor_tensor(out=qn[0:T, :, :], in0=qn[0:T, :, :],
                                                 in1=gq_b[0:T, :].unsqueeze(1).to_broadcast([T, NH, ND + RD]), op=ALU.mult),
             reads=qnk + ["gq_b0", "gq_b1"], writes=["qn_g"])
        S.op("gpsimd", lambda e: e.tensor_copy(out=Qb[0:T, :, 0:ND], in_=qn[0:T, :, 0:ND]), reads=["qn_g"], writes=["Qb_n"])
        cosb = cosT[0:T, ti, :].unsqueeze(1).to_broadcast([T, NH, 16])
        sinb = sinT[0:T, ti, :].unsqueeze(1).to_broadcast([T, NH, 16])
        rope(Qb[0:T, :, ND:ND + 16], Qb[0:T, :, ND + 16:ND + 32], qn[0:T, :, ND:ND + 16], qn[0:T, :, ND + 16:ND + 32],
             cosb, sinb, [qrt[0:T, i, :, :] for i in range(4)], ["qn_g", "cosT", "sinT"], ["Qb_r0", "Qb_r1"], "qrt")

        S.op("scalar", lambda e: e.activation(out=ksq[0:T, 0:512], in_=ps[5][0:T, :], func=AF.Square),
             reads=["ps5"], writes=["ksq"])
        S.op("vector", lambda e: e.reduce_sum(out=ST[0:T, 64:72], in_=ksq[0:T, 0:512].rearrange("p (h d) -> p h d", h=NH), axis=AX.X),
             reads=["ksq"], writes=[("st", sl, 64)])
        rstd_from_ss(ST[0:T, 64:72], ST[0:T, 72:80], ND, T, [("st", sl, 64)], [("st", sl, 72)], ST[0:T, 80:88], ("st", sl, 80))
        S.op("vector", lambda e: e.tensor_tensor(out=kn_t[0:T, :, :], in0=ps[5][0:T, :].rearrange("p (h d) -> p h d", h=NH),
                                                 in1=ST[0:T, 72:80].unsqueeze(2).to_broadcast([T, NH, ND]), op=ALU.mult),
             reads=["ps5", ("st", sl, 72)], writes=["kn_t"])
        S.op("vector", lambda e: e.tensor_tensor(out=Kb[0:T, :, 0:ND], in0=kn_t[0:T, :, :],
                                                 in1=gkn_b[0:T, :].unsqueeze(1).to_broadcast([T, NH, ND]), op=ALU.mult),
             reads=["kn_t", "gkn_b"], writes=["Kb_n"])
        S.op("gpsimd", lambda e: e.tensor_copy(out=Kb[0:T, :, ND:ND + RD], in_=KR[0:T, :].unsqueeze(1).to_broadcast([T, NH, RD])),
             reads=[("kr_f", sl, 0), ("kr_f", sl, 1)], writes=["Kb_r"])
        if ti < 16:
            S.op("scalar", lambda e: e.copy(out=Vaug[:, ti, :, 0:VD], in_=ps[6][:, :].rearrange("p (h d) -> p h d", h=NH)),
                 reads=["ps6"], writes=[("Vaug", ti)])

        pQ = psb(7)

        def tr_Q(e):
            ins = None
            for h in range(NH):
                ins = e.transpose(out=pQ[0:96, h * 128:h * 128 + T], in_=Qb[0:T, h, :], identity=ident[0:T, 0:T])
            return ins
        S.op("tensor", tr_Q, reads=["Qb_n", "Qb_r0", "Qb_r1", "ident"], writes=["ps7"])
        S.op("vector", lambda e: e.tensor_copy(out=QT[0:96, :, tok0:tok0 + T],
                                               in_=pQ[0:96, 0:1024].rearrange("p (h t) -> p h t", h=NH)[:, :, 0:T]),
             reads=["ps7"], writes=[("QT", ti)])
        pK = psb(0)

        def tr_K(e):
            ins = None
            for h in range(NH):
                ins = e.transpose(out=pK[0:96, h * 128:h * 128 + T], in_=Kb[0:T, h, :], identity=ident[0:T, 0:T])
            return ins
        S.op("tensor", tr_K, reads=["Kb_n", "Kb_r", "ident"], writes=["ps0"])
        S.op("scalar", lambda e: e.copy(out=KT[0:96, :, tok0:tok0 + T],
                                        in_=pK[0:96, 0:1024].rearrange("p (h t) -> p h t", h=NH)[:, :, 0:T]),
             reads=["ps0"], writes=[("KT", ti)])
        S.mute = False

    def conv_chunk(ci):
        if ci < 8:
            nseg, L = 1, 256
            hTc = hT[ci % 2]
            hkeys = [("hT", ci % 2, j) for j in range(2)]
            tok0 = ci * 256
        else:
            nseg, L = 4, 8
            hTc = hT[0]
            hkeys = [("hT", 0, 0)]
            tok0 = SEQ
        N = nseg * L
        for c in range(4):
            U = ubuf[c] if ci < 8 else usam[c]
            ukey = ("ubuf", c) if ci < 8 else ("usam", c)
            ukeys = [ukey] if ci < 8 else [("usam", c, q_) for q_ in range(4)]
            cwk = [("convw_c", k) for k in range(3)]
            if ci < 8:
                ufull = U[:, :].rearrange("p (s l) -> p s l", s=1)
            else:
                ufull = U[:, :, :]

            if S.win is not None:
                S.cur_stream = 2 + c
            pc = c % 2
            bk = (1, 2, 3, 4) if pc == 0 else (5, 6, 7, 0)
            gc_, vc_, yc_, cs_, cr_ = gc_sbL[pc], vconvL[pc], yconvL[pc], csqL[pc], crsL[pc]
            kx = lambda nm: (nm, pc)

            def mm(e, c=c, bk=bk):
                ins = None
                for j, off in enumerate((0, 2 * CW, CW)):
                    for k in range(8):
                        ins = e.matmul(ps[bk[j]][:, 0:N], lhsT=w_in_sb[:, k, off + c * 128:off + (c + 1) * 128],
                                       rhs=hTc[:, k, 0:N], start=(k == 0), stop=(k == 7))
                return ins
            S.op("tensor", mm, reads=hkeys + [("w_in", k) for k in range(8)], writes=["ps%d" % bk[0], "ps%d" % bk[1], "ps%d" % bk[2]])
            S.op("scalar", lambda e, bk=bk, gc_=gc_: e.copy(out=gc_[:, 0:N], in_=ps[bk[1]][:, 0:N]), reads=["ps%d" % bk[1]], writes=[kx("gc_sb")])
            S.op("vector", lambda e, ufull=ufull, bk=bk, gc_=gc_: e.tensor_tensor(
                out=ufull[:, :, 2:2 + L], in0=ps[bk[0]][:, 0:N].rearrange("p (s l) -> p s l", s=nseg),
                in1=gc_[:, 0:N].rearrange("p (s l) -> p s l", s=nseg), op=ALU.mult),
                reads=["ps%d" % bk[0], kx("gc_sb")], writes=[(ukey, "body")])
            v3 = vc_[:, 0:N].rearrange("p (s l) -> p s l", s=nseg)
            S.op("vector", lambda e, c=c, ufull=ufull, v3=v3: e.tensor_scalar(
                out=v3, in0=ufull[:, :, 2:2 + L], scalar1=convw_c[:, 2, c:c + 1], scalar2=convb_c[:, c:c + 1],
                op0=ALU.mult, op1=ALU.add), reads=[(ukey, "body")] + cwk + ["convb_c"], writes=[kx("vconv")])
            for tap in (1, 0):
                S.op("vector", lambda e, c=c, tap=tap, ufull=ufull, v3=v3: e.scalar_tensor_tensor(
                    out=v3, in0=ufull[:, :, tap:tap + L], scalar=convw_c[:, tap, c:c + 1], in1=v3,
                    op0=ALU.mult, op1=ALU.add), reads=[(ukey, "body"), kx("vconv")] + ukeys + cwk, writes=[kx("vconv")])
            S.op("vector", lambda e, bk=bk, yc_=yc_, vc_=vc_: e.tensor_tensor(out=yc_[:, 0:N], in0=ps[bk[2]][:, 0:N], in1=vc_[:, 0:N], op=ALU.mult),
                 reads=["ps%d" % bk[2], kx("vconv")], writes=[kx("yconv")])
            S.op("scalar", lambda e, cs_=cs_, yc_=yc_: e.activation(out=cs_[:, 0:N], in_=yc_[:, 0:N], func=AF.Square),
                 reads=[kx("yconv")], writes=[kx("csq")])
            S.op("tensor", lambda e, bk=bk, cs_=cs_: e.matmul(ps[bk[3]][:, 0:N], lhsT=bconv[:, :], rhs=cs_[:, 0:N], start=True, stop=True),
                 reads=[kx("csq"), "bconv"], writes=["ps%d" % bk[3]])
            S.op("scalar", lambda e, bk=bk, cr_=cr_: e.activation(out=cr_[:, 0:N], in_=ps[bk[3]][:, 0:N], func=AF.Ln, bias=eps_c[:, :], scale=1.0),
                 reads=["ps%d" % bk[3], "eps_c"], writes=[kx("crs")])
            S.op("scalar", lambda e, cr_=cr_: e.activation(out=cr_[:, 0:N], in_=cr_[:, 0:N], func=AF.Exp, scale=-0.5),
                 reads=[kx("crs")], writes=[kx("crs")])
            S.op("vector", lambda e, c=c, yc_=yc_, cr_=cr_: e.scalar_tensor_tensor(out=yconvT[:, c, tok0:tok0 + N], in0=yc_[:, 0:N],
                                                                                  scalar=gout_c[:, c:c + 1], in1=cr_[:, 0:N],
                                                                                  op0=ALU.mult, op1=ALU.mult),
                 reads=[kx("yconv"), kx("crs"), "gout_c"], writes=[("yconvT", c, ci)])
            if True:
                if ci == 7:
                    out_recs.append(S.dma("sync", "st", o_conv_p[:, c * 128:(c + 1) * 128].rearrange("t p -> p t"),
                                          U[:, 256:258], reads=[(ukey, "body")], allow_slow_non_contiguous=True))
                elif ci == 8:
                    for q_ in range(4):
                        out_recs.append(S.dma("sync", "st", o_conv_s[q_, :, c * 128:(c + 1) * 128].rearrange("t p -> p t"),
                                              U[:, q_, 8:10], reads=[(ukey, "body")], allow_slow_non_contiguous=True))
            if ci < 7:
                S.op("gpsimd", lambda e, U=U: e.tensor_copy(out=U[:, 0:2], in_=U[:, 256:258]),
                     reads=[(ukey, "body")], writes=[ukey])

    token_tile(16, "A")
    token_tile(16, "B")
    conv_chunk(8)
    token_tile(0, "A")
    for ti in range(16):
        S.window_begin()
        if ti + 1 < 16:
            S.cur_stream = 0
            token_tile(ti + 1, "A")
        S.cur_stream = 1
        token_tile(ti, "B")
        if ti % 2 == 1:
            S.cur_stream = 2
            conv_chunk(ti // 2)
        S.cur_stream = 0
        S.window_end()

    S.barrier()
    arena_reset()
    RG = 16
    c2ckv = cache_ckv.rearrange("n (r t) d -> (n r) (t d)", r=RG)
    c2kr = cache_kr.rearrange("n (r t) d -> (n r) (t d)", r=2)
    idx_i = wk("idx_i", [128, 4], I32)
    idx16 = wk("idx16", [128, 4], I32)
    idx2 = wk("idx2", [128, 4], I32)
    riota = wk("riota", [128, RG], I32)
    idxc = wk("idxc", [128, 4, RG], I32)
    idxk = wk("idxk", [128, 4, 2], I32)
    ckv_g = [wk("ckv_g%d" % i, [128, 1024], BF16) for i in range(3)]
    kr_g = [wk("kr_g%d" % i, [128, 2048], BF16) for i in range(2)]
    cgx = [wk("cgx%d" % i, [128, 8, KVR + 1], BF16) for i in range(2)]
    ckvT_g = [wk("ckvT_g%d" % i, [128, 1024], BF16) for i in range(2)]
    krT_g = [wk("krT_g%d" % i, [128, 1024], BF16) for i in range(2)]
    ksq_s = [wk("ksq_s%d" % i, [128, 2048], BF16) for i in range(2)]
    ss_s = wk("ss_s", [128, 64], F32)
    ln_s = wk("ln_s", [128, 64], F32)
    rs_s = [wk("rs_s%d" % i, [128, 64], F32) for i in range(2)]
    tmpS = [wk("tmpS%d" % i, [128, 512], F32) for i in range(2)]
    Es = [wk("Es%d" % i, [128, 512], BF16) for i in range(2)]
    QabsT = wk("QabsT", [128, 4, 64], BF16)
    QrT = wk("QrT", [128, 4, 64], BF16)
    Qg = wk("Qg", [128, NH, NS], BF16)
    w_ukT = wk("w_ukT", [128, NH, 128], BF16)
    En = wk("En", [128, NH, NS], F32)
    En_s = wk("En_s", [128, 4, 64], BF16)
    mk_i = wk("mk_i", [128, 4], I32)
    mk_f = wk("mk_f", [128, 4], F32)
    mcol_i = wk("mcol_i", [128, 2, NS], I32)
    mcol_f = wk("mcol_f", [128, 2, NS], F32)
    mnew = wk("mnew", [128, 2, NS], F32)
    rsum = wk("rsum", [128, 1], F32)
    ctxn = wk("ctxn", [128, 128], BF16)
    ctxT = wk("ctxT", [128, 64], BF16)
    ysq = wk("ysq", [128, 256], BF16)
    yr = wk("yr", [128, 256], F32)
    battn_s = wk("battn_s", [128, 64], BF16)

    V = "vector"
    S.dma("sync", "ld", idx_i[:, :], ptab.rearrange("s p -> p s"), writes=["idx_i"], allow_slow_non_contiguous=True)
    S.op("gpsimd", lambda e: e.iota(riota[:, :], pattern=[[1, RG]], base=0, channel_multiplier=0), writes=["riota"])
    S.op("gpsimd", lambda e: e.memset(battn_s[:, :], 1.0 / 64), writes=["battn_s"])
    for q_ in range(2):
        S.op("gpsimd", lambda e, q_=q_: e.memset(cgx[q_][:, :, KVR:KVR + 1], 1.0), writes=[("cgx1", q_)])
    idx_f = wk("idx_f", [128, 4], F32)
    riota_f = wk("riota_f", [128, RG], F32)
    idxc_f = wk("idxc_f", [128, 4, RG], F32)
    idxk_f = wk("idxk_f", [128, 4, 2], F32)
    S.op(V, lambda e: e.tensor_copy(out=idx_f[:, :], in_=idx_i[:, :]), reads=["idx_i"], writes=["idx_f"])
    S.op(V, lambda e: e.tensor_copy(out=riota_f[:, :], in_=riota[:, :]), reads=["riota"], writes=["riota_f"])
    for q_ in range(4):
        S.op(V, lambda e, q_=q_: e.scalar_tensor_tensor(out=idxc_f[:, q_, :], in0=idx_f[:, q_:q_ + 1].to_broadcast([128, RG]),
                                                        scalar=float(RG), in1=riota_f[:, :], op0=ALU.mult, op1=ALU.add),
             reads=["idx_f", "riota_f"], writes=[("idxc_f", q_)])
        S.op(V, lambda e, q_=q_: e.scalar_tensor_tensor(out=idxk_f[:, q_, :], in0=idx_f[:, q_:q_ + 1].to_broadcast([128, 2]),
                                                        scalar=2.0, in1=riota_f[:, 0:2], op0=ALU.mult, op1=ALU.add),
             reads=["idx_f", "riota_f"], writes=[("idxk_f", q_)])
    S.op(V, lambda e: e.tensor_copy(out=idxc[:, :, :], in_=idxc_f[:, :, :]), reads=[("idxc_f", q_) for q_ in range(4)], writes=["idxc"])
    S.op(V, lambda e: e.tensor_copy(out=idxk[:, :, :], in_=idxk_f[:, :, :]), reads=[("idxk_f", q_) for q_ in range(4)], writes=["idxk"])

    def tr_wuk(e):
        ins = None
        for h in range(NH):
            ins = e.transpose(out=psb(0)[0:64, h * 128:(h + 1) * 128], in_=w_uk_sb[:, h * 64:(h + 1) * 64], identity=ident[:, :])
        return ins
    S.op("tensor", tr_wuk, reads=["w_uk", "ident"], writes=["ps0"])
    S.op(V, lambda e: e.tensor_copy(out=w_ukT[0:64, :, :], in_=psb(0)[0:64, 0:1024].rearrange("p (h d) -> p h d", h=NH)),
         reads=["ps0"], writes=["w_ukT"])
    S.op(V, lambda e: e.tensor_scalar(out=Qg[0:64, :, :], in0=QT[0:64, :, SEQ:SEQ + NS], scalar1=gkn_c[0:64, 0:1], scalar2=None,
                                      op0=ALU.mult), reads=[("QT", 16), "gkn_c"], writes=["Qg"])

    def mm_qabs(e):
        ins = None
        for h in range(NH):
            ins = e.matmul(ps[2][:, h * NS:(h + 1) * NS], lhsT=w_ukT[0:64, h, :], rhs=Qg[0:64, h, :], start=True, stop=True)
        return ins
    S.op("tensor", mm_qabs, reads=["w_ukT", "Qg"], writes=["ps2"])
    S.op(V, lambda e: e.tensor_copy(out=QabsT[:, :, :].rearrange("p s (h t) -> p h s t", h=NH),
                                    in_=ps[2][:, 0:NH * NS].rearrange("p (h s t) -> p h s t", h=NH, s=4)),
         reads=["ps2"], writes=["QabsT"])
    S.op(V, lambda e: e.tensor_copy(out=QrT[0:32, :, :].rearrange("p s (h t) -> p h s t", h=NH),
                                    in_=QT[64:96, :, SEQ:SEQ + NS].rearrange("p h (s t) -> p h s t", s=4)),
         reads=[("QT", 16)], writes=["QrT"])

    def mm_new(e):
        ins = None
        for h in range(NH):
            ins = e.matmul(ps[3][0:NS, h * NS:(h + 1) * NS], lhsT=KT[0:96, h, SEQ:SEQ + NS], rhs=QT[0:96, h, SEQ:SEQ + NS],
                           start=True, stop=True)
        return ins
    S.op("tensor", mm_new, reads=[("KT", 16), ("QT", 16)], writes=["ps3"])
    S.op("scalar", lambda e: e.activation(out=En[0:NS, :, :], in_=ps[3][0:NS, 0:NH * NS].rearrange("p (h q) -> p h q", h=NH),
                                          func=AF.Exp, scale=SCALE), reads=["ps3"], writes=["En"])
    S.op("gpsimd", lambda e: e.iota(mk_i[:, 0:1], pattern=[[0, 1]], base=0, channel_multiplier=1), writes=["mk_i0"])
    S.op(V, lambda e: e.tensor_single_scalar(out=mk_i[:, 1:2], in_=mk_i[:, 0:1], scalar=3, op=ALU.arith_shift_right),
         reads=["mk_i0"], writes=["mk_i1"])
    S.op(V, lambda e: e.tensor_single_scalar(out=mk_i[:, 2:3], in_=mk_i[:, 0:1], scalar=7, op=ALU.bitwise_and),
         reads=["mk_i0"], writes=["mk_i2"])
    S.op(V, lambda e: e.tensor_copy(out=mk_f[:, :], in_=mk_i[:, :]), reads=["mk_i0", "mk_i1", "mk_i2"], writes=["mk_f"])
    S.op("gpsimd", lambda e: e.iota(mcol_i[:, 0, :], pattern=[[1, 4], [0, 8]], base=0, channel_multiplier=0), writes=["mcol0"])
    S.op("gpsimd", lambda e: e.iota(mcol_i[:, 1, :], pattern=[[0, 4], [1, 8]], base=0, channel_multiplier=0), writes=["mcol1"])
    S.op(V, lambda e: e.tensor_copy(out=mcol_f[:, :, :], in_=mcol_i[:, :, :]), reads=["mcol0", "mcol1"], writes=["mcol_f"])
    S.op(V, lambda e: e.tensor_scalar(out=mnew[:, 0, :], in0=mcol_f[:, 0, :], scalar1=mk_f[:, 1:2], scalar2=None, op0=ALU.is_equal),
         reads=["mcol_f", "mk_f"], writes=["mnew0"])
    S.op(V, lambda e: e.tensor_scalar(out=mnew[:, 1, :], in0=mcol_f[:, 1, :], scalar1=mk_f[:, 2:3], scalar2=None, op0=ALU.is_ge),
         reads=["mcol_f", "mk_f"], writes=["mnew1"])
    S.op(V, lambda e: e.tensor_tensor(out=mnew[:, 0, :], in0=mnew[:, 0, :], in1=mnew[:, 1, :], op=ALU.mult),
         reads=["mnew0", "mnew1"], writes=["mnew0"])
    S.op(V, lambda e: e.tensor_tensor(out=En[0:NS, :, :], in0=En[0:NS, :, :],
                                      in1=mnew[0:NS, 0, :].unsqueeze(1).to_broadcast([NS, NH, NS]), op=ALU.mult),
         reads=["En", "mnew0"], writes=["En"])
    S.op(V, lambda e: e.tensor_copy(out=En_s[0:NS, :, :].rearrange("p s (h t) -> p h s t", h=NH),
                                    in_=En[0:NS, :, :].rearrange("p h (s t) -> p h s t", s=4)),
         reads=["En"], writes=["En_s"])

    NG = 4 * RG

    def gather_group(g):
        s_, r = divmod(g, RG)
        cg = ckv_g[g % 3]
        S.op("gpsimd", lambda e: e.indirect_dma_start(
            out=cg[:, :], out_offset=None, in_=c2ckv[:, :],
            in_offset=bass.IndirectOffsetOnAxis(ap=idxc[:, s_, r:r + 1], axis=0)),
            reads=["idxc"], writes=[("ckv_g", g % 3)], dma="gc%d" % (g % 3))
        if r % 8 == 0:
            kb = (g // 8) % 2
            S.op("gpsimd", lambda e: e.indirect_dma_start(
                out=kr_g[kb][:, :], out_offset=None, in_=c2kr[:, :],
                in_offset=bass.IndirectOffsetOnAxis(ap=idxk[:, s_, r // 8:r // 8 + 1], axis=0)),
                reads=["idxk"], writes=[("kr_g", kb)], dma="gk%d" % kb)

    def front(g):
        s_, r = divmod(g, RG)
        if g + 2 < NG:
            gather_group(g + 2)
        if g % 3 == 2:
            issue_conv(1)
        cg = ckv_g[g % 3]
        kb = (g // 8) % 2
        cT, kT_ = ckvT_g[g % 2], krT_g[g % 2]
        rs_ = rs_s[g % 2]
        cx = cgx[g % 2]
        S.op("gpsimd", lambda e: e.tensor_copy(out=cx[:, :, 0:KVR], in_=cg[:, :].rearrange("p (t d) -> p t d", t=8)),
             reads=[("ckv_g", g % 3)], writes=[("cgx", g % 2)])

        def tr_c(e):
            ins = None
            for t in range(8):
                ins = e.transpose(out=psb(0)[:, t * 128:(t + 1) * 128], in_=cg[:, t * 128:(t + 1) * 128], identity=ident[:, :])
            return ins
        S.op("tensor", tr_c, reads=[("ckv_g", g % 3), "ident"], writes=["ps0"])
        S.op("scalar", lambda e: e.copy(out=cT[:, :], in_=psb(0)[:, 0:1024]), reads=["ps0"], writes=[("ckvT_g", g % 2)])

        def tr_k(e):
            ins = None
            for t in range(8):
                tl = (r % 8) * 8 + t
                ins = e.transpose(out=psb(1)[0:32, t * 128:(t + 1) * 128], in_=kr_g[kb][:, tl * 32:(tl + 1) * 32], identity=ident[:, :])
            return ins
        S.op("tensor", tr_k, reads=[("kr_g", kb), "ident"], writes=["ps1"])
        S.op(V, lambda e: e.tensor_copy(out=kT_[0:32, :], in_=psb(1)[0:32, 0:1024]), reads=["ps1"], writes=[("krT_g", g % 2)])
        for rd in range(4):
            b0 = 2 + 2 * (rd % 2)

            def mm_kn(e, rd=rd, b0=b0):
                ins = None
                for tt_ in range(2):
                    t = rd * 2 + tt_
                    ins = e.matmul(ps[b0 + tt_][:, :], lhsT=cT[:, t * 128:(t + 1) * 128], rhs=w_uk_sb[:, :], start=True, stop=True)
                return ins
            S.op("tensor", mm_kn, reads=[("ckvT_g", g % 2), "w_uk"], writes=["ps%d" % b0, "ps%d" % (b0 + 1)])
            S.op("scalar", lambda e, rd=rd, b0=b0: e.activation(out=ksq_s[rd % 2][:, 0:1024], in_=ps_big[:, b0 * 512:(b0 + 2) * 512],
                                                                func=AF.Square),
                 reads=["ps%d" % b0, "ps%d" % (b0 + 1)], writes=[("ksq_s", rd % 2)])
            S.op(V, lambda e, rd=rd: e.reduce_sum(out=ss_s[:, rd * 16:(rd + 1) * 16],
                                                  in_=ksq_s[rd % 2][:, 0:1024].rearrange("p (a d) -> p a d", d=ND), axis=AX.X),
                 reads=[("ksq_s", rd % 2)], writes=[("ss_s", rd)])
        S.op("scalar", lambda e: e.activation(out=ln_s[:, :], in_=ss_s[:, :], func=AF.Ln, bias=eps_c[:, :], scale=1.0 / ND),
             reads=[("ss_s", q_) for q_ in range(4)] + ["eps_c"], writes=["ln_s"])
        S.op("scalar", lambda e: e.activation(out=rs_[:, :], in_=ln_s[:, :], func=AF.Exp, scale=-0.5),
             reads=["ln_s"], writes=[("rs_s", g % 2)])

    def back(g):
        s_, r = divmod(g, RG)
        cT, kT_ = ckvT_g[g % 2], krT_g[g % 2]
        rs_ = rs_s[g % 2]
        cx = cgx[g % 2]
        if r == 0:
            S.op("tensor", lambda e: e.matmul(ps[7][0:64, 0:KVR + 1], lhsT=En_s[0:NS, s_, :], rhs=ckv_s_b[0:NS, :], start=True, stop=False),
                 reads=["En_s", "ckv_s_b", "ckv_s_b1"], writes=["ps7"])

        def mm_sc(e):
            ins = None
            for t in range(8):
                e.matmul(ps[6][:, t * 64:(t + 1) * 64], lhsT=cT[:, t * 128:(t + 1) * 128], rhs=QabsT[:, s_, :], start=True, stop=True)
            for t in range(8):
                ins = e.matmul(ps[1][:, t * 64:(t + 1) * 64], lhsT=kT_[0:32, t * 128:(t + 1) * 128], rhs=QrT[0:32, s_, :],
                               start=True, stop=True)
            return ins
        S.op("tensor", mm_sc, reads=[("ckvT_g", g % 2), ("krT_g", g % 2), "QabsT", "QrT"], writes=["ps6", "ps1"])
        tS, E_ = tmpS[g % 2], Es[g % 2]
        S.op(V, lambda e: e.tensor_tensor(out=tS[:, :].rearrange("p (a t) -> p a t", t=8),
                                          in0=ps[6][:, :].rearrange("p (a t) -> p a t", t=8),
                                          in1=rs_[:, :].unsqueeze(2).to_broadcast([128, 64, 8]), op=ALU.mult),
             reads=["ps6", ("rs_s", g % 2)], writes=[("tmpS", g % 2)])
        S.op(V, lambda e: e.tensor_tensor(out=tS[:, :], in0=ps[1][:, :], in1=tS[:, :], op=ALU.add),
             reads=["ps1", ("tmpS", g % 2)], writes=[("tmpS", g % 2)])
        S.op("scalar", lambda e: e.activation(out=E_[:, :], in_=tS[:, :], func=AF.Exp, scale=SCALE),
             reads=[("tmpS", g % 2)], writes=[("Es", g % 2)])

        def mm_ctx(e):
            ins = None
            for t in range(8):
                last = (r == RG - 1 and t == 7)
                ins = e.matmul(ps[7][0:64, 0:KVR + 1], lhsT=E_[:, t * 64:(t + 1) * 64], rhs=cx[:, t, :], start=False, stop=last)
            return ins
        S.op("tensor", mm_ctx, reads=[("Es", g % 2), ("cgx", g % 2), ("cgx1", g % 2)], writes=["ps7"])
        if r == RG - 1:
            S.op(V, lambda e: e.reciprocal(out=rsum[0:64, :], in_=ps[7][0:64, 128:129]), reads=["ps7"], writes=["rsum"])
            S.op(V, lambda e: e.tensor_scalar(out=ctxn[0:64, :], in0=ps[7][0:64, 0:128], scalar1=rsum[0:64, 0:1], scalar2=None, op0=ALU.mult),
                 reads=["ps7", "rsum"], writes=["ctxn"])
            S.op("tensor", lambda e: e.transpose(out=ps_ct[:, 0:64], in_=ctxn[0:64, :], identity=ident[0:64, 0:64]),
                 reads=["ctxn", "ident"], writes=["ps7b"])
            S.op(V, lambda e: e.tensor_copy(out=ctxT[:, :], in_=ps_ct[:, 0:64]), reads=["ps7b"], writes=["ctxT"])

            def mm_yv(e):
                ins = None
                for h in range(NH):
                    ins = e.matmul(ps_yv[0:64, h * NS + s_ * 8:h * NS + s_ * 8 + 8], lhsT=w_uv_sb[:, h * 64:(h + 1) * 64],
                                   rhs=ctxT[:, h * 8:(h + 1) * 8], start=True, stop=True)
                return ins
            S.op("tensor", mm_yv, reads=["ctxT", "w_uv"], writes=[("psyv", s_)])

    ps_yv = ps_big[:, 7 * 512 + 256:7 * 512 + 512]
    ps_ct = ps[7].bitcast(BF16)[:, 320:384]
    gather_group(0)
    gather_group(1)
    front(0)
    for g in range(NG):
        S.window_begin()
        if g + 1 < NG:
            S.cur_stream = 0
            front(g + 1)
        S.cur_stream = 1
        back(g)
        S.cur_stream = 0
        S.window_end()
    S.op("scalar", lambda e: e.activation(out=ysq[0:64, :], in_=ps_yv[0:64, 0:256], func=AF.Square),
         reads=[("psyv", q) for q in range(4)], writes=["ysq"])
    S.op("tensor", lambda e: e.matmul(ps[2][0:64, 0:256], lhsT=battn_s[0:64, 0:64], rhs=ysq[0:64, :], start=True, stop=True),
         reads=["ysq", "battn_s"], writes=["ps2"])
    S.op("scalar", lambda e: e.activation(out=yr[0:64, :], in_=ps[2][0:64, 0:256], func=AF.Ln, bias=eps_c[0:64, :], scale=1.0),
         reads=["ps2", "eps_c"], writes=["yr"])
    S.op("scalar", lambda e: e.activation(out=yr[0:64, :], in_=yr[0:64, :], func=AF.Exp, scale=-0.5), reads=["yr"], writes=["yr"])
    for h in range(NH):
        po = (h % 2) * 64
        S.op(V, lambda e, h=h, po=po: e.scalar_tensor_tensor(out=ya_s[po:po + 64, h // 2, :], in0=ps_yv[0:64, h * NS:(h + 1) * NS],
                                                             scalar=gattn_c[0:64, h:h + 1], in1=yr[0:64, h * NS:(h + 1) * NS],
                                                             op0=ALU.mult, op1=ALU.mult),
             reads=[("psyv", q) for q in range(4)] + ["yr", "gattn_c"], writes=["ya_s"])

    S.barrier()
    arena_reset()
    x1s = nc.dram_tensor("x1s", [NTOK, D], F32, kind="Internal").ap()
    OH = wk("OH", [128, 17, 64], F32)
    RK = wk("RK", [128, 17, 4], F32)
    carry = wk("carry", [128, NE], F32)
    P23_MARK = arena_off[0]
    LG = wk("LG", [128, 17, 36], F32)
    MARK_R = arena_off[0]
    H2 = nc.dram_tensor("H2", [NTOK, D], BF16, kind="Internal").ap()
    w_out_sb = wk("w_out_sb", [128, 8, D], BF16)
    Et = [wk("Et%d" % i, [128, 512], BF16) for i in range(6)]
    sqa = wk("sqa", [128, 512], BF16)
    lnr = wk("lnr", [128, 512], F32)
    yattnT = [wk("yattnT%d" % i, [128, 4, 512], BF16) for i in range(2)]
    xr = [wk("xr%d" % i, [128, D], F32) for i in range(2)]
    h2b = wk("h2b", [128, D], BF16)
    junk2 = wk("junk2", [128, D], BF16)
    st2 = [wk("st2_%d" % i, [128, 32], F32) for i in range(2)]
    brb = wk("brb", [128, 36], F32)
    wr_sb = wk("wr_sb", [128, 8, 36], BF16)
    battn = wk("battn", [128, 64], BF16)

    S.dma("gpsimd", "wo", w_out_sb, w_out.rearrange("(k p) n -> p k n", p=128), writes=["w_out"])
    S.dma("gpsimd", "wr0", wr_sb[:, :, 0:4], w_rg.rearrange("(k p) n -> p k n", p=128), writes=["wr0"])
    S.dma("gpsimd", "wr1", wr_sb[:, :, 4:36], w_re.rearrange("(k p) n -> p k n", p=128), writes=["wr1"])
    S.dma("sync", "lg", gmix_b[:], g_ffn[0:1, :].to_broadcast([128, D]), writes=["gffn_b"])
    S.dma("sync", "lb0", brb[:, 0:4], b_rg[0:1, :].to_broadcast([128, 4]), writes=["brb0"])
    S.dma("sync", "lb1", brb[:, 4:36], b_re[0:1, :].to_broadcast([128, NE]), writes=["brb1"])
    S.op("gpsimd", lambda e: e.memset(battn[0:64, :], 1.0 / 64), writes=["battn0"])
    S.op("gpsimd", lambda e: e.memset(battn[64:65, :], EPS), writes=["battn1"])

    def attention_chunk(c):
        ya = yattnT[c % 2]
        nj = 4 * c + 4
        S.window_begin()
        for h in range(NH):
            S.cur_stream = h % 2
            ob = 2 + (h % 2)
            sb0 = 0 if h % 2 == 0 else 5
            eb0 = 3 * (h % 2)

            def q0n(j):
                q0 = max(c * 512, j * 128)
                return q0, (c + 1) * 512 - q0

            def issue_S(j, h=h, sb0=sb0):
                q0, n = q0n(j)
                bank = sb0 + j % 2
                S.op("tensor", lambda e: e.matmul(ps[bank][:, 0:n], lhsT=KT[0:96, h, j * 128:(j + 1) * 128],
                                                  rhs=QT[0:96, h, q0:q0 + n], start=True, stop=True),
                     reads=[("KT", j)] + [("QT", t) for t in range(q0 // 128, 4 * c + 4)], writes=["ps%d" % bank])
            issue_S(0)
            for j in range(nj):
                if j + 1 < nj:
                    issue_S(j + 1)
                q0, n = q0n(j)
                bank, eb = sb0 + j % 2, eb0 + j % 3
                S.op("scalar", lambda e, n=n, bank=bank, eb=eb: e.activation(out=Et[eb][:, 0:n], in_=ps[bank][:, 0:n],
                                                                             func=AF.Exp, scale=SCALE),
                     reads=["ps%d" % bank], writes=[("E", eb)])
                if j >= 4 * c:
                    S.op("gpsimd", lambda e, eb=eb: e.tensor_tensor(out=Et[eb][:, 0:128], in0=Et[eb][:, 0:128], in1=tri[:, :],
                                                                    op=ALU.mult),
                         reads=[("E", eb), "tri"], writes=[("E", eb)])
                S.op("tensor", lambda e, j=j, h=h, n=n, q0=q0, eb=eb, ob=ob: e.matmul(
                    ps[ob][0:65, q0 - c * 512:512], lhsT=Vaug[:, j, h, :], rhs=Et[eb][:, 0:n],
                    start=(j == 0), stop=(j == nj - 1)),
                    reads=[("E", eb), ("Vaug", j), "Vaug_ones"], writes=["ps%d" % ob])
            S.op("scalar", lambda e, ob=ob: e.activation(out=sqa[0:65, :], in_=ps[ob][0:65, :], func=AF.Square),
                 reads=["ps%d" % ob], writes=["sqa"])
            S.op("tensor", lambda e: e.matmul(ps[4][0:64, :], lhsT=battn[0:65, :], rhs=sqa[0:65, :], start=True, stop=True),
                 reads=["sqa", "battn0", "battn1"], writes=["ps4"])
            S.op("scalar", lambda e: e.activation(out=lnr[0:64, :], in_=ps[4][0:64, :], func=AF.Ln),
                 reads=["ps4", "lnr", "lnr2"], writes=["lnr"])
            S.op("scalar", lambda e: e.activation(out=lnr[0:64, :], in_=lnr[0:64, :], func=AF.Exp, scale=-0.5),
                 reads=["lnr"], writes=["lnr"])
            po = (h % 2) * 64
            S.op("vector", lambda e, h=h, ob=ob, po=po, ya=ya: e.scalar_tensor_tensor(
                out=ya[po:po + 64, h // 2, :], in0=ps[ob][0:64, :], scalar=gattn_c[0:64, h:h + 1], in1=lnr[0:64, :],
                op0=ALU.mult, op1=ALU.mult),
                reads=["ps%d" % ob, "lnr", "gattn_c"], writes=[("ya", c % 2, h)])
        S.cur_stream = 0
        S.window_end()

    def merge_tile(ti):
        T = 128 if ti < 16 else NS
        tok0 = ti * 128
        sl = ti % 2
        X, X1, ST = xr[sl], xr[sl], st2[sl]
        src = x_p[tok0:tok0 + 128, :] if ti < 16 else x_s[:, :]
        if ti == 0:
            S.dma("sync", "ldr%d" % sl, X[0:T, :], src, writes=[("xr", sl), ("x1b", sl, 0), ("x1b", sl, 1)])
        if ti < 16:
            nt_ = ti + 1
            nsl = nt_ % 2
            nT = 128 if nt_ < 16 else NS
            nsrc = x_p[nt_ * 128:(nt_ + 1) * 128, :] if nt_ < 16 else x_s[:, :]
            S.dma("sync", "ldr%d" % nsl, xr[nsl][0:nT, :], nsrc, writes=[("xr", nsl), ("x1b", nsl, 0), ("x1b", nsl, 1)])
        if ti < 16:
            ya = yattnT[(ti // 4) % 2]
            acol = (ti % 4) * 128
            yakeys = [("ya", (ti // 4) % 2, h) for h in range(NH)]
        else:
            ya = ya_s
            acol = 0
            yakeys = ["ya_s"]

        def mm(e):
            ins = None
            for half in range(2):
                for k in range(4):
                    ins = e.matmul(ps[5 + half][0:T, :], lhsT=yconvT[:, k, tok0:tok0 + T],
                                   rhs=w_out_sb[:, k, half * 512:(half + 1) * 512], start=(k == 0), stop=False)
                for k in range(4):
                    ins = e.matmul(ps[5 + half][0:T, :], lhsT=ya[:, k, acol:acol + T],
                                   rhs=w_out_sb[:, 4 + k, half * 512:(half + 1) * 512], start=False, stop=(k == 3))
            return ins
        S.op("tensor", mm, reads=yakeys + ["w_out"] + [("yconvT", c, ci) for c in range(4) for ci in range(9)],
             writes=["ps5", "ps6"])
        for half in range(2):
            S.op("vector", lambda e, half=half: e.tensor_tensor(out=X1[0:T, half * 512:(half + 1) * 512],
                                                                in0=ps[5 + half][0:T, :], in1=X[0:T, half * 512:(half + 1) * 512],
                                                                op=ALU.add),
                 reads=["ps%d" % (5 + half), ("xr", sl)], writes=[("x1b", sl, half)])
        x1k = [("x1b", sl, 0), ("x1b", sl, 1)]
        S.dma("sync", "stx%d" % sl, x1s[tok0:tok0 + T, :], X1[0:T, :], reads=x1k, writes=[("x1s", ti)])
        S.op("scalar", lambda e: e.activation(out=junk2[0:T, :], in_=X1[0:T, :], func=AF.Square, accum_out=ST[0:T, 0:1]),
             reads=x1k, writes=[("st2", sl, 0)])
        rstd_from_ss(ST[0:T, 0:1], ST[0:T, 1:2], D, T, [("st2", sl, 0)], [("st2", sl, 1)], ST[0:T, 2:3], ("st2", sl, 2))
        S.op("vector", lambda e: e.scalar_tensor_tensor(out=h2b[0:T, :], in0=X1[0:T, :], scalar=ST[0:T, 1:2],
                                                        in1=gmix_b[0:T, :], op0=ALU.mult, op1=ALU.mult),
             reads=x1k + [("st2", sl, 1), "gffn_b"], writes=["h2b"])
        S.dma("sync", "sth", H2[tok0:tok0 + T, :], h2b[0:T, :], reads=["h2b"], writes=[("H2", ti)])
        pT = psb(7)

        def tr(e):
            ins = None
            for k in range(8):
                ins = e.transpose(out=pT[:, k * 128:k * 128 + T], in_=h2b[0:T, k * 128:(k + 1) * 128], identity=ident[0:T, 0:T])
            return ins
        S.op("tensor", tr, reads=["h2b", "ident"], writes=["ps7"])
        S.op("scalar", lambda e: e.copy(out=QT[:, :, tok0:tok0 + T],
                                        in_=pT[:, 0:1024].rearrange("p (k t) -> p k t", k=8)[:, :, 0:T]),
             reads=["ps7"], writes=[("QT", ti)])

        def mmr(e):
            ins = None
            for k in range(8):
                ins = e.matmul(ps[7][0:T, 0:36], lhsT=QT[:, k, tok0:tok0 + T], rhs=wr_sb[:, k, :], start=(k == 0), stop=(k == 7))
            return ins
        S.op("tensor", mmr, reads=[("QT", ti), "wr0", "wr1"], writes=["ps7"])
        S.op("vector", lambda e: e.tensor_tensor(out=LG[0:T, ti, :], in0=ps[7][0:T, 0:36], in1=brb[0:T, :], op=ALU.add),
             reads=["ps7", "brb0", "brb1"], writes=[("LG", ti)])

    for c in range(4):
        attention_chunk(c)
        S.window_begin()
        for t in range(4):
            S.cur_stream = t % 2
            merge_tile(4 * c + t)
        S.cur_stream = 0
        S.window_end()
    merge_tile(16)
    issue_conv(100)

    S.barrier()
    arena_off[0] = MARK_R
    V = "vector"
    NTL = 17
    g4 = wk("g4", [128, NTL, 4], F32)
    goh = wk("goh", [128, NTL, 4], F32)
    pen = wk("pen", [128, NTL, 4], F32)
    sc = wk("sc", [128, 12, NTL], F32)
    em = wk("em", [128, NTL, NE], F32)
    em2 = wk("em2", [128, NTL, NE], F32)
    R7 = wk("R7", [128, NTL, NE], F32)
    CAR = wk("CAR", [128, NTL, NE], F32)
    Mb = wk("Mb", [128, NTL, NE], BF16)
    lst_b = wk("lst_b", [128, 128], BF16)
    S.op("gpsimd", lambda e: e.affine_select(out=lst_b[:, :], in_=onesb[:, :], pattern=[[1, 128]], compare_op=ALU.is_gt, fill=0.0,
                                            base=0, channel_multiplier=-1), reads=["onesb"], writes=["lst_b"])
    SC = lambda i: sc[:, i, :]
    bc4 = lambda ap: ap.unsqueeze(2).to_broadcast([128, NTL, 4])
    bc32 = lambda ap: ap.unsqueeze(2).to_broadcast([128, NTL, NE])
    OH1a, OH2a = OH[:, :, 0:32], OH[:, :, 32:64]
    S.op(V, lambda e: e.reduce_max(out=SC(0), in_=LG[:, :, 0:4], axis=AX.X), writes=["sc0"])
    S.op(V, lambda e: e.tensor_tensor(out=goh[:, :, :], in0=LG[:, :, 0:4], in1=bc4(SC(0)), op=ALU.is_equal), reads=["sc0"], writes=["goh"])
    S.op(V, lambda e: e.tensor_tensor(out=g4[:, :, :], in0=LG[:, :, 0:4], in1=bc4(SC(0)), op=ALU.subtract), reads=["sc0"], writes=["g4"])
    S.op("scalar", lambda e: e.activation(out=g4[:, :, :], in_=g4[:, :, :], func=AF.Exp), reads=["g4"], writes=["g4"])
    S.op(V, lambda e: e.reduce_sum(out=SC(1), in_=g4[:, :, :], axis=AX.X), reads=["g4"], writes=["sc1"])
    S.op(V, lambda e: e.reciprocal(out=SC(2), in_=SC(1)), reads=["sc1"], writes=["sc2"])
    S.op(V, lambda e: e.tensor_scalar(out=pen[:, :, :], in0=goh[:, :, :], scalar1=-1.0, scalar2=1e30, op0=ALU.add, op1=ALU.mult),
         reads=["goh"], writes=["pen"])
    S.op(V, lambda e: e.tensor_tensor(out=em[:, :, :].rearrange("p t (g x) -> p t g x", g=4),
                                      in0=LG[:, :, 4:36].rearrange("p t (g x) -> p t g x", g=4),
                                      in1=pen[:, :, :].unsqueeze(3).to_broadcast([128, NTL, 4, 8]), op=ALU.add),
         reads=["pen"], writes=["em"])
    S.op(V, lambda e: e.reduce_max(out=SC(3), in_=em[:, :, :], axis=AX.X), reads=["em"], writes=["sc3"])
    S.op(V, lambda e: e.tensor_tensor(out=OH1a, in0=em[:, :, :], in1=bc32(SC(3)), op=ALU.is_equal), reads=["em", "sc3"], writes=["oh1"])
    S.op(V, lambda e: e.scalar_tensor_tensor(out=em2[:, :, :], in0=OH1a, scalar=-1e30, in1=em[:, :, :], op0=ALU.mult, op1=ALU.add),
         reads=["oh1", "em"], writes=["em2"])
    S.op(V, lambda e: e.reduce_max(out=SC(4), in_=em2[:, :, :], axis=AX.X), reads=["em2"], writes=["sc4"])
    S.op(V, lambda e: e.tensor_tensor(out=OH2a, in0=em2[:, :, :], in1=bc32(SC(4)), op=ALU.is_equal), reads=["em2", "sc4"], writes=["oh2"])
    S.op(V, lambda e: e.tensor_tensor(out=SC(5), in0=SC(4), in1=SC(3), op=ALU.subtract), reads=["sc3", "sc4"], writes=["sc5"])
    S.op("scalar", lambda e: e.activation(out=SC(6), in_=SC(5), func=AF.Exp), reads=["sc5"], writes=["sc6"])
    S.op(V, lambda e: e.tensor_scalar(out=SC(7), in0=SC(6), scalar1=1.0, scalar2=None, op0=ALU.add), reads=["sc6"], writes=["sc7"])
    S.op(V, lambda e: e.reciprocal(out=SC(8), in_=SC(7)), reads=["sc7"], writes=["sc8"])
    S.op(V, lambda e: e.tensor_tensor(out=RK[:, :, 2], in0=SC(8), in1=SC(2), op=ALU.mult), reads=["sc8", "sc2"], writes=["rk2"])
    S.op(V, lambda e: e.tensor_tensor(out=RK[:, :, 3], in0=SC(2), in1=RK[:, :, 2], op=ALU.subtract), reads=["rk2", "sc2"], writes=["rk3"])
    S.op(V, lambda e: e.tensor_tensor(out=Mb[:, :, :], in0=OH1a, in1=OH2a, op=ALU.add), reads=["oh1", "oh2"], writes=["Mb"])

    def mmc(e):
        ins = None
        for ti in range(NTL):
            T = 128 if ti < 16 else NS
            if ti < 16:
                e.matmul(ps[6][:, ti * NE:(ti + 1) * NE], lhsT=lst_b[0:T, 0:T], rhs=Mb[0:T, ti, :], start=True, stop=True)
                ins = e.matmul(ps[5][:, ti * NE:(ti + 1) * NE], lhsT=onesb[0:T, :], rhs=Mb[0:T, ti, :], start=True, stop=True)
            else:
                e.matmul(ps[7][0:T, 0:NE], lhsT=lst_b[0:T, 0:T], rhs=Mb[0:T, ti, :], start=True, stop=True)
                ins = e.matmul(ps[7][:, 64:64 + NE], lhsT=onesb[0:T, :], rhs=Mb[0:T, ti, :], start=True, stop=True)
        return ins
    S.op("tensor", mmc, reads=["Mb", "lst_b", "onesb"], writes=["ps5", "ps6", "ps7"])
    S.op("gpsimd", lambda e: e.memset(CAR[:, 0, :], 0.0), writes=[("car", 0)])
    for ti in range(16):
        S.op(V, lambda e, ti=ti: e.tensor_tensor(out=CAR[:, ti + 1, :], in0=ps[5][:, ti * NE:(ti + 1) * NE], in1=CAR[:, ti, :], op=ALU.add),
             reads=["ps5", ("car", ti)], writes=[("car", ti + 1)])
    S.op(V, lambda e: e.tensor_tensor(out=carry[:, :], in0=ps[7][:, 64:64 + NE], in1=CAR[:, 16, :], op=ALU.add),
         reads=["ps7", ("car", 16)], writes=["carry"])
    cark = [("car", t) for t in range(17)]
    S.op(V, lambda e: e.tensor_tensor(out=R7[:, 0:16, :], in0=ps[6][:, :].rearrange("p (t x) -> p t x", t=16), in1=CAR[:, 0:16, :], op=ALU.add),
         reads=["ps6"] + cark, writes=["R7a"])
    S.op(V, lambda e: e.tensor_tensor(out=R7[0:NS, 16, :], in0=ps[7][0:NS, 0:NE], in1=CAR[0:NS, 16, :], op=ALU.add),
         reads=["ps7"] + cark, writes=["R7b"])
    S.op(V, lambda e: e.tensor_tensor(out=em[:, :, :], in0=R7[:, :, :], in1=OH1a, op=ALU.mult), reads=["R7a", "R7b", "oh1", "em2"], writes=["em"])
    S.op(V, lambda e: e.reduce_sum(out=RK[:, :, 0], in_=em[:, :, :], axis=AX.X), reads=["em"], writes=["rk0"])
    S.op(V, lambda e: e.tensor_tensor(out=em2[:, :, :], in0=R7[:, :, :], in1=OH2a, op=ALU.mult), reads=["R7a", "R7b", "oh2"], writes=["em2"])
    S.op(V, lambda e: e.reduce_sum(out=RK[:, :, 1], in_=em2[:, :, :], axis=AX.X), reads=["em2"], writes=["rk1"])

    S.barrier()
    arena_off[0] = P23_MARK
    TM = 256
    NT = (2 * NTOK + TM - 1) // TM + NE
    NSL = NT * TM
    Xs = nc.dram_tensor("Xs", [NSL, D], BF16, kind="Internal").ap()
    Ys = nc.dram_tensor("Ys", [NSL, D], BF16, kind="Internal").ap()
    V = "vector"
    cnt_i = wk("cnt_i", [128, NE], I32)
    nt_f = wk("nt_f", [128, NE], F32)
    scn = [wk("scn%d" % i, [128, NE], F32) for i in range(2)]
    bt = wk("bt", [128, NE], F32)
    bslot = wk("bslot", [128, NE], F32)
    ee_i = wk("ee_i", [128, NE], I32)
    ee_f = wk("ee_f", [128, NE], F32)
    QTf = QT[:, :, :].rearrange("p h t -> p (h t)").bitcast(F32)
    ii_f = QTf[:, 0:NT * NE].rearrange("p (i e) -> p i e", i=NT)
    ii_i = QTf[:, NT * NE:2 * NT * NE].bitcast(I32).rearrange("p (i e) -> p i e", i=NT)
    indA = QTf[:, 2 * NT * NE:3 * NT * NE].rearrange("p (i e) -> p i e", i=NT)
    texp = wk("texp", [128, NT], F32)
    tval = wk("tval", [128, NT], F32)
    pp_i = wk("pp_i", [128, 1], I32)
    pp_f = wk("pp_f", [128, 1], F32)
    idxw_f = wk("idxw_f", [128, NT], F32)
    idxw = wk("idxw", [128, NT], I32)
    POSf = wk("POSf", [128, 17, 2], F32)
    POS = wk("POS", [128, 17, 2], I32)
    ptmp = wk("ptmp", [128, NE], F32)

    S.op(V, lambda e: e.tensor_copy(out=cnt_i[:, :], in_=carry[:, :]), reads=["carry"], writes=["cnt_i"])
    S.op(V, lambda e: e.tensor_single_scalar(out=cnt_i[:, :], in_=cnt_i[:, :], scalar=TM - 1, op=ALU.add), reads=["cnt_i"], writes=["cnt_i"])
    S.op(V, lambda e: e.tensor_single_scalar(out=cnt_i[:, :], in_=cnt_i[:, :], scalar=8, op=ALU.arith_shift_right),
         reads=["cnt_i"], writes=["cnt_i"])
    S.op(V, lambda e: e.tensor_copy(out=nt_f[:, :], in_=cnt_i[:, :]), reads=["cnt_i"], writes=["nt_f"])
    cur, curk = nt_f, ["nt_f"]
    for si, sh in enumerate((1, 2, 4, 8, 16)):
        nxt = scn[si % 2]
        k0, k1 = ("scn", si % 2, 0), ("scn", si % 2, 1)
        S.op(V, lambda e, cur=cur, nxt=nxt, sh=sh: e.tensor_copy(out=nxt[:, 0:sh], in_=cur[:, 0:sh]), reads=curk, writes=[k0])
        S.op(V, lambda e, cur=cur, nxt=nxt, sh=sh: e.tensor_tensor(out=nxt[:, sh:NE], in0=cur[:, sh:NE], in1=cur[:, 0:NE - sh], op=ALU.add),
             reads=curk, writes=[k1])
        cur, curk = nxt, [k0, k1]
    bti, btik = cur, curk
    S.op(V, lambda e: e.tensor_tensor(out=bt[:, :], in0=bti[:, :], in1=nt_f[:, :], op=ALU.subtract), reads=btik + ["nt_f"], writes=["bt"])
    S.op(V, lambda e: e.tensor_single_scalar(out=bslot[:, :], in_=bt[:, :], scalar=float(TM), op=ALU.mult), reads=["bt"], writes=["bslot"])
    S.op("gpsimd", lambda e: e.iota(ee_i[:, :], pattern=[[1, NE]], base=0, channel_multiplier=0), writes=["ee_i"])
    S.op("gpsimd", lambda e: e.iota(ii_i[:, :, :], pattern=[[1, NT], [0, NE]], base=0, channel_multiplier=0), writes=["ii_i"])
    S.op("gpsimd", lambda e: e.iota(pp_i[:, :], pattern=[[0, 1]], base=0, channel_multiplier=1), writes=["pp_i"])
    S.op(V, lambda e: e.tensor_copy(out=ee_f[:, :], in_=ee_i[:, :]), reads=["ee_i"], writes=["ee_f"])
    S.op(V, lambda e: e.tensor_copy(out=ii_f[:, :, :], in_=ii_i[:, :, :]), reads=["ii_i"], writes=["ii_f"])
    S.op(V, lambda e: e.tensor_copy(out=pp_f[:, :], in_=pp_i[:, :]), reads=["pp_i"], writes=["pp_f"])
    S.op(V, lambda e: e.tensor_tensor(out=indA[:, :, :], in0=ii_f[:, :, :], in1=bt[:, :].unsqueeze(1).to_broadcast([128, NT, NE]), op=ALU.is_ge),
         reads=["ii_f", "bt"], writes=["indA"])
    S.op(V, lambda e: e.tensor_tensor(out=ii_f[:, :, :], in0=ii_f[:, :, :], in1=bti[:, :].unsqueeze(1).to_broadcast([128, NT, NE]), op=ALU.is_lt),
         reads=["ii_f"] + btik, writes=["ii_f"])
    S.op(V, lambda e: e.tensor_tensor(out=indA[:, :, :], in0=indA[:, :, :], in1=ii_f[:, :, :], op=ALU.mult), reads=["indA", "ii_f"], writes=["indA"])
    S.op(V, lambda e: e.reduce_sum(out=tval[:, :], in_=indA[:, :, :], axis=AX.X), reads=["indA"], writes=["tval"])
    S.op(V, lambda e: e.tensor_tensor(out=indA[:, :, :], in0=indA[:, :, :], in1=ee_f[:, :].unsqueeze(1).to_broadcast([128, NT, NE]), op=ALU.mult),
         reads=["indA", "ee_f", "tval"], writes=["indA"])
    S.op(V, lambda e: e.reduce_sum(out=texp[:, :], in_=indA[:, :, :], axis=AX.X), reads=["indA"], writes=["texp"])
    S.op(V, lambda e: e.tensor_scalar(out=idxw_f[:, :], in0=texp[:, :], scalar1=128.0, scalar2=pp_f[:, 0:1], op0=ALU.mult, op1=ALU.add),
         reads=["texp", "pp_f"], writes=["idxw_f"])
    S.op(V, lambda e: e.tensor_copy(out=idxw[:, :], in_=idxw_f[:, :]), reads=["idxw_f"], writes=["idxw"])

    h2t = [wk("h2t%d" % i, [128, D], BF16) for i in range(4)]
    last_sc = None
    for ti in range(17):
        T = 128 if ti < 16 else NS
        for k in range(2):
            S.op(V, lambda e, ti=ti, k=k, T=T: e.tensor_tensor(out=ptmp[0:T, :], in0=OH[0:T, ti, 32 * k:32 * k + 32], in1=bslot[0:T, :], op=ALU.mult),
                 reads=["bslot"], writes=["ptmp"])
            S.op(V, lambda e, ti=ti, k=k, T=T: e.reduce_sum(out=POSf[0:T, ti, k:k + 1], in_=ptmp[0:T, :], axis=AX.X),
                 reads=["ptmp"], writes=[("posf", ti, k)])
        S.op(V, lambda e, ti=ti, T=T: e.tensor_tensor(out=POSf[0:T, ti, :], in0=POSf[0:T, ti, :], in1=RK[0:T, ti, 0:2], op=ALU.add),
             reads=[("posf", ti, 0), ("posf", ti, 1)], writes=[("posf2", ti)])
        S.op(V, lambda e, ti=ti, T=T: e.tensor_copy(out=POS[0:T, ti, :], in_=POSf[0:T, ti, :]), reads=[("posf2", ti)], writes=[("pos", ti)])
        sl = ti % 4
        tok0 = ti * 128
        S.dma("sync", "lh%d" % sl, h2t[sl][0:T, :], H2[tok0:tok0 + T, :], writes=[("h2t", sl)])
        if ti == 0:
            dbg("h2t0", h2t[0][:, :], [("h2t", 0)])
        for k in range(2):
            last_sc = S.op("gpsimd", lambda e, ti=ti, k=k, T=T, sl=sl: e.indirect_dma_start(
                out=Xs[:, :], out_offset=bass.IndirectOffsetOnAxis(ap=POS[0:T, ti, k:k + 1], axis=0),
                in_=h2t[sl][0:T, :], in_offset=None), reads=[("h2t", sl), ("pos", ti)], writes=[("xs_sc", ti, k)], dma="sc%d" % sl)
    S.barrier()

    NWS = 5
    WSZ = 8 * DE + 8 * DE + 2 * D
    wslots = [arenaX[:, i * WSZ:(i + 1) * WSZ] for i in range(2)] + [arenaK[:, i * WSZ:(i + 1) * WSZ] for i in range(3)]
    xs = [wk("xs%d" % i, [128, 2, D], BF16) for i in range(2)]
    xsT = [wk("xsT%d" % i, [128, 8, TM], BF16) for i in range(2)]
    sgm = [wk("sgm%d" % i, [128, TM], F32) for i in range(2)]
    aTm = [wk("aTm%d" % i, [128, 2, TM], BF16) for i in range(2)]
    ysb = [wk("ysb%d" % i, [128, 2, D], BF16) for i in range(2)]
    ys_recs = []

    def wviews(slot):
        a = wslots[slot]
        return (a[:, 0:8 * DE], a[:, 8 * DE:16 * DE], a[:, 16 * DE:16 * DE + 2 * D])

    def fetch_tile(i):
        slot = i % NWS
        wg2, wu2, wd2 = wviews(slot)
        for j, (dst, srcw) in enumerate(((wg2, wgb), (wu2, wub), (wd2, wdb))):
            S.op("gpsimd", lambda e, dst=dst, srcw=srcw, i=i: e.indirect_dma_start(
                out=dst, out_offset=None, in_=srcw[:, :], in_offset=bass.IndirectOffsetOnAxis(ap=idxw[:, i:i + 1], axis=0)),
                reads=["idxw"], writes=[("wsl", slot, j)], dma="we%d_%d" % (slot, j))

    def fetch_x(i):
        sl = i % 2
        S.dma("sync", "lx%d" % sl, xs[sl][:, :, :], Xs[i * TM:(i + 1) * TM, :].rearrange("(h p) d -> p h d", p=128), writes=[("xs", sl)])

    def moe_tile(i):
        slot, sl = i % NWS, i % 2
        wg2, wu2, wd2 = wviews(slot)
        wg = wg2.rearrange("p (k n) -> p k n", k=8)
        wu = wu2.rearrange("p (k n) -> p k n", k=8)
        wd = wd2.rearrange("p (k n) -> p k n", k=2)
        X, XT, A, Y = xs[sl], xsT[sl], aTm[sl], ysb[sl]
        for hh in range(2):
            def tr(e, hh=hh):
                ins = None
                for k in range(8):
                    ins = e.transpose(out=psb(hh)[:, k * 128:(k + 1) * 128], in_=X[:, hh, k * 128:(k + 1) * 128], identity=ident[:, :])
                return ins
            S.op("tensor", tr, reads=[("xs", sl), "ident"], writes=["ps%d" % hh])
            eng = "scalar" if hh == 0 else "vector"
            if hh == 0:
                S.op("scalar", lambda e, hh=hh: e.copy(out=XT[:, :, hh * 128:(hh + 1) * 128],
                                                       in_=psb(hh)[:, 0:1024].rearrange("p (k t) -> p k t", k=8)),
                     reads=["ps%d" % hh], writes=[("xsT", sl, hh)])
            else:
                S.op("vector", lambda e, hh=hh: e.tensor_copy(out=XT[:, :, hh * 128:(hh + 1) * 128],
                                                              in_=psb(hh)[:, 0:1024].rearrange("p (k t) -> p k t", k=8)),
                     reads=["ps%d" % hh], writes=[("xsT", sl, hh)])
        for kc in range(2):
            def mm(e, kc=kc):
                ins = None
                for k in range(8):
                    ins = e.matmul(ps[2 + kc][:, 0:TM], lhsT=wg[:, k, kc * 128:(kc + 1) * 128], rhs=XT[:, k, :], start=(k == 0), stop=(k == 7))
                for k in range(8):
                    ins = e.matmul(ps[2 + kc][:, TM:2 * TM], lhsT=wu[:, k, kc * 128:(kc + 1) * 128], rhs=XT[:, k, :], start=(k == 0), stop=(k == 7))
                return ins
            S.op("tensor", mm, reads=[("xsT", sl, 0), ("xsT", sl, 1), ("wsl", slot, 0), ("wsl", slot, 1)], writes=["ps%d" % (2 + kc)])
            S.op("scalar", lambda e, kc=kc: e.activation(out=sgm[kc][:, :], in_=ps[2 + kc][:, 0:TM], func=AF.Silu),
                 reads=["ps%d" % (2 + kc)], writes=[("sgm", kc)])
            S.op("vector", lambda e, kc=kc: e.tensor_tensor(out=A[:, kc, :], in0=ps[2 + kc][:, TM:2 * TM], in1=sgm[kc][:, :], op=ALU.mult),
                 reads=["ps%d" % (2 + kc), ("sgm", kc)], writes=[("aTm", sl, kc)])
        for hh in range(2):
            for half in range(2):
                pb = 4 + hh * 2 + half

                def mmd(e, hh=hh, half=half, pb=pb):
                    ins = None
                    for kc in range(2):
                        ins = e.matmul(ps[pb][:, :], lhsT=A[:, kc, hh * 128:(hh + 1) * 128], rhs=wd[:, kc, half * 512:(half + 1) * 512],
                                       start=(kc == 0), stop=(kc == 1))
                    return ins
                S.op("tensor", mmd, reads=[("aTm", sl, 0), ("aTm", sl, 1), ("wsl", slot, 2)], writes=["ps%d" % pb])
                if half == 0:
                    S.op("scalar", lambda e, hh=hh, half=half, pb=pb: e.copy(out=Y[:, hh, half * 512:(half + 1) * 512], in_=ps[pb][:, :]),
                         reads=["ps%d" % pb], writes=[("ysb", sl, hh, half)])
                else:
                    S.op("vector", lambda e, hh=hh, half=half, pb=pb: e.tensor_copy(out=Y[:, hh, half * 512:(half + 1) * 512], in_=ps[pb][:, :]),
                         reads=["ps%d" % pb], writes=[("ysb", sl, hh, half)])
        ys_recs.append(S.dma("sync", "sys%d" % sl, Ys[i * TM:(i + 1) * TM, :].rearrange("(h p) d -> p h d", p=128), Y[:, :, :],
                             reads=[("ysb", sl, hh, half) for hh in range(2) for half in range(2)]))

    PRE = 3
    for i in range(min(PRE, NT)):
        fetch_tile(i)
    fetch_x(0)
    dbg("xs_t0", xs[0][:, :, :], [("xs", 0)])
    dbg("wg_t0", wslots[0][:, 0:2048], [("wsl", 0, 0)])
    dbg("wd_t0", wslots[0][:, 4096:6144], [("wsl", 0, 2)])
    for i in range(NT):
        if i + PRE < NT:
            fetch_tile(i + PRE)
        if i + 1 < NT:
            fetch_x(i + 1)
        moe_tile(i)
        if i == 0:
            dbg("ysb_t0", ysb[0][:, :, :], [("ysb", 0, hh, half) for hh in range(2) for half in range(2)])
            dbg("xsT_t0", xsT[0][:, :, :], [("xsT", 0, 0), ("xsT", 0, 1)])
            dbg("aT_t0", aTm[0][:, :, :], [("aTm", 0, 0), ("aTm", 0, 1)])
    S.barrier()

    xf = [QTf[:, i * D:(i + 1) * D] for i in range(2)]
    QTb = QT[:, :, :].rearrange("p h t -> p (h t)")
    g1 = [QTb[:, (4 + i) * D:(5 + i) * D] for i in range(2)]
    g2 = [QTb[:, (6 + i) * D:(7 + i) * D] for i in range(2)]
    for ti in range(17):
        if ti % 2 == 0:
            S.window_begin()
        S.cur_stream = ti % 2
        T = 128 if ti < 16 else NS
        tok0 = ti * 128
        sl = ti % 2
        S.dma("sync", "lf%d" % sl, xf[sl][0:T, :], x1s[tok0:tok0 + T, :], writes=[("xf", sl)])
        for k, G in enumerate((g1, g2)):
            S.op("gpsimd", lambda e, ti=ti, k=k, T=T, G=G, sl=sl: e.indirect_dma_start(
                out=G[sl][0:T, :], out_offset=None, in_=Ys[:, :],
                in_offset=bass.IndirectOffsetOnAxis(ap=POS[0:T, ti, k:k + 1], axis=0)),
                reads=[("pos", ti)], writes=[("gg", k, sl)], dma="gy%d_%d" % (k, sl))
        if ti == 0:
            dbg("g1_0", g1[0][:, :], [("gg", 0, 0)])
            dbg("g2_0", g2[0][:, :], [("gg", 1, 0)])
            dbg("xf_0", xf[0][:, :], [("xf", 0)])
        S.op(V, lambda e, ti=ti, T=T, sl=sl: e.scalar_tensor_tensor(out=xf[sl][0:T, :], in0=g1[sl][0:T, :], scalar=RK[0:T, ti, 2:3],
                                                                    in1=xf[sl][0:T, :], op0=ALU.mult, op1=ALU.add),
             reads=[("gg", 0, sl), ("xf", sl)], writes=[("xf", sl)])
        S.op(V, lambda e, ti=ti, T=T, sl=sl: e.scalar_tensor_tensor(out=xf[sl][0:T, :], in0=g2[sl][0:T, :], scalar=RK[0:T, ti, 3:4],
                                                                    in1=xf[sl][0:T, :], op0=ALU.mult, op1=ALU.add),
             reads=[("gg", 1, sl), ("xf", sl)], writes=[("xf", sl)])
        dst = y_p[tok0:tok0 + 128, :] if ti < 16 else y_s[:, :]
        out_recs.append(S.dma("sync", "sf%d" % sl, dst, xf[sl][0:T, :], reads=[("xf", sl)]))
        if ti % 2 == 1 or ti == 16:
            S.cur_stream = 0
            S.window_end()

    dbg("OH", OH[:, :, :], [])
    dbg("RK", RK[:, :, :], [])
    dbg("carry", carry[:, :], [])
    dbg("nt_f", nt_f[:, :], [])
    dbg("bt", bt[:, :], [])
    dbg("bti", bti[:, :], [])
    dbg("texp", texp[:, :], [])
    dbg("idxw", idxw[:, :], [])
    dbg("POS", POS[:, :, :], [])
    dbg("xsT0", xsT[0][:, :, :], [])
    dbg("ysb0", ysb[0][:, :, :], [])
    S.op("sync", None, reads=[], writes=[])
    fin = S.ops["sync"][-1]
    fin.deps = [r_ for r_ in out_recs if r_.eng is not None]
    for d in fin.deps:
        d.needed = True

    S.emit(nc, es)
    es.close()
    return nc


_CACHE = {}


def kernel(**inputs):
    f = lambda a: np.ascontiguousarray(a)
    nc = _CACHE.get("nc")
    if nc is None:
        nc = build_program()
        _CACHE["nc"] = nc
    shared = {
        "cache_ckv": f(inputs["cache_ckv"][0]),
        "cache_kr": f(inputs["cache_krope"][0]),
        "g_mix": f(inputs["g_mix"]), "w_in": f(inputs["w_in"][0]),
        "conv_w": f(inputs["conv_w"][0]), "conv_b": f(inputs["conv_b"]),
        "g_q_lat": f(inputs["g_q_lat"]), "w_uq": f(inputs["w_uq"][0]),
        "g_kv_lat": f(inputs["g_kv_lat"]), "w_uk": f(inputs["w_uk"][0]), "w_uv": f(inputs["w_uv"][0]),
        "g_q_nope": f(inputs["g_q_nope"]), "g_q_rope": f(inputs["g_q_rope"]),
        "g_k_nope": f(inputs["g_k_nope"]), "g_k_rope": f(inputs["g_k_rope"]),
        "g_out": f(inputs["g_out"]), "w_out": f(inputs["w_out"][0]), "g_ffn": f(inputs["g_ffn"]),
        "w_rg": f(inputs["w_router_group"][0]), "b_rg": f(inputs["b_router_group"]),
        "w_re": f(inputs["w_router_expert"][0]), "b_re": f(inputs["b_router_expert"]),
        "w_gate": f(inputs["w_gate"][0].reshape(NE, 8, 128, DE).transpose(0, 2, 1, 3).reshape(NE * 128, 8 * DE)),
        "w_up": f(inputs["w_up"][0].reshape(NE, 8, 128, DE).transpose(0, 2, 1, 3).reshape(NE * 128, 8 * DE)),
        "w_down": f(inputs["w_down"][0].reshape(NE, 2, 128, D).transpose(0, 2, 1, 3).reshape(NE * 128, 2 * D)),
    }
    in_maps = []
    for c in range(NCORES):
        m = dict(shared)
        m["x_p"] = f(inputs["x_prompt"][c])
        m["x_s"] = f(inputs["x_sample"][4 * c:4 * c + 4].reshape(NS, D))
        m["st_conv"] = f(inputs["state_conv"][0, 4 * c:4 * c + 4])
        m["ptab"] = f(inputs["page_table"][4 * c:4 * c + 4])
        in_maps.append(m)
    res = run_bass_kernel_spmd(nc, in_maps, core_ids=list(range(NCORES)))
    R = res.results
    if DEBUG:
        _CACHE["dbg"] = R
    y_p = np.stack([R[c]["y_p"] for c in range(NCORES)], 0)
    y_s = np.concatenate([R[c]["y_s"].reshape(4, 8, D) for c in range(NCORES)], 0)
    ckv_p = np.stack([R[c]["o_ckv_p"] for c in range(NCORES)], 0)[None]
    kr_p = np.stack([R[c]["o_kr_p"] for c in range(NCORES)], 0)[None]
    conv_p = np.stack([R[c]["o_conv_p"] for c in range(NCORES)], 0)[None]
    ckv_s = np.concatenate([R[c]["o_ckv_s"].reshape(4, 8, KVR) for c in range(NCORES)], 0)[None]
    kr_s = np.concatenate([R[c]["o_kr_s"].reshape(4, 8, RD) for c in range(NCORES)], 0)[None]
    conv_s = np.concatenate([R[c]["o_conv_s"] for c in range(NCORES)], 0)[None]
    return (y_p, y_s, ckv_p, kr_p, conv_p, ckv_s, kr_s, conv_s)
```

```python
import numpy as np
from contextlib import ExitStack
import concourse.bass as bass
import concourse.mybir as mybir
from concourse.bass_utils import run_bass_kernel_spmd

F32 = mybir.dt.float32
BF16 = mybir.dt.bfloat16
I32 = mybir.dt.int32
ALU = mybir.AluOpType
AF = mybir.ActivationFunctionType
AX = mybir.AxisListType

NCORES = 8
D = 1024
SEQ = 2048
NS = 32
NTOK = SEQ + NS
CW = 512
QR = 256
KVR = 128
RD = 32
NH = 8
ND = 64
VD = 64
PW = 3 * CW + QR + KVR + RD
NE = 32
DE = 256
EPS = 1e-6
DEBUG = False
NPAGE = 128
PAGE = 128
NPOOL = 5120
PAST = NPAGE * PAGE
SCALE = float((ND + RD) ** -0.5)


class Rec:
    __slots__ = ("eng", "fn", "deps", "dma", "needed", "sem", "val", "seq", "stream", "pdeps")

    def __init__(self, eng, fn, deps, dma):
        self.eng, self.fn, self.deps, self.dma = eng, fn, deps, dma
        self.needed = False
        self.sem = None
        self.val = 0
        self.seq = -1
        self.stream = 0
        self.pdeps = ()


class Sched:
    ENGS = ("sync", "gpsimd", "vector", "scalar", "tensor")

    def __init__(self):
        self.ops = {e: [] for e in self.ENGS}
        self.buf = {}
        self.streams = {}
        self.mute = False
        self.seq = 0
        self.cur_stream = 0
        self.win = None

    def op(self, eng, fn, reads=(), writes=(), dma=None):
        if self.mute:
            return Rec(None, None, [], None)
        deps = []
        seen = set()

        def add(r):
            if r is not None and id(r) not in seen:
                seen.add(id(r))
                deps.append(r)

        for k in reads:
            st = self.buf.get(k)
            if st is not None:
                add(st[0])
        for k in writes:
            st = self.buf.get(k)
            if st is not None:
                add(st[0])
                for r in st[1]:
                    add(r)
        pdeps = ()
        if eng == "tensor":
            pdeps = [d for d in deps if d.eng == "tensor" and not d.dma]
            deps = [d for d in deps if d.eng != "tensor" or d.dma]
        rec = Rec(eng, fn, deps, dma)
        rec.pdeps = pdeps
        rec.seq = self.seq
        rec.stream = self.cur_stream
        self.seq += 1
        for d in deps:
            d.needed = True
        self.ops[eng].append(rec)
        if self.win is not None:
            self.win.append(rec)
        for k in reads:
            self.buf.setdefault(k, [None, []])[1].append(rec)
        for k in writes:
            self.buf[k] = [rec, []]
        if dma is not None:
            self.streams.setdefault(dma, 0)
        return rec

    def window_begin(self):
        self.win = []

    def window_end(self):
        win, self.win = self.win, None
        if not win:
            return
        inwin = set(id(r) for r in win)
        first_seq = win[0].seq
        for e in self.ENGS:
            self.ops[e] = [r for r in self.ops[e] if id(r) not in inwin]
        by_stream = {}
        for r in win:
            by_stream.setdefault(r.stream, []).append(r)
        keys = sorted(by_stream)
        ptr = {k: 0 for k in keys}
        pe_order = [r for r in win if r.eng == "tensor"]
        pe_next = 0
        done = set()
        out = []
        turn = 0
        total = len(win)

        def ready(r):
            for d in r.deps:
                if id(d) in inwin and id(d) not in done:
                    return False
            for d in r.pdeps:
                if id(d) in inwin and id(d) not in done:
                    return False
            return True
        while len(out) < total:
            picked = None
            for off in range(len(keys)):
                k = keys[(turn + off) % len(keys)]
                if ptr[k] < len(by_stream[k]) and ready(by_stream[k][ptr[k]]):
                    picked = k
                    break
            assert picked is not None, "window_end: no ready op (dependency cycle?)"
            r = by_stream[picked][ptr[picked]]
            ptr[picked] += 1
            if r.eng == "tensor":
                pe_next += 1
            done.add(id(r))
            out.append(r)
            turn = (keys.index(picked) + 1) % len(keys)
        for r in out:
            self.ops[r.eng].append(r)

    def barrier(self):
        lasts = []
        last_dma = {}
        for e in self.ENGS:
            last_c = None
            for r in self.ops[e]:
                if r.dma is not None:
                    last_dma[r.dma] = r
                elif r.fn is not None:
                    last_c = r
            if last_c is not None:
                lasts.append(last_c)
        lasts += list(last_dma.values())
        for d in lasts:
            d.needed = True
        for e in self.ENGS:
            rec = Rec(e, None, list(lasts), None)
            self.ops[e].append(rec)
        self.buf = {}

    def dma(self, eng, stream, out, in_, reads=(), writes=(), **kw):
        return self.op(eng, lambda e: e.dma_start(out=out, in_=in_, **kw), reads, writes, dma=stream)

    def emit(self, nc, es):
        sems = {}
        for e in self.ENGS:
            sems[e] = es.enter_context(nc.semaphore("p_" + e))
        dsem = {}
        for s in self.streams:
            dsem[s] = es.enter_context(nc.semaphore("d_" + s))
        cnt = {s: 0 for s in self.streams}
        for e in self.ENGS:
            c = 0
            for r in self.ops[e]:
                if r.dma is not None:
                    cnt[r.dma] += 16
                    r.sem, r.val = dsem[r.dma], cnt[r.dma]
                elif r.needed:
                    c += 1
                    r.sem, r.val = sems[e], c
        block = es.enter_context(nc.Block())

        def run(eng_name):
            def body(eng):
                waited = {}
                for r in self.ops[eng_name]:
                    need = {}
                    for d in r.deps:
                        key = id(d.sem)
                        if key not in need or need[key][1] < d.val:
                            need[key] = (d.sem, d.val)
                    for key, (sem_, val_) in need.items():
                        if waited.get(key, 0) < val_:
                            eng.wait_ge(sem_, val_)
                            waited[key] = val_
                    if r.fn is None:
                        continue
                    ins = r.fn(eng)
                    if r.dma is not None:
                        ins.then_inc(r.sem, 16)
                    elif r.needed:
                        ins.then_inc(r.sem, 1)
            return body

        block.sync(run("sync"))
        block.gpsimd(run("gpsimd"))
        block.vector(run("vector"))
        block.scalar(run("scalar"))
        block.tensor(run("tensor"))


def build_program():
    nc = bass.Bass("TRN2", target_bir_lowering=False)
    S = Sched()
    es = ExitStack()

    def din(name, shape, dt=F32):
        return nc.dram_tensor(name, list(shape), dt, kind="ExternalInput").ap()

    def dout(name, shape, dt=F32):
        return nc.dram_tensor(name, list(shape), dt, kind="ExternalOutput").ap()

    x_p = din("x_p", [SEQ, D])
    x_s = din("x_s", [NS, D])
    st_conv = din("st_conv", [4, 2, CW])
    cache_ckv = din("cache_ckv", [NPOOL, PAGE, KVR])
    cache_kr = din("cache_kr", [NPOOL, PAGE, RD])
    ptab = din("ptab", [4, NPAGE], I32)
    g_mix = din("g_mix", [1, D])
    w_in = din("w_in", [D, PW])
    conv_w = din("conv_w", [3, CW])
    conv_b = din("conv_b", [1, CW])
    g_q_lat = din("g_q_lat", [1, QR])
    w_uq = din("w_uq", [QR, NH * (ND + RD)])
    g_kv_lat = din("g_kv_lat", [1, KVR])
    w_uk = din("w_uk", [KVR, NH * ND])
    w_uv = din("w_uv", [KVR, NH * VD])
    g_q_nope = din("g_q_nope", [1, ND])
    g_q_rope = din("g_q_rope", [1, RD])
    g_k_nope = din("g_k_nope", [1, ND])
    g_k_rope = din("g_k_rope", [1, RD])
    g_out = din("g_out", [1, D])
    w_out = din("w_out", [D, D])
    g_ffn = din("g_ffn", [1, D])
    w_rg = din("w_rg", [D, 4])
    b_rg = din("b_rg", [1, 4])
    w_re = din("w_re", [D, NE])
    b_re = din("b_re", [1, NE])
    w_gate = din("w_gate", [NE * 128, 8 * DE])
    w_up = din("w_up", [NE * 128, 8 * DE])
    w_down = din("w_down", [NE * 128, 2 * D])

    y_p = dout("y_p", [SEQ, D])
    y_s = dout("y_s", [NS, D])
    o_ckv_p = dout("o_ckv_p", [SEQ, KVR])
    o_kr_p = dout("o_kr_p", [SEQ, RD])
    o_conv_p = dout("o_conv_p", [2, CW])
    o_ckv_s = dout("o_ckv_s", [NS, KVR])
    o_kr_s = dout("o_kr_s", [NS, RD])
    o_conv_s = dout("o_conv_s", [4, 2, CW])

    def sb(name, shape, dt=F32):
        return es.enter_context(nc.sbuf_tensor(name, list(shape), dt))

    def dbg(name, ap, keys):
        if not DEBUG:
            return
        shp = list(ap.shape)
        o = nc.dram_tensor("dbg_" + name, shp, ap.dtype, kind="ExternalOutput").ap()
        out_recs.append(S.dma("sync", "st", o, ap, reads=keys))

    out_recs = []

    ARENA_BYTES = 61440
    arena_t = sb("arena", [128, ARENA_BYTES // 4], F32)
    arena_off = [0]

    def arena_reset():
        arena_off[0] = 0

    def wk(name, shape, dt=F32):
        esz = 2 if dt == BF16 else 4
        n = 1
        for d in shape[1:]:
            n *= d
        nb = (n * esz + 31) // 32 * 32
        off = arena_off[0]
        assert off + nb <= ARENA_BYTES, (name, off, nb)
        arena_off[0] = off + nb
        ap = arena_t[:, off // 4:(off + nb) // 4]
        if dt != F32:
            ap = ap.bitcast(dt)
        ap = ap[:, 0:n]
        if len(shape) == 3:
            ap = ap.rearrange("p (a b) -> p a b", a=shape[1])
        elif len(shape) == 4:
            ap = ap.rearrange("p (a b c) -> p a b c", a=shape[1], b=shape[2])
        return ap

    ps_big_t = es.enter_context(nc.psum_tensor("ps_big", [128, 4096], F32))
    ps_big = ps_big_t[:, :]
    ps = [ps_big[:, i * 512:(i + 1) * 512] for i in range(8)]

    def psb(i):
        return ps[i].bitcast(BF16)

    ident = sb("ident", [128, 128], BF16)
    tri = sb("tri", [128, 128], BF16)
    bconv = sb("bconv", [128, 128], BF16)
    onesb = sb("onesb", [128, 128], BF16)
    zero_c = sb("zero_c", [128, 1], F32)
    eps_c = sb("eps_c", [128, 1], F32)
    gmix_b = sb("gmix_b", [128, D], F32)
    gql_b = sb("gql_b", [128, QR], F32)
    gkv_b = sb("gkv_b", [128, KVR], F32)
    gq_b = sb("gq_b", [128, ND + RD], F32)
    gkn_b = sb("gkn_b", [128, ND], F32)
    gkr_b = sb("gkr_b", [128, RD], F32)
    convw_c = sb("convw_c", [128, 3, 4], F32)
    convb_c = sb("convb_c", [128, 4], F32)
    gout_c = sb("gout_c", [128, 8], F32)
    gattn_c = sb("gattn_c", [64, 8], F32)
    gkn_c = sb("gkn_c", [64, 1], F32)
    cosT = sb("cosT", [128, 17, 16], F32)
    sinT = sb("sinT", [128, 17, 16], F32)

    ckv_s_b = sb("ckv_s_b", [NS, KVR + 1], BF16)
    ya_s = sb("ya_s", [128, 4, NS], BF16)
    arenaX = sb("arenaX", [128, 8 * NTOK], BF16)
    w_in_sb = arenaX[:, 0:8 * PW].rearrange("p (k n) -> p k n", k=8)
    w_uq_sb = sb("w_uq_sb", [128, 2, NH * (ND + RD)], BF16)
    w_uk_sb = sb("w_uk_sb", [128, NH * ND], BF16)
    w_uv_sb = sb("w_uv_sb", [128, NH * VD], BF16)

    QT = sb("QT", [128, NH, NTOK], BF16)
    arenaK = sb("arenaK", [128, NH * NTOK + 16 * NH * (VD + 1) + 4 * NTOK], BF16)
    KT = arenaK[:, 0:NH * NTOK].rearrange("p (h t) -> p h t", h=NH)
    _o = NH * NTOK
    Vaug = arenaK[:, _o:_o + 16 * NH * (VD + 1)].rearrange("p (j h d) -> p j h d", j=16, h=NH)
    _o += 16 * NH * (VD + 1)
    yconvT = arenaK[:, _o:_o + 4 * NTOK].rearrange("p (c t) -> p c t", c=4)

    xin = [wk("xin%d" % i, [128, D], F32) for i in range(2)]
    junk = wk("junk", [128, D], BF16)
    hbf = [wk("hbf%d" % i, [128, D], BF16) for i in range(2)]
    hT = [wk("hT%d" % i, [128, 8, 256], BF16) for i in range(2)]
    st1 = [wk("st1_%d" % i, [128, 96], F32) for i in range(2)]
    qln = wk("qln", [128, QR], BF16)
    qlnT = wk("qlnT", [128, 2, 128], BF16)
    ckv_f = [wk("ckv_f%d" % i, [128, KVR], F32) for i in range(2)]
    ckv_b = wk("ckv_b", [128, KVR], BF16)
    ckvT = wk("ckvT", [128, 128], BF16)
    krn = wk("krn", [128, RD], F32)
    kr_f = [wk("kr_f%d" % i, [128, RD], F32) for i in range(2)]
    rtmp = wk("rtmp", [128, 4, 16], F32)
    qsq = wk("qsq", [128, 768], F32)
    ksq = wk("ksq", [128, 512], F32)
    qn = wk("qn", [128, NH, ND + RD], F32)
    qrt = wk("qrt", [128, 4, NH, 16], F32)
    Qb = wk("Qb", [128, NH, ND + RD], BF16)
    Kb = wk("Kb", [128, NH, ND + RD], BF16)
    kn_t = wk("kn_t", [128, NH, ND], F32)
    gc_sb = wk("gc_sb", [128, 256], F32)
    ubuf = [wk("ubuf%d" % c, [128, 2 + 256], F32) for c in range(4)]
    usam = [wk("usam%d" % c, [128, 4, 10], F32) for c in range(4)]
    vconv = wk("vconv", [128, 256], F32)
    yconv = wk("yconv", [128, 256], F32)
    csq = wk("csq", [128, 256], BF16)
    crs = wk("crs", [128, 256], F32)

    S.op("gpsimd", lambda e: e.memset(ident[:], 0.0), writes=["ident"])
    S.op("gpsimd", lambda e: e.affine_select(out=ident[:], in_=ident[:], pattern=[[-1, 128]],
                                            compare_op=ALU.not_equal, fill=1.0, base=0,
                                            channel_multiplier=1), reads=["ident"], writes=["ident"])
    S.op("gpsimd", lambda e: e.memset(onesb[:], 1.0), writes=["onesb"])
    S.op("gpsimd", lambda e: e.affine_select(out=tri[:], in_=onesb[:], pattern=[[1, 128]],
                                            compare_op=ALU.is_ge, fill=0.0, base=0,
                                            channel_multiplier=-1), reads=["onesb"], writes=["tri"])
    S.op("gpsimd", lambda e: e.memset(bconv[:], 0.0), writes=["bconv"])
    S.op("gpsimd", lambda e: e.memset(bconv[0:64, 0:64], 1.0 / 64), reads=["bconv"], writes=["bconv"])
    S.op("gpsimd", lambda e: e.memset(bconv[64:128, 64:128], 1.0 / 64), reads=["bconv"], writes=["bconv"])
    S.op("gpsimd", lambda e: e.memset(zero_c[:], 0.0), writes=["zero_c"])
    S.op("gpsimd", lambda e: e.memset(eps_c[:], EPS), writes=["eps_c"])
    S.op("gpsimd", lambda e: e.memset(Vaug[:, :, :, VD:VD + 1], 1.0), writes=["Vaug_ones"])
    for c in range(4):
        S.op("gpsimd", lambda e, c=c: e.memset(ubuf[c][:, 0:2], 0.0), writes=[("ubuf", c)])

    def bload(dst, src, n, key):
        S.dma("sync", "ld", dst[:], src[0:1, :].to_broadcast([128, n]), writes=[key])

    bload(gmix_b, g_mix, D, "gmix_b")
    bload(gql_b, g_q_lat, QR, "gql_b")
    bload(gkv_b, g_kv_lat, KVR, "gkv_b")
    bload(gkn_b, g_k_nope, ND, "gkn_b")
    bload(gkr_b, g_k_rope, RD, "gkr_b")
    S.dma("sync", "ld", gq_b[:, 0:ND], g_q_nope[0:1, :].to_broadcast([128, ND]), writes=["gq_b0"])
    S.dma("sync", "ld", gq_b[:, ND:ND + RD], g_q_rope[0:1, :].to_broadcast([128, RD]), writes=["gq_b1"])
    if True:
        for k in range(3):
            S.dma("sync", "ld", convw_c[:, k, :], conv_w[k:k + 1, :].rearrange("o (c p) -> p (o c)", p=128),
                  writes=[("convw_c", k)], allow_slow_non_contiguous=True)
        S.dma("sync", "ld", convb_c[:], conv_b.rearrange("o (c p) -> p (o c)", p=128), writes=["convb_c"], allow_slow_non_contiguous=True)
        S.dma("sync", "ld", gout_c[:], g_out.rearrange("o (c p) -> p (o c)", p=128), writes=["gout_c"], allow_slow_non_contiguous=True)
        S.dma("sync", "ld", gattn_c[:], g_out[:, CW:D].rearrange("o (h p) -> p (o h)", p=64), writes=["gattn_c"], allow_slow_non_contiguous=True)
        S.dma("sync", "ld", gkn_c[:], g_k_nope.rearrange("o p -> p o"), writes=["gkn_c"], allow_slow_non_contiguous=True)
        for c in range(4):
            for sq_ in range(4):
                S.dma("sync", "ld", usam[c][:, sq_, 0:2],
                      st_conv[sq_, :, c * 128:(c + 1) * 128].rearrange("t p -> p t"), writes=[("usam", c, sq_)],
                      allow_slow_non_contiguous=True)

    w_in_v = w_in.rearrange("(k p) n -> p k n", p=128)
    for k in range(8):
        S.dma("gpsimd", "wg", w_in_sb[:, k, :], w_in_v[:, k, :], writes=[("w_in", k)])
    S.dma("gpsimd", "wg", w_uq_sb[:], w_uq.rearrange("(k p) n -> p k n", p=128), writes=["w_uq"])
    S.dma("gpsimd", "wg", w_uk_sb[:], w_uk[:, :], writes=["w_uk"])
    S.dma("gpsimd", "wg", w_uv_sb[:], w_uv[:, :], writes=["w_uv"])

    wall = nc.dram_tensor("wall", [NE * 128, 16 * DE + 2 * D], BF16, kind="Internal").ap()
    conv_jobs = [(srcw, c0, q_) for srcw, c0 in ((w_gate, 0), (w_up, 8 * DE), (w_down, 16 * DE)) for q_ in range(8)]

    def issue_conv(n):
        for _ in range(n):
            if conv_jobs:
                srcw, c0, q_ = conv_jobs.pop(0)
                S.dma("gpsimd", "cv", wall[q_ * 512:(q_ + 1) * 512, c0:c0 + 2048], srcw[q_ * 512:(q_ + 1) * 512, :], writes=[("wconv", c0, q_)])

    MARK_SETUP = arena_off[0]
    posf = wk("posf", [128, 17], F32)
    posi = wk("posi", [128, 17], I32)
    invf = wk("invf", [128, 16], F32)
    ang = wk("ang", [128, 17, 16], F32)
    ang2 = wk("ang2", [128, 17, 16], F32)
    S.op("gpsimd", lambda e: e.iota(posi[:, 0:16], pattern=[[128, 16]], base=0, channel_multiplier=1),
         writes=["posi0"])
    S.op("gpsimd", lambda e: e.iota(posi[:, 16:17], pattern=[[0, 1]], base=0, channel_multiplier=1),
         writes=["posi1"])
    S.op("vector", lambda e: e.tensor_single_scalar(out=posi[:, 16:17], in_=posi[:, 16:17], scalar=7,
                                                   op=ALU.bitwise_and), reads=["posi1"], writes=["posi1"])
    S.op("vector", lambda e: e.tensor_single_scalar(out=posi[:, 16:17], in_=posi[:, 16:17], scalar=PAST,
                                                   op=ALU.add), reads=["posi1"], writes=["posi1"])
    S.op("vector", lambda e: e.tensor_copy(out=posf[:], in_=posi[:]), reads=["posi0", "posi1"], writes=["posf"])
    invf_np = (np.float32(10000.0) ** (-(np.arange(16, dtype=np.float32) / np.float32(16)))).astype(np.float32)
    for i in range(16):
        S.op("gpsimd", lambda e, i=i: e.memset(invf[:, i:i + 1], float(invf_np[i])), writes=[("invf", i)])
    invk = [("invf", i) for i in range(16)]
    for t in range(17):
        S.op("vector", lambda e, t=t: e.tensor_scalar(out=ang[:, t, :], in0=invf[:], scalar1=posf[:, t:t + 1],
                                                      scalar2=None, op0=ALU.mult),
             reads=invk + ["posf"], writes=[("ang", t)])
    angkeys = [("ang", t) for t in range(17)]
    PI = float(np.pi)
    C1 = 6.28125
    C2 = float(2 * np.pi - 6.28125)
    angk = wk("angk", [128, 17, 16], I32)
    angkf = wk("angkf", [128, 17, 16], F32)
    angm = wk("angm", [128, 17, 16], F32)
    S.op("vector", lambda e: e.tensor_scalar(out=ang2[:], in0=ang[:], scalar1=float(1.0 / (2 * np.pi)), scalar2=None,
                                             op0=ALU.mult), reads=angkeys, writes=["ang2"])
    S.op("vector", lambda e: e.tensor_copy(out=angk[:], in_=ang2[:]), reads=["ang2"], writes=["angk"])
    S.op("vector", lambda e: e.tensor_copy(out=angkf[:], in_=angk[:]), reads=["angk"], writes=["angkf"])
    S.op("vector", lambda e: e.scalar_tensor_tensor(out=ang2[:], in0=angkf[:], scalar=-C1, in1=ang[:],
                                                    op0=ALU.mult, op1=ALU.add), reads=["angkf"] + angkeys, writes=["ang2"])
    S.op("vector", lambda e: e.scalar_tensor_tensor(out=ang2[:], in0=angkf[:], scalar=-C2, in1=ang2[:],
                                                    op0=ALU.mult, op1=ALU.add), reads=["angkf", "ang2"], writes=["ang2"])

    def wrap_and_sin(dst, key):
        S.op("vector", lambda e: e.tensor_single_scalar(out=angm[:], in_=ang2[:], scalar=PI, op=ALU.is_gt),
             reads=["ang2"], writes=["angm"])
        S.op("vector", lambda e: e.scalar_tensor_tensor(out=ang2[:], in0=angm[:], scalar=-2 * PI, in1=ang2[:],
                                                        op0=ALU.mult, op1=ALU.add), reads=["angm", "ang2"], writes=["ang2"])
        S.op("vector", lambda e: e.tensor_scalar(out=angm[:], in0=ang2[:], scalar1=-PI, scalar2=PI,
                                                 op0=ALU.max, op1=ALU.min), reads=["ang2"], writes=["angm"])
        S.op("scalar", lambda e: e.activation(out=dst[:], in_=angm[:], func=AF.Sin), reads=["angm"], writes=[key])

    wrap_and_sin(sinT, "sinT")
    S.op("vector", lambda e: e.tensor_scalar(out=ang2[:], in0=ang2[:], scalar1=PI / 2, scalar2=None, op0=ALU.add),
         reads=["ang2", "sinT"], writes=["ang2"])
    wrap_and_sin(cosT, "cosT")

    S.barrier()
    arena_off[0] = MARK_SETUP
    gc_sbL = [gc_sb, wk("gc_sb2", [128, 256], F32)]
    vconvL = [vconv, wk("vconv2", [128, 256], F32)]
    yconvL = [yconv, wk("yconv2", [128, 256], F32)]
    csqL = [csq, wk("csq2", [128, 256], BF16)]
    crsL = [crs, wk("crs2", [128, 256], F32)]
    qlnTL = [qlnT, wk("qlnT2", [128, 2, 128], BF16)]
    ckvTL = [ckvT, wk("ckvT2", [128, 128], BF16)]
    def rstd_from_ss(ss_ap, out_ap, n, T, keys_r, keys_w, tmp_ap, tmpkey):
        S.op("scalar", lambda e: e.activation(out=tmp_ap, in_=ss_ap, func=AF.Ln, bias=eps_c[0:T, :], scale=1.0 / n),
             reads=keys_r + ["eps_c"], writes=[tmpkey])
        S.op("scalar", lambda e: e.activation(out=out_ap, in_=tmp_ap, func=AF.Exp, scale=-0.5),
             reads=[tmpkey], writes=keys_w)

    def rope(dst1, dst2, x1, x2, cos, sin, tmp, rkeys, wkeys, tkey):
        S.op("vector", lambda e: e.tensor_tensor(out=tmp[0], in0=x1, in1=cos, op=ALU.mult), reads=rkeys, writes=[(tkey, 0)])
        S.op("vector", lambda e: e.tensor_tensor(out=tmp[1], in0=x2, in1=sin, op=ALU.mult), reads=rkeys, writes=[(tkey, 1)])
        S.op("vector", lambda e: e.tensor_tensor(out=tmp[2], in0=x1, in1=sin, op=ALU.mult), reads=rkeys, writes=[(tkey, 2)])
        S.op("vector", lambda e: e.tensor_tensor(out=tmp[3], in0=x2, in1=cos, op=ALU.mult), reads=rkeys, writes=[(tkey, 3)])
        S.op("vector", lambda e: e.tensor_tensor(out=dst1, in0=tmp[0], in1=tmp[1], op=ALU.subtract),
             reads=[(tkey, 0), (tkey, 1)], writes=[wkeys[0]])
        S.op("vector", lambda e: e.tensor_tensor(out=dst2, in0=tmp[2], in1=tmp[3], op=ALU.add),
             reads=[(tkey, 2), (tkey, 3)], writes=[wkeys[1]])

    def token_tile(ti, part):
        S.mute = (part == "B")
        T = 128 if ti < 16 else NS
        tok0 = ti * 128
        sl = ti % 2
        src = x_p[tok0:tok0 + 128, :] if ti < 16 else x_s[:, :]
        X, H, ST = xin[sl], hbf[sl], st1[sl]
        hTc = hT[(ti // 2) % 2]
        col0 = (ti % 2) * 128
        if ti in (16, 0):
            S.dma("sync", "ldx%d" % sl, X[0:T, :], src, writes=[("xin", sl)])
        if ti < 15:
            nsl = (ti + 1) % 2
            S.dma("sync", "ldx%d" % nsl, xin[nsl][:, :], x_p[(ti + 1) * 128:(ti + 2) * 128, :], writes=[("xin", nsl)])
        S.op("scalar", lambda e: e.activation(out=junk[0:T, :], in_=X[0:T, :], func=AF.Square, accum_out=ST[0:T, 0:1]),
             reads=[("xin", sl)], writes=[("st", sl, 0)])
        rstd_from_ss(ST[0:T, 0:1], ST[0:T, 1:2], D, T, [("st", sl, 0)], [("st", sl, 1)], ST[0:T, 2:3], ("st", sl, 2))
        S.op("vector", lambda e: e.scalar_tensor_tensor(out=H[0:T, :], in0=X[0:T, :], scalar=ST[0:T, 1:2],
                                                        in1=gmix_b[0:T, :], op0=ALU.mult, op1=ALU.mult),
             reads=[("xin", sl), ("st", sl, 1), "gmix_b"], writes=[("hbf", sl)])
        pT = psb(0)

        def tr_h(e):
            ins = None
            for k in range(8):
                ins = e.transpose(out=pT[:, k * 128:k * 128 + T], in_=H[0:T, k * 128:(k + 1) * 128], identity=ident[0:T, 0:T])
            return ins
        S.op("tensor", tr_h, reads=[("hbf", sl), "ident"], writes=["ps0"])
        S.op("scalar", lambda e: e.copy(out=hTc[:, :, col0:col0 + T],
                                        in_=pT[:, 0:1024].rearrange("p (k t) -> p k t", k=8)[:, :, 0:T]),
             reads=["ps0"], writes=[("hT", (ti // 2) % 2, ti % 2)])
        hkey = ("hT", (ti // 2) % 2, ti % 2)

        def mm_small(e):
            ins = None
            for k in range(8):
                ins = e.matmul(ps[1][0:T, 0:416], lhsT=hTc[:, k, col0:col0 + T], rhs=w_in_sb[:, k, 3 * CW:PW],
                               start=(k == 0), stop=(k == 7))
            return ins
        S.op("tensor", mm_small, reads=[hkey] + [("w_in", k) for k in range(8)], writes=["ps1"])
        zs = ps[1]
        for j, (a, b) in enumerate(((0, QR), (QR, QR + KVR), (QR + KVR, QR + KVR + RD))):
            S.op("scalar", lambda e, a=a, b=b, j=j: e.activation(out=junk[0:T, a:b], in_=zs[0:T, a:b], func=AF.Square,
                                                                 accum_out=ST[0:T, 4 + j:5 + j]),
                 reads=["ps1"], writes=[("st", sl, 4 + j)])
        rstd_from_ss(ST[0:T, 4:5], ST[0:T, 8:9], QR, T, [("st", sl, 4)], [("st", sl, 8)], ST[0:T, 12:13], ("st", sl, 12))
        rstd_from_ss(ST[0:T, 5:6], ST[0:T, 9:10], KVR, T, [("st", sl, 5)], [("st", sl, 9)], ST[0:T, 13:14], ("st", sl, 13))
        rstd_from_ss(ST[0:T, 6:7], ST[0:T, 10:11], RD, T, [("st", sl, 6)], [("st", sl, 10)], ST[0:T, 14:15], ("st", sl, 14))
        S.op("vector", lambda e: e.scalar_tensor_tensor(out=qln[0:T, :], in0=zs[0:T, 0:QR], scalar=ST[0:T, 8:9],
                                                        in1=gql_b[0:T, :], op0=ALU.mult, op1=ALU.mult),
             reads=["ps1", ("st", sl, 8), "gql_b"], writes=["qln"])
        CK = ckv_f[sl]
        S.op("vector", lambda e: e.scalar_tensor_tensor(out=CK[0:T, :], in0=zs[0:T, QR:QR + KVR], scalar=ST[0:T, 9:10],
                                                        in1=gkv_b[0:T, :], op0=ALU.mult, op1=ALU.mult),
             reads=["ps1", ("st", sl, 9), "gkv_b"], writes=[("ckv_f", sl)])
        S.op("vector", lambda e: e.scalar_tensor_tensor(out=krn[0:T, :], in0=zs[0:T, QR + KVR:QR + KVR + RD],
                                                        scalar=ST[0:T, 10:11], in1=gkr_b[0:T, :], op0=ALU.mult, op1=ALU.mult),
             reads=["ps1", ("st", sl, 10), "gkr_b"], writes=["krn"])
        dst_ckv = o_ckv_p[tok0:tok0 + 128, :] if ti < 16 else o_ckv_s[:, :]
        out_recs.append(S.dma("sync", "stc%d" % sl, dst_ckv, CK[0:T, :], reads=[("ckv_f", sl)]))
        S.op("gpsimd", lambda e: e.tensor_copy(out=ckv_b[0:T, :], in_=CK[0:T, :]), reads=[("ckv_f", sl)], writes=["ckv_b"])
        if ti == 16:
            S.op("gpsimd", lambda e: e.tensor_copy(out=ckv_s_b[0:T, 0:KVR], in_=CK[0:T, :]), reads=[("ckv_f", sl)], writes=["ckv_s_b"])
            S.op("gpsimd", lambda e: e.memset(ckv_s_b[0:T, KVR:KVR + 1], 1.0), writes=["ckv_s_b1"])
        KR = kr_f[sl]
        rope(KR[0:T, 0:16], KR[0:T, 16:32], krn[0:T, 0:16], krn[0:T, 16:32], cosT[0:T, ti, :], sinT[0:T, ti, :],
             [rtmp[0:T, i, :] for i in range(4)], ["krn", "cosT", "sinT"], [("kr_f", sl, 0), ("kr_f", sl, 1)], "rtmp")
        dst_kr = o_kr_p[tok0:tok0 + 128, :] if ti < 16 else o_kr_s[:, :]
        out_recs.append(S.dma("sync", "stk%d" % sl, dst_kr, KR[0:T, :], reads=[("kr_f", sl, 0), ("kr_f", sl, 1)]))

        pT2 = psb(2)

        def tr_q(e):
            e.transpose(out=pT2[:, 0:T], in_=qln[0:T, 0:128], identity=ident[0:T, 0:T])
            e.transpose(out=pT2[:, 128:128 + T], in_=qln[0:T, 128:256], identity=ident[0:T, 0:T])
            return e.transpose(out=pT2[:, 256:256 + T], in_=ckv_b[0:T, :], identity=ident[0:T, 0:T])
        S.op("tensor", tr_q, reads=["qln", "ckv_b", "ident"], writes=["ps2"])
        qlnT, ckvT = qlnTL[sl], ckvTL[sl]
        S.op("vector", lambda e: e.tensor_copy(out=qlnT[:, :, 0:T],
                                               in_=pT2[:, 0:256].rearrange("p (k t) -> p k t", k=2)[:, :, 0:T]),
             reads=["ps2"], writes=[("qlnT", sl)])
        S.op("vector", lambda e: e.tensor_copy(out=ckvT[:, 0:T], in_=pT2[:, 256:256 + T]), reads=["ps2"], writes=[("ckvT", sl)])
        S.mute = (part == "A")

        def mm_q(e):
            ins = None
            for half in range(2):
                for k in range(2):
                    ins = e.matmul(ps[3 + half][0:T, 0:384], lhsT=qlnT[:, k, 0:T], rhs=w_uq_sb[:, k, half * 384:(half + 1) * 384],
                                   start=(k == 0), stop=(k == 1))
            return ins
        S.op("tensor", mm_q, reads=[("qlnT", sl), "w_uq"], writes=["ps3", "ps4"])

        def mm_kv(e):
            e.matmul(ps[5][0:T, :], lhsT=ckvT[:, 0:T], rhs=w_uk_sb[:, :], start=True, stop=True)
            return e.matmul(ps[6][0:T, :], lhsT=ckvT[:, 0:T], rhs=w_uv_sb[:, :], start=True, stop=True)
        S.op("tensor", mm_kv, reads=[("ckvT", sl), "w_uk", "w_uv"], writes=["ps5", "ps6"])

        for half in range(2):
            S.op("scalar", lambda e, half=half: e.activation(out=qsq[0:T, half * 384:(half + 1) * 384],
                                                             in_=ps[3 + half][0:T, 0:384], func=AF.Square),
                 reads=["ps%d" % (3 + half)], writes=[("qsq", half)])
        qsq_v = qsq[:, :].rearrange("p (h d) -> p h d", h=NH)
        S.op("vector", lambda e: e.reduce_sum(out=ST[0:T, 16:24], in_=qsq_v[0:T, :, 0:ND], axis=AX.X),
             reads=[("qsq", 0), ("qsq", 1)], writes=[("st", sl, 16)])
        S.op("vector", lambda e: e.reduce_sum(out=ST[0:T, 24:32], in_=qsq_v[0:T, :, ND:ND + RD], axis=AX.X),
             reads=[("qsq", 0), ("qsq", 1)], writes=[("st", sl, 24)])
        rstd_from_ss(ST[0:T, 16:24], ST[0:T, 32:40], ND, T, [("st", sl, 16)], [("st", sl, 32)], ST[0:T, 48:56], ("st", sl, 48))
        rstd_from_ss(ST[0:T, 24:32], ST[0:T, 40:48], RD, T, [("st", sl, 24)], [("st", sl, 40)], ST[0:T, 56:64], ("st", sl, 56))
        for half in range(2):
            hs = slice(half * 4, half * 4 + 4)
            qps = ps[3 + half][0:T, 0:384].rearrange("p (h d) -> p h d", h=4)
            S.op("vector", lambda e, hs=hs, qps=qps: e.tensor_tensor(
                out=qn[0:T, hs, 0:ND], in0=qps[:, :, 0:ND],
                in1=ST[0:T, 32 + hs.start:32 + hs.stop].unsqueeze(2).to_broadcast([T, 4, ND]), op=ALU.mult),
                reads=["ps%d" % (3 + half), ("st", sl, 32)], writes=[("qn", half, 0)])
            S.op("vector", lambda e, hs=hs, qps=qps: e.tensor_tensor(
                out=qn[0:T, hs, ND:ND + RD], in0=qps[:, :, ND:ND + RD],
                in1=ST[0:T, 40 + hs.start:40 + hs.stop].unsqueeze(2).to_broadcast([T, 4, RD]), op=ALU.mult),
                reads=["ps%d" % (3 + half), ("st", sl, 40)], writes=[("qn", half, 1)])
        qnk = [("qn", h2, j) for h2 in range(2) for j in range(2)]
        S.op("vector", lambda e: e.tensor_tensor(out=qn[0:T, :, :], in0=qn[0:T, :, :],
                                                 in1=gq_b[0:T, :].unsqueeze(1).to_broadcast([T, NH, ND + RD]), op=ALU.mult),
             reads=qnk + ["gq_b0", "gq_b1"], writes=["qn_g"])
        S.op("gpsimd", lambda e: e.tensor_copy(out=Qb[0:T, :, 0:ND], in_=qn[0:T, :, 0:ND]), reads=["qn_g"], writes=["Qb_n"])
        cosb = cosT[0:T, ti, :].unsqueeze(1).to_broadcast([T, NH, 16])
        sinb = sinT[0:T, ti, :].unsqueeze(1).to_broadcast([T, NH, 16])
        rope(Qb[0:T, :, ND:ND + 16], Qb[0:T, :, ND + 16:ND + 32], qn[0:T, :, ND:ND + 16], qn[0:T, :, ND + 16:ND + 32],
             cosb, sinb, [qrt[0:T, i, :, :] for i in range(4)], ["qn_g", "cosT", "sinT"], ["Qb_r0", "Qb_r1"], "qrt")

        S.op("scalar", lambda e: e.activation(out=ksq[0:T, 0:512], in_=ps[5][0:T, :], func=AF.Square),
             reads=["ps5"], writes=["ksq"])
        S.op("vector", lambda e: e.reduce_sum(out=ST[0:T, 64:72], in_=ksq[0:T, 0:512].rearrange("p (h d) -> p h d", h=NH), axis=AX.X),
             reads=["ksq"], writes=[("st", sl, 64)])
        rstd_from_ss(ST[0:T, 64:72], ST[0:T, 72:80], ND, T, [("st", sl, 64)], [("st", sl, 72)], ST[0:T, 80:88], ("st", sl, 80))
        S.op("vector", lambda e: e.tensor_tensor(out=kn_t[0:T, :, :], in0=ps[5][0:T, :].rearrange("p (h d) -> p h d", h=NH),
                                                 in1=ST[0:T, 72:80].unsqueeze(2).to_broadcast([T, NH, ND]), op=ALU.mult),
             reads=["ps5", ("st", sl, 72)], writes=["kn_t"])
        S.op("vector", lambda e: e.tensor_tensor(out=Kb[0:T, :, 0:ND], in0=kn_t[0:T, :, :],
                                                 in1=gkn_b[0:T, :].unsqueeze(1).to_broadcast([T, NH, ND]), op=ALU.mult),
             reads=["kn_t", "gkn_b"], writes=["Kb_n"])
        S.op("gpsimd", lambda e: e.tensor_copy(out=Kb[0:T, :, ND:ND + RD], in_=KR[0:T, :].unsqueeze(1).to_broadcast([T, NH, RD])),
             reads=[("kr_f", sl, 0), ("kr_f", sl, 1)], writes=["Kb_r"])
        if ti < 16:
            S.op("scalar", lambda e: e.copy(out=Vaug[:, ti, :, 0:VD], in_=ps[6][:, :].rearrange("p (h d) -> p h d", h=NH)),
                 reads=["ps6"], writes=[("Vaug", ti)])

        pQ = psb(7)

        def tr_Q(e):
            ins = None
            for h in range(NH):
                ins = e.transpose(out=pQ[0:96, h * 128:h * 128 + T], in_=Qb[0:T, h, :], identity=ident[0:T, 0:T])
            return ins
        S.op("tensor", tr_Q, reads=["Qb_n", "Qb_r0", "Qb_r1", "ident"], writes=["ps7"])
        S.op("vector", lambda e: e.tensor_copy(out=QT[0:96, :, tok0:tok0 + T],
                                               in_=pQ[0:96, 0:1024].rearrange("p (h t) -> p h t", h=NH)[:, :, 0:T]),
             reads=["ps7"], writes=[("QT", ti)])
        pK = psb(0)

        def tr_K(e):
            ins = None
            for h in range(NH):
                ins = e.transpose(out=pK[0:96, h * 128:h * 128 + T], in_=Kb[0:T, h, :], identity=ident[0:T, 0:T])
            return ins
        S.op("tensor", tr_K, reads=["Kb_n", "Kb_r", "ident"], writes=["ps0"])
        S.op("scalar", lambda e: e.copy(out=KT[0:96, :, tok0:tok0 + T],
                                        in_=pK[0:96, 0:1024].rearrange("p (h t) -> p h t", h=NH)[:, :, 0:T]),
             reads=["ps0"], writes=[("KT", ti)])
        S.mute = False

    def conv_chunk(ci):
        if ci < 8:
            nseg, L = 1, 256
            hTc = hT[ci % 2]
            hkeys = [("hT", ci % 2, j) for j in range(2)]
            tok0 = ci * 256
        else:
            nseg, L = 4, 8
            hTc = hT[0]
            hkeys = [("hT", 0, 0)]
            tok0 = SEQ
        N = nseg * L
        for c in range(4):
            U = ubuf[c] if ci < 8 else usam[c]
            ukey = ("ubuf", c) if ci < 8 else ("usam", c)
            ukeys = [ukey] if ci < 8 else [("usam", c, q_) for q_ in range(4)]
            cwk = [("convw_c", k) for k in range(3)]
            if ci < 8:
                ufull = U[:, :].rearrange("p (s l) -> p s l", s=1)
            else:
                ufull = U[:, :, :]

            if S.win is not None:
                S.cur_stream = 2 + c
            pc = c % 2
            bk = (1, 2, 3, 4) if pc == 0 else (5, 6, 7, 0)
            gc_, vc_, yc_, cs_, cr_ = gc_sbL[pc], vconvL[pc], yconvL[pc], csqL[pc], crsL[pc]
            kx = lambda nm: (nm, pc)

            def mm(e, c=c, bk=bk):
                ins = None
                for j, off in enumerate((0, 2 * CW, CW)):
                    for k in range(8):
                        ins = e.matmul(ps[bk[j]][:, 0:N], lhsT=w_in_sb[:, k, off + c * 128:off + (c + 1) * 128],
                                       rhs=hTc[:, k, 0:N], start=(k == 0), stop=(k == 7))
                return ins
            S.op("tensor", mm, reads=hkeys + [("w_in", k) for k in range(8)], writes=["ps%d" % bk[0], "ps%d" % bk[1], "ps%d" % bk[2]])
            S.op("scalar", lambda e, bk=bk, gc_=gc_: e.copy(out=gc_[:, 0:N], in_=ps[bk[1]][:, 0:N]), reads=["ps%d" % bk[1]], writes=[kx("gc_sb")])
            S.op("vector", lambda e, ufull=ufull, bk=bk, gc_=gc_: e.tensor_tensor(
                out=ufull[:, :, 2:2 + L], in0=ps[bk[0]][:, 0:N].rearrange("p (s l) -> p s l", s=nseg),
                in1=gc_[:, 0:N].rearrange("p (s l) -> p s l", s=nseg), op=ALU.mult),
                reads=["ps%d" % bk[0], kx("gc_sb")], writes=[(ukey, "body")])
            v3 = vc_[:, 0:N].rearrange("p (s l) -> p s l", s=nseg)
            S.op("vector", lambda e, c=c, ufull=ufull, v3=v3: e.tensor_scalar(
                out=v3, in0=ufull[:, :, 2:2 + L], scalar1=convw_c[:, 2, c:c + 1], scalar2=convb_c[:, c:c + 1],
                op0=ALU.mult, op1=ALU.add), reads=[(ukey, "body")] + cwk + ["convb_c"], writes=[kx("vconv")])
            for tap in (1, 0):
                S.op("vector", lambda e, c=c, tap=tap, ufull=ufull, v3=v3: e.scalar_tensor_tensor(
                    out=v3, in0=ufull[:, :, tap:tap + L], scalar=convw_c[:, tap, c:c + 1], in1=v3,
                    op0=ALU.mult, op1=ALU.add), reads=[(ukey, "body"), kx("vconv")] + ukeys + cwk, writes=[kx("vconv")])
            S.op("vector", lambda e, bk=bk, yc_=yc_, vc_=vc_: e.tensor_tensor(out=yc_[:, 0:N], in0=ps[bk[2]][:, 0:N], in1=vc_[:, 0:N], op=ALU.mult),
                 reads=["ps%d" % bk[2], kx("vconv")], writes=[kx("yconv")])
            S.op("scalar", lambda e, cs_=cs_, yc_=yc_: e.activation(out=cs_[:, 0:N], in_=yc_[:, 0:N], func=AF.Square),
                 reads=[kx("yconv")], writes=[kx("csq")])
            S.op("tensor", lambda e, bk=bk, cs_=cs_: e.matmul(ps[bk[3]][:, 0:N], lhsT=bconv[:, :], rhs=cs_[:, 0:N], start=True, stop=True),
                 reads=[kx("csq"), "bconv"], writes=["ps%d" % bk[3]])
            S.op("scalar", lambda e, bk=bk, cr_=cr_: e.activation(out=cr_[:, 0:N], in_=ps[bk[3]][:, 0:N], func=AF.Ln, bias=eps_c[:, :], scale=1.0),
                 reads=["ps%d" % bk[3], "eps_c"], writes=[kx("crs")])
            S.op("scalar", lambda e, cr_=cr_: e.activation(out=cr_[:, 0:N], in_=cr_[:, 0:N], func=AF.Exp, scale=-0.5),
                 reads=[kx("crs")], writes=[kx("crs")])
            S.op("vector", lambda e, c=c, yc_=yc_, cr_=cr_: e.scalar_tensor_tensor(out=yconvT[:, c, tok0:tok0 + N], in0=yc_[:, 0:N],
                                                                                  scalar=gout_c[:, c:c + 1], in1=cr_[:, 0:N],
                                                                                  op0=ALU.mult, op1=ALU.mult),
                 reads=[kx("yconv"), kx("crs"), "gout_c"], writes=[("yconvT", c, ci)])
            if True:
                if ci == 7:
                    out_recs.append(S.dma("sync", "st", o_conv_p[:, c * 128:(c + 1) * 128].rearrange("t p -> p t"),
                                          U[:, 256:258], reads=[(ukey, "body")], allow_slow_non_contiguous=True))
                elif ci == 8:
                    for q_ in range(4):
                        out_recs.append(S.dma("sync", "st", o_conv_s[q_, :, c * 128:(c + 1) * 128].rearrange("t p -> p t"),
                                              U[:, q_, 8:10], reads=[(ukey, "body")], allow_slow_non_contiguous=True))
            if ci < 7:
                S.op("gpsimd", lambda e, U=U: e.tensor_copy(out=U[:, 0:2], in_=U[:, 256:258]),
                     reads=[(ukey, "body")], writes=[ukey])

    token_tile(16, "A")
    token_tile(16, "B")
    conv_chunk(8)
    token_tile(0, "A")
    for ti in range(16):
        S.window_begin()
        if ti + 1 < 16:
            S.cur_stream = 0
            token_tile(ti + 1, "A")
        S.cur_stream = 1
        token_tile(ti, "B")
        if ti % 2 == 1:
            S.cur_stream = 2
            conv_chunk(ti // 2)
        S.cur_stream = 0
        S.window_end()

    S.barrier()
    arena_reset()
    RG = 16
    c2ckv = cache_ckv.rearrange("n (r t) d -> (n r) (t d)", r=RG)
    c2kr = cache_kr.rearrange("n (r t) d -> (n r) (t d)", r=2)
    idx_i = wk("idx_i", [128, 4], I32)
    idx16 = wk("idx16", [128, 4], I32)
    idx2 = wk("idx2", [128, 4], I32)
    riota = wk("riota", [128, RG], I32)
    idxc = wk("idxc", [128, 4, RG], I32)
    idxk = wk("idxk", [128, 4, 2], I32)
    ckv_g = [wk("ckv_g%d" % i, [128, 1024], BF16) for i in range(3)]
    kr_g = [wk("kr_g%d" % i, [128, 2048], BF16) for i in range(2)]
    cgx = [wk("cgx%d" % i, [128, 8, KVR + 1], BF16) for i in range(2)]
    ckvT_g = [wk("ckvT_g%d" % i, [128, 1024], BF16) for i in range(2)]
    krT_g = [wk("krT_g%d" % i, [128, 1024], BF16) for i in range(2)]
    ksq_s = [wk("ksq_s%d" % i, [128, 2048], BF16) for i in range(2)]
    ss_s = wk("ss_s", [128, 64], F32)
    ln_s = wk("ln_s", [128, 64], F32)
    rs_s = [wk("rs_s%d" % i, [128, 64], F32) for i in range(2)]
    tmpS = [wk("tmpS%d" % i, [128, 512], F32) for i in range(2)]
    Es = [wk("Es%d" % i, [128, 512], BF16) for i in range(2)]
    QabsT = wk("QabsT", [128, 4, 64], BF16)
    QrT = wk("QrT", [128, 4, 64], BF16)
    Qg = wk("Qg", [128, NH, NS], BF16)
    w_ukT = wk("w_ukT", [128, NH, 128], BF16)
    En = wk("En", [128, NH, NS], F32)
    En_s = wk("En_s", [128, 4, 64], BF16)
    mk_i = wk("mk_i", [128, 4], I32)
    mk_f = wk("mk_f", [128, 4], F32)
    mcol_i = wk("mcol_i", [128, 2, NS], I32)
    mcol_f = wk("mcol_f", [128, 2, NS], F32)
    mnew = wk("mnew", [128, 2, NS], F32)
    rsum = wk("rsum", [128, 1], F32)
    ctxn = wk("ctxn", [128, 128], BF16)
    ctxT = wk("ctxT", [128, 64], BF16)
    ysq = wk("ysq", [128, 256], BF16)
    yr = wk("yr", [128, 256], F32)
    battn_s = wk("battn_s", [128, 64], BF16)

    V = "vector"
    S.dma("sync", "ld", idx_i[:, :], ptab.rearrange("s p -> p s"), writes=["idx_i"], allow_slow_non_contiguous=True)
    S.op("gpsimd", lambda e: e.iota(riota[:, :], pattern=[[1, RG]], base=0, channel_multiplier=0), writes=["riota"])
    S.op("gpsimd", lambda e: e.memset(battn_s[:, :], 1.0 / 64), writes=["battn_s"])
    for q_ in range(2):
        S.op("gpsimd", lambda e, q_=q_: e.memset(cgx[q_][:, :, KVR:KVR + 1], 1.0), writes=[("cgx1", q_)])
    idx_f = wk("idx_f", [128, 4], F32)
    riota_f = wk("riota_f", [128, RG], F32)
    idxc_f = wk("idxc_f", [128, 4, RG], F32)
    idxk_f = wk("idxk_f", [128, 4, 2], F32)
    S.op(V, lambda e: e.tensor_copy(out=idx_f[:, :], in_=idx_i[:, :]), reads=["idx_i"], writes=["idx_f"])
    S.op(V, lambda e: e.tensor_copy(out=riota_f[:, :], in_=riota[:, :]), reads=["riota"], writes=["riota_f"])
    for q_ in range(4):
        S.op(V, lambda e, q_=q_: e.scalar_tensor_tensor(out=idxc_f[:, q_, :], in0=idx_f[:, q_:q_ + 1].to_broadcast([128, RG]),
                                                        scalar=float(RG), in1=riota_f[:, :], op0=ALU.mult, op1=ALU.add),
             reads=["idx_f", "riota_f"], writes=[("idxc_f", q_)])
        S.op(V, lambda e, q_=q_: e.scalar_tensor_tensor(out=idxk_f[:, q_, :], in0=idx_f[:, q_:q_ + 1].to_broadcast([128, 2]),
                                                        scalar=2.0, in1=riota_f[:, 0:2], op0=ALU.mult, op1=ALU.add),
             reads=["idx_f", "riota_f"], writes=[("idxk_f", q_)])
    S.op(V, lambda e: e.tensor_copy(out=idxc[:, :, :], in_=idxc_f[:, :, :]), reads=[("idxc_f", q_) for q_ in range(4)], writes=["idxc"])
    S.op(V, lambda e: e.tensor_copy(out=idxk[:, :, :], in_=idxk_f[:, :, :]), reads=[("idxk_f", q_) for q_ in range(4)], writes=["idxk"])

    def tr_wuk(e):
        ins = None
        for h in range(NH):
            ins = e.transpose(out=psb(0)[0:64, h * 128:(h + 1) * 128], in_=w_uk_sb[:, h * 64:(h + 1) * 64], identity=ident[:, :])
        return ins
    S.op("tensor", tr_wuk, reads=["w_uk", "ident"], writes=["ps0"])
    S.op(V, lambda e: e.tensor_copy(out=w_ukT[0:64, :, :], in_=psb(0)[0:64, 0:1024].rearrange("p (h d) -> p h d", h=NH)),
         reads=["ps0"], writes=["w_ukT"])
    S.op(V, lambda e: e.tensor_scalar(out=Qg[0:64, :, :], in0=QT[0:64, :, SEQ:SEQ + NS], scalar1=gkn_c[0:64, 0:1], scalar2=None,
                                      op0=ALU.mult), reads=[("QT", 16), "gkn_c"], writes=["Qg"])

    def mm_qabs(e):
        ins = None
        for h in range(NH):
            ins = e.matmul(ps[2][:, h * NS:(h + 1) * NS], lhsT=w_ukT[0:64, h, :], rhs=Qg[0:64, h, :], start=True, stop=True)
        return ins
    S.op("tensor", mm_qabs, reads=["w_ukT", "Qg"], writes=["ps2"])
    S.op(V, lambda e: e.tensor_copy(out=QabsT[:, :, :].rearrange("p s (h t) -> p h s t", h=NH),
                                    in_=ps[2][:, 0:NH * NS].rearrange("p (h s t) -> p h s t", h=NH, s=4)),
         reads=["ps2"], writes=["QabsT"])
    S.op(V, lambda e: e.tensor_copy(out=QrT[0:32, :, :].rearrange("p s (h t) -> p h s t", h=NH),
                                    in_=QT[64:96, :, SEQ:SEQ + NS].rearrange("p h (s t) -> p h s t", s=4)),
         reads=[("QT", 16)], writes=["QrT"])

    def mm_new(e):
        ins = None
        for h in range(NH):
            ins = e.matmul(ps[3][0:NS, h * NS:(h + 1) * NS], lhsT=KT[0:96, h, SEQ:SEQ + NS], rhs=QT[0:96, h, SEQ:SEQ + NS],
                           start=True, stop=True)
        return ins
    S.op("tensor", mm_new, reads=[("KT", 16), ("QT", 16)], writes=["ps3"])
    S.op("scalar", lambda e: e.activation(out=En[0:NS, :, :], in_=ps[3][0:NS, 0:NH * NS].rearrange("p (h q) -> p h q", h=NH),
                                          func=AF.Exp, scale=SCALE), reads=["ps3"], writes=["En"])
    S.op("gpsimd", lambda e: e.iota(mk_i[:, 0:1], pattern=[[0, 1]], base=0, channel_multiplier=1), writes=["mk_i0"])
    S.op(V, lambda e: e.tensor_single_scalar(out=mk_i[:, 1:2], in_=mk_i[:, 0:1], scalar=3, op=ALU.arith_shift_right),
         reads=["mk_i0"], writes=["mk_i1"])
    S.op(V, lambda e: e.tensor_single_scalar(out=mk_i[:, 2:3], in_=mk_i[:, 0:1], scalar=7, op=ALU.bitwise_and),
         reads=["mk_i0"], writes=["mk_i2"])
    S.op(V, lambda e: e.tensor_copy(out=mk_f[:, :], in_=mk_i[:, :]), reads=["mk_i0", "mk_i1", "mk_i2"], writes=["mk_f"])
    S.op("gpsimd", lambda e: e.iota(mcol_i[:, 0, :], pattern=[[1, 4], [0, 8]], base=0, channel_multiplier=0), writes=["mcol0"])
    S.op("gpsimd", lambda e: e.iota(mcol_i[:, 1, :], pattern=[[0, 4], [1, 8]], base=0, channel_multiplier=0), writes=["mcol1"])
    S.op(V, lambda e: e.tensor_copy(out=mcol_f[:, :, :], in_=mcol_i[:, :, :]), reads=["mcol0", "mcol1"], writes=["mcol_f"])
    S.op(V, lambda e: e.tensor_scalar(out=mnew[:, 0, :], in0=mcol_f[:, 0, :], scalar1=mk_f[:, 1:2], scalar2=None, op0=ALU.is_equal),
         reads=["mcol_f", "mk_f"], writes=["mnew0"])
    S.op(V, lambda e: e.tensor_scalar(out=mnew[:, 1, :], in0=mcol_f[:, 1, :], scalar1=mk_f[:, 2:3], scalar2=None, op0=ALU.is_ge),
         reads=["mcol_f", "mk_f"], writes=["mnew1"])
    S.op(V, lambda e: e.tensor_tensor(out=mnew[:, 0, :], in0=mnew[:, 0, :], in1=mnew[:, 1, :], op=ALU.mult),
         reads=["mnew0", "mnew1"], writes=["mnew0"])
    S.op(V, lambda e: e.tensor_tensor(out=En[0:NS, :, :], in0=En[0:NS, :, :],
                                      in1=mnew[0:NS, 0, :].unsqueeze(1).to_broadcast([NS, NH, NS]), op=ALU.mult),
         reads=["En", "mnew0"], writes=["En"])
    S.op(V, lambda e: e.tensor_copy(out=En_s[0:NS, :, :].rearrange("p s (h t) -> p h s t", h=NH),
                                    in_=En[0:NS, :, :].rearrange("p h (s t) -> p h s t", s=4)),
         reads=["En"], writes=["En_s"])

    NG = 4 * RG

    def gather_group(g):
        s_, r = divmod(g, RG)
        cg = ckv_g[g % 3]
        S.op("gpsimd", lambda e: e.indirect_dma_start(
            out=cg[:, :], out_offset=None, in_=c2ckv[:, :],
            in_offset=bass.IndirectOffsetOnAxis(ap=idxc[:, s_, r:r + 1], axis=0)),
            reads=["idxc"], writes=[("ckv_g", g % 3)], dma="gc%d" % (g % 3))
        if r % 8 == 0:
            kb = (g // 8) % 2
            S.op("gpsimd", lambda e: e.indirect_dma_start(
                out=kr_g[kb][:, :], out_offset=None, in_=c2kr[:, :],
                in_offset=bass.IndirectOffsetOnAxis(ap=idxk[:, s_, r // 8:r // 8 + 1], axis=0)),
                reads=["idxk"], writes=[("kr_g", kb)], dma="gk%d" % kb)

    def front(g):
        s_, r = divmod(g, RG)
        if g + 2 < NG:
            gather_group(g + 2)
        if g % 3 == 2:
            issue_conv(1)
        cg = ckv_g[g % 3]
        kb = (g // 8) % 2
        cT, kT_ = ckvT_g[g % 2], krT_g[g % 2]
        rs_ = rs_s[g % 2]
        cx = cgx[g % 2]
        S.op("gpsimd", lambda e: e.tensor_copy(out=cx[:, :, 0:KVR], in_=cg[:, :].rearrange("p (t d) -> p t d", t=8)),
             reads=[("ckv_g", g % 3)], writes=[("cgx", g % 2)])

        def tr_c(e):
            ins = None
            for t in range(8):
                ins = e.transpose(out=psb(0)[:, t * 128:(t + 1) * 128], in_=cg[:, t * 128:(t + 1) * 128], identity=ident[:, :])
            return ins
        S.op("tensor", tr_c, reads=[("ckv_g", g % 3), "ident"], writes=["ps0"])
        S.op("scalar", lambda e: e.copy(out=cT[:, :], in_=psb(0)[:, 0:1024]), reads=["ps0"], writes=[("ckvT_g", g % 2)])

        def tr_k(e):
            ins = None
            for t in range(8):
                tl = (r % 8) * 8 + t
                ins = e.transpose(out=psb(1)[0:32, t * 128:(t + 1) * 128], in_=kr_g[kb][:, tl * 32:(tl + 1) * 32], identity=ident[:, :])
            return ins
        S.op("tensor", tr_k, reads=[("kr_g", kb), "ident"], writes=["ps1"])
        S.op(V, lambda e: e.tensor_copy(out=kT_[0:32, :], in_=psb(1)[0:32, 0:1024]), reads=["ps1"], writes=[("krT_g", g % 2)])
        for rd in range(4):
            b0 = 2 + 2 * (rd % 2)

            def mm_kn(e, rd=rd, b0=b0):
                ins = None
                for tt_ in range(2):
                    t = rd * 2 + tt_
                    ins = e.matmul(ps[b0 + tt_][:, :], lhsT=cT[:, t * 128:(t + 1) * 128], rhs=w_uk_sb[:, :], start=True, stop=True)
                return ins
            S.op("tensor", mm_kn, reads=[("ckvT_g", g % 2), "w_uk"], writes=["ps%d" % b0, "ps%d" % (b0 + 1)])
            S.op("scalar", lambda e, rd=rd, b0=b0: e.activation(out=ksq_s[rd % 2][:, 0:1024], in_=ps_big[:, b0 * 512:(b0 + 2) * 512],
                                                                func=AF.Square),
                 reads=["ps%d" % b0, "ps%d" % (b0 + 1)], writes=[("ksq_s", rd % 2)])
            S.op(V, lambda e, rd=rd: e.reduce_sum(out=ss_s[:, rd * 16:(rd + 1) * 16],
                                                  in_=ksq_s[rd % 2][:, 0:1024].rearrange("p (a d) -> p a d", d=ND), axis=AX.X),
                 reads=[("ksq_s", rd % 2)], writes=[("ss_s", rd)])
        S.op("scalar", lambda e: e.activation(out=ln_s[:, :], in_=ss_s[:, :], func=AF.Ln, bias=eps_c[:, :], scale=1.0 / ND),
             reads=[("ss_s", q_) for q_ in range(4)] + ["eps_c"], writes=["ln_s"])
        S.op("scalar", lambda e: e.activation(out=rs_[:, :], in_=ln_s[:, :], func=AF.Exp, scale=-0.5),
             reads=["ln_s"], writes=[("rs_s", g % 2)])

    def back(g):
        s_, r = divmod(g, RG)
        cT, kT_ = ckvT_g[g % 2], krT_g[g % 2]
        rs_ = rs_s[g % 2]
        cx = cgx[g % 2]
        if r == 0:
            S.op("tensor", lambda e: e.matmul(ps[7][0:64, 0:KVR + 1], lhsT=En_s[0:NS, s_, :], rhs=ckv_s_b[0:NS, :], start=True, stop=False),
                 reads=["En_s", "ckv_s_b", "ckv_s_b1"], writes=["ps7"])

        def mm_sc(e):
            ins = None
            for t in range(8):
                e.matmul(ps[6][:, t * 64:(t + 1) * 64], lhsT=cT[:, t * 128:(t + 1) * 128], rhs=QabsT[:, s_, :], start=True, stop=True)
            for t in range(8):
                ins = e.matmul(ps[1][:, t * 64:(t + 1) * 64], lhsT=kT_[0:32, t * 128:(t + 1) * 128], rhs=QrT[0:32, s_, :],
                               start=True, stop=True)
            return ins
        S.op("tensor", mm_sc, reads=[("ckvT_g", g % 2), ("krT_g", g % 2), "QabsT", "QrT"], writes=["ps6", "ps1"])
        tS, E_ = tmpS[g % 2], Es[g % 2]
        S.op(V, lambda e: e.tensor_tensor(out=tS[:, :].rearrange("p (a t) -> p a t", t=8),
                                          in0=ps[6][:, :].rearrange("p (a t) -> p a t", t=8),
                                          in1=rs_[:, :].unsqueeze(2).to_broadcast([128, 64, 8]), op=ALU.mult),
             reads=["ps6", ("rs_s", g % 2)], writes=[("tmpS", g % 2)])
        S.op(V, lambda e: e.tensor_tensor(out=tS[:, :], in0=ps[1][:, :], in1=tS[:, :], op=ALU.add),
             reads=["ps1", ("tmpS", g % 2)], writes=[("tmpS", g % 2)])
        S.op("scalar", lambda e: e.activation(out=E_[:, :], in_=tS[:, :], func=AF.Exp, scale=SCALE),
             reads=[("tmpS", g % 2)], writes=[("Es", g % 2)])

        def mm_ctx(e):
            ins = None
            for t in range(8):
                last = (r == RG - 1 and t == 7)
                ins = e.matmul(ps[7][0:64, 0:KVR + 1], lhsT=E_[:, t * 64:(t + 1) * 64], rhs=cx[:, t, :], start=False, stop=last)
            return ins
        S.op("tensor", mm_ctx, reads=[("Es", g % 2), ("cgx", g % 2), ("cgx1", g % 2)], writes=["ps7"])
        if r == RG - 1:
            S.op(V, lambda e: e.reciprocal(out=rsum[0:64, :], in_=ps[7][0:64, 128:129]), reads=["ps7"], writes=["rsum"])
            S.op(V, lambda e: e.tensor_scalar(out=ctxn[0:64, :], in0=ps[7][0:64, 0:128], scalar1=rsum[0:64, 0:1], scalar2=None, op0=ALU.mult),
                 reads=["ps7", "rsum"], writes=["ctxn"])
            S.op("tensor", lambda e: e.transpose(out=ps_ct[:, 0:64], in_=ctxn[0:64, :], identity=ident[0:64, 0:64]),
                 reads=["ctxn", "ident"], writes=["ps7b"])
            S.op(V, lambda e: e.tensor_copy(out=ctxT[:, :], in_=ps_ct[:, 0:64]), reads=["ps7b"], writes=["ctxT"])

            def mm_yv(e):
                ins = None
                for h in range(NH):
                    ins = e.matmul(ps_yv[0:64, h * NS + s_ * 8:h * NS + s_ * 8 + 8], lhsT=w_uv_sb[:, h * 64:(h + 1) * 64],
                                   rhs=ctxT[:, h * 8:(h + 1) * 8], start=True, stop=True)
                return ins
            S.op("tensor", mm_yv, reads=["ctxT", "w_uv"], writes=[("psyv", s_)])

    ps_yv = ps_big[:, 7 * 512 + 256:7 * 512 + 512]
    ps_ct = ps[7].bitcast(BF16)[:, 320:384]
    gather_group(0)
    gather_group(1)
    front(0)
    for g in range(NG):
        S.window_begin()
        if g + 1 < NG:
            S.cur_stream = 0
            front(g + 1)
        S.cur_stream = 1
        back(g)
        S.cur_stream = 0
        S.window_end()
    S.op("scalar", lambda e: e.activation(out=ysq[0:64, :], in_=ps_yv[0:64, 0:256], func=AF.Square),
         reads=[("psyv", q) for q in range(4)], writes=["ysq"])
    S.op("tensor", lambda e: e.matmul(ps[2][0:64, 0:256], lhsT=battn_s[0:64, 0:64], rhs=ysq[0:64, :], start=True, stop=True),
         reads=["ysq", "battn_s"], writes=["ps2"])
    S.op("scalar", lambda e: e.activation(out=yr[0:64, :], in_=ps[2][0:64, 0:256], func=AF.Ln, bias=eps_c[0:64, :], scale=1.0),
         reads=["ps2", "eps_c"], writes=["yr"])
    S.op("scalar", lambda e: e.activation(out=yr[0:64, :], in_=yr[0:64, :], func=AF.Exp, scale=-0.5), reads=["yr"], writes=["yr"])
    for h in range(NH):
        po = (h % 2) * 64
        S.op(V, lambda e, h=h, po=po: e.scalar_tensor_tensor(out=ya_s[po:po + 64, h // 2, :], in0=ps_yv[0:64, h * NS:(h + 1) * NS],
                                                             scalar=gattn_c[0:64, h:h + 1], in1=yr[0:64, h * NS:(h + 1) * NS],
                                                             op0=ALU.mult, op1=ALU.mult),
             reads=[("psyv", q) for q in range(4)] + ["yr", "gattn_c"], writes=["ya_s"])

    S.barrier()
    arena_reset()
    x1s = nc.dram_tensor("x1s", [NTOK, D], F32, kind="Internal").ap()
    OH = wk("OH", [128, 17, 64], F32)
    RK = wk("RK", [128, 17, 4], F32)
    carry = wk("carry", [128, NE], F32)
    P23_MARK = arena_off[0]
    LG = wk("LG", [128, 17, 36], F32)
    MARK_R = arena_off[0]
    H2 = nc.dram_tensor("H2", [NTOK, D], BF16, kind="Internal").ap()
    w_out_sb = wk("w_out_sb", [128, 8, D], BF16)
    Et = [wk("Et%d" % i, [128, 512], BF16) for i in range(6)]
    sqa = wk("sqa", [128, 512], BF16)
    lnr = wk("lnr", [128, 512], F32)
    yattnT = [wk("yattnT%d" % i, [128, 4, 512], BF16) for i in range(2)]
    xr = [wk("xr%d" % i, [128, D], F32) for i in range(2)]
    h2b = wk("h2b", [128, D], BF16)
    junk2 = wk("junk2", [128, D], BF16)
    st2 = [wk("st2_%d" % i, [128, 32], F32) for i in range(2)]
    brb = wk("brb", [128, 36], F32)
    wr_sb = wk("wr_sb", [128, 8, 36], BF16)
    battn = wk("battn", [128, 64], BF16)

    S.dma("gpsimd", "wo", w_out_sb, w_out.rearrange("(k p) n -> p k n", p=128), writes=["w_out"])
    S.dma("gpsimd", "wr0", wr_sb[:, :, 0:4], w_rg.rearrange("(k p) n -> p k n", p=128), writes=["wr0"])
    S.dma("gpsimd", "wr1", wr_sb[:, :, 4:36], w_re.rearrange("(k p) n -> p k n", p=128), writes=["wr1"])
    S.dma("sync", "lg", gmix_b[:], g_ffn[0:1, :].to_broadcast([128, D]), writes=["gffn_b"])
    S.dma("sync", "lb0", brb[:, 0:4], b_rg[0:1, :].to_broadcast([128, 4]), writes=["brb0"])
    S.dma("sync", "lb1", brb[:, 4:36], b_re[0:1, :].to_broadcast([128, NE]), writes=["brb1"])
    S.op("gpsimd", lambda e: e.memset(battn[0:64, :], 1.0 / 64), writes=["battn0"])
    S.op("gpsimd", lambda e: e.memset(battn[64:65, :], EPS), writes=["battn1"])

    def attention_chunk(c):
        ya = yattnT[c % 2]
        nj = 4 * c + 4
        S.window_begin()
        for h in range(NH):
            S.cur_stream = h % 2
            ob = 2 + (h % 2)
            sb0 = 0 if h % 2 == 0 else 5
            eb0 = 3 * (h % 2)

            def q0n(j):
                q0 = max(c * 512, j * 128)
                return q0, (c + 1) * 512 - q0

            def issue_S(j, h=h, sb0=sb0):
                q0, n = q0n(j)
                bank = sb0 + j % 2
                S.op("tensor", lambda e: e.matmul(ps[bank][:, 0:n], lhsT=KT[0:96, h, j * 128:(j + 1) * 128],
                                                  rhs=QT[0:96, h, q0:q0 + n], start=True, stop=True),
                     reads=[("KT", j)] + [("QT", t) for t in range(q0 // 128, 4 * c + 4)], writes=["ps%d" % bank])
            issue_S(0)
            for j in range(nj):
                if j + 1 < nj:
                    issue_S(j + 1)
                q0, n = q0n(j)
                bank, eb = sb0 + j % 2, eb0 + j % 3
                S.op("scalar", lambda e, n=n, bank=bank, eb=eb: e.activation(out=Et[eb][:, 0:n], in_=ps[bank][:, 0:n],
                                                                             func=AF.Exp, scale=SCALE),
                     reads=["ps%d" % bank], writes=[("E", eb)])
                if j >= 4 * c:
                    S.op("gpsimd", lambda e, eb=eb: e.tensor_tensor(out=Et[eb][:, 0:128], in0=Et[eb][:, 0:128], in1=tri[:, :],
                                                                    op=ALU.mult),
                         reads=[("E", eb), "tri"], writes=[("E", eb)])
                S.op("tensor", lambda e, j=j, h=h, n=n, q0=q0, eb=eb, ob=ob: e.matmul(
                    ps[ob][0:65, q0 - c * 512:512], lhsT=Vaug[:, j, h, :], rhs=Et[eb][:, 0:n],
                    start=(j == 0), stop=(j == nj - 1)),
                    reads=[("E", eb), ("Vaug", j), "Vaug_ones"], writes=["ps%d" % ob])
            S.op("scalar", lambda e, ob=ob: e.activation(out=sqa[0:65, :], in_=ps[ob][0:65, :], func=AF.Square),
                 reads=["ps%d" % ob], writes=["sqa"])
            S.op("tensor", lambda e: e.matmul(ps[4][0:64, :], lhsT=battn[0:65, :], rhs=sqa[0:65, :], start=True, stop=True),
                 reads=["sqa", "battn0", "battn1"], writes=["ps4"])
            S.op("scalar", lambda e: e.activation(out=lnr[0:64, :], in_=ps[4][0:64, :], func=AF.Ln),
                 reads=["ps4", "lnr", "lnr2"], writes=["lnr"])
            S.op("scalar", lambda e: e.activation(out=lnr[0:64, :], in_=lnr[0:64, :], func=AF.Exp, scale=-0.5),
                 reads=["lnr"], writes=["lnr"])
            po = (h % 2) * 64
            S.op("vector", lambda e, h=h, ob=ob, po=po, ya=ya: e.scalar_tensor_tensor(
                out=ya[po:po + 64, h // 2, :], in0=ps[ob][0:64, :], scalar=gattn_c[0:64, h:h + 1], in1=lnr[0:64, :],
                op0=ALU.mult, op1=ALU.mult),
                reads=["ps%d" % ob, "lnr", "gattn_c"], writes=[("ya", c % 2, h)])
        S.cur_stream = 0
        S.window_end()

    def merge_tile(ti):
        T = 128 if ti < 16 else NS
        tok0 = ti * 128
        sl = ti % 2
        X, X1, ST = xr[sl], xr[sl], st2[sl]
        src = x_p[tok0:tok0 + 128, :] if ti < 16 else x_s[:, :]
        if ti == 0:
            S.dma("sync", "ldr%d" % sl, X[0:T, :], src, writes=[("xr", sl), ("x1b", sl, 0), ("x1b", sl, 1)])
        if ti < 16:
            nt_ = ti + 1
            nsl = nt_ % 2
            nT = 128 if nt_ < 16 else NS
            nsrc = x_p[nt_ * 128:(nt_ + 1) * 128, :] if nt_ < 16 else x_s[:, :]
            S.dma("sync", "ldr%d" % nsl, xr[nsl][0:nT, :], nsrc, writes=[("xr", nsl), ("x1b", nsl, 0), ("x1b", nsl, 1)])
        if ti < 16:
            ya = yattnT[(ti // 4) % 2]
            acol = (ti % 4) * 128
            yakeys = [("ya", (ti // 4) % 2, h) for h in range(NH)]
        else:
            ya = ya_s
            acol = 0
            yakeys = ["ya_s"]

        def mm(e):
            ins = None
            for half in range(2):
                for k in range(4):
                    ins = e.matmul(ps[5 + half][0:T, :], lhsT=yconvT[:, k, tok0:tok0 + T],
                                   rhs=w_out_sb[:, k, half * 512:(half + 1) * 512], start=(k == 0), stop=False)
                for k in range(4):
                    ins = e.matmul(ps[5 + half][0:T, :], lhsT=ya[:, k, acol:acol + T],
                                   rhs=w_out_sb[:, 4 + k, half * 512:(half + 1) * 512], start=False, stop=(k == 3))
            return ins
        S.op("tensor", mm, reads=yakeys + ["w_out"] + [("yconvT", c, ci) for c in range(4) for ci in range(9)],
             writes=["ps5", "ps6"])
        for half in range(2):
            S.op("vector", lambda e, half=half: e.tensor_tensor(out=X1[0:T, half * 512:(half + 1) * 512],
                                                                in0=ps[5 + half][0:T, :], in1=X[0:T, half * 512:(half + 1) * 512],
                                                                op=ALU.add),
                 reads=["ps%d" % (5 + half), ("xr", sl)], writes=[("x1b", sl, half)])
        x1k = [("x1b", sl, 0), ("x1b", sl, 1)]
        S.dma("sync", "stx%d" % sl, x1s[tok0:tok0 + T, :], X1[0:T, :], reads=x1k, writes=[("x1s", ti)])
        S.op("scalar", lambda e: e.activation(out=junk2[0:T, :], in_=X1[0:T, :], func=AF.Square, accum_out=ST[0:T, 0:1]),
             reads=x1k, writes=[("st2", sl, 0)])
        rstd_from_ss(ST[0:T, 0:1], ST[0:T, 1:2], D, T, [("st2", sl, 0)], [("st2", sl, 1)], ST[0:T, 2:3], ("st2", sl, 2))
        S.op("vector", lambda e: e.scalar_tensor_tensor(out=h2b[0:T, :], in0=X1[0:T, :], scalar=ST[0:T, 1:2],
                                                        in1=gmix_b[0:T, :], op0=ALU.mult, op1=ALU.mult),
             reads=x1k + [("st2", sl, 1), "gffn_b"], writes=["h2b"])
        S.dma("sync", "sth", H2[tok0:tok0 + T, :], h2b[0:T, :], reads=["h2b"], writes=[("H2", ti)])
        pT = psb(7)

        def tr(e):
            ins = None
            for k in range(8):
                ins = e.transpose(out=pT[:, k * 128:k * 128 + T], in_=h2b[0:T, k * 128:(k + 1) * 128], identity=ident[0:T, 0:T])
            return ins
        S.op("tensor", tr, reads=["h2b", "ident"], writes=["ps7"])
        S.op("scalar", lambda e: e.copy(out=QT[:, :, tok0:tok0 + T],
                                        in_=pT[:, 0:1024].rearrange("p (k t) -> p k t", k=8)[:, :, 0:T]),
             reads=["ps7"], writes=[("QT", ti)])

        def mmr(e):
            ins = None
            for k in range(8):
                ins = e.matmul(ps[7][0:T, 0:36], lhsT=QT[:, k, tok0:tok0 + T], rhs=wr_sb[:, k, :], start=(k == 0), stop=(k == 7))
            return ins
        S.op("tensor", mmr, reads=[("QT", ti), "wr0", "wr1"], writes=["ps7"])
        S.op("vector", lambda e: e.tensor_tensor(out=LG[0:T, ti, :], in0=ps[7][0:T, 0:36], in1=brb[0:T, :], op=ALU.add),
             reads=["ps7", "brb0", "brb1"], writes=[("LG", ti)])

    for c in range(4):
        attention_chunk(c)
        S.window_begin()
        for t in range(4):
            S.cur_stream = t % 2
            merge_tile(4 * c + t)
        S.cur_stream = 0
        S.window_end()
    merge_tile(16)
    issue_conv(100)

    S.barrier()
    arena_off[0] = MARK_R
    V = "vector"
    NTL = 17
    g4 = wk("g4", [128, NTL, 4], F32)
    goh = wk("goh", [128, NTL, 4], F32)
    pen = wk("pen", [128, NTL, 4], F32)
    sc = wk("sc", [128, 12, NTL], F32)
    em = wk("em", [128, NTL, NE], F32)
    em2 = wk("em2", [128, NTL, NE], F32)
    R7 = wk("R7", [128, NTL, NE], F32)
    CAR = wk("CAR", [128, NTL, NE], F32)
    Mb = wk("Mb", [128, NTL, NE], BF16)
    lst_b = wk("lst_b", [128, 128], BF16)
    S.op("gpsimd", lambda e: e.affine_select(out=lst_b[:, :], in_=onesb[:, :], pattern=[[1, 128]], compare_op=ALU.is_gt, fill=0.0,
                                            base=0, channel_multiplier=-1), reads=["onesb"], writes=["lst_b"])
    SC = lambda i: sc[:, i, :]
    bc4 = lambda ap: ap.unsqueeze(2).to_broadcast([128, NTL, 4])
    bc32 = lambda ap: ap.unsqueeze(2).to_broadcast([128, NTL, NE])
    OH1a, OH2a = OH[:, :, 0:32], OH[:, :, 32:64]
    S.op(V, lambda e: e.reduce_max(out=SC(0), in_=LG[:, :, 0:4], axis=AX.X), writes=["sc0"])
    S.op(V, lambda e: e.tensor_tensor(out=goh[:, :, :], in0=LG[:, :, 0:4], in1=bc4(SC(0)), op=ALU.is_equal), reads=["sc0"], writes=["goh"])
    S.op(V, lambda e: e.tensor_tensor(out=g4[:, :, :], in0=LG[:, :, 0:4], in1=bc4(SC(0)), op=ALU.subtract), reads=["sc0"], writes=["g4"])
    S.op("scalar", lambda e: e.activation(out=g4[:, :, :], in_=g4[:, :, :], func=AF.Exp), reads=["g4"], writes=["g4"])
    S.op(V, lambda e: e.reduce_sum(out=SC(1), in_=g4[:, :, :], axis=AX.X), reads=["g4"], writes=["sc1"])
    S.op(V, lambda e: e.reciprocal(out=SC(2), in_=SC(1)), reads=["sc1"], writes=["sc2"])
    S.op(V, lambda e: e.tensor_scalar(out=pen[:, :, :], in0=goh[:, :, :], scalar1=-1.0, scalar2=1e30, op0=ALU.add, op1=ALU.mult),
         reads=["goh"], writes=["pen"])
    S.op(V, lambda e: e.tensor_tensor(out=em[:, :, :].rearrange("p t (g x) -> p t g x", g=4),
                                      in0=LG[:, :, 4:36].rearrange("p t (g x) -> p t g x", g=4),
                                      in1=pen[:, :, :].unsqueeze(3).to_broadcast([128, NTL, 4, 8]), op=ALU.add),
         reads=["pen"], writes=["em"])
    S.op(V, lambda e: e.reduce_max(out=SC(3), in_=em[:, :, :], axis=AX.X), reads=["em"], writes=["sc3"])
    S.op(V, lambda e: e.tensor_tensor(out=OH1a, in0=em[:, :, :], in1=bc32(SC(3)), op=ALU.is_equal), reads=["em", "sc3"], writes=["oh1"])
    S.op(V, lambda e: e.scalar_tensor_tensor(out=em2[:, :, :], in0=OH1a, scalar=-1e30, in1=em[:, :, :], op0=ALU.mult, op1=ALU.add),
         reads=["oh1", "em"], writes=["em2"])
    S.op(V, lambda e: e.reduce_max(out=SC(4), in_=em2[:, :, :], axis=AX.X), reads=["em2"], writes=["sc4"])
    S.op(V, lambda e: e.tensor_tensor(out=OH2a, in0=em2[:, :, :], in1=bc32(SC(4)), op=ALU.is_equal), reads=["em2", "sc4"], writes=["oh2"])
    S.op(V, lambda e: e.tensor_tensor(out=SC(5), in0=SC(4), in1=SC(3), op=ALU.subtract), reads=["sc3", "sc4"], writes=["sc5"])
    S.op("scalar", lambda e: e.activation(out=SC(6), in_=SC(5), func=AF.Exp), reads=["sc5"], writes=["sc6"])
    S.op(V, lambda e: e.tensor_scalar(out=SC(7), in0=SC(6), scalar1=1.0, scalar2=None, op0=ALU.add), reads=["sc6"], writes=["sc7"])
    S.op(V, lambda e: e.reciprocal(out=SC(8), in_=SC(7)), reads=["sc7"], writes=["sc8"])
    S.op(V, lambda e: e.tensor_tensor(out=RK[:, :, 2], in0=SC(8), in1=SC(2), op=ALU.mult), reads=["sc8", "sc2"], writes=["rk2"])
    S.op(V, lambda e: e.tensor_tensor(out=RK[:, :, 3], in0=SC(2), in1=RK[:, :, 2], op=ALU.subtract), reads=["rk2", "sc2"], writes=["rk3"])
    S.op(V, lambda e: e.tensor_tensor(out=Mb[:, :, :], in0=OH1a, in1=OH2a, op=ALU.add), reads=["oh1", "oh2"], writes=["Mb"])

    def mmc(e):
        ins = None
        for ti in range(NTL):
            T = 128 if ti < 16 else NS
            if ti < 16:
                e.matmul(ps[6][:, ti * NE:(ti + 1) * NE], lhsT=lst_b[0:T, 0:T], rhs=Mb[0:T, ti, :], start=True, stop=True)
                ins = e.matmul(ps[5][:, ti * NE:(ti + 1) * NE], lhsT=onesb[0:T, :], rhs=Mb[0:T, ti, :], start=True, stop=True)
            else:
                e.matmul(ps[7][0:T, 0:NE], lhsT=lst_b[0:T, 0:T], rhs=Mb[0:T, ti, :], start=True, stop=True)
                ins = e.matmul(ps[7][:, 64:64 + NE], lhsT=onesb[0:T, :], rhs=Mb[0:T, ti, :], start=True, stop=True)
        return ins
    S.op("tensor", mmc, reads=["Mb", "lst_b", "onesb"], writes=["ps5", "ps6", "ps7"])
    S.op("gpsimd", lambda e: e.memset(CAR[:, 0, :], 0.0), writes=[("car", 0)])
    for ti in range(16):
        S.op(V, lambda e, ti=ti: e.tensor_tensor(out=CAR[:, ti + 1, :], in0=ps[5][:, ti * NE:(ti + 1) * NE], in1=CAR[:, ti, :], op=ALU.add),
             reads=["ps5", ("car", ti)], writes=[("car", ti + 1)])
    S.op(V, lambda e: e.tensor_tensor(out=carry[:, :], in0=ps[7][:, 64:64 + NE], in1=CAR[:, 16, :], op=ALU.add),
         reads=["ps7", ("car", 16)], writes=["carry"])
    cark = [("car", t) for t in range(17)]
    S.op(V, lambda e: e.tensor_tensor(out=R7[:, 0:16, :], in0=ps[6][:, :].rearrange("p (t x) -> p t x", t=16), in1=CAR[:, 0:16, :], op=ALU.add),
         reads=["ps6"] + cark, writes=["R7a"])
    S.op(V, lambda e: e.tensor_tensor(out=R7[0:NS, 16, :], in0=ps[7][0:NS, 0:NE], in1=CAR[0:NS, 16, :], op=ALU.add),
         reads=["ps7"] + cark, writes=["R7b"])
    S.op(V, lambda e: e.tensor_tensor(out=em[:, :, :], in0=R7[:, :, :], in1=OH1a, op=ALU.mult), reads=["R7a", "R7b", "oh1", "em2"], writes=["em"])
    S.op(V, lambda e: e.reduce_sum(out=RK[:, :, 0], in_=em[:, :, :], axis=AX.X), reads=["em"], writes=["rk0"])
    S.op(V, lambda e: e.tensor_tensor(out=em2[:, :, :], in0=R7[:, :, :], in1=OH2a, op=ALU.mult), reads=["R7a", "R7b", "oh2"], writes=["em2"])
    S.op(V, lambda e: e.reduce_sum(out=RK[:, :, 1], in_=em2[:, :, :], axis=AX.X), reads=["em2"], writes=["rk1"])

    S.barrier()
    arena_off[0] = P23_MARK
    TM = 256
    NT = (2 * NTOK + TM - 1) // TM + NE
    NSL = NT * TM
    Xs = nc.dram_tensor("Xs", [NSL, D], BF16, kind="Internal").ap()
    Ys = nc.dram_tensor("Ys", [NSL, D], BF16, kind="Internal").ap()
    V = "vector"
    cnt_i = wk("cnt_i", [128, NE], I32)
    nt_f = wk("nt_f", [128, NE], F32)
    scn = [wk("scn%d" % i, [128, NE], F32) for i in range(2)]
    bt = wk("bt", [128, NE], F32)
    bslot = wk("bslot", [128, NE], F32)
    ee_i = wk("ee_i", [128, NE], I32)
    ee_f = wk("ee_f", [128, NE], F32)
    QTf = QT[:, :, :].rearrange("p h t -> p (h t)").bitcast(F32)
    ii_f = QTf[:, 0:NT * NE].rearrange("p (i e) -> p i e", i=NT)
    ii_i = QTf[:, NT * NE:2 * NT * NE].bitcast(I32).rearrange("p (i e) -> p i e", i=NT)
    indA = QTf[:, 2 * NT * NE:3 * NT * NE].rearrange("p (i e) -> p i e", i=NT)
    texp = wk("texp", [128, NT], F32)
    tval = wk("tval", [128, NT], F32)
    pp_i = wk("pp_i", [128, 1], I32)
    pp_f = wk("pp_f", [128, 1], F32)
    idxw_f = wk("idxw_f", [128, NT], F32)
    idxw = wk("idxw", [128, NT], I32)
    POSf = wk("POSf", [128, 17, 2], F32)
    POS = wk("POS", [128, 17, 2], I32)
    ptmp = wk("ptmp", [128, NE], F32)

    S.op(V, lambda e: e.tensor_copy(out=cnt_i[:, :], in_=carry[:, :]), reads=["carry"], writes=["cnt_i"])
    S.op(V, lambda e: e.tensor_single_scalar(out=cnt_i[:, :], in_=cnt_i[:, :], scalar=TM - 1, op=ALU.add), reads=["cnt_i"], writes=["cnt_i"])
    S.op(V, lambda e: e.tensor_single_scalar(out=cnt_i[:, :], in_=cnt_i[:, :], scalar=8, op=ALU.arith_shift_right),
         reads=["cnt_i"], writes=["cnt_i"])
    S.op(V, lambda e: e.tensor_copy(out=nt_f[:, :], in_=cnt_i[:, :]), reads=["cnt_i"], writes=["nt_f"])
    cur, curk = nt_f, ["nt_f"]
    for si, sh in enumerate((1, 2, 4, 8, 16)):
        nxt = scn[si % 2]
        k0, k1 = ("scn", si % 2, 0), ("scn", si % 2, 1)
        S.op(V, lambda e, cur=cur, nxt=nxt, sh=sh: e.tensor_copy(out=nxt[:, 0:sh], in_=cur[:, 0:sh]), reads=curk, writes=[k0])
        S.op(V, lambda e, cur=cur, nxt=nxt, sh=sh: e.tensor_tensor(out=nxt[:, sh:NE], in0=cur[:, sh:NE], in1=cur[:, 0:NE - sh], op=ALU.add),
             reads=curk, writes=[k1])
        cur, curk = nxt, [k0, k1]
    bti, btik = cur, curk
    S.op(V, lambda e: e.tensor_tensor(out=bt[:, :], in0=bti[:, :], in1=nt_f[:, :], op=ALU.subtract), reads=btik + ["nt_f"], writes=["bt"])
    S.op(V, lambda e: e.tensor_single_scalar(out=bslot[:, :], in_=bt[:, :], scalar=float(TM), op=ALU.mult), reads=["bt"], writes=["bslot"])
    S.op("gpsimd", lambda e: e.iota(ee_i[:, :], pattern=[[1, NE]], base=0, channel_multiplier=0), writes=["ee_i"])
    S.op("gpsimd", lambda e: e.iota(ii_i[:, :, :], pattern=[[1, NT], [0, NE]], base=0, channel_multiplier=0), writes=["ii_i"])
    S.op("gpsimd", lambda e: e.iota(pp_i[:, :], pattern=[[0, 1]], base=0, channel_multiplier=1), writes=["pp_i"])
    S.op(V, lambda e: e.tensor_copy(out=ee_f[:, :], in_=ee_i[:, :]), reads=["ee_i"], writes=["ee_f"])
    S.op(V, lambda e: e.tensor_copy(out=ii_f[:, :, :], in_=ii_i[:, :, :]), reads=["ii_i"], writes=["ii_f"])
    S.op(V, lambda e: e.tensor_copy(out=pp_f[:, :], in_=pp_i[:, :]), reads=["pp_i"], writes=["pp_f"])
    S.op(V, lambda e: e.tensor_tensor(out=indA[:, :, :], in0=ii_f[:, :, :], in1=bt[:, :].unsqueeze(1).to_broadcast([128, NT, NE]), op=ALU.is_ge),
         reads=["ii_f", "bt"], writes=["indA"])
    S.op(V, lambda e: e.tensor_tensor(out=ii_f[:, :, :], in0=ii_f[:, :, :], in1=bti[:, :].unsqueeze(1).to_broadcast([128, NT, NE]), op=ALU.is_lt),
         reads=["ii_f"] + btik, writes=["ii_f"])
    S.op(V, lambda e: e.tensor_tensor(out=indA[:, :, :], in0=indA[:, :, :], in1=ii_f[:, :, :], op=ALU.mult), reads=["indA", "ii_f"], writes=["indA"])
    S.op(V, lambda e: e.reduce_sum(out=tval[:, :], in_=indA[:, :, :], axis=AX.X), reads=["indA"], writes=["tval"])
    S.op(V, lambda e: e.tensor_tensor(out=indA[:, :, :], in0=indA[:, :, :], in1=ee_f[:, :].unsqueeze(1).to_broadcast([128, NT, NE]), op=ALU.mult),
         reads=["indA", "ee_f", "tval"], writes=["indA"])
    S.op(V, lambda e: e.reduce_sum(out=texp[:, :], in_=indA[:, :, :], axis=AX.X), reads=["indA"], writes=["texp"])
    S.op(V, lambda e: e.tensor_scalar(out=tval[:, :], in0=tval[:, :], scalar1=-1000.0, scalar2=1000.0, op0=ALU.mult, op1=ALU.add),
         reads=["tval"], writes=["tval"])
    S.op(V, lambda e: e.tensor_tensor(out=texp[:, :], in0=texp[:, :], in1=tval[:, :], op=ALU.add), reads=["texp", "tval"], writes=["texp"])
    S.op(V, lambda e: e.tensor_scalar(out=idxw_f[:, :], in0=texp[:, :], scalar1=128.0, scalar2=pp_f[:, 0:1], op0=ALU.mult, op1=ALU.add),
         reads=["texp", "pp_f"], writes=["idxw_f"])
    S.op(V, lambda e: e.tensor_copy(out=idxw[:, :], in_=idxw_f[:, :]), reads=["idxw_f"], writes=["idxw"])

    h2t = [wk("h2t%d" % i, [128, D], BF16) for i in range(4)]
    last_sc = None
    for ti in range(17):
        T = 128 if ti < 16 else NS
        for k in range(2):
            S.op(V, lambda e, ti=ti, k=k, T=T: e.tensor_tensor(out=ptmp[0:T, :], in0=OH[0:T, ti, 32 * k:32 * k + 32], in1=bslot[0:T, :], op=ALU.mult),
                 reads=["bslot"], writes=["ptmp"])
            S.op(V, lambda e, ti=ti, k=k, T=T: e.reduce_sum(out=POSf[0:T, ti, k:k + 1], in_=ptmp[0:T, :], axis=AX.X),
                 reads=["ptmp"], writes=[("posf", ti, k)])
        S.op(V, lambda e, ti=ti, T=T: e.tensor_tensor(out=POSf[0:T, ti, :], in0=POSf[0:T, ti, :], in1=RK[0:T, ti, 0:2], op=ALU.add),
             reads=[("posf", ti, 0), ("posf", ti, 1)], writes=[("posf2", ti)])
        S.op(V, lambda e, ti=ti, T=T: e.tensor_copy(out=POS[0:T, ti, :], in_=POSf[0:T, ti, :]), reads=[("posf2", ti)], writes=[("pos", ti)])
        sl = ti % 4
        tok0 = ti * 128
        S.dma("sync", "lh%d" % sl, h2t[sl][0:T, :], H2[tok0:tok0 + T, :], writes=[("h2t", sl)])
        if ti == 0:
            dbg("h2t0", h2t[0][:, :], [("h2t", 0)])
        for k in range(2):
            last_sc = S.op("gpsimd", lambda e, ti=ti, k=k, T=T, sl=sl: e.indirect_dma_start(
                out=Xs[:, :], out_offset=bass.IndirectOffsetOnAxis(ap=POS[0:T, ti, k:k + 1], axis=0),
                in_=h2t[sl][0:T, :], in_offset=None), reads=[("h2t", sl), ("pos", ti)], writes=[("xs_sc", ti, k)], dma="sc%d" % sl)
    S.barrier()

    NWS = 5
    WSZ = 8 * DE + 8 * DE + 2 * D
    wslots = [arenaX[:, i * WSZ:(i + 1) * WSZ] for i in range(2)] + [arenaK[:, i * WSZ:(i + 1) * WSZ] for i in range(3)]
    xs = [wk("xs%d" % i, [128, 2, D], BF16) for i in range(2)]
    xsT = [wk("xsT%d" % i, [128, 8, TM], BF16) for i in range(2)]
    sgm = [wk("sgm%d" % i, [128, TM], F32) for i in range(2)]
    aTm = [wk("aTm%d" % i, [128, 2, TM], BF16) for i in range(2)]
    ysb = [wk("ysb%d" % i, [128, 2, D], BF16) for i in range(2)]
    ys_recs = []

    def wviews(slot):
        a = wslots[slot]
        return (a[:, 0:8 * DE], a[:, 8 * DE:16 * DE], a[:, 16 * DE:16 * DE + 2 * D])

    def fetch_tile(i):
        slot = i % NWS
        kw = dict(bounds_check=NE * 128 - 1, oob_is_err=False) if i >= 17 else {}
        S.op("gpsimd", lambda e: e.indirect_dma_start(
            out=wslots[slot][:, :], out_offset=None, in_=wall[:, :], in_offset=bass.IndirectOffsetOnAxis(ap=idxw[:, i:i + 1], axis=0), **kw),
            reads=["idxw"], writes=[("wsl", slot)], dma="we%d" % slot)

    def fetch_x(i):
        sl = i % 2
        S.dma("sync", "lx%d" % sl, xs[sl][:, :, :], Xs[i * TM:(i + 1) * TM, :].rearrange("(h p) d -> p h d", p=128), writes=[("xs", sl)])

    def moe_tile(i):
        slot, sl = i % NWS, i % 2
        wg2, wu2, wd2 = wviews(slot)
        wg = wg2.rearrange("p (k n) -> p k n", k=8)
        wu = wu2.rearrange("p (k n) -> p k n", k=8)
        wd = wd2.rearrange("p (k n) -> p k n", k=2)
        X, XT, A, Y = xs[sl], xsT[sl], aTm[sl], ysb[sl]
        for hh in range(2):
            def tr(e, hh=hh):
                ins = None
                for k in range(8):
                    ins = e.transpose(out=psb(hh)[:, k * 128:(k + 1) * 128], in_=X[:, hh, k * 128:(k + 1) * 128], identity=ident[:, :])
                return ins
            S.op("tensor", tr, reads=[("xs", sl), "ident"], writes=["ps%d" % hh])
            eng = "scalar" if hh == 0 else "vector"
            if hh == 0:
                S.op("scalar", lambda e, hh=hh: e.copy(out=XT[:, :, hh * 128:(hh + 1) * 128],
                                                       in_=psb(hh)[:, 0:1024].rearrange("p (k t) -> p k t", k=8)),
                     reads=["ps%d" % hh], writes=[("xsT", sl, hh)])
            else:
                S.op("vector", lambda e, hh=hh: e.tensor_copy(out=XT[:, :, hh * 128:(hh + 1) * 128],
                                                              in_=psb(hh)[:, 0:1024].rearrange("p (k t) -> p k t", k=8)),
                     reads=["ps%d" % hh], writes=[("xsT", sl, hh)])
        for kc in range(2):
            def mm(e, kc=kc):
                ins = None
                for k in range(8):
                    ins = e.matmul(ps[2 + kc][:, 0:TM], lhsT=wg[:, k, kc * 128:(kc + 1) * 128], rhs=XT[:, k, :], start=(k == 0), stop=(k == 7))
                for k in range(8):
                    ins = e.matmul(ps[2 + kc][:, TM:2 * TM], lhsT=wu[:, k, kc * 128:(kc + 1) * 128], rhs=XT[:, k, :], start=(k == 0), stop=(k == 7))
                return ins
            S.op("tensor", mm, reads=[("xsT", sl, 0), ("xsT", sl, 1), ("wsl", slot)], writes=["ps%d" % (2 + kc)])
            S.op("scalar", lambda e, kc=kc: e.activation(out=sgm[kc][:, :], in_=ps[2 + kc][:, 0:TM], func=AF.Silu),
                 reads=["ps%d" % (2 + kc)], writes=[("sgm", kc)])
            S.op("vector", lambda e, kc=kc: e.tensor_tensor(out=A[:, kc, :], in0=ps[2 + kc][:, TM:2 * TM], in1=sgm[kc][:, :], op=ALU.mult),
                 reads=["ps%d" % (2 + kc), ("sgm", kc)], writes=[("aTm", sl, kc)])
        for hh in range(2):
            for half in range(2):
                pb = 4 + hh * 2 + half

                def mmd(e, hh=hh, half=half, pb=pb):
                    ins = None
                    for kc in range(2):
                        ins = e.matmul(ps[pb][:, :], lhsT=A[:, kc, hh * 128:(hh + 1) * 128], rhs=wd[:, kc, half * 512:(half + 1) * 512],
                                       start=(kc == 0), stop=(kc == 1))
                    return ins
                S.op("tensor", mmd, reads=[("aTm", sl, 0), ("aTm", sl, 1), ("wsl", slot)], writes=["ps%d" % pb])
                if half == 0:
                    S.op("scalar", lambda e, hh=hh, half=half, pb=pb: e.copy(out=Y[:, hh, half * 512:(half + 1) * 512], in_=ps[pb][:, :]),
                         reads=["ps%d" % pb], writes=[("ysb", sl, hh, half)])
                else:
                    S.op("vector", lambda e, hh=hh, half=half, pb=pb: e.tensor_copy(out=Y[:, hh, half * 512:(half + 1) * 512], in_=ps[pb][:, :]),
                         reads=["ps%d" % pb], writes=[("ysb", sl, hh, half)])
        ys_recs.append(S.dma("sync", "sys%d" % sl, Ys[i * TM:(i + 1) * TM, :].rearrange("(h p) d -> p h d", p=128), Y[:, :, :],
                             reads=[("ysb", sl, hh, half) for hh in range(2) for half in range(2)]))

    PRE = 3
    for i in range(min(PRE, NT)):
        fetch_tile(i)
    fetch_x(0)
    dbg("xs_t0", xs[0][:, :, :], [("xs", 0)])
    dbg("wg_t0", wslots[0][:, 0:2048], [("wsl", 0, 0)])
    dbg("wd_t0", wslots[0][:, 4096:6144], [("wsl", 0, 2)])
    for i in range(NT):
        if i + PRE < NT:
            fetch_tile(i + PRE)
        if i + 1 < NT:
            fetch_x(i + 1)
        moe_tile(i)
        if i == 0:
            dbg("ysb_t0", ysb[0][:, :, :], [("ysb", 0, hh, half) for hh in range(2) for half in range(2)])
            dbg("xsT_t0", xsT[0][:, :, :], [("xsT", 0, 0), ("xsT", 0, 1)])
            dbg("aT_t0", aTm[0][:, :, :], [("aTm", 0, 0), ("aTm", 0, 1)])
    S.barrier()

    xf = [QTf[:, i * D:(i + 1) * D] for i in range(2)]
    QTb = QT[:, :, :].rearrange("p h t -> p (h t)")
    g1 = [QTb[:, (4 + i) * D:(5 + i) * D] for i in range(2)]
    g2 = [QTb[:, (6 + i) * D:(7 + i) * D] for i in range(2)]
    for ti in range(17):
        if ti % 2 == 0:
            S.window_begin()
        S.cur_stream = ti % 2
        T = 128 if ti < 16 else NS
        tok0 = ti * 128
        sl = ti % 2
        S.dma("sync", "lf%d" % sl, xf[sl][0:T, :], x1s[tok0:tok0 + T, :], writes=[("xf", sl)])
        for k, G in enumerate((g1, g2)):
            S.op("gpsimd", lambda e, ti=ti, k=k, T=T, G=G, sl=sl: e.indirect_dma_start(
                out=G[sl][0:T, :], out_offset=None, in_=Ys[:, :],
                in_offset=bass.IndirectOffsetOnAxis(ap=POS[0:T, ti, k:k + 1], axis=0)),
                reads=[("pos", ti)], writes=[("gg", k, sl)], dma="gy%d_%d" % (k, sl))
        if ti == 0:
            dbg("g1_0", g1[0][:, :], [("gg", 0, 0)])
            dbg("g2_0", g2[0][:, :], [("gg", 1, 0)])
            dbg("xf_0", xf[0][:, :], [("xf", 0)])
        S.op(V, lambda e, ti=ti, T=T, sl=sl: e.scalar_tensor_tensor(out=xf[sl][0:T, :], in0=g1[sl][0:T, :], scalar=RK[0:T, ti, 2:3],
                                                                    in1=xf[sl][0:T, :], op0=ALU.mult, op1=ALU.add),
             reads=[("gg", 0, sl), ("xf", sl)], writes=[("xf", sl)])
        S.op(V, lambda e, ti=ti, T=T, sl=sl: e.scalar_tensor_tensor(out=xf[sl][0:T, :], in0=g2[sl][0:T, :], scalar=RK[0:T, ti, 3:4],
                                                                    in1=xf[sl][0:T, :], op0=ALU.mult, op1=ALU.add),
             reads=[("gg", 1, sl), ("xf", sl)], writes=[("xf", sl)])
        dst = y_p[tok0:tok0 + 128, :] if ti < 16 else y_s[:, :]
        out_recs.append(S.dma("sync", "sf%d" % sl, dst, xf[sl][0:T, :], reads=[("xf", sl)]))
        if ti % 2 == 1 or ti == 16:
            S.cur_stream = 0
            S.window_end()

    dbg("OH", OH[:, :, :], [])
    dbg("RK", RK[:, :, :], [])
    dbg("carry", carry[:, :], [])
    dbg("nt_f", nt_f[:, :], [])
    dbg("bt", bt[:, :], [])
    dbg("bti", bti[:, :], [])
    dbg("texp", texp[:, :], [])
    dbg("idxw", idxw[:, :], [])
    dbg("POS", POS[:, :, :], [])
    dbg("xsT0", xsT[0][:, :, :], [])
    dbg("ysb0", ysb[0][:, :, :], [])
    S.op("sync", None, reads=[], writes=[])
    fin = S.ops["sync"][-1]
    fin.deps = [r_ for r_ in out_recs if r_.eng is not None]
    for d in fin.deps:
        d.needed = True

    S.emit(nc, es)
    es.close()
    return nc


_CACHE = {}


def kernel(**inputs):
    f = lambda a: np.ascontiguousarray(a)
    nc = _CACHE.get("nc")
    if nc is None:
        nc = build_program()
        _CACHE["nc"] = nc
    shared = {
        "cache_ckv": f(inputs["cache_ckv"][0]),
        "cache_kr": f(inputs["cache_krope"][0]),
        "g_mix": f(inputs["g_mix"]), "w_in": f(inputs["w_in"][0]),
        "conv_w": f(inputs["conv_w"][0]), "conv_b": f(inputs["conv_b"]),
        "g_q_lat": f(inputs["g_q_lat"]), "w_uq": f(inputs["w_uq"][0]),
        "g_kv_lat": f(inputs["g_kv_lat"]), "w_uk": f(inputs["w_uk"][0]), "w_uv": f(inputs["w_uv"][0]),
        "g_q_nope": f(inputs["g_q_nope"]), "g_q_rope": f(inputs["g_q_rope"]),
        "g_k_nope": f(inputs["g_k_nope"]), "g_k_rope": f(inputs["g_k_rope"]),
        "g_out": f(inputs["g_out"]), "w_out": f(inputs["w_out"][0]), "g_ffn": f(inputs["g_ffn"]),
        "w_rg": f(inputs["w_router_group"][0]), "b_rg": f(inputs["b_router_group"]),
        "w_re": f(inputs["w_router_expert"][0]), "b_re": f(inputs["b_router_expert"]),
        "w_gate": f(inputs["w_gate"][0].reshape(NE, 8, 128, DE).transpose(0, 2, 1, 3).reshape(NE * 128, 8 * DE)),
        "w_up": f(inputs["w_up"][0].reshape(NE, 8, 128, DE).transpose(0, 2, 1, 3).reshape(NE * 128, 8 * DE)),
        "w_down": f(inputs["w_down"][0].reshape(NE, 2, 128, D).transpose(0, 2, 1, 3).reshape(NE * 128, 2 * D)),
    }
    in_maps = []
    for c in range(NCORES):
        m = dict(shared)
        m["x_p"] = f(inputs["x_prompt"][c])
        m["x_s"] = f(inputs["x_sample"][4 * c:4 * c + 4].reshape(NS, D))
        m["st_conv"] = f(inputs["state_conv"][0, 4 * c:4 * c + 4])
        m["ptab"] = f(inputs["page_table"][4 * c:4 * c + 4])
        in_maps.append(m)
    res = run_bass_kernel_spmd(nc, in_maps, core_ids=list(range(NCORES)))
    R = res.results
    if DEBUG:
        _CACHE["dbg"] = R
    y_p = np.stack([R[c]["y_p"] for c in range(NCORES)], 0)
    y_s = np.concatenate([R[c]["y_s"].reshape(4, 8, D) for c in range(NCORES)], 0)
    ckv_p = np.stack([R[c]["o_ckv_p"] for c in range(NCORES)], 0)[None]
    kr_p = np.stack([R[c]["o_kr_p"] for c in range(NCORES)], 0)[None]
    conv_p = np.stack([R[c]["o_conv_p"] for c in range(NCORES)], 0)[None]
    ckv_s = np.concatenate([R[c]["o_ckv_s"].reshape(4, 8, KVR) for c in range(NCORES)], 0)[None]
    kr_s = np.concatenate([R[c]["o_kr_s"].reshape(4, 8, RD) for c in range(NCORES)], 0)[None]
    conv_s = np.concatenate([R[c]["o_conv_s"] for c in range(NCORES)], 0)[None]
    return (y_p, y_s, ckv_p, kr_p, conv_p, ckv_s, kr_s, conv_s)
```
